# Optimizing a Trainium2 kernel written in Bass

```python
import numpy as np
import jax
import jax.numpy as jnp
from jax import lax

D_MODEL = 4096
BATCH = 2
SEQ = 8192
DEPTH = 1

N_MEM = 256
N_BRANCH = 3
BRANCH_W = D_MODEL // 2
GLA_HEADS = 4
GLA_DV = BRANCH_W // GLA_HEADS
GLA_DK = GLA_DV // 2
GLA_K_W = GLA_HEADS * GLA_DK
GLA_LOWRANK = 16
GLA_TAU = 16.0
GLA_CHUNK = 64
NSA_HEADS = 16
NSA_GROUPS = 4
NSA_REP = NSA_HEADS // NSA_GROUPS
NSA_HD = BRANCH_W // NSA_HEADS
NSA_KV_W = NSA_GROUPS * NSA_HD
CMP_LEN = 32
CMP_STRIDE = 16
SEL_LEN = 64
SEL_TOPK = 16
WINDOW = 512
Q_BLOCK = 128
FORCE_SCORE = 1e4
MEM_HEADS = 4
MEM_HD = BRANCH_W // MEM_HEADS
LN_EPS = 1e-5
RMS_EPS = 1e-6
NEG_INF = -1e30
DEEPNORM_ALPHA = (2 * DEPTH) ** 0.25
DEEPNORM_BETA = (8 * DEPTH) ** -0.25

IN_SIZES = (
    GLA_K_W, GLA_K_W, BRANCH_W, BRANCH_W, GLA_LOWRANK,
    BRANCH_W, NSA_KV_W, NSA_KV_W, NSA_KV_W, NSA_KV_W, NSA_KV_W, NSA_KV_W,
    BRANCH_W, 3 * NSA_HEADS,
    BRANCH_W, BRANCH_W,
    N_BRANCH * D_MODEL,
)
D_IN = sum(IN_SIZES)

kernel_name = 'hybrid_gla_nsa_memory_deepnorm'


def alibi_slopes(n):
    return jnp.exp2(-8.0 * (jnp.arange(n, dtype=jnp.float32) + 1.0) / n)


def layer_norm(z, g, b):
    zf = z.astype(jnp.float32)
    mu = jnp.mean(zf, -1, keepdims=True)
    zc = zf - mu
    var = jnp.mean(zc * zc, -1, keepdims=True)
    return (zc * lax.rsqrt(var + LN_EPS) * g + b).astype(z.dtype)


def gla_mixer(q, k, v, a_lr, w_a2, b_a, norm_g):
    B, S, _ = q.shape
    H, DK, DV, C = GLA_HEADS, GLA_DK, GLA_DV, GLA_CHUNK
    N = S // C
    f32 = jnp.float32
    log_a = jax.nn.log_sigmoid((a_lr @ w_a2 + b_a).astype(f32)) / GLA_TAU

    def to_chunks(t, d):
        return t.astype(f32).reshape(B, N, C, H, d).transpose(1, 0, 3, 2, 4)

    qc = to_chunks(q, DK) * DK ** -0.5
    kc = to_chunks(k, DK)
    vc = to_chunks(v, DV)
    ac = to_chunks(log_a, DK)
    causal = jnp.tril(jnp.ones((C, C), dtype=bool))

    def step(state, inp):
        qn, kn, vn, an = inp
        bcum = jnp.cumsum(an, axis=-2)
        b_last = bcum[..., -1:, :]
        q_d = qn * jnp.exp(bcum)
        k_d = kn * jnp.exp(-bcum)
        att = jnp.where(causal, jnp.einsum('bhcd,bhjd->bhcj', q_d, k_d), 0.0)
        o = att @ vn + jnp.einsum('bhcd,bhde->bhce', q_d, state)
        k_end = kn * jnp.exp(b_last - bcum)
        state = jnp.exp(b_last[..., 0, :])[..., None] * state + jnp.einsum('bhcd,bhce->bhde', k_end, vn)
        return state, o

    s0 = jnp.zeros((B, H, DK, DV), f32)
    _, o = lax.scan(step, s0, (qc, kc, vc, ac))
    o = o.transpose(1, 0, 3, 2, 4).reshape(B, S, H, DV)
    o = o * lax.rsqrt(jnp.mean(o * o, -1, keepdims=True) + RMS_EPS) * norm_g.astype(f32)
    return o.reshape(B, S, H * DV)


def nsa_mixer(q, kc, vc, ks, vs, kw, vw, gate_logits, pe_k, pe_v, wk1, wk2, wv1, wv2):
    B, S, _ = q.shape
    G, R, HD = NSA_GROUPS, NSA_REP, NSA_HD
    f32 = jnp.float32
    q = q.reshape(B, S, G, R, HD) * HD ** -0.5
    kc, vc, ks, vs, kw, vw = [t.reshape(B, S, G, HD) for t in (kc, vc, ks, vs, kw, vw)]
    slopes = alibi_slopes(NSA_HEADS).reshape(G, R)

    n_cmp = (S - CMP_LEN) // CMP_STRIDE + 1
    tok_idx = jnp.arange(n_cmp)[:, None] * CMP_STRIDE + jnp.arange(CMP_LEN)[None, :]
    cmp_start = tok_idx[:, 0]
    cmp_end = tok_idx[:, -1]

    def compress(t, pe, w1, w2):
        blk = t[:, tok_idx] + pe[None, None, :, None, :]
        blk = blk.transpose(0, 1, 3, 2, 4).reshape(B, n_cmp, G, CMP_LEN * HD)
        return jax.nn.silu(blk @ w1) @ w2

    k_cmp = compress(kc, pe_k, wk1, wk2)
    v_cmp = compress(vc, pe_v, wv1, wv2)

    n_sel = S // SEL_LEN
    top_k = min(SEL_TOPK, n_sel)
    ks_blk = ks.reshape(B, n_sel, SEL_LEN, G, HD).transpose(0, 3, 1, 2, 4)
    vs_blk = vs.reshape(B, n_sel, SEL_LEN, G, HD).transpose(0, 3, 1, 2, 4)
    sel_start = jnp.arange(n_sel) * SEL_LEN
    overlap = ((cmp_start[:, None] < sel_start[None, :] + SEL_LEN)
               & (cmp_end[:, None] >= sel_start[None, :])).astype(f32)
    blk_ids = jnp.arange(n_sel)

    pad = ((0, 0), (WINDOW, 0), (0, 0), (0, 0))
    kw_p = jnp.pad(kw, pad)
    vw_p = jnp.pad(vw, pad)

    b_ix = jnp.arange(B)[:, None, None, None]
    g_ix = jnp.arange(G)[None, :, None, None]

    def attend_block(qb):
        start = qb * Q_BLOCK
        t = start + jnp.arange(Q_BLOCK)
        qq = lax.dynamic_slice_in_dim(q, start, Q_BLOCK, axis=1)

        dist_c = t[:, None] - cmp_end[None, :]
        ok_c = dist_c >= 0
        s_c = (jnp.einsum('bqgrd,bngd->bgrqn', qq, k_cmp).astype(f32)
               - slopes[:, :, None, None] * dist_c.astype(f32))
        s_c = jnp.where(ok_c, s_c, NEG_INF)
        p_c = jax.nn.softmax(s_c, axis=-1) * jnp.any(ok_c, -1)[:, None]
        o_cmp = jnp.einsum('bgrqn,bngd->bqgrd', p_c.astype(v_cmp.dtype), v_cmp)

        imp = jnp.einsum('bgrqn,nj->bgqj', p_c, overlap)
        cur = t // SEL_LEN
        forced = ((blk_ids[None, :] == 0) | (blk_ids[None, :] == cur[:, None])
                  | (blk_ids[None, :] == cur[:, None] - 1))
        causal_blk = sel_start[None, :] <= t[:, None]
        score = jnp.where(forced, FORCE_SCORE, jnp.where(causal_blk, imp, -1.0))
        _, sel = lax.top_k(score, top_k)
        k_g = ks_blk[b_ix, g_ix, sel]
        v_g = vs_blk[b_ix, g_ix, sel]
        pos = sel[..., None] * SEL_LEN + jnp.arange(SEL_LEN)
        dist_s = (t[None, None, :, None, None] - pos)[:, :, None]
        s_s = (jnp.einsum('bqgrd,bgqkld->bgrqkl', qq, k_g).astype(f32)
               - slopes[None, :, :, None, None, None] * dist_s.astype(f32))
        s_s = jnp.where(dist_s >= 0, s_s, NEG_INF).reshape(B, G, R, Q_BLOCK, top_k * SEL_LEN)
        p_s = jax.nn.softmax(s_s, axis=-1).reshape(B, G, R, Q_BLOCK, top_k, SEL_LEN)
        o_sel = jnp.einsum('bgrqkl,bgqkld->bqgrd', p_s.astype(v_g.dtype), v_g)

        kk = lax.dynamic_slice_in_dim(kw_p, start, Q_BLOCK + WINDOW, axis=1)
        vv = lax.dynamic_slice_in_dim(vw_p, start, Q_BLOCK + WINDOW, axis=1)
        s_pos = start - WINDOW + jnp.arange(Q_BLOCK + WINDOW)
        dist_w = t[:, None] - s_pos[None, :]
        ok_w = (dist_w >= 0) & (dist_w < WINDOW) & (s_pos[None, :] >= 0)
        s_w = (jnp.einsum('bqgrd,bkgd->bgrqk', qq, kk).astype(f32)
               - slopes[:, :, None, None] * dist_w.astype(f32))
        s_w = jnp.where(ok_w, s_w, NEG_INF)
        p_w = jax.nn.softmax(s_w, axis=-1)
        o_win = jnp.einsum('bgrqk,bkgd->bqgrd', p_w.astype(vv.dtype), vv)
        return o_cmp, o_sel, o_win

    o_cmp, o_sel, o_win = lax.map(attend_block, jnp.arange(S // Q_BLOCK))

    def unblock(o):
        return o.transpose(1, 0, 2, 3, 4, 5).reshape(B, S, NSA_HEADS, HD)

    g = jax.nn.sigmoid(gate_logits).reshape(B, S, NSA_HEADS, 3)
    o = (g[..., 0:1] * unblock(o_cmp) + g[..., 1:2] * unblock(o_sel)
         + g[..., 2:3] * unblock(o_win))
    return o.reshape(B, S, NSA_HEADS * HD)


def memory_attention(q, mem, w_kv):
    B, S, _ = q.shape
    M = mem.shape[1]
    kv = (mem @ w_kv).reshape(B, M, 2, MEM_HEADS, MEM_HD)
    k, v = kv[:, :, 0], kv[:, :, 1]
    q = q.reshape(B, S, MEM_HEADS, MEM_HD) * MEM_HD ** -0.5
    s = jnp.einsum('bshd,bmhd->bhsm', q, k).astype(jnp.float32)
    p = jax.nn.softmax(s, axis=-1).astype(v.dtype)
    return jnp.einsum('bhsm,bmhd->bshd', p, v).reshape(B, S, MEM_HEADS * MEM_HD)


def hybrid_layer(x, mem, w_in, b_merge, gla_w_a2, gla_b_a, gla_norm_g, nsa_pe_k, nsa_pe_v,
                 nsa_wk1, nsa_wk2, nsa_wv1, nsa_wv2, w_mem_kv, w_br_gla, w_br_nsa, w_br_mem,
                 w_out, ln_g, ln_b):
    B, S, D = x.shape
    h = x @ w_in
    (gq, gk, gv, gz, ga, nq, nkc, nvc, nks, nvs, nkw, nvw, nz, nbg, mq, mz, mrg) = jnp.split(
        h, np.cumsum(IN_SIZES)[:-1].tolist(), axis=-1)
    o_gla = gla_mixer(gq, gk, gv, ga, gla_w_a2, gla_b_a, gla_norm_g).astype(x.dtype)
    y_gla = (o_gla * jax.nn.silu(gz)) @ w_br_gla
    o_nsa = nsa_mixer(nq, nkc, nvc, nks, nvs, nkw, nvw, nbg, nsa_pe_k, nsa_pe_v,
                      nsa_wk1, nsa_wk2, nsa_wv1, nsa_wv2)
    y_nsa = (o_nsa * jax.nn.silu(nz)) @ w_br_nsa
    o_mem = memory_attention(mq, mem, w_mem_kv)
    y_mem = (o_mem * jax.nn.silu(mz)) @ w_br_mem
    a = jax.nn.sigmoid(mrg + b_merge).reshape(B, S, N_BRANCH, D)
    merged = a[:, :, 0] * y_gla + a[:, :, 1] * y_nsa + a[:, :, 2] * y_mem
    out = merged @ w_out
    return layer_norm(DEEPNORM_ALPHA * x + out, ln_g, ln_b)


def setup_inputs(seed: int = 0) -> dict:
    key = jax.random.key(seed)
    ks = jax.random.split(key, 20)
    L, D = DEPTH, D_MODEL

    def nrm(k, shape, scale):
        return jax.random.normal(k, shape, jnp.float32) * scale

    return {
        'x': nrm(ks[0], (BATCH, SEQ, D), 1.0),
        'mem': nrm(ks[1], (BATCH, N_MEM, D), 1.0),
        'w_in': nrm(ks[2], (L, D, D_IN), D ** -0.5),
        'b_merge': nrm(ks[3], (L, N_BRANCH * D), 0.01),
        'gla_w_a2': nrm(ks[4], (L, GLA_LOWRANK, GLA_K_W), GLA_LOWRANK ** -0.5),
        'gla_b_a': nrm(ks[5], (L, GLA_K_W), 0.1),
        'gla_norm_g': 1.0 + nrm(ks[6], (L, GLA_DV), 0.02),
        'nsa_pe_k': nrm(ks[7], (L, CMP_LEN, NSA_HD), 0.1),
        'nsa_pe_v': nrm(ks[8], (L, CMP_LEN, NSA_HD), 0.1),
        'nsa_wk1': nrm(ks[9], (L, CMP_LEN * NSA_HD, NSA_HD), (CMP_LEN * NSA_HD) ** -0.5),
        'nsa_wk2': nrm(ks[10], (L, NSA_HD, NSA_HD), NSA_HD ** -0.5),
        'nsa_wv1': nrm(ks[11], (L, CMP_LEN * NSA_HD, NSA_HD), (CMP_LEN * NSA_HD) ** -0.5),
        'nsa_wv2': nrm(ks[12], (L, NSA_HD, NSA_HD), NSA_HD ** -0.5),
        'w_mem_kv': nrm(ks[13], (L, D, 2 * MEM_HEADS * MEM_HD), D ** -0.5),
        'w_br_gla': nrm(ks[14], (L, BRANCH_W, D), BRANCH_W ** -0.5 * DEEPNORM_BETA),
        'w_br_nsa': nrm(ks[15], (L, BRANCH_W, D), BRANCH_W ** -0.5 * DEEPNORM_BETA),
        'w_br_mem': nrm(ks[16], (L, BRANCH_W, D), BRANCH_W ** -0.5 * DEEPNORM_BETA),
        'w_out': nrm(ks[17], (L, D, D), D ** -0.5 * DEEPNORM_BETA),
        'ln_g': 1.0 + nrm(ks[18], (L, D), 0.02),
        'ln_b': nrm(ks[19], (L, D), 0.02),
    }


def reference(x, mem, w_in, b_merge, gla_w_a2, gla_b_a, gla_norm_g, nsa_pe_k, nsa_pe_v,
              nsa_wk1, nsa_wk2, nsa_wv1, nsa_wv2, w_mem_kv, w_br_gla, w_br_nsa, w_br_mem,
              w_out, ln_g, ln_b):
    for l in range(DEPTH):
        x = hybrid_layer(x, mem, w_in[l], b_merge[l], gla_w_a2[l], gla_b_a[l], gla_norm_g[l],
                         nsa_pe_k[l], nsa_pe_v[l], nsa_wk1[l], nsa_wk2[l], nsa_wv1[l], nsa_wv2[l],
                         w_mem_kv[l], w_br_gla[l], w_br_nsa[l], w_br_mem[l], w_out[l],
                         ln_g[l], ln_b[l])
    return x
```

```python
import numpy as np
import ml_dtypes
import concourse.bass as bass
import concourse.mybir as mybir
from concourse.bass_utils import run_bass_kernel_spmd

F32 = mybir.dt.float32
BF16 = mybir.dt.bfloat16
AF = mybir.ActivationFunctionType
ALU = mybir.AluOpType
AX = mybir.AxisListType
NPBF = ml_dtypes.bfloat16


class Sched:
    ENGS = ("pe", "act", "dve", "pool", "sp")
    SEM_SPAN = 12000

    def __init__(self, nc):
        self.nc = nc
        self.ops = []
        self.last_w = {}
        self.readers = {}

    def op(self, eng, fn, r=(), w=(), dma=None, inc=16):
        idx = len(self.ops)
        deps = set()
        raw = set()
        for k in r:
            if k in self.last_w:
                deps.add(self.last_w[k])
                raw.add(self.last_w[k])
        for k in w:
            if k in self.last_w:
                deps.add(self.last_w[k])
            rd = self.readers.get(k)
            if rd:
                deps.update(rd.values())
        for k in r:
            d = self.readers.setdefault(k, {})
            d[("dma", idx) if dma is not None else eng] = idx
        for k in w:
            self.last_w[k] = idx
            self.readers[k] = {}
        deps.discard(idx)
        self.ops.append(dict(eng=eng, fn=fn, deps=deps, raw=raw, dma=dma, sig=False, inc=inc))
        return idx

    def setup_phased(self, scr_tiles, tok_src, tok_dst):
        import contextlib
        self.stack = contextlib.ExitStack()
        self.sems = {}
        self.cnt = {e: 0 for e in self.ENGS}
        self.dma_n = {e: 0 for e in self.ENGS}
        self.dma_cnt = {}
        self.emitted = 0
        self.scr = scr_tiles
        self.tok_src, self.tok_dst = tok_src, tok_dst
        self.bar = {}
        self.nphase = 0

    def _sem(self, key):
        if key not in self.sems:
            self.sems[key] = self.stack.enter_context(self.nc.semaphore("s%d" % len(self.sems)))
        return self.sems[key]

    def flush(self, final=False):
        nc = self.nc
        ops = self.ops
        lo = self.emitted
        cur = ops[lo:]
        self.emitted = len(ops)
        npool = {"sp": 6, "pool": 5, "act": 4, "dve": 1, "pe": 1}
        toks = []
        for e in ("act", "dve", "pool"):
            scr = self.scr[e]
            if e == "act":
                fn = (lambda eng, scr=scr: eng.copy(out=scr[:, 0:1], in_=scr[:, 1:2]))
            else:
                fn = (lambda eng, scr=scr: eng.memset(scr[:, 0:1], 0.0))
            o = dict(eng=e, fn=fn, deps=set(), raw=set(), dma=None, sig=True, inc=1, tok=True)
            toks.append(o)
        o = dict(eng="sp", fn=(lambda eng: eng.dma_start(out=self.tok_dst, in_=self.tok_src)), deps=set(), raw=set(), dma="tok", sig=False, inc=16, tok=True)
        toks.append(o)
        for i, o in enumerate(cur):
            nd = set()
            for d in o["deps"]:
                if d < lo:
                    continue
                od = ops[d]
                if od["dma"] is None and od["eng"] == o["eng"] and o["dma"] is None and o["eng"] == "pe":
                    continue
                nd.add(d)
                if od["dma"] is None:
                    od["sig"] = True
            o["deps"] = nd
        pe_ops = [o for o in cur if o["eng"] == "pe"]
        if pe_ops:
            pe_ops[-1]["sig"] = True
        allops = cur + toks
        for o in allops:
            if o["dma"] is not None:
                q = o["eng"]
                if o["dma"] == "cc":
                    k = (q, "cc")
                else:
                    k = (q, self.dma_n[q] % npool[q])
                    self.dma_n[q] += 1
                o["prev"] = ("d", k, self.dma_cnt.get(k, 0))
                self.dma_cnt[k] = self.dma_cnt.get(k, 0) + o["inc"]
                o["semv"] = ("d", k, self.dma_cnt[k])
            elif o["sig"]:
                e = o["eng"]
                c = self.cnt[e]
                self.cnt[e] += 1
                o["semv"] = ("e", (e, c // self.SEM_SPAN), c % self.SEM_SPAN + 1)
        per_eng = {e: [o for o in allops if o["eng"] == e] for e in self.ENGS}
        newbar = {}
        for o in toks:
            t, k, v = o["semv"]
            newbar[(t, k)] = v
        if pe_ops:
            t, k, v = pe_ops[-1]["semv"]
            newbar[(t, k)] = v
        oldbar = self.bar
        block = nc.Block()
        with block:
            def run(engname, engobj):
                waited = {}
                for key, v in oldbar.items():
                    waited[key] = v
                    engobj.wait_ge(self._sem(key), v)
                myops = per_eng[engname]
                for o in myops:
                    need = {}
                    if o.get("tok"):
                        for (q, j), v in self.dma_cnt.items():
                            if q == engname and not (o["dma"] is not None and ("d", (q, j)) == o["semv"][:2]):
                                need[("d", (q, j))] = v
                            elif q == engname:
                                need[("d", (q, j))] = o["prev"][2]
                    for d in o["deps"]:
                        t, k, v = ops[d]["semv"]
                        key = (t, k)
                        if need.get(key, 0) < v:
                            need[key] = v
                    if o["dma"] is not None:
                        t, k, v = o["prev"]
                        if v > 0 and need.get((t, k), 0) < v:
                            need[(t, k)] = v
                    for key, v in need.items():
                        if v <= 0 or waited.get(key, 0) >= v:
                            continue
                        waited[key] = v
                        engobj.wait_ge(self._sem(key), v)
                    ins = o["fn"](engobj)
                    if o["dma"] is not None:
                        t, k, v = o["semv"]
                        ins.then_inc(self._sem((t, k)), o["inc"])
                    elif o["sig"]:
                        t, k, v = o["semv"]
                        ins.then_inc(self._sem((t, k)), 1)
                if final:
                    for (q, j), v in self.dma_cnt.items():
                        if q == engname:
                            engobj.wait_ge(self._sem(("d", (q, j))), v)

            @block.tensor
            def _(e):
                run("pe", e)

            @block.scalar
            def _(e):
                run("act", e)

            @block.vector
            def _(e):
                run("dve", e)

            @block.gpsimd
            def _(e):
                run("pool", e)

            @block.sync
            def _(e):
                run("sp", e)
        t, k, v = toks[-1]["semv"]
        newbar[(t, k)] = v
        self.bar = dict(oldbar)
        self.bar.update(newbar)
        self.nphase += 1
        print("phase %d: %d ops, %d sems" % (self.nphase, len(cur), len(self.sems)))
        self.last_w = {}
        self.readers = {}

    def close(self):
        self.stack.close()

    def emit(self):
        nc = self.nc
        ops = self.ops
        for o in ops:
            nd = set()
            for d in o["deps"]:
                od = ops[d]
                if od["dma"] is None and od["eng"] == o["eng"] and o["dma"] is None:
                    if o["eng"] == "pe":
                        continue
                nd.add(d)
                if od["dma"] is None:
                    od["sig"] = True
            o["deps"] = nd
        cnt = {e: 0 for e in self.ENGS}
        npool = {"sp": 6, "pool": 5, "act": 4, "dve": 1, "pe": 1}
        dma_n = {e: 0 for e in self.ENGS}
        dma_cnt = {}
        for o in ops:
            if o["dma"] is not None:
                q = o["eng"]
                k = (q, dma_n[q] % npool[q])
                dma_n[q] += 1
                o["prev"] = ("d", k, dma_cnt.get(k, 0))
                dma_cnt[k] = dma_cnt.get(k, 0) + o["inc"]
                o["semv"] = ("d", k, dma_cnt[k])
            elif o["sig"]:
                e = o["eng"]
                c = cnt[e]
                cnt[e] += 1
                o["semv"] = ("e", (e, c // self.SEM_SPAN), c % self.SEM_SPAN + 1)
        sem_names = []
        for e in self.ENGS:
            for j in range((cnt[e] + self.SEM_SPAN - 1) // self.SEM_SPAN):
                sem_names.append(("e", (e, j)))
        for k in dma_cnt:
            sem_names.append(("d", k))
        import contextlib
        with contextlib.ExitStack() as st:
            sems = {}
            for i, sn in enumerate(sem_names):
                sems[sn] = st.enter_context(nc.semaphore("s%d" % i))
            block = st.enter_context(nc.Block())
            per_eng = {e: [o for o in ops if o["eng"] == e] for e in self.ENGS}

            def run(engname, engobj):
                waited = {}
                for o in per_eng[engname]:
                    need = {}
                    for d in o["deps"]:
                        t, k, v = ops[d]["semv"]
                        key = (t, k)
                        if need.get(key, 0) < v:
                            need[key] = v
                    if o["dma"] is not None:
                        t, k, v = o["prev"]
                        if v > 0 and need.get((t, k), 0) < v:
                            need[(t, k)] = v
                    for key, v in need.items():
                        if waited.get(key, 0) >= v:
                            continue
                        waited[key] = v
                        engobj.wait_ge(sems[key], v)
                    ins = o["fn"](engobj)
                    if o["dma"] is not None:
                        t, k, v = o["semv"]
                        ins.then_inc(sems[(t, k)], o["inc"])
                    elif o["sig"]:
                        t, k, v = o["semv"]
                        ins.then_inc(sems[(t, k)], 1)
                fin = {}
                for o in per_eng[engname]:
                    if o["dma"] is not None:
                        t, k, v = o["semv"]
                        fin[(t, k)] = max(fin.get((t, k), 0), v)
                for key, v in fin.items():
                    engobj.wait_ge(sems[key], v)

            @block.tensor
            def _(e):
                run("pe", e)

            @block.scalar
            def _(e):
                run("act", e)

            @block.vector
            def _(e):
                run("dve", e)

            @block.gpsimd
            def _(e):
                run("pool", e)

            @block.sync
            def _(e):
                run("sp", e)
        print("sched: %d ops, %d sems" % (len(ops), len(sem_names)))


def cast_pass(S, pool, src, dst, tag, engs=("dve", "pool"), stq="act", chunk=4096, nbuf=2):
    R, C = src.shape
    nrow = R // 128
    sv = src.rearrange("(n p) c -> p n c", p=128)
    dv = dst.rearrange("(n p) c -> p n c", p=128)
    if C >= chunk:
        assert C % chunk == 0
        steps = [(n, 1, c0, chunk) for n in range(nrow) for c0 in range(0, C, chunk)]
    else:
        nn = max(1, chunk // C)
        steps = [(n, min(nn, nrow - n), 0, C) for n in range(0, nrow, nn)]
    stg, obf, pn = pool["stg"], pool["obf"], pool["name"]
    keys = []
    for i, (n, k, c0, cw) in enumerate(steps):
        sl = i % nbuf
        sa = stg[sl][:, 0:k * cw].rearrange("p (k c) -> p k c", k=k)
        oa = obf[sl][:, 0:k * cw].rearrange("p (k c) -> p k c", k=k)
        S.op("sp", lambda e, sa=sa, n=n, k=k, c0=c0, cw=cw: e.dma_start(out=sa, in_=sv[:, n:n + k, c0:c0 + cw]),
             w=[(pn, "stg", sl)], dma=(pn, "stg", sl))
        ce = engs[i % len(engs)]
        if ce == "act":
            S.op("act", lambda e, sa=sa, oa=oa: e.copy(out=oa, in_=sa), r=[(pn, "stg", sl)], w=[(pn, "obf", sl)])
        else:
            S.op(ce, lambda e, sa=sa, oa=oa: e.tensor_copy(out=oa, in_=sa), r=[(pn, "stg", sl)], w=[(pn, "obf", sl)])
        S.op(stq, lambda e, oa=oa, n=n, k=k, c0=c0, cw=cw: e.dma_start(out=dv[:, n:n + k, c0:c0 + cw], in_=oa),
             r=[(pn, "obf", sl)], w=[(tag, "dram", i)], dma=(pn, "obf", sl))
        keys.append((tag, "dram", i))
    return keys


def gemm(S, tag, aT, bm, M, K, blocks, bufs, a_dep, b_dep, stq="act", ldq=("sp", "pool")):
    KB = K // 128
    MT = 512
    A, B, O, PS = bufs["A"], bufs["B"], bufs["O"], bufs["PS"]
    out_tag = tag
    tag = bufs["name"]
    av = aT.rearrange("(kb p) m -> p kb m", p=128)
    bv = bm.rearrange("(kb p) n -> p kb n", p=128)
    keys = []
    ai = 0
    oi = 0
    pi = 0
    for bi, (mode, n0, nw, dst) in enumerate(blocks):
        half = KB // 2
        S.op("sp", lambda e, n0=n0, nw=nw: e.dma_start(out=B[:, 0:half, 0:nw], in_=bv[:, 0:half, n0:n0 + nw]),
             r=[b_dep], w=[(tag, "B", 0)], dma=(tag, "B", 0))
        S.op(ldq[1], lambda e, n0=n0, nw=nw: e.dma_start(out=B[:, half:KB, 0:nw], in_=bv[:, half:KB, n0:n0 + nw]),
             r=[b_dep], w=[(tag, "B", 1)], dma=(tag, "B", 1))
        for m0 in range(0, M, MT):
            sl = ai % 2
            ai += 1
            At = A[sl]
            S.op("sp", lambda e, At=At, m0=m0: e.dma_start(out=At[:, 0:half, :], in_=av[:, 0:half, m0:m0 + MT]),
                 r=[a_dep], w=[(tag, "A", sl, 0)], dma=(tag, "A", sl, 0))
            S.op(ldq[1], lambda e, At=At, m0=m0: e.dma_start(out=At[:, half:KB, :], in_=av[:, half:KB, m0:m0 + MT]),
                 r=[a_dep], w=[(tag, "A", sl, 1)], dma=(tag, "A", sl, 1))
            for s0 in range(0, nw, 512):
                sw = min(512, nw - s0)
                osl = oi % 2
                oi += 1
                Ot = O[osl]
                for j in range(4):
                    if mode == "FM" and j * 128 >= sw:
                        continue
                    pk = pi % 8
                    pi += 1
                    ps = PS[pk]

                    def mm(e, ps=ps, At=At, j=j, s0=s0, sw=sw, mode=mode):
                        ins = None
                        for kb in range(KB):
                            if mode == "TM":
                                ins = e.matmul(ps[:, 0:sw], lhsT=At[:, kb, j * 128:(j + 1) * 128],
                                               rhs=B[:, kb, s0:s0 + sw], start=(kb == 0), stop=(kb == KB - 1))
                            else:
                                ins = e.matmul(ps[:, 0:MT], lhsT=B[:, kb, s0 + j * 128:s0 + (j + 1) * 128],
                                               rhs=At[:, kb, :], start=(kb == 0), stop=(kb == KB - 1))
                        return ins
                    S.op("pe", mm, r=[(tag, "A", sl, 0), (tag, "A", sl, 1), (tag, "B", 0), (tag, "B", 1)],
                         w=[(tag, "PS", pk)])
                    ww = sw if mode == "TM" else MT
                    if j % 2 == 0:
                        S.op("act", lambda e, ps=ps, Ot=Ot, j=j, ww=ww: e.copy(out=Ot[:, j, 0:ww], in_=ps[:, 0:ww]),
                             r=[(tag, "PS", pk)], w=[(tag, "O", osl, j)])
                    else:
                        S.op("dve", lambda e, ps=ps, Ot=Ot, j=j, ww=ww: e.tensor_copy(out=Ot[:, j, 0:ww], in_=ps[:, 0:ww]),
                             r=[(tag, "PS", pk)], w=[(tag, "O", osl, j)])
                if mode == "TM":
                    dv = dst[m0:m0 + MT, s0:s0 + sw].rearrange("(j p) n -> p j n", p=128)
                    src = lambda Ot=Ot, sw=sw: Ot[:, :, 0:sw]
                else:
                    nj = sw // 128
                    dv = dst[s0:s0 + sw, m0:m0 + MT].rearrange("(j p) m -> p j m", p=128)
                    src = lambda Ot=Ot, nj=nj: Ot[:, 0:nj, :]
                k = (out_tag, "out", bi, m0, s0)
                S.op(stq, lambda e, dv=dv, src=src: e.dma_start(out=dv, in_=src()),
                     r=[(tag, "O", osl, j) for j in range(4)], w=[k], dma=(tag, "O", osl))
                keys.append(k)
    return keys


def gla_consts():
    j = np.arange(128)[:, None]
    c = np.arange(128)[None, :]
    tri = np.where(j <= c, -1.0 / 16.0, 0.0).astype(np.float32)
    tri2 = np.where(j > c, -1.0 / 16.0, 0.0).astype(np.float32)
    sel = np.full((128, 1), -1.0 / 16.0, np.float32)
    mask = (j <= c).astype(np.float32)
    return np.concatenate([tri, tri2, mask, sel, np.zeros((128, 127), np.float32)], axis=1)


def phase_gla(S, nc, st, SEQ, fmq, fmk, fmga, tmk, tmv, tmgz, wa2aug, normg_b, gconst, ident, outT, dep):
    def sb(name, shape, dt):
        return st.enter_context(nc.sbuf_tensor("gla_" + name, shape, dt))

    def psum(name, shape, dt=F32):
        return st.enter_context(nc.psum_tensor("gla_" + name, shape, dt))
    NT = SEQ // 128
    qT = [sb("qT%d" % i, [128, 2, 512], BF16) for i in range(2)]
    kT = [sb("kT%d" % i, [128, 2, 512], BF16) for i in range(2)]
    gaT = [sb("gaT%d" % i, [32, 512], BF16) for i in range(2)]
    ktm = [sb("ktm%d" % i, [128, 4, 256], BF16) for i in range(2)]
    vtm = [sb("vtm%d" % i, [128, 4, 512], BF16) for i in range(2)]
    gz = [sb("gz%d" % i, [128, 4, 512], BF16) for i in range(2)]
    wa_f = sb("wa_f", [32, 256], F32)
    wa = sb("wa", [32, 256], BF16)
    ng = sb("ng", [128, 512], F32)
    gc = sb("gc", [128, 512], F32)
    idn = sb("idn", [128, 128], BF16)
    e1 = sb("e1", [128, 256], F32)
    la = sb("la", [128, 256], F32)
    Eq = sb("Eq", [128, 2, 128], F32)
    Ek = sb("Ek", [128, 2, 128], F32)
    Eend = sb("Eend", [128, 256], F32)
    dec = sb("dec", [128, 2], F32)
    qd = sb("qd", [128, 2, 128], BF16)
    kd = sb("kd", [128, 2, 128], BF16)
    kend = sb("kend", [128, 256], BF16)
    att = sb("att", [128, 128], BF16)
    S32 = sb("S32", [128, 2, 512], F32)
    Sbf = sb("Sbf", [128, 2, 512], BF16)
    sq = sb("sq", [128, 512], F32)
    ss = sb("ss", [128, 1], F32)
    rstd = sb("rstd", [128, 1], F32)
    eps_t = sb("eps_t", [128, 1], F32)
    Gz = sb("Gz", [128, 512], F32)
    actt = sb("actt", [128, 512], BF16)
    oT = [sb("oT%d" % i, [128, 4, 512], BF16) for i in range(2)]
    P_lg = psum("P_lg", [128, 512])
    P_bc = psum("P_bc", [128, 512])
    P_bT = psum("P_bT", [128, 512])
    P_att = psum("P_att", [128, 512])
    P_o = psum("P_o", [128, 512])
    P_kv = [psum("P_kv%d" % i, [128, 512]) for i in range(2)]
    P_tr = psum("P_tr", [128, 1024], BF16)

    K = lambda *a: ("gla",) + a
    S.op("sp", lambda e: e.dma_start(out=wa_f[0:17, :], in_=wa2aug), w=[K("wa_f")], dma=K("wa_f"))
    S.op("sp", lambda e: e.dma_start(out=ng[:], in_=normg_b), w=[K("ng")], dma=K("ng"))
    S.op("sp", lambda e: e.dma_start(out=gc[:], in_=gconst), w=[K("gc")], dma=K("gc"))
    S.op("sp", lambda e: e.dma_start(out=idn[:], in_=ident), w=[K("idn")], dma=K("idn"))
    S.op("dve", lambda e: e.tensor_copy(out=wa[0:17, :], in_=wa_f[0:17, :]), r=[K("wa_f")], w=[K("wa")])
    for i in range(2):
        S.op("dve", lambda e, i=i: e.memset(gaT[i][0:1, :], 1.0), w=[K("gaT1", i)])
    S.op("dve", lambda e: e.memset(eps_t[:], 1e-6), w=[K("eps")])
    S.op("dve", lambda e: e.memset(S32[:], 0.0), w=[K("S32")])
    S.op("dve", lambda e: e.memset(Sbf[:], 0.0), w=[K("Sbf")])
    tri = gc[:, 0:128]
    tri2 = gc[:, 128:256]
    msk = gc[:, 256:384]
    sel = gc[:, 384:385]
    keys = []
    for t in range(NT):
        sup, j = divmod(t, 4)
        sl = sup % 2
        t0 = sup * 512
        if j == 0:
            S.op("sp", lambda e, sl=sl, t0=t0: e.dma_start(out=qT[sl][:], in_=fmq[:, t0:t0 + 512].rearrange("(b p) t -> p b t", p=128)),
                 r=[dep], w=[K("qT", sl)], dma=K("qT", sl))
            S.op("sp", lambda e, sl=sl, t0=t0: e.dma_start(out=kT[sl][:], in_=fmk[:, t0:t0 + 512].rearrange("(b p) t -> p b t", p=128)),
                 r=[dep], w=[K("kT", sl)], dma=K("kT", sl))
            S.op("sp", lambda e, sl=sl, t0=t0: e.dma_start(out=gaT[sl][1:17, :], in_=fmga[:, t0:t0 + 512]),
                 r=[dep, K("gaT1", sl)], w=[K("gaT", sl)], dma=K("gaT", sl))
            S.op("pool", lambda e, sl=sl, t0=t0: e.dma_start(out=ktm[sl][:], in_=tmk[t0:t0 + 512, :].rearrange("(j p) c -> p j c", p=128)),
                 r=[dep], w=[K("ktm", sl)], dma=K("ktm", sl))
            S.op("pool", lambda e, sl=sl, t0=t0: e.dma_start(out=vtm[sl][:], in_=tmv[t0:t0 + 512, :].rearrange("(j p) c -> p j c", p=128)),
                 r=[dep], w=[K("vtm", sl)], dma=K("vtm", sl))
            S.op("pool", lambda e, sl=sl, t0=t0: e.dma_start(out=gz[sl][:], in_=tmgz[t0:t0 + 512, :].rearrange("(j p) c -> p j c", p=128)),
                 r=[dep], w=[K("gz", sl)], dma=K("gz", sl))
        c0 = j * 128
        S.op("pe", lambda e, sl=sl, c0=c0: e.matmul(P_lg[:, 0:256], lhsT=gaT[sl][0:17, c0:c0 + 128], rhs=wa[0:17, :], start=True, stop=True),
             r=[K("gaT", sl), K("wa")], w=[K("P_lg")])
        S.op("act", lambda e: e.activation(out=e1[:], in_=P_lg[:, 0:256], func=AF.Exp, scale=-1.0), r=[K("P_lg")], w=[K("e1")])
        S.op("act", lambda e: e.activation(out=la[:], in_=e1[:], func=AF.Ln, bias=1.0), r=[K("e1")], w=[K("la")])
        def mm_bc(e):
            e.matmul(P_bc[:, 0:256], lhsT=tri2, rhs=la[:], start=True, stop=True)
            e.matmul(P_bT[:, 0:128], lhsT=la[:, 0:128], rhs=tri, start=True, stop=True)
            e.matmul(P_bT[:, 128:256], lhsT=la[:, 128:256], rhs=tri, start=True, stop=True)
            e.matmul(P_bT[:, 256:257], lhsT=la[:, 0:128], rhs=sel, start=True, stop=True)
            return e.matmul(P_bT[:, 257:258], lhsT=la[:, 128:256], rhs=sel, start=True, stop=True)
        S.op("pe", mm_bc, r=[K("la"), K("gc")], w=[K("P_bc"), K("P_bT")])
        S.op("act", lambda e: e.activation(out=Eq[:], in_=P_bT[:, 0:256].rearrange("p (b c) -> p b c", b=2), func=AF.Exp),
             r=[K("P_bT")], w=[K("Eq")])
        S.op("act", lambda e: e.activation(out=Ek[:], in_=P_bT[:, 0:256].rearrange("p (b c) -> p b c", b=2), func=AF.Exp, scale=-1.0),
             r=[K("P_bT")], w=[K("Ek")])
        S.op("act", lambda e: e.activation(out=dec[:], in_=P_bT[:, 256:258], func=AF.Exp), r=[K("P_bT")], w=[K("dec")])
        S.op("act", lambda e: e.activation(out=Eend[:], in_=P_bc[:, 0:256], func=AF.Exp), r=[K("P_bc")], w=[K("Eend")])
        S.op("dve", lambda e, sl=sl, c0=c0: e.scalar_tensor_tensor(out=qd[:], in0=qT[sl][:, :, c0:c0 + 128], scalar=1.0 / 16.0, in1=Eq[:],
                                                                   op0=ALU.mult, op1=ALU.mult),
             r=[K("qT", sl), K("Eq")], w=[K("qd")])
        S.op("dve", lambda e, sl=sl, c0=c0: e.tensor_tensor(out=kd[:], in0=kT[sl][:, :, c0:c0 + 128], in1=Ek[:], op=ALU.mult),
             r=[K("kT", sl), K("Ek")], w=[K("kd")])
        S.op("pool", lambda e, sl=sl, j=j: e.tensor_tensor(out=kend[:], in0=ktm[sl][:, j, :], in1=Eend[:], op=ALU.mult),
             r=[K("ktm", sl), K("Eend")], w=[K("kend")])
        def mm_att(e):
            e.matmul(P_att[:, 0:128], lhsT=kd[:, 0, :], rhs=qd[:, 0, :], start=True, stop=False)
            return e.matmul(P_att[:, 0:128], lhsT=kd[:, 1, :], rhs=qd[:, 1, :], start=False, stop=True)
        S.op("pe", mm_att, r=[K("kd"), K("qd")], w=[K("P_att")])
        S.op("dve", lambda e: e.tensor_tensor(out=att[:], in0=P_att[:, 0:128], in1=msk, op=ALU.mult), r=[K("P_att"), K("gc")], w=[K("att")])
        def mm_o(e, sl=sl, j=j):
            e.matmul(P_o[:], lhsT=att[:], rhs=vtm[sl][:, j, :], start=True, stop=False)
            e.matmul(P_o[:], lhsT=qd[:, 0, :], rhs=Sbf[:, 0, :], start=False, stop=False)
            return e.matmul(P_o[:], lhsT=qd[:, 1, :], rhs=Sbf[:, 1, :], start=False, stop=True)
        S.op("pe", mm_o, r=[K("att"), K("vtm", sl), K("qd"), K("Sbf")], w=[K("P_o")])
        for b in range(2):
            S.op("pe", lambda e, b=b, sl=sl, j=j: e.matmul(P_kv[b][:], lhsT=kend[:, b * 128:(b + 1) * 128], rhs=vtm[sl][:, j, :], start=True, stop=True),
                 r=[K("kend"), K("vtm", sl)], w=[K("P_kv", b)])
            S.op("dve", lambda e, b=b: e.scalar_tensor_tensor(out=S32[:, b, :], in0=S32[:, b, :], scalar=dec[:, b:b + 1], in1=P_kv[b][:],
                                                              op0=ALU.mult, op1=ALU.add),
                 r=[K("P_kv", b), K("dec"), K("S32")], w=[K("S32")])
        S.op("act", lambda e: e.copy(out=Sbf[:], in_=S32[:]), r=[K("S32")], w=[K("Sbf")])
        S.op("act", lambda e: e.activation(out=sq[:], in_=P_o[:], func=AF.Square, accum_out=ss[:]), r=[K("P_o")], w=[K("sq"), K("ss")])
        S.op("act", lambda e: e.activation(out=rstd[:], in_=ss[:], func=AF.Sqrt, scale=1.0 / 512.0, bias=eps_t[:, 0:1]), r=[K("ss"), K("eps")], w=[K("rstd")])
        S.op("dve", lambda e: e.reciprocal(out=rstd[:], in_=rstd[:]), r=[K("rstd")], w=[K("rstd")])
        S.op("act", lambda e, sl=sl, j=j: e.activation(out=Gz[:], in_=gz[sl][:, j, :], func=AF.Silu), r=[K("gz", sl)], w=[K("Gz")])
        S.op("pool", lambda e: e.tensor_tensor(out=Gz[:], in0=Gz[:], in1=ng[:], op=ALU.mult), r=[K("Gz"), K("ng")], w=[K("Gz")])
        S.op("dve", lambda e: e.scalar_tensor_tensor(out=actt[:], in0=P_o[:], scalar=rstd[:, 0:1], in1=Gz[:], op0=ALU.mult, op1=ALU.mult),
             r=[K("P_o"), K("rstd"), K("Gz")], w=[K("actt")])
        def mm_tr(e):
            ins = None
            for b in range(4):
                ins = e.transpose(P_tr[:, b * 128:(b + 1) * 128], actt[:, b * 128:(b + 1) * 128], idn[:])
            return ins
        S.op("pe", mm_tr, r=[K("actt"), K("idn")], w=[K("P_tr")])
        S.op("act", lambda e, sl=sl, c0=c0: e.copy(out=oT[sl][:, :, c0:c0 + 128], in_=P_tr[:, 0:512].rearrange("p (b c) -> p b c", b=4)),
             r=[K("P_tr")], w=[K("oT", sl)])
        if j == 3:
            k = K("out", sup)
            S.op("sp", lambda e, sl=sl, t0=t0: e.dma_start(out=outT[:, t0:t0 + 512].rearrange("(b p) t -> p b t", p=128), in_=oT[sl][:]),
                 r=[K("oT", sl)], w=[k], dma=K("oT", sl))
            keys.append(k)
    return keys


def finish_tile(S, nc, Kf, P_tr, actt_key, actt, idn, idn_key, oT, sl, j, outT, t0, keys, tag, stq="sp"):
    def mm_tr(e):
        ins = None
        for b in range(4):
            ins = e.transpose(P_tr[:, b * 128:(b + 1) * 128], actt[:, b * 128:(b + 1) * 128], idn[:])
        return ins
    S.op("pe", mm_tr, r=[actt_key, idn_key], w=[Kf("P_tr")])
    c0 = j * 128
    S.op("act", lambda e: e.copy(out=oT[sl][:, :, c0:c0 + 128], in_=P_tr[:, 0:512].rearrange("p (b c) -> p b c", b=4)),
         r=[Kf("P_tr")], w=[Kf("oT", sl)])
    if j == 3:
        k = Kf("out", t0)
        S.op(stq, lambda e: e.dma_start(out=outT[:, t0:t0 + 512].rearrange("(b p) t -> p b t", p=128), in_=oT[sl][:]),
             r=[Kf("oT", sl)], w=[k], dma=Kf("oT", sl))
        keys.append(k)


def phase_mem(S, nc, st, SEQ, fmmq, tmmz, memT_bf, wk_bf, wv_bf, ident, outT, dep, wdep):
    def sb(name, shape, dt):
        return st.enter_context(nc.sbuf_tensor("mem_" + name, shape, dt))

    def psum(name, shape, dt=F32):
        return st.enter_context(nc.psum_tensor("mem_" + name, shape, dt))
    K = lambda *a: ("mem",) + a
    mT = sb("mT", [128, 32, 256], BF16)
    wkv = sb("wkv", [128, 32, 512], BF16)
    kT = sb("kT", [128, 4, 256], BF16)
    vv = sb("vv", [128, 2, 512], BF16)
    ones = sb("ones", [128, 1], BF16)
    idn = sb("idn", [128, 128], BF16)
    qT = [sb("qT%d" % i, [128, 4, 512], BF16) for i in range(2)]
    mz = [sb("mz%d" % i, [128, 4, 512], BF16) for i in range(2)]
    pT = sb("pT", [128, 2, 512], BF16)
    sg = sb("sg", [128, 512], F32)
    rz = sb("rz", [128, 1], F32)
    actt = sb("actt", [128, 512], BF16)
    oT = [sb("oT%d" % i, [128, 4, 512], BF16) for i in range(2)]
    P_s = [psum("P_s%d" % i, [128, 512]) for i in range(2)]
    P_o = [psum("P_o%d" % i, [128, 512]) for i in range(2)]
    P_z = psum("P_z", [128, 512])
    P_tr = psum("P_tr", [128, 1024], BF16)
    S.op("sp", lambda e: e.dma_start(out=idn[:], in_=ident), w=[K("idn")], dma=K("idn"))
    S.op("dve", lambda e: e.memset(ones[:], 1.0), w=[K("ones")])
    S.op("sp", lambda e: e.dma_start(out=mT[:], in_=memT_bf.rearrange("(kb p) m -> p kb m", p=128)), r=[wdep], w=[K("mT")], dma=K("mT"))
    S.op("sp", lambda e: e.dma_start(out=wkv[:], in_=wk_bf.rearrange("(kb p) n -> p kb n", p=128)), r=[wdep], w=[K("wkv")], dma=K("wkv"))
    for db in range(4):
        def mmk(e, db=db):
            ins = None
            for kb in range(32):
                ins = e.matmul(P_s[db % 2][:, 0:256], lhsT=wkv[:, kb, db * 128:(db + 1) * 128], rhs=mT[:, kb, :], start=(kb == 0), stop=(kb == 31))
            return ins
        S.op("pe", mmk, r=[K("wkv"), K("mT")], w=[K("P_s", db % 2)])
        S.op("act", lambda e, db=db: e.activation(out=kT[:, db, :], in_=P_s[db % 2][:, 0:256], func=AF.Copy, scale=512.0 ** -0.5),
             r=[K("P_s", db % 2)], w=[K("kT")])
    S.op("sp", lambda e: e.dma_start(out=wkv[:], in_=wv_bf.rearrange("(kb p) n -> p kb n", p=128)), r=[wdep], w=[K("wkv")], dma=K("wkv"))
    for mt in range(2):
        def mmv(e, mt=mt):
            ins = None
            for kb in range(32):
                ins = e.matmul(P_o[mt][:], lhsT=mT[:, kb, mt * 128:(mt + 1) * 128], rhs=wkv[:, kb, :], start=(kb == 0), stop=(kb == 31))
            return ins
        S.op("pe", mmv, r=[K("wkv"), K("mT")], w=[K("P_o", mt)])
        S.op("act", lambda e, mt=mt: e.copy(out=vv[:, mt, :], in_=P_o[mt][:]), r=[K("P_o", mt)], w=[K("vv")])
    keys = []
    for sup in range(SEQ // 512):
        sl = sup % 2
        t0 = sup * 512
        S.op("sp", lambda e, sl=sl, t0=t0: e.dma_start(out=qT[sl][:], in_=fmmq[:, t0:t0 + 512].rearrange("(b p) t -> p b t", p=128)),
             r=[dep], w=[K("qT", sl)], dma=K("qT", sl))
        S.op("pool", lambda e, sl=sl, t0=t0: e.dma_start(out=mz[sl][:], in_=tmmz[t0:t0 + 512, :].rearrange("(j p) c -> p j c", p=128)),
             r=[dep], w=[K("mz", sl)], dma=K("mz", sl))
        for mt in range(2):
            def mms(e, mt=mt, sl=sl):
                ins = None
                for db in range(4):
                    ins = e.matmul(P_s[mt][:], lhsT=kT[:, db, mt * 128:(mt + 1) * 128], rhs=qT[sl][:, db, :], start=(db == 0), stop=(db == 3))
                return ins
            S.op("pe", mms, r=[K("kT"), K("qT", sl)], w=[K("P_s", mt)])
            S.op("act", lambda e, mt=mt: e.activation(out=pT[:, mt, :], in_=P_s[mt][:], func=AF.Exp), r=[K("P_s", mt)], w=[K("pT", mt)])
        for j in range(4):
            pj = j % 2
            def mmo(e, j=j, pj=pj):
                e.matmul(P_o[pj][:], lhsT=pT[:, 0, j * 128:(j + 1) * 128], rhs=vv[:, 0, :], start=True, stop=False)
                e.matmul(P_o[pj][:], lhsT=pT[:, 1, j * 128:(j + 1) * 128], rhs=vv[:, 1, :], start=False, stop=True)
                e.matmul(P_z[:, j:j + 1], lhsT=pT[:, 0, j * 128:(j + 1) * 128], rhs=ones[:], start=True, stop=False)
                return e.matmul(P_z[:, j:j + 1], lhsT=pT[:, 1, j * 128:(j + 1) * 128], rhs=ones[:], start=False, stop=True)
            S.op("pe", mmo, r=[K("pT", 0), K("pT", 1), K("vv"), K("ones")], w=[K("P_o", pj), K("P_z")])
            S.op("dve", lambda e, j=j: e.reciprocal(out=rz[:], in_=P_z[:, j:j + 1]), r=[K("P_z")], w=[K("rz")])
            S.op("act", lambda e, j=j, sl=sl: e.activation(out=sg[:], in_=mz[sl][:, j, :], func=AF.Silu), r=[K("mz", sl)], w=[K("sg")])
            S.op("dve", lambda e, pj=pj: e.scalar_tensor_tensor(out=actt[:], in0=P_o[pj][:], scalar=rz[:, 0:1], in1=sg[:], op0=ALU.mult, op1=ALU.mult),
                 r=[K("P_o", pj), K("rz"), K("sg")], w=[K("actt")])
            finish_tile(S, nc, K, P_tr, K("actt"), actt, idn, K("idn"), oT, sl, j, outT, t0, keys, "mem")
    return keys


def nsa_consts(SEQ, g):
    NT = SEQ // 128
    NSEL = SEQ // 64
    NCP = ((SEQ // 16 - 1 + 127) // 128) * 128
    NCT = NCP // 128
    slopes = (2.0 ** (-8.0 * (np.arange(16) + 1.0) / 16))[4 * g:4 * g + 4].astype(np.float64)
    nrel = np.arange(128)[:, None]
    m = np.arange(NCT)[None, :, None]
    i = np.arange(NT)[None, None, :]
    n = 128 * m + nrel[:, :, None]
    arg = 16 * n + 31 - 128 * i - 64
    fut = (16 * n + 31) > (128 * i + 127)
    biasc = np.stack([np.where(fut | (s * arg < -70.0), -30000.0, s * arg) for s in slopes], axis=1)
    biasc = biasc.reshape(128, 4 * NCT * NT).astype(np.float32)
    D = np.arange(NT)[None, :]
    bs = np.stack([np.where(s * (nrel - 64 - 128 * D) < -70.0, -30000.0, s * (nrel - 64 - 128 * D)) for s in slopes], axis=1)
    bs = bs.reshape(128, 4 * NT).astype(np.float32)
    trel = np.arange(128)[None, :]
    masks = []
    for k in range(16):
        masks.append(((16 * (nrel - 8 * k) + 31) <= trel))
    masks.append(((16 * (nrel - 128) + 31) <= trel))
    masks.append(nrel <= trel)
    masks.append(nrel > trel)
    maskc = np.concatenate(masks, axis=1).astype(np.float32).astype(NPBF)
    nn = np.arange(NCP)[:, None]
    jj = np.arange(NSEL)[None, :]
    ov = ((16 * nn < 64 * jj + 64) & (16 * nn + 31 >= 64 * jj) & (nn < SEQ // 16 - 1)).astype(np.float32)
    ovl = np.concatenate([ov, np.ones((NCP, 1), np.float32), np.zeros((NCP, 1), np.float32)], axis=1).astype(NPBF)
    ovl = np.ascontiguousarray(ovl.reshape(NCT, 128, NSEL + 2).transpose(1, 0, 2))
    E = np.zeros((128, SEQ), np.float32)
    kk = np.arange(SEQ)
    if NSEL <= 128:
        E[kk // 64, kk] = 1.0
    E = E.astype(NPBF)
    t = (128 * np.arange(NT)[:, None] + np.arange(128)[None, :])[:, :, None]
    jb = np.arange(NSEL)[None, None, :]
    cur = t // 64
    causal = (jb <= cur).astype(np.float32)
    forced = ((jb == 0) | (jb == cur) | (jb == cur - 1)).astype(np.float32)
    add = (causal - 1.0) + 1e4 * forced
    tk = np.stack([causal, add], axis=2).astype(np.float32)
    return dict(biasc=biasc, bs=bs, maskc=maskc, ovl=ovl, E=E, tk=tk)


def phase_nsa(S, nc, st, SEQ, fmq, fmkc, fmvc, fmks, fmkw, tmvs, tmvw, tmnz, tmbg,
              w1k, w1v, w2k, w2v, pekT, pevT, cst, ident, outT, dep, dbg=None):
    def sb(name, shape, dt):
        return st.enter_context(nc.sbuf_tensor("nsa_" + name, shape, dt))

    def psum(name, shape, dt=F32):
        return st.enter_context(nc.psum_tensor("nsa_" + name, shape, dt))
    K = lambda *a: ("nsa",) + a
    NT = SEQ // 128
    NSEL = SEQ // 64
    NC = SEQ // 16 - 1
    NCT = (NC + 127) // 128
    NCP = NCT * 128
    WR = 128 + NSEL + 1
    SC = 128.0 ** -0.5
    assert NSEL <= 128
    idn = sb("idn", [128, 128], BF16)
    ksT = sb("ksT", [128, SEQ], BF16)
    kwT = sb("kwT", [128, SEQ], BF16)
    kcT = sb("kcT", [128, SEQ], BF16)
    vsa = sb("vsa", [128, NT, 130], BF16)
    vwa = sb("vwa", [128, NT, 130], BF16)
    Emat = sb("E", [128, SEQ], BF16)
    kcmpT = sb("kcmpT", [128, NCP], BF16)
    Rc = sb("Rc", [128, NCT, WR + 1], BF16)
    biasc = sb("biasc", [128, 4 * NCT * NT], F32)
    bs = sb("bs", [128, 4 * NT], F32)
    maskc = sb("maskc", [128, 19 * 128], BF16)
    w1f = sb("w1f", [128, 32, 128], F32)
    w1 = sb("w1", [128, 32, 128], BF16)
    w2f = sb("w2f", [128, 128], F32)
    w2 = sb("w2", [128, 128], BF16)
    pef = sb("pef", [128, 32], F32)
    peb = sb("peb", [128, 32], BF16)
    cb = sb("cb", [128, 1], F32)
    hc = sb("hc", [128, NCP], BF16)
    S.op("sp", lambda e: e.dma_start(out=idn[:], in_=ident), w=[K("idn")], dma=K("idn"))
    S.op("sp", lambda e: e.dma_start(out=ksT[:], in_=fmks), r=[dep], w=[K("ksT")], dma=K("ksT"))
    S.op("sp", lambda e: e.dma_start(out=kwT[:], in_=fmkw), r=[dep], w=[K("kwT")], dma=K("kwT"))
    S.op("pool", lambda e: e.dma_start(out=Emat[:], in_=cst["E"]), w=[K("E")], dma=K("E"))
    S.op("pool", lambda e: e.dma_start(out=biasc[:], in_=cst["biasc"]), w=[K("biasc")], dma=K("biasc"))
    S.op("pool", lambda e: e.dma_start(out=bs[:], in_=cst["bs"]), w=[K("bs")], dma=K("bs"))
    S.op("pool", lambda e: e.dma_start(out=maskc[:], in_=cst["maskc"]), w=[K("maskc")], dma=K("maskc"))
    S.op("dve", lambda e: e.memset(vsa[:], 1.0), w=[K("vsa")])
    S.op("dve", lambda e: e.memset(vwa[:], 1.0), w=[K("vwa")])
    S.op("sp", lambda e: e.dma_start(out=vsa[:, :, 0:128], in_=tmvs.rearrange("(j p) c -> p j c", p=128)), r=[dep, K("vsa")], w=[K("vsa")], dma=K("vsa"))
    S.op("sp", lambda e: e.dma_start(out=vwa[:, :, 0:128], in_=tmvw.rearrange("(j p) c -> p j c", p=128)), r=[dep, K("vwa")], w=[K("vwa")], dma=K("vwa"))
    S.op("dve", lambda e: e.memset(kcmpT[:], 0.0), w=[K("kcmpT")])
    S.op("dve", lambda e: e.memset(Rc[:], 0.0), w=[K("Rc")])
    S.op("dve", lambda e: e.memset(hc[:], 0.0), w=[K("hc")])
    S.op("pool", lambda e: e.dma_start(out=Rc[:, :, 128:WR + 1], in_=cst["ovl"]), r=[K("Rc")], w=[K("Rc")], dma=K("Rc"))
    P_big = [psum("P_big%d" % i, [128, 512]) for i in range(2)]
    P_sc = [psum("P_sc%d" % i, [128, 512]) for i in range(3)]
    P_sw = [psum("P_sw%d" % i, [128, 512]) for i in range(2)]
    P_trm = psum("P_trm", [128, 1024], BF16)
    P_tr = P_trm[:, 0:512]
    P_mT = P_trm[:, 512:1024]
    for which, (fmx, w1d, w2d, ped) in enumerate(((fmkc, w1k, w2k, pekT), (fmvc, w1v, w2v, pevT))):
        S.op("sp", lambda e, fmx=fmx: e.dma_start(out=kcT[:], in_=fmx), r=[dep], w=[K("kcT")], dma=K("kcT"))
        S.op("sp", lambda e, w1d=w1d: e.dma_start(out=w1f[:], in_=w1d.rearrange("(l d) o -> d l o", d=128)), w=[K("w1f")], dma=K("w1f"))
        S.op("sp", lambda e, w2d=w2d: e.dma_start(out=w2f[:], in_=w2d), w=[K("w2f")], dma=K("w2f"))
        S.op("sp", lambda e, ped=ped: e.dma_start(out=pef[:], in_=ped), w=[K("pef")], dma=K("pef"))
        S.op("dve", lambda e: e.tensor_copy(out=w1[:], in_=w1f[:]), r=[K("w1f")], w=[K("w1")])
        S.op("dve", lambda e: e.tensor_copy(out=w2[:], in_=w2f[:]), r=[K("w2f")], w=[K("w2")])
        S.op("dve", lambda e: e.tensor_copy(out=peb[:], in_=pef[:]), r=[K("pef")], w=[K("peb")])
        def mm_c(e):
            ins = None
            for l in range(32):
                ins = e.matmul(P_big[1][:, 0:1], lhsT=w1[:, l, :], rhs=peb[:, l:l + 1], start=(l == 0), stop=(l == 31))
            return ins
        S.op("pe", mm_c, r=[K("w1"), K("peb")], w=[K("P_big", 1)])
        S.op("act", lambda e: e.copy(out=cb[:], in_=P_big[1][:, 0:1]), r=[K("P_big", 1)], w=[K("cb")])
        def mm_pre(e):
            ins = None
            for l in range(32):
                ins = e.matmul(P_big[0][:, 0:NC], lhsT=w1[:, l, :], rhs=kcT[:, l:l + 16 * (NC - 1) + 1:16], start=(l == 0), stop=(l == 31))
            return ins
        S.op("pe", mm_pre, r=[K("w1"), K("kcT")], w=[K("P_big", 0)])
        S.op("act", lambda e: e.activation(out=hc[:, 0:NC], in_=P_big[0][:, 0:NC], func=AF.Silu, bias=cb[:, 0:1]), r=[K("P_big", 0), K("cb")], w=[K("hc")])
        if which == 0:
            S.op("pe", lambda e: e.matmul(P_big[0][:, 0:NC], lhsT=w2[:], rhs=hc[:, 0:NC], start=True, stop=True), r=[K("w2"), K("hc")], w=[K("P_big", 0)])
            S.op("act", lambda e: e.copy(out=kcmpT[:, 0:NC], in_=P_big[0][:, 0:NC]), r=[K("P_big", 0)], w=[K("kcmpT")])
        else:
            for nt in range(NCT):
                S.op("pe", lambda e, nt=nt: e.matmul(P_big[nt % 2][:, 0:128], lhsT=hc[:, nt * 128:(nt + 1) * 128], rhs=w2[:], start=True, stop=True),
                     r=[K("w2"), K("hc")], w=[K("P_big", nt % 2)])
                S.op("act", lambda e, nt=nt: e.copy(out=Rc[:, nt, 0:128], in_=P_big[nt % 2][:, 0:128]), r=[K("P_big", nt % 2)], w=[K("Rc")])
    qT = [sb("qT%d" % i, [128, 4, 128], BF16) for i in range(2)]
    nz = [sb("nz%d" % i, [128, 512], BF16) for i in range(2)]
    bgl = [sb("bg%d" % i, [128, 12], BF16) for i in range(2)]
    tkc = [sb("tk%d" % i, [128, 2, NSEL], F32) for i in range(2)]
    gates = sb("gates", [128, 12], F32)
    pT = [sb("pT%d" % i, [128, 128], BF16) for i in range(6)]
    zc = sb("zc", [128, 4], F32)
    imp = sb("imp", [128, NSEL], F32)
    score = sb("score", [128, NSEL], F32)
    score2 = sb("score2", [128, NSEL], F32)
    mx8 = sb("mx8", [128, 8], F32)
    mx8b = sb("mx8b", [128, 8], F32)
    msel = sb("msel", [128, 128], BF16)
    negm = sb("negm", [128, 128], BF16)
    ocmp = sb("ocmp", [128, 4, 128], F32)
    cf = sb("cf", [128, 8], F32)
    tmp = sb("tmp", [128, 128], F32)
    sg = sb("sg", [128, 512], F32)
    actt = sb("actt", [128, 512], BF16)
    oT = [sb("oT%d" % i, [128, 4, 512], BF16) for i in range(2)]
    S.op("dve", lambda e: e.memset(msel[:], 0.0), w=[K("msel")])
    dbgt = sb("dbgt", [128, 512], F32) if dbg is not None else None
    keys = []
    pcount = [0]
    sccount = [0]

    def score_exp(lhsT_fn, lk, r, sl, bias_ap, bias_key, mask_ap, extra=None):
        si = sccount[0] % 3
        sccount[0] += 1
        ps = P_sc[si][:, 0:128]
        pk = K("P_sc", si)

        def mm(e):
            ins = e.matmul(ps, lhsT=lhsT_fn(), rhs=qT[sl][:, r, :], start=True, stop=(extra is None))
            if extra is not None:
                ins = e.matmul(ps, lhsT=extra(), rhs=negm[:], start=False, stop=True)
            return ins
        S.op("pe", mm, r=list(lk) + [K("qT", sl)] + ([K("negm"), K("E")] if extra is not None else []), w=[pk])
        pi = pcount[0] % 6
        pcount[0] += 1
        S.op("act", lambda e: e.activation(out=pT[pi][:], in_=ps, func=AF.Exp, scale=SC, bias=bias_ap), r=[pk, bias_key], w=[K("pT", pi)])
        if mask_ap is not None:
            S.op("dve", lambda e: e.tensor_tensor(out=pT[pi][:], in0=pT[pi][:], in1=mask_ap, op=ALU.mult), r=[K("pT", pi), K("maskc")], w=[K("pT", pi)])
        return pi

    for i in range(NT):
        sl = i % 2
        t0 = i * 128
        sup, j4 = divmod(i, 4)
        osl = sup % 2
        S.op("sp", lambda e, sl=sl, t0=t0: e.dma_start(out=qT[sl][:], in_=fmq[:, t0:t0 + 128].rearrange("(b p) t -> p b t", p=128)),
             r=[dep], w=[K("qT", sl)], dma=K("qT", sl))
        S.op("sp", lambda e, sl=sl, t0=t0: e.dma_start(out=nz[sl][:], in_=tmnz[t0:t0 + 128, :]), r=[dep], w=[K("nz", sl)], dma=K("nz", sl))
        S.op("sp", lambda e, sl=sl, t0=t0: e.dma_start(out=bgl[sl][:], in_=tmbg[t0:t0 + 128, :]), r=[dep], w=[K("bg", sl)], dma=K("bg", sl))
        S.op("pool", lambda e, sl=sl, i=i: e.dma_start(out=tkc[sl][:], in_=cst["tk"][i]), w=[K("tk", sl)], dma=K("tk", sl))
        S.op("act", lambda e, sl=sl: e.activation(out=gates[:], in_=bgl[sl][:], func=AF.Sigmoid), r=[K("bg", sl)], w=[K("gates")])
        mb = min((8 * i + 6) // 128, NCT - 1)
        for r in range(4):
            pis = []
            for m in range(mb + 1):
                mask_ap = None
                if m == mb:
                    kq = i % 16
                    mask_ap = maskc[:, kq * 128:(kq + 1) * 128]
                elif m == mb - 1 and i % 16 == 0:
                    mask_ap = maskc[:, 16 * 128:17 * 128]
                bcol = (r * NCT + m) * NT + i
                pi = score_exp(lambda m=m: kcmpT[:, m * 128:(m + 1) * 128], [K("kcmpT")], r, sl, biasc[:, bcol:bcol + 1], K("biasc"), mask_ap)
                pis.append((m, pi))
            pb = P_big[r % 2]

            def mm_pv(e, pis=pis, pb=pb):
                ins = None
                for q, (m, pi) in enumerate(pis):
                    ins = e.matmul(pb[:, 0:WR], lhsT=pT[pi][:], rhs=Rc[:, m, 0:WR], start=(q == 0), stop=(q == len(pis) - 1))
                return ins
            S.op("pe", mm_pv, r=[K("pT", pi) for _, pi in pis] + [K("Rc")], w=[K("P_big", r % 2)])
            S.op("dve", lambda e, r=r, pb=pb: e.tensor_scalar(out=zc[:, r:r + 1], in0=pb[:, WR - 1:WR], scalar1=1e-30, scalar2=None, op0=ALU.add),
                 r=[K("P_big", r % 2)], w=[K("zc", r)])
            S.op("dve", lambda e, r=r: e.reciprocal(out=zc[:, r:r + 1], in_=zc[:, r:r + 1]), r=[K("zc", r)], w=[K("zc", r)])
            if r == 0:
                S.op("dve", lambda e, pb=pb: e.tensor_scalar(out=imp[:], in0=pb[:, 128:128 + NSEL], scalar1=zc[:, 0:1], scalar2=None, op0=ALU.mult),
                     r=[K("P_big", 0), K("zc", 0)], w=[K("imp")])
            else:
                S.op("dve", lambda e, r=r, pb=pb: e.scalar_tensor_tensor(out=imp[:], in0=pb[:, 128:128 + NSEL], scalar=zc[:, r:r + 1], in1=imp[:], op0=ALU.mult, op1=ALU.add),
                     r=[K("P_big", r % 2), K("zc", r), K("imp")], w=[K("imp")])
            S.op("dve", lambda e, r=r: e.tensor_tensor(out=cf[:, r:r + 1], in0=zc[:, r:r + 1], in1=gates[:, 3 * r:3 * r + 1], op=ALU.mult),
                 r=[K("zc", r), K("gates")], w=[K("cf", r)])
            S.op("act", lambda e, r=r, pb=pb: e.activation(out=ocmp[:, r, :], in_=pb[:, 0:128], func=AF.Copy, scale=cf[:, r:r + 1]),
                 r=[K("P_big", r % 2), K("cf", r)], w=[K("ocmp", r)])
        S.op("dve", lambda e, sl=sl: e.tensor_tensor(out=score[:], in0=imp[:], in1=tkc[sl][:, 0, :], op=ALU.mult), r=[K("imp"), K("tk", sl)], w=[K("score")])
        S.op("dve", lambda e, sl=sl: e.tensor_tensor(out=score[:], in0=score[:], in1=tkc[sl][:, 1, :], op=ALU.add), r=[K("score"), K("tk", sl)], w=[K("score")])
        if NSEL > 16:
            S.op("dve", lambda e: e.max(out=mx8[:], in_=score[:]), r=[K("score")], w=[K("mx8")])
            S.op("dve", lambda e: e.match_replace(out=score2[:], in_to_replace=mx8[:], in_values=score[:], imm_value=-1e30), r=[K("mx8"), K("score")], w=[K("score2")])
            S.op("dve", lambda e: e.max(out=mx8b[:], in_=score2[:]), r=[K("score2")], w=[K("mx8b")])
            S.op("dve", lambda e: e.tensor_scalar(out=msel[:, 0:NSEL], in0=score[:], scalar1=mx8b[:, 7:8], scalar2=None, op0=ALU.is_ge),
                 r=[K("score"), K("mx8b")], w=[K("msel")])
        else:
            S.op("dve", lambda e: e.memset(msel[:, 0:NSEL], 1.0), r=[K("score")], w=[K("msel")])
        S.op("pe", lambda e: e.transpose(P_mT[:, 0:128], msel[:], idn[:]), r=[K("msel"), K("idn")], w=[K("P_tr")])
        S.op("dve", lambda e: e.tensor_scalar(out=negm[:], in0=P_mT[:, 0:128], scalar1=-1.0, scalar2=30000.0 / SC, op0=ALU.add, op1=ALU.mult),
             r=[K("P_tr")], w=[K("negm")])
        if dbg is not None:
            if "imp" in dbg:
                S.op("pool", lambda e, i=i: e.dma_start(out=dbg["imp"][i], in_=imp[:]), r=[K("imp")], w=[K("dbg", "imp", i)], dma=K("dbg1"))
            if "msel" in dbg:
                S.op("pool", lambda e, i=i: e.dma_start(out=dbg["msel"][i], in_=msel[:]), r=[K("msel")], w=[K("dbg", "msel", i)], dma=K("dbg2"))
            if "ocmp" in dbg:
                S.op("pool", lambda e, i=i: e.dma_start(out=dbg["ocmp"][i], in_=ocmp[:]), r=[K("ocmp", r) for r in range(4)], w=[K("dbg", "ocmp", i)], dma=K("dbg3"))
            if "zc" in dbg:
                S.op("pool", lambda e, i=i: e.dma_start(out=dbg["zc"][i], in_=zc[:]), r=[K("zc", r) for r in range(4)], w=[K("dbg", "zc", i)], dma=K("dbg4"))
            if i == 0 and "kcmpT" in dbg:
                S.op("pool", lambda e: e.dma_start(out=dbg["kcmpT"], in_=kcmpT[:]), r=[K("kcmpT")], w=[K("dbg", "kcmpT")], dma=K("dbg5"))
                S.op("pool", lambda e: e.dma_start(out=dbg["Rc"], in_=Rc[:, 0, 0:WR - 1]), r=[K("Rc")], w=[K("dbg", "Rc")], dma=K("dbg6"))
        S.op("act", lambda e, sl=sl: e.activation(out=sg[:], in_=nz[sl][:], func=AF.Silu), r=[K("nz", sl)], w=[K("sg")])
        for r in range(4):
            psw = P_sw[r % 2]
            pis = []
            for J in range(i + 1):
                mask_ap = maskc[:, 17 * 128:18 * 128] if J == i else None
                bcol = r * NT + (i - J)
                pi = score_exp(lambda J=J: ksT[:, J * 128:(J + 1) * 128], [K("ksT")], r, sl, bs[:, bcol:bcol + 1], K("bs"), mask_ap,
                               extra=lambda J=J: Emat[:, J * 128:(J + 1) * 128])
                S.op("pe", lambda e, J=J, pi=pi, psw=psw, i=i: e.matmul(psw[:, 0:129], lhsT=pT[pi][:], rhs=vsa[:, J, 0:129], start=(J == 0), stop=(J == i)),
                     r=[K("pT", pi), K("vsa")], w=[K("P_sw", r % 2)])
            J0 = max(0, i - 4)
            for J in range(J0, i + 1):
                mask_ap = None
                if J == i:
                    mask_ap = maskc[:, 17 * 128:18 * 128]
                elif J == i - 4:
                    mask_ap = maskc[:, 18 * 128:19 * 128]
                bcol = r * NT + (i - J)
                pi = score_exp(lambda J=J: kwT[:, J * 128:(J + 1) * 128], [K("kwT")], r, sl, bs[:, bcol:bcol + 1], K("bs"), mask_ap)
                S.op("pe", lambda e, J=J, pi=pi, psw=psw, i=i, J0=J0: e.matmul(psw[:, 256:385], lhsT=pT[pi][:], rhs=vwa[:, J, 0:129], start=(J == J0), stop=(J == i)),
                     r=[K("pT", pi), K("vwa")], w=[K("P_sw", r % 2)])
            S.op("dve", lambda e, r=r, psw=psw: e.reciprocal(out=cf[:, 4:5], in_=psw[:, 128:129]), r=[K("P_sw", r % 2)], w=[K("cf4")])
            S.op("dve", lambda e, r=r: e.tensor_tensor(out=cf[:, 4:5], in0=cf[:, 4:5], in1=gates[:, 3 * r + 1:3 * r + 2], op=ALU.mult), r=[K("cf4"), K("gates")], w=[K("cf4")])
            S.op("dve", lambda e, r=r, psw=psw: e.reciprocal(out=cf[:, 5:6], in_=psw[:, 384:385]), r=[K("P_sw", r % 2)], w=[K("cf5")])
            S.op("dve", lambda e, r=r: e.tensor_tensor(out=cf[:, 5:6], in0=cf[:, 5:6], in1=gates[:, 3 * r + 2:3 * r + 3], op=ALU.mult), r=[K("cf5"), K("gates")], w=[K("cf5")])
            S.op("dve", lambda e, r=r, psw=psw: e.scalar_tensor_tensor(out=tmp[:], in0=psw[:, 0:128], scalar=cf[:, 4:5], in1=ocmp[:, r, :], op0=ALU.mult, op1=ALU.add),
                 r=[K("P_sw", r % 2), K("cf4"), K("ocmp", r)], w=[K("tmp")])
            S.op("dve", lambda e, r=r, psw=psw: e.scalar_tensor_tensor(out=tmp[:], in0=psw[:, 256:384], scalar=cf[:, 5:6], in1=tmp[:], op0=ALU.mult, op1=ALU.add),
                 r=[K("P_sw", r % 2), K("cf5"), K("tmp")], w=[K("tmp")])
            if dbg is not None and "sw" in dbg:
                S.op("act", lambda e, psw=psw: e.copy(out=dbgt[:], in_=psw[:]), r=[K("P_sw", r % 2), K("P_sw", r % 2)], w=[K("dbgt")])
                S.op("pool", lambda e, i=i, r=r: e.dma_start(out=dbg["sw"][i, r], in_=dbgt[:]), r=[K("dbgt")], w=[K("dbg", "sw", i, r)], dma=K("dbg7"))
            S.op("pool", lambda e, r=r: e.tensor_tensor(out=actt[:, r * 128:(r + 1) * 128], in0=tmp[:], in1=sg[:, r * 128:(r + 1) * 128], op=ALU.mult),
                 r=[K("tmp"), K("sg")], w=[K("actt")])
        finish_tile(S, nc, K, P_tr, K("actt"), actt, idn, K("idn"), oT, osl, j4, outT, sup * 512, keys, "nsa")
    return keys


def dram_cast(S, src, dst, tag, nchunk=8):
    R, C = src.shape
    step = (R + nchunk - 1) // nchunk
    keys = []
    for i, r0 in enumerate(range(0, R, step)):
        r1 = min(R, r0 + step)
        k = (tag, "dram", i)
        S.op("pool", lambda e, r0=r0, r1=r1: e.dma_start(out=dst[r0:r1, :], in_=src[r0:r1, :]), w=[k], dma=k)
        keys.append(k)
    return keys


def phase_out(S, nc, st, TT, xT_bf, x_tok, actT, wm_bf, wbr_bf, wout_bf, bmT, lng, lnb, out, dep, alpha):
    def sb(name, shape, dt):
        return st.enter_context(nc.sbuf_tensor("po_" + name, shape, dt))

    def psum(name, shape, dt=F32):
        return st.enter_context(nc.psum_tensor("po_" + name, shape, dt))
    K = lambda *a: ("po",) + a
    TL = 512
    xT = sb("xT", [128, 32, TL], BF16)
    act = sb("act", [128, 16, TL], BF16)
    mg = sb("mg", [128, 32, TL], BF16)
    Wm = [sb("Wm%d" % i, [128, 32, 128], BF16) for i in range(2)]
    Wb = [sb("Wb%d" % i, [128, 16, 128], BF16) for i in range(2)]
    Wo = [sb("Wo%d" % i, [128, 32, 256], BF16) for i in range(2)]
    z = sb("z", [128, 4096], F32)
    gch = [sb("gch%d" % i, [128, 1024], F32) for i in range(2)]
    bch = [sb("bch%d" % i, [128, 1024], F32) for i in range(2)]
    at = [sb("at%d" % i, [128, TL], F32) for i in range(2)]
    bm = sb("bm", [128, 96], F32)
    st6 = sb("st6", [128, 16], F32)
    junk = sb("junk", [128, 1024], F32)
    eps_t = sb("eps", [128, 1], F32)
    P_m = [psum("P_m%d" % i, [128, 512]) for i in range(2)]
    P_y = [psum("P_y%d" % i, [128, 512]) for i in range(2)]
    P_o = [psum("P_o%d" % i, [128, 512]) for i in range(4)]
    S.op("sp", lambda e: e.dma_start(out=bm[:], in_=bmT), w=[K("bm")], dma=K("bm"))
    S.op("dve", lambda e: e.memset(eps_t[:], 1e-5), w=[K("eps")])
    wmv = wm_bf.rearrange("(kb p) n -> p kb n", p=128)
    wov = wout_bf.rearrange("(kb p) n -> p kb n", p=128)
    wi = 0
    woi = 0
    gi = 0
    keys = []
    for tt in range(TT // TL):
        t0 = tt * TL
        S.op("sp", lambda e, t0=t0: e.dma_start(out=xT[:], in_=xT_bf[:, t0:t0 + TL].rearrange("(kb p) t -> p kb t", p=128)),
             r=[dep], w=[K("xT")], dma=K("xT"))
        for br in range(3):
            S.op("act", lambda e, t0=t0, br=br: e.dma_start(out=act[:], in_=actT[br][:, t0:t0 + TL].rearrange("(fb p) t -> p fb t", p=128)),
                 r=[dep], w=[K("act")], dma=K("act"))
            wbv = wbr_bf[br].rearrange("(fb p) n -> p fb n", p=128)
            for cb in range(32):
                ws = wi % 2
                wi += 1
                col = br * 4096 + cb * 128
                S.op("sp", lambda e, ws=ws, col=col: e.dma_start(out=Wm[ws][:], in_=wmv[:, :, col:col + 128]), r=[dep], w=[K("Wm", ws)], dma=K("Wm", ws))
                S.op("act", lambda e, ws=ws, cb=cb, wbv=wbv: e.dma_start(out=Wb[ws][:], in_=wbv[:, :, cb * 128:(cb + 1) * 128]), r=[dep], w=[K("Wb", ws)], dma=K("Wb", ws))

                def mm1(e, ws=ws):
                    ins = None
                    for kb in range(32):
                        ins = e.matmul(P_m[ws][:], lhsT=Wm[ws][:, kb, :], rhs=xT[:, kb, :], start=(kb == 0), stop=(kb == 31))
                    return ins
                S.op("pe", mm1, r=[K("Wm", ws), K("xT")], w=[K("P_m", ws)])

                def mm2(e, ws=ws):
                    ins = None
                    for fb in range(16):
                        ins = e.matmul(P_y[ws][:], lhsT=Wb[ws][:, fb, :], rhs=act[:, fb, :], start=(fb == 0), stop=(fb == 15))
                    return ins
                S.op("pe", mm2, r=[K("Wb", ws), K("act")], w=[K("P_y", ws)])
                bcol = br * 32 + cb
                S.op("act", lambda e, ws=ws, bcol=bcol: e.activation(out=at[ws][:], in_=P_m[ws][:], func=AF.Sigmoid, bias=bm[:, bcol:bcol + 1]),
                     r=[K("P_m", ws), K("bm")], w=[K("at", ws)])
                if br == 0:
                    S.op("dve", lambda e, ws=ws, cb=cb: e.tensor_tensor(out=mg[:, cb, :], in0=P_y[ws][:], in1=at[ws][:], op=ALU.mult),
                         r=[K("P_y", ws), K("at", ws)], w=[K("mg", cb)])
                else:
                    S.op("dve", lambda e, ws=ws: e.tensor_tensor(out=at[ws][:], in0=P_y[ws][:], in1=at[ws][:], op=ALU.mult),
                         r=[K("P_y", ws), K("at", ws)], w=[K("at", ws)])
                    S.op("pool", lambda e, ws=ws, cb=cb: e.tensor_tensor(out=mg[:, cb, :], in0=mg[:, cb, :], in1=at[ws][:], op=ALU.add),
                         r=[K("mg", cb), K("at", ws)], w=[K("mg", cb)])
        for j in range(4):
            r0 = t0 + j * 128
            S.op("sp", lambda e, r0=r0: e.dma_start(out=z[:], in_=x_tok[r0:r0 + 128, :]), w=[K("z")], dma=K("z"))
            for nb in range(16):
                wos = woi % 2
                woi += 1
                pk = woi % 4
                S.op("sp", lambda e, wos=wos, nb=nb: e.dma_start(out=Wo[wos][:], in_=wov[:, :, nb * 256:(nb + 1) * 256]), r=[dep], w=[K("Wo", wos)], dma=K("Wo", wos))

                def mm3(e, wos=wos, pk=pk, j=j):
                    ins = None
                    for cb in range(32):
                        ins = e.matmul(P_o[pk][:, 0:256], lhsT=mg[:, cb, j * 128:(j + 1) * 128], rhs=Wo[wos][:, cb, :], start=(cb == 0), stop=(cb == 31))
                    return ins
                S.op("pe", mm3, r=[K("Wo", wos)] + [K("mg", cb) for cb in range(32)], w=[K("P_o", pk)])
                S.op("dve", lambda e, pk=pk, nb=nb: e.scalar_tensor_tensor(out=z[:, nb * 256:(nb + 1) * 256], in0=z[:, nb * 256:(nb + 1) * 256], scalar=alpha,
                                                                         in1=P_o[pk][:, 0:256], op0=ALU.mult, op1=ALU.add),
                     r=[K("P_o", pk), K("z")], w=[K("z")])
            for c in range(4):
                S.op("act", lambda e, c=c: e.activation(out=junk[:], in_=z[:, c * 1024:(c + 1) * 1024], func=AF.Copy, accum_out=st6[:, c:c + 1]),
                     r=[K("z")], w=[K("junk"), K("st6", c)])
                S.op("act", lambda e, c=c: e.activation(out=junk[:], in_=z[:, c * 1024:(c + 1) * 1024], func=AF.Square, accum_out=st6[:, 4 + c:5 + c]),
                     r=[K("z")], w=[K("junk"), K("st6", 4 + c)])
            stk = [K("st6", c) for c in range(8)]
            S.op("dve", lambda e: e.tensor_reduce(out=st6[:, 8:9], in_=st6[:, 0:4], axis=AX.X, op=ALU.add), r=stk, w=[K("mean")])
            S.op("dve", lambda e: e.tensor_reduce(out=st6[:, 9:10], in_=st6[:, 4:8], axis=AX.X, op=ALU.add), r=stk, w=[K("ex2")])
            S.op("dve", lambda e: e.tensor_scalar(out=st6[:, 8:9], in0=st6[:, 8:9], scalar1=1.0 / 4096.0, scalar2=None, op0=ALU.mult), r=[K("mean")], w=[K("mean")])
            S.op("dve", lambda e: e.tensor_scalar(out=st6[:, 9:10], in0=st6[:, 9:10], scalar1=1.0 / 4096.0, scalar2=None, op0=ALU.mult), r=[K("ex2")], w=[K("ex2")])
            S.op("dve", lambda e: e.tensor_tensor(out=st6[:, 10:11], in0=st6[:, 8:9], in1=st6[:, 8:9], op=ALU.mult), r=[K("mean")], w=[K("m2")])
            S.op("dve", lambda e: e.tensor_tensor(out=st6[:, 11:12], in0=st6[:, 9:10], in1=st6[:, 10:11], op=ALU.subtract), r=[K("ex2"), K("m2")], w=[K("var")])
            S.op("act", lambda e: e.activation(out=st6[:, 12:13], in_=st6[:, 11:12], func=AF.Sqrt, bias=eps_t[:, 0:1]), r=[K("var"), K("eps")], w=[K("rstd")])
            S.op("dve", lambda e: e.reciprocal(out=st6[:, 13:14], in_=st6[:, 12:13]), r=[K("rstd")], w=[K("rstd2")])
            for c in range(4):
                gs = gi % 2
                gi += 1
                S.op("sp", lambda e, gs=gs, c=c: e.dma_start(out=gch[gs][:], in_=lng[:, c * 1024:(c + 1) * 1024]), w=[K("gch", gs)], dma=K("gch", gs))
                S.op("sp", lambda e, gs=gs, c=c: e.dma_start(out=bch[gs][:], in_=lnb[:, c * 1024:(c + 1) * 1024]), w=[K("bch", gs)], dma=K("bch", gs))
                zc = z[:, c * 1024:(c + 1) * 1024]
                S.op("dve", lambda e, zc=zc: e.tensor_scalar(out=zc, in0=zc, scalar1=st6[:, 8:9], scalar2=st6[:, 13:14], op0=ALU.subtract, op1=ALU.mult),
                     r=[K("z"), K("mean"), K("rstd2")], w=[K("z")])
                S.op("pool", lambda e, zc=zc, gs=gs: e.tensor_tensor(out=zc, in0=zc, in1=gch[gs][:], op=ALU.mult), r=[K("z"), K("gch", gs)], w=[K("z")])
                S.op("dve", lambda e, zc=zc, gs=gs: e.tensor_tensor(out=zc, in0=zc, in1=bch[gs][:], op=ALU.add), r=[K("z"), K("bch", gs)], w=[K("z")])
            k = K("out", r0)
            S.op("sp", lambda e, r0=r0: e.dma_start(out=out[r0:r0 + 128, :], in_=z[:]), r=[K("z")], w=[k], dma=K("zout"))
            keys.append(k)
    return keys


def phase_select(S, nc, st, gathered, myact, selm, NR, SEQ, TT):
    def sb(name, shape, dt):
        return st.enter_context(nc.sbuf_tensor("sel_" + name, shape, dt))
    K = lambda *a: ("sel",) + a
    C = [sb("C%d" % i, [128, 16, 512], BF16) for i in range(2)]
    acc = sb("acc", [128, 16, 512], BF16)
    m = sb("m", [128, 8], F32)
    S.op("sp", lambda e: e.dma_start(out=m[:], in_=selm), w=[K("m")], dma=K("m"))
    NB = NR // 4
    NQ = SEQ // TT
    keys = []
    ci = 0
    for br in range(3):
        for tt in range(TT // 512):
            for k in range(NB * NQ):
                bb, q = divmod(k, NQ)
                sl = ci % 2
                ci += 1
                for g in range(4):
                    r0 = (4 * bb + g) * 1536 + br * 512
                    c0 = q * TT + tt * 512
                    S.op("sp" if g % 2 == 0 else "act", lambda e, sl=sl, g=g, r0=r0, c0=c0: e.dma_start(
                        out=C[sl][:, 4 * g:4 * g + 4, :], in_=gathered[r0:r0 + 512, c0:c0 + 512].rearrange("(fb p) t -> p fb t", p=128)),
                        w=[K("C", sl, g)], dma=K("C", sl, g))
                rk = [K("C", sl, g) for g in range(4)] + [K("m")]
                if k == 0:
                    S.op("dve", lambda e, sl=sl, k=k: e.tensor_scalar(out=acc[:], in0=C[sl][:], scalar1=m[:, k:k + 1], scalar2=None, op0=ALU.mult),
                         r=rk, w=[K("acc")])
                else:
                    S.op("dve", lambda e, sl=sl, k=k: e.scalar_tensor_tensor(out=acc[:], in0=C[sl][:], scalar=m[:, k:k + 1], in1=acc[:], op0=ALU.mult, op1=ALU.add),
                         r=rk + [K("acc")], w=[K("acc")])
            kk = K("out", br, tt)
            S.op("sp", lambda e, br=br, tt=tt: e.dma_start(out=myact[br][:, tt * 512:(tt + 1) * 512].rearrange("(fb p) t -> p fb t", p=128), in_=acc[:]),
                 r=[K("acc")], w=[kk], dma=K("accst"))
            keys.append(kk)
    return keys


D_MODEL = 4096
IN_SIZES = (1024, 1024, 2048, 2048, 16, 2048, 512, 512, 512, 512, 512, 512, 2048, 48, 2048, 2048, 12288)
OFF = np.concatenate([[0], np.cumsum(IN_SIZES)]).astype(np.int64)
NF = 2176
NTM = 2576
FM_Q, FM_K, FM_NQ, FM_KC, FM_VC, FM_KS, FM_KW, FM_MQ, FM_GA = 0, 256, 512, 1024, 1152, 1280, 1408, 1536, 2048
TM_K, TM_V, TM_GZ, TM_VS, TM_VW, TM_NZ, TM_MZ, TM_BG = 0, 256, 768, 1280, 1408, 1536, 2048, 2560


def proj_cols(g):
    fm = np.concatenate([
        OFF[0] + g * 256 + np.arange(256), OFF[1] + g * 256 + np.arange(256), OFF[5] + g * 512 + np.arange(512),
        OFF[6] + g * 128 + np.arange(128), OFF[7] + g * 128 + np.arange(128), OFF[8] + g * 128 + np.arange(128),
        OFF[10] + g * 128 + np.arange(128), OFF[14] + g * 512 + np.arange(512), OFF[4] + np.arange(16)])
    tm = np.concatenate([
        OFF[1] + g * 256 + np.arange(256), OFF[2] + g * 512 + np.arange(512), OFF[3] + g * 512 + np.arange(512),
        OFF[9] + g * 128 + np.arange(128), OFF[11] + g * 128 + np.arange(128), OFF[12] + g * 512 + np.arange(512),
        OFF[15] + g * 512 + np.arange(512), OFF[13] + g * 12 + np.arange(12)])
    return fm, tm


def _ctx():
    import contextlib
    return contextlib.ExitStack()


def build_proj(SEQ):
    nc = bass.Bass("TRN2", target_bir_lowering=False)
    xT = nc.dram_tensor("xT", [4096, SEQ], F32, kind="ExternalInput").ap()
    w = nc.dram_tensor("w", [4096, NF + NTM], F32, kind="ExternalInput").ap()
    fm = nc.dram_tensor("fm", [NF, SEQ], BF16, kind="ExternalOutput").ap()
    tm = nc.dram_tensor("tm", [SEQ, NTM], BF16, kind="ExternalOutput").ap()
    xb = nc.dram_tensor("xb", [4096, SEQ], BF16).ap()
    wb = nc.dram_tensor("wb", [4096, NF + NTM], BF16).ap()
    S = Sched(nc)
    with _ctx() as st:
        sb = lambda name, shape, dt: st.enter_context(nc.sbuf_tensor(name, shape, dt))
        bufs = dict(name="gb", A=[sb("A%d" % i, [128, 32, 512], BF16) for i in range(2)], B=sb("B", [128, 32, 1024], BF16),
                    O=[sb("O%d" % i, [128, 4, 512], BF16) for i in range(2)],
                    PS=[st.enter_context(nc.psum_tensor("ps%d" % i, [128, 512], F32)) for i in range(8)])
        dummy = sb("dummyt", [128, 8], F32)
        k2 = dram_cast(S, w, wb, "cw", 8)
        k1 = dram_cast(S, xT, xb, "cx", 16)
        S.op("pool", lambda e: e.memset(dummy[:, 0:1], 0.0), r=k1, w=["xb_done"])
        S.op("pool", lambda e: e.memset(dummy[:, 1:2], 0.0), r=k2, w=["wb_done"])
        blocks = [("FM", 0, 1024, fm[0:1024, :]), ("FM", 1024, 1024, fm[1024:2048, :]), ("FM", 2048, 128, fm[2048:2176, :]),
                  ("TM", NF, 1024, tm[:, 0:1024]), ("TM", NF + 1024, 1024, tm[:, 1024:2048]), ("TM", NF + 2048, 528, tm[:, 2048:2576])]
        gemm(S, "g", xb, wb, SEQ, 4096, blocks, bufs, "xb_done", "wb_done")
        S.emit()
    return nc


def build_gla(SEQ):
    nc = bass.Bass("TRN2", target_bir_lowering=False)
    di = lambda n, s, d: nc.dram_tensor(n, list(s), d, kind="ExternalInput").ap()
    fm = di("fm", [NF, SEQ], BF16)
    tm = di("tm", [SEQ, NTM], BF16)
    wa2 = di("wa2", [17, 256], F32)
    ngb = di("ngb", [128, 512], F32)
    gcn = di("gcn", [128, 512], F32)
    idn = di("idn", [128, 128], BF16)
    o_gla = nc.dram_tensor("o_gla", [512, SEQ], BF16, kind="ExternalOutput").ap()
    S = Sched(nc)
    with _ctx() as st:
        phase_gla(S, nc, st, SEQ, fm[FM_Q:FM_Q + 256, :], fm[FM_K:FM_K + 256, :], fm[FM_GA:FM_GA + 16, :],
                  tm[:, TM_K:TM_K + 256], tm[:, TM_V:TM_V + 512], tm[:, TM_GZ:TM_GZ + 512], wa2, ngb, gcn, idn, o_gla, "nodep")
        S.emit()
    return nc


def build_mem(SEQ):
    nc = bass.Bass("TRN2", target_bir_lowering=False)
    di = lambda n, s, d: nc.dram_tensor(n, list(s), d, kind="ExternalInput").ap()
    fm = di("fm", [NF, SEQ], BF16)
    tm = di("tm", [SEQ, NTM], BF16)
    idn = di("idn", [128, 128], BF16)
    memT = di("memT", [4096, 256], F32)
    wk = di("wk", [4096, 512], F32)
    wv = di("wv", [4096, 512], F32)
    memTb = nc.dram_tensor("memTb", [4096, 256], BF16).ap()
    wkb = nc.dram_tensor("wkb", [4096, 512], BF16).ap()
    wvb = nc.dram_tensor("wvb", [4096, 512], BF16).ap()
    o_mem = nc.dram_tensor("o_mem", [512, SEQ], BF16, kind="ExternalOutput").ap()
    S = Sched(nc)
    with _ctx() as st:
        dummy = st.enter_context(nc.sbuf_tensor("dummyt", [128, 8], F32))
        ks = dram_cast(S, memT, memTb, "cm", 2) + dram_cast(S, wk, wkb, "ck", 2) + dram_cast(S, wv, wvb, "cv", 2)
        S.op("pool", lambda e: e.memset(dummy[:, 0:1], 0.0), r=ks, w=["w_done"])
        phase_mem(S, nc, st, SEQ, fm[FM_MQ:FM_MQ + 512, :], tm[:, TM_MZ:TM_MZ + 512], memTb, wkb, wvb, idn, o_mem, "nodep", "w_done")
        S.emit()
    return nc


def build_nsa(SEQ, cn):
    nc = bass.Bass("TRN2", target_bir_lowering=False)
    di = lambda n, s, d: nc.dram_tensor(n, list(s), d, kind="ExternalInput").ap()
    fm = di("fm", [NF, SEQ], BF16)
    tm = di("tm", [SEQ, NTM], BF16)
    w = {k: di(k, s, F32) for k, s in (("w1k", [4096, 128]), ("w1v", [4096, 128]), ("w2k", [128, 128]), ("w2v", [128, 128]),
                                       ("pekT", [128, 32]), ("pevT", [128, 32]))}
    cst = {k: di("c_" + k, v.shape, BF16 if v.dtype == NPBF else F32) for k, v in cn.items()}
    idn = di("idn", [128, 128], BF16)
    o_nsa = nc.dram_tensor("o_nsa", [512, SEQ], BF16, kind="ExternalOutput").ap()
    S = Sched(nc)
    with _ctx() as st:
        phase_nsa(S, nc, st, SEQ, fm[FM_NQ:FM_NQ + 512, :], fm[FM_KC:FM_KC + 128, :], fm[FM_VC:FM_VC + 128, :], fm[FM_KS:FM_KS + 128, :],
                  fm[FM_KW:FM_KW + 128, :], tm[:, TM_VS:TM_VS + 128], tm[:, TM_VW:TM_VW + 128], tm[:, TM_NZ:TM_NZ + 512], tm[:, TM_BG:TM_BG + 12],
                  w["w1k"], w["w1v"], w["w2k"], w["w2v"], w["pekT"], w["pevT"], cst, idn, o_nsa, "nodep")
        S.emit()
    return nc


def build_out(TT, alpha):
    nc = bass.Bass("TRN2", target_bir_lowering=False)
    di = lambda n, s, d: nc.dram_tensor(n, list(s), d, kind="ExternalInput").ap()
    dt = lambda n, s, d: nc.dram_tensor(n, list(s), d).ap()
    xT = di("xT", [4096, TT], F32)
    xtok = di("xtok", [TT, 4096], F32)
    actT = [di("act%d" % i, [2048, TT], BF16) for i in range(3)]
    wm = di("wm", [4096, 12288], F32)
    wbr = [di("wbr%d" % i, [2048, 4096], F32) for i in range(3)]
    wout = di("wout", [4096, 4096], F32)
    bmT = di("bmT", [128, 96], F32)
    lng = di("lng", [128, 4096], F32)
    lnb = di("lnb", [128, 4096], F32)
    out = nc.dram_tensor("out", [TT, 4096], F32, kind="ExternalOutput").ap()
    xTb = dt("xTb", [4096, TT], BF16)
    wmb = dt("wmb", [4096, 12288], BF16)
    wbrb = [dt("wbrb%d" % i, [2048, 4096], BF16) for i in range(3)]
    woutb = dt("woutb", [4096, 4096], BF16)
    S = Sched(nc)
    with _ctx() as st:
        dummy = st.enter_context(nc.sbuf_tensor("dummyt", [128, 8], F32))
        ks = dram_cast(S, xT, xTb, "cx") + dram_cast(S, wm, wmb, "cwm", 16) + dram_cast(S, wout, woutb, "cwo")
        for i in range(3):
            ks += dram_cast(S, wbr[i], wbrb[i], "cwb%d" % i)
        S.op("pool", lambda e: e.memset(dummy[:, 0:1], 0.0), r=ks, w=["w_done"])
        phase_out(S, nc, st, TT, xTb, xtok, actT, wmb, wbrb, woutb, bmT, lng, lnb, out, "w_done", alpha)
        S.emit()
    return nc


def kernel_multi(x, mem, w_in, b_merge, gla_w_a2, gla_b_a, gla_norm_g, nsa_pe_k, nsa_pe_v, nsa_wk1, nsa_wk2, nsa_wv1, nsa_wv2,
           w_mem_kv, w_br_gla, w_br_nsa, w_br_mem, w_out, ln_g, ln_b):
    f32 = lambda a: np.ascontiguousarray(np.asarray(a, dtype=np.float32))
    x = np.asarray(x, dtype=np.float32)
    B, SEQ, D = x.shape
    NCORE = 4 * B
    cores = list(range(NCORE))
    w_in0 = np.asarray(w_in, dtype=np.float32)[0]
    alpha = float((2 * 1) ** 0.25)
    ident = np.eye(128, dtype=np.float32).astype(NPBF)
    xTs = [f32(x[b].T) for b in range(B)]
    ins = []
    for c in cores:
        b, g = divmod(c, 4)
        fmc, tmc = proj_cols(g)
        wc = np.zeros((4096, NF + NTM), np.float32)
        wc[:, 0:len(fmc)] = w_in0[:, fmc]
        wc[:, NF:NF + len(tmc)] = w_in0[:, tmc]
        ins.append(dict(xT=xTs[b], w=wc))
    res = run_bass_kernel_spmd(build_proj(SEQ), ins, core_ids=cores)
    fms = [np.asarray(r["fm"]) for r in res.results]
    tms = [np.asarray(r["tm"]) for r in res.results]
    del ins, res
    gcn = gla_consts()
    ins = []
    for c in cores:
        b, g = divmod(c, 4)
        wa2 = np.concatenate([np.asarray(gla_b_a, np.float32)[0][None, g * 256:(g + 1) * 256],
                              np.asarray(gla_w_a2, np.float32)[0][:, g * 256:(g + 1) * 256]], axis=0)
        ins.append(dict(fm=fms[c], tm=tms[c], wa2=f32(wa2),
                        ngb=f32(np.broadcast_to(np.asarray(gla_norm_g, np.float32)[0][None, :], (128, 512))), gcn=gcn, idn=ident))
    res = run_bass_kernel_spmd(build_gla(SEQ), ins, core_ids=cores)
    o_gla = [np.asarray(r["o_gla"]) for r in res.results]
    del ins, res
    wkv = np.asarray(w_mem_kv, dtype=np.float32)[0]
    memTs = [f32(np.asarray(mem, dtype=np.float32)[b].T) for b in range(B)]
    ins = []
    for c in cores:
        b, g = divmod(c, 4)
        ins.append(dict(fm=fms[c], tm=tms[c], idn=ident, memT=memTs[b], wk=f32(wkv[:, g * 512:(g + 1) * 512]),
                        wv=f32(wkv[:, 2048 + g * 512:2048 + (g + 1) * 512])))
    res = run_bass_kernel_spmd(build_mem(SEQ), ins, core_ids=cores)
    o_mem = [np.asarray(r["o_mem"]) for r in res.results]
    del ins, res
    cns = [nsa_consts(SEQ, g) for g in range(4)]
    ins = []
    for c in cores:
        b, g = divmod(c, 4)
        d = dict(fm=fms[c], tm=tms[c], w1k=f32(np.asarray(nsa_wk1)[0]), w1v=f32(np.asarray(nsa_wv1)[0]), w2k=f32(np.asarray(nsa_wk2)[0]),
                 w2v=f32(np.asarray(nsa_wv2)[0]), pekT=f32(np.asarray(nsa_pe_k, np.float32)[0].T), pevT=f32(np.asarray(nsa_pe_v, np.float32)[0].T), idn=ident)
        for k, v in cns[g].items():
            d["c_" + k] = v
        ins.append(d)
    res = run_bass_kernel_spmd(build_nsa(SEQ, cns[0]), ins, core_ids=cores)
    o_nsa = [np.asarray(r["o_nsa"]) for r in res.results]
    del ins, res, fms, tms
    TT = B * SEQ // NCORE
    per_b = SEQ // TT
    wm = f32(w_in0[:, OFF[16]:OFF[17]])
    bmT = f32(np.asarray(b_merge, np.float32)[0].reshape(96, 128).T)
    lng = f32(np.broadcast_to(np.asarray(ln_g, np.float32)[0][None], (128, 4096)))
    lnb = f32(np.broadcast_to(np.asarray(ln_b, np.float32)[0][None], (128, 4096)))
    wbrs = [f32(np.asarray(wb)[0]) for wb in (w_br_gla, w_br_nsa, w_br_mem)]
    wo = f32(np.asarray(w_out)[0])
    ins = []
    for c in cores:
        b, q = divmod(c, per_b)
        sl = slice(q * TT, (q + 1) * TT)
        d = dict(xT=f32(xTs[b][:, sl]), xtok=f32(x[b, sl]), wm=wm, wbr0=wbrs[0], wbr1=wbrs[1], wbr2=wbrs[2], wout=wo, bmT=bmT, lng=lng, lnb=lnb)
        for i, oo in enumerate((o_gla, o_nsa, o_mem)):
            d["act%d" % i] = np.ascontiguousarray(np.concatenate([oo[b * 4 + g][:, sl] for g in range(4)], axis=0))
        ins.append(d)
    res = run_bass_kernel_spmd(build_out(TT, alpha), ins, core_ids=cores)
    out = np.concatenate([np.asarray(r["out"]).astype(np.float32) for r in res.results], axis=0).reshape(B, SEQ, D)
    return out


def build_fused(SEQ, NR, cn, alpha):
    import contextlib
    TT = SEQ // 4
    nc = bass.Bass("TRN2", target_bir_lowering=False)
    di = lambda n, s, d: nc.dram_tensor(n, list(s), d, kind="ExternalInput").ap()
    dt = lambda n, s, d: nc.dram_tensor(n, list(s), d).ap()
    xT = di("xT", [4096, SEQ], F32)
    w = di("w", [4096, NF + NTM], F32)
    wa2 = di("wa2", [17, 256], F32)
    ngb = di("ngb", [128, 512], F32)
    gcn = di("gcn", [128, 512], F32)
    idn = di("idn", [128, 128], BF16)
    memT = di("memT", [4096, 256], F32)
    wk = di("wk", [4096, 512], F32)
    wv = di("wv", [4096, 512], F32)
    nw = {k: di(k, s, F32) for k, s in (("w1k", [4096, 128]), ("w1v", [4096, 128]), ("w2k", [128, 128]), ("w2v", [128, 128]),
                                        ("pekT", [128, 32]), ("pevT", [128, 32]))}
    cst = {k: di("c_" + k, v.shape, BF16 if v.dtype == NPBF else F32) for k, v in cn.items()}
    xTq = di("xTq", [4096, TT], F32)
    xtok = di("xtok", [TT, 4096], F32)
    wm = di("wm", [4096, 12288], F32)
    wbr = [di("wbr%d" % i, [2048, 4096], F32) for i in range(3)]
    wout = di("wout", [4096, 4096], F32)
    bmT = di("bmT", [128, 96], F32)
    lng = di("lng", [128, 4096], F32)
    lnb = di("lnb", [128, 4096], F32)
    selm = di("selm", [128, 8], F32)
    out = nc.dram_tensor("out", [TT, 4096], F32, kind="ExternalOutput").ap()
    xb = dt("xb", [4096, SEQ], BF16)
    wb = dt("wb", [4096, NF + NTM], BF16)
    fm = dt("fm", [NF, SEQ], BF16)
    tm = dt("tm", [SEQ, NTM], BF16)
    memTb = dt("memTb", [4096, 256], BF16)
    wkb = dt("wkb", [4096, 512], BF16)
    wvb = dt("wvb", [4096, 512], BF16)
    acts = dt("acts", [1536, SEQ], BF16)
    gathered = dt("gathered", [NR * 1536, SEQ], BF16)
    myact = [dt("myact%d" % i, [2048, TT], BF16) for i in range(3)]
    xTqb = dt("xTqb", [4096, TT], BF16)
    wmb = dt("wmb", [4096, 12288], BF16)
    wbrb = [dt("wbrb%d" % i, [2048, 4096], BF16) for i in range(3)]
    woutb = dt("woutb", [4096, 4096], BF16)
    tok_src = dt("tok_src", [1, 16], F32)
    tok_dst = dt("tok_dst", [1, 16], F32)
    S = Sched(nc)
    with contextlib.ExitStack() as top:
        scr = {e: top.enter_context(nc.sbuf_tensor("scr_" + e, [1, 2], F32)) for e in ("act", "dve", "pool")}
        S.setup_phased({e: scr[e][:] for e in scr}, tok_src, tok_dst)
        with contextlib.ExitStack() as st:
            sb = lambda name, shape, dty: st.enter_context(nc.sbuf_tensor(name, shape, dty))
            bufs = dict(name="gb", A=[sb("A%d" % i, [128, 32, 512], BF16) for i in range(2)], B=sb("B", [128, 32, 1024], BF16),
                        O=[sb("O%d" % i, [128, 4, 512], BF16) for i in range(2)],
                        PS=[st.enter_context(nc.psum_tensor("ps%d" % i, [128, 512], F32)) for i in range(8)])
            dummy = sb("dummyt", [128, 8], F32)
            k2 = dram_cast(S, w, wb, "cw", 8)
            k1 = dram_cast(S, xT, xb, "cx", 16)
            S.op("pool", lambda e: e.memset(dummy[:, 0:1], 0.0), r=k1, w=["xb_done"])
            S.op("pool", lambda e: e.memset(dummy[:, 1:2], 0.0), r=k2, w=["wb_done"])
            dram_cast(S, memT, memTb, "cm", 2)
            dram_cast(S, wk, wkb, "ck", 2)
            dram_cast(S, wv, wvb, "cv", 2)
            dram_cast(S, xTq, xTqb, "cxq", 4)
            dram_cast(S, wm, wmb, "cwm", 16)
            dram_cast(S, wout, woutb, "cwo", 8)
            for i in range(3):
                dram_cast(S, wbr[i], wbrb[i], "cwb%d" % i, 4)
            blocks = [("FM", 0, 1024, fm[0:1024, :]), ("FM", 1024, 1024, fm[1024:2048, :]), ("FM", 2048, 128, fm[2048:2176, :]),
                      ("TM", NF, 1024, tm[:, 0:1024]), ("TM", NF + 1024, 1024, tm[:, 1024:2048]), ("TM", NF + 2048, 528, tm[:, 2048:2576])]
            gemm(S, "g", xb, wb, SEQ, 4096, blocks, bufs, "xb_done", "wb_done", stq="sp", ldq=("sp", "act"))
            S.flush()
        with contextlib.ExitStack() as st:
            phase_gla(S, nc, st, SEQ, fm[FM_Q:FM_Q + 256, :], fm[FM_K:FM_K + 256, :], fm[FM_GA:FM_GA + 16, :],
                      tm[:, TM_K:TM_K + 256], tm[:, TM_V:TM_V + 512], tm[:, TM_GZ:TM_GZ + 512], wa2, ngb, gcn, idn, acts[0:512, :], "nodep")
            S.flush()
        with contextlib.ExitStack() as st:
            phase_mem(S, nc, st, SEQ, fm[FM_MQ:FM_MQ + 512, :], tm[:, TM_MZ:TM_MZ + 512], memTb, wkb, wvb, idn, acts[1024:1536, :], "nodep", "nodep")
            S.flush()
        with contextlib.ExitStack() as st:
            kn = phase_nsa(S, nc, st, SEQ, fm[FM_NQ:FM_NQ + 512, :], fm[FM_KC:FM_KC + 128, :], fm[FM_VC:FM_VC + 128, :], fm[FM_KS:FM_KS + 128, :],
                           fm[FM_KW:FM_KW + 128, :], tm[:, TM_VS:TM_VS + 128], tm[:, TM_VW:TM_VW + 128], tm[:, TM_NZ:TM_NZ + 512],
                           tm[:, TM_BG:TM_BG + 12], nw["w1k"], nw["w1v"], nw["w2k"], nw["w2v"], nw["pekT"], nw["pevT"], cst, idn, acts[512:1024, :], "nodep")
            S.op("pool", lambda e: e.collective_compute("AllGather", ALU.bypass, replica_groups=[list(range(NR))], ins=[acts.opt()], outs=[gathered.opt()]),
                 r=kn, w=["gathered"], dma="cc", inc=1)
            S.flush()
        with contextlib.ExitStack() as st:
            phase_select(S, nc, st, gathered, myact, selm, NR, SEQ, TT)
            S.flush()
        with contextlib.ExitStack() as st:
            phase_out(S, nc, st, TT, xTqb, xtok, myact, wmb, wbrb, woutb, bmT, lng, lnb, out, "nodep", alpha)
            S.flush(final=True)
        S.close()
    return nc


def kernel(x, mem, w_in, b_merge, gla_w_a2, gla_b_a, gla_norm_g, nsa_pe_k, nsa_pe_v, nsa_wk1, nsa_wk2, nsa_wv1, nsa_wv2,
                 w_mem_kv, w_br_gla, w_br_nsa, w_br_mem, w_out, ln_g, ln_b):
    f32 = lambda a: np.ascontiguousarray(np.asarray(a, dtype=np.float32))
    x = np.asarray(x, dtype=np.float32)
    B, SEQ, D = x.shape
    NR = 4 * B
    TT = SEQ // 4
    cores = list(range(NR))
    w_in0 = np.asarray(w_in, dtype=np.float32)[0]
    alpha = float((2 * 1) ** 0.25)
    ident = np.eye(128, dtype=np.float32).astype(NPBF)
    xTs = [f32(x[b].T) for b in range(B)]
    memTs = [f32(np.asarray(mem, dtype=np.float32)[b].T) for b in range(B)]
    wkv = np.asarray(w_mem_kv, dtype=np.float32)[0]
    gcn = gla_consts()
    cns = [nsa_consts(SEQ, g) for g in range(4)]
    wm = f32(w_in0[:, OFF[16]:OFF[17]])
    bmT = f32(np.asarray(b_merge, np.float32)[0].reshape(96, 128).T)
    lng = f32(np.broadcast_to(np.asarray(ln_g, np.float32)[0][None], (128, 4096)))
    lnb = f32(np.broadcast_to(np.asarray(ln_b, np.float32)[0][None], (128, 4096)))
    wbrs = [f32(np.asarray(wb_)[0]) for wb_ in (w_br_gla, w_br_nsa, w_br_mem)]
    wo = f32(np.asarray(w_out)[0])
    ngb = f32(np.broadcast_to(np.asarray(gla_norm_g, np.float32)[0][None, :], (128, 512)))
    shared = dict(idn=ident, gcn=gcn, ngb=ngb, w1k=f32(np.asarray(nsa_wk1)[0]), w1v=f32(np.asarray(nsa_wv1)[0]), w2k=f32(np.asarray(nsa_wk2)[0]),
                  w2v=f32(np.asarray(nsa_wv2)[0]), pekT=f32(np.asarray(nsa_pe_k, np.float32)[0].T), pevT=f32(np.asarray(nsa_pe_v, np.float32)[0].T),
                  wm=wm, wbr0=wbrs[0], wbr1=wbrs[1], wbr2=wbrs[2], wout=wo, bmT=bmT, lng=lng, lnb=lnb)
    ins = []
    for c in cores:
        b, g = divmod(c, 4)
        fmc, tmc = proj_cols(g)
        wc = np.zeros((4096, NF + NTM), np.float32)
        wc[:, 0:len(fmc)] = w_in0[:, fmc]
        wc[:, NF:NF + len(tmc)] = w_in0[:, tmc]
        wa2 = np.concatenate([np.asarray(gla_b_a, np.float32)[0][None, g * 256:(g + 1) * 256],
                              np.asarray(gla_w_a2, np.float32)[0][:, g * 256:(g + 1) * 256]], axis=0)
        sl = slice(g * TT, (g + 1) * TT)
        selm = np.zeros((128, 8), np.float32)
        selm[:, b * 4 + g] = 1.0
        d = dict(shared)
        d.update(xT=xTs[b], w=wc, wa2=f32(wa2), memT=memTs[b], wk=f32(wkv[:, g * 512:(g + 1) * 512]),
                 wv=f32(wkv[:, 2048 + g * 512:2048 + (g + 1) * 512]), xTq=f32(xTs[b][:, sl]), xtok=f32(x[b, sl]), selm=selm)
        for k, v in cns[g].items():
            d["c_" + k] = v
        ins.append(d)
    res = run_bass_kernel_spmd(build_fused(SEQ, NR, cns[0], alpha), ins, core_ids=cores)
    out = np.concatenate([np.asarray(r["out"]).astype(np.float32) for r in res.results], axis=0).reshape(B, SEQ, D)
    return out
```

```python
import numpy as np
import ml_dtypes
import concourse.bass as bass
import concourse.mybir as mybir
from concourse.bass_utils import run_bass_kernel_spmd

F32 = mybir.dt.float32
BF16 = mybir.dt.bfloat16
AF = mybir.ActivationFunctionType
ALU = mybir.AluOpType
AX = mybir.AxisListType
NPBF = ml_dtypes.bfloat16


class Sched:
    ENGS = ("pe", "act", "dve", "pool", "sp")
    SEM_SPAN = 12000

    def __init__(self, nc):
        self.nc = nc
        self.ops = []
        self.last_w = {}
        self.readers = {}

    def op(self, eng, fn, r=(), w=(), dma=None, inc=16):
        idx = len(self.ops)
        deps = set()
        raw = set()
        for k in r:
            if k in self.last_w:
                deps.add(self.last_w[k])
                raw.add(self.last_w[k])
        for k in w:
            if k in self.last_w:
                deps.add(self.last_w[k])
            rd = self.readers.get(k)
            if rd:
                deps.update(rd.values())
        for k in r:
            d = self.readers.setdefault(k, {})
            d[("dma", idx) if dma is not None else eng] = idx
        for k in w:
            self.last_w[k] = idx
            self.readers[k] = {}
        deps.discard(idx)
        self.ops.append(dict(eng=eng, fn=fn, deps=deps, raw=raw, dma=dma, sig=False, inc=inc))
        return idx

    def setup_phased(self, scr_tiles, tok_src, tok_dst):
        import contextlib
        self.stack = contextlib.ExitStack()
        self.sems = {}
        self.cnt = {e: 0 for e in self.ENGS}
        self.dma_n = {e: 0 for e in self.ENGS}
        self.dma_cnt = {}
        self.emitted = 0
        self.scr = scr_tiles
        self.tok_src, self.tok_dst = tok_src, tok_dst
        self.bar = {}
        self.nphase = 0

    def _sem(self, key):
        if key not in self.sems:
            self.sems[key] = self.stack.enter_context(self.nc.semaphore("s%d" % len(self.sems)))
        return self.sems[key]

    def flush(self, final=False):
        nc = self.nc
        ops = self.ops
        lo = self.emitted
        cur = ops[lo:]
        self.emitted = len(ops)
        npool = {"sp": 6, "pool": 5, "act": 4, "dve": 1, "pe": 1}
        toks = []
        for e in ("act", "dve", "pool"):
            scr = self.scr[e]
            if e == "act":
                fn = (lambda eng, scr=scr: eng.copy(out=scr[:, 0:1], in_=scr[:, 1:2]))
            else:
                fn = (lambda eng, scr=scr: eng.memset(scr[:, 0:1], 0.0))
            o = dict(eng=e, fn=fn, deps=set(), raw=set(), dma=None, sig=True, inc=1, tok=True)
            toks.append(o)
        o = dict(eng="sp", fn=(lambda eng: eng.dma_start(out=self.tok_dst, in_=self.tok_src)), deps=set(), raw=set(), dma="tok", sig=False, inc=16, tok=True)
        toks.append(o)
        for i, o in enumerate(cur):
            nd = set()
            for d in o["deps"]:
                if d < lo:
                    continue
                od = ops[d]
                if od["dma"] is None and od["eng"] == o["eng"] and o["dma"] is None and o["eng"] == "pe":
                    continue
                nd.add(d)
                if od["dma"] is None:
                    od["sig"] = True
            o["deps"] = nd
        pe_ops = [o for o in cur if o["eng"] == "pe"]
        if pe_ops:
            pe_ops[-1]["sig"] = True
        allops = cur + toks
        for o in allops:
            if o["dma"] is not None:
                q = o["eng"]
                if o["dma"] == "cc":
                    k = (q, "cc")
                else:
                    k = (q, self.dma_n[q] % npool[q])
                    self.dma_n[q] += 1
                o["prev"] = ("d", k, self.dma_cnt.get(k, 0))
                self.dma_cnt[k] = self.dma_cnt.get(k, 0) + o["inc"]
                o["semv"] = ("d", k, self.dma_cnt[k])
            elif o["sig"]:
                e = o["eng"]
                c = self.cnt[e]
                self.cnt[e] += 1
                o["semv"] = ("e", (e, c // self.SEM_SPAN), c % self.SEM_SPAN + 1)
        per_eng = {e: [o for o in allops if o["eng"] == e] for e in self.ENGS}
        newbar = {}
        for o in toks:
            t, k, v = o["semv"]
            newbar[(t, k)] = v
        if pe_ops:
            t, k, v = pe_ops[-1]["semv"]
            newbar[(t, k)] = v
        oldbar = self.bar
        block = nc.Block()
        with block:
            def run(engname, engobj):
                waited = {}
                for key, v in oldbar.items():
                    waited[key] = v
                    engobj.wait_ge(self._sem(key), v)
                myops = per_eng[engname]
                for o in myops:
                    need = {}
                    if o.get("tok"):
                        for (q, j), v in self.dma_cnt.items():
                            if q == engname and not (o["dma"] is not None and ("d", (q, j)) == o["semv"][:2]):
                                need[("d", (q, j))] = v
                            elif q == engname:
                                need[("d", (q, j))] = o["prev"][2]
                    for d in o["deps"]:
                        t, k, v = ops[d]["semv"]
                        key = (t, k)
                        if need.get(key, 0) < v:
                            need[key] = v
                    if o["dma"] is not None:
                        t, k, v = o["prev"]
                        if v > 0 and need.get((t, k), 0) < v:
                            need[(t, k)] = v
                    for key, v in need.items():
                        if v <= 0 or waited.get(key, 0) >= v:
                            continue
                        waited[key] = v
                        engobj.wait_ge(self._sem(key), v)
                    ins = o["fn"](engobj)
                    if o["dma"] is not None:
                        t, k, v = o["semv"]
                        ins.then_inc(self._sem((t, k)), o["inc"])
                    elif o["sig"]:
                        t, k, v = o["semv"]
                        ins.then_inc(self._sem((t, k)), 1)
                if final:
                    for (q, j), v in self.dma_cnt.items():
                        if q == engname:
                            engobj.wait_ge(self._sem(("d", (q, j))), v)

            @block.tensor
            def _(e):
                run("pe", e)

            @block.scalar
            def _(e):
                run("act", e)

            @block.vector
            def _(e):
                run("dve", e)

            @block.gpsimd
            def _(e):
                run("pool", e)

            @block.sync
            def _(e):
                run("sp", e)
        t, k, v = toks[-1]["semv"]
        newbar[(t, k)] = v
        self.bar = dict(oldbar)
        self.bar.update(newbar)
        self.nphase += 1
        print("phase %d: %d ops, %d sems" % (self.nphase, len(cur), len(self.sems)))
        self.last_w = {}
        self.readers = {}

    def close(self):
        self.stack.close()

    def emit(self):
        nc = self.nc
        ops = self.ops
        for o in ops:
            nd = set()
            for d in o["deps"]:
                od = ops[d]
                if od["dma"] is None and od["eng"] == o["eng"] and o["dma"] is None:
                    if o["eng"] == "pe":
                        continue
                nd.add(d)
                if od["dma"] is None:
                    od["sig"] = True
            o["deps"] = nd
        cnt = {e: 0 for e in self.ENGS}
        npool = {"sp": 6, "pool": 5, "act": 4, "dve": 1, "pe": 1}
        dma_n = {e: 0 for e in self.ENGS}
        dma_cnt = {}
        for o in ops:
            if o["dma"] is not None:
                q = o["eng"]
                k = (q, dma_n[q] % npool[q])
                dma_n[q] += 1
                o["prev"] = ("d", k, dma_cnt.get(k, 0))
                dma_cnt[k] = dma_cnt.get(k, 0) + o["inc"]
                o["semv"] = ("d", k, dma_cnt[k])
            elif o["sig"]:
                e = o["eng"]
                c = cnt[e]
                cnt[e] += 1
                o["semv"] = ("e", (e, c // self.SEM_SPAN), c % self.SEM_SPAN + 1)
        sem_names = []
        for e in self.ENGS:
            for j in range((cnt[e] + self.SEM_SPAN - 1) // self.SEM_SPAN):
                sem_names.append(("e", (e, j)))
        for k in dma_cnt:
            sem_names.append(("d", k))
        import contextlib
        with contextlib.ExitStack() as st:
            sems = {}
            for i, sn in enumerate(sem_names):
                sems[sn] = st.enter_context(nc.semaphore("s%d" % i))
            block = st.enter_context(nc.Block())
            per_eng = {e: [o for o in ops if o["eng"] == e] for e in self.ENGS}

            def run(engname, engobj):
                waited = {}
                for o in per_eng[engname]:
                    need = {}
                    for d in o["deps"]:
                        t, k, v = ops[d]["semv"]
                        key = (t, k)
                        if need.get(key, 0) < v:
                            need[key] = v
                    if o["dma"] is not None:
                        t, k, v = o["prev"]
                        if v > 0 and need.get((t, k), 0) < v:
                            need[(t, k)] = v
                    for key, v in need.items():
                        if waited.get(key, 0) >= v:
                            continue
                        waited[key] = v
                        engobj.wait_ge(sems[key], v)
                    ins = o["fn"](engobj)
                    if o["dma"] is not None:
                        t, k, v = o["semv"]
                        ins.then_inc(sems[(t, k)], o["inc"])
                    elif o["sig"]:
                        t, k, v = o["semv"]
                        ins.then_inc(sems[(t, k)], 1)
                fin = {}
                for o in per_eng[engname]:
                    if o["dma"] is not None:
                        t, k, v = o["semv"]
                        fin[(t, k)] = max(fin.get((t, k), 0), v)
                for key, v in fin.items():
                    engobj.wait_ge(sems[key], v)

            @block.tensor
            def _(e):
                run("pe", e)

            @block.scalar
            def _(e):
                run("act", e)

            @block.vector
            def _(e):
                run("dve", e)

            @block.gpsimd
            def _(e):
                run("pool", e)

            @block.sync
            def _(e):
                run("sp", e)
        print("sched: %d ops, %d sems" % (len(ops), len(sem_names)))


def cast_pass(S, pool, src, dst, tag, engs=("dve", "pool"), stq="act", chunk=4096, nbuf=2):
    R, C = src.shape
    nrow = R // 128
    sv = src.rearrange("(n p) c -> p n c", p=128)
    dv = dst.rearrange("(n p) c -> p n c", p=128)
    if C >= chunk:
        assert C % chunk == 0
        steps = [(n, 1, c0, chunk) for n in range(nrow) for c0 in range(0, C, chunk)]
    else:
        nn = max(1, chunk // C)
        steps = [(n, min(nn, nrow - n), 0, C) for n in range(0, nrow, nn)]
    stg, obf, pn = pool["stg"], pool["obf"], pool["name"]
    keys = []
    for i, (n, k, c0, cw) in enumerate(steps):
        sl = i % nbuf
        sa = stg[sl][:, 0:k * cw].rearrange("p (k c) -> p k c", k=k)
        oa = obf[sl][:, 0:k * cw].rearrange("p (k c) -> p k c", k=k)
        S.op("sp", lambda e, sa=sa, n=n, k=k, c0=c0, cw=cw: e.dma_start(out=sa, in_=sv[:, n:n + k, c0:c0 + cw]),
             w=[(pn, "stg", sl)], dma=(pn, "stg", sl))
        ce = engs[i % len(engs)]
        if ce == "act":
            S.op("act", lambda e, sa=sa, oa=oa: e.copy(out=oa, in_=sa), r=[(pn, "stg", sl)], w=[(pn, "obf", sl)])
        else:
            S.op(ce, lambda e, sa=sa, oa=oa: e.tensor_copy(out=oa, in_=sa), r=[(pn, "stg", sl)], w=[(pn, "obf", sl)])
        S.op(stq, lambda e, oa=oa, n=n, k=k, c0=c0, cw=cw: e.dma_start(out=dv[:, n:n + k, c0:c0 + cw], in_=oa),
             r=[(pn, "obf", sl)], w=[(tag, "dram", i)], dma=(pn, "obf", sl))
        keys.append((tag, "dram", i))
    return keys


def gemm(S, tag, aT, bm, M, K, blocks, bufs, a_dep, b_dep, stq="act", ldq=("sp", "pool")):
    KB = K // 128
    MT = 512
    A, B, O, PS = bufs["A"], bufs["B"], bufs["O"], bufs["PS"]
    out_tag = tag
    tag = bufs["name"]
    av = aT.rearrange("(kb p) m -> p kb m", p=128)
    bv = bm.rearrange("(kb p) n -> p kb n", p=128)
    keys = []
    ai = 0
    oi = 0
    pi = 0
    for bi, (mode, n0, nw, dst) in enumerate(blocks):
        half = KB // 2
        S.op("sp", lambda e, n0=n0, nw=nw: e.dma_start(out=B[:, 0:half, 0:nw], in_=bv[:, 0:half, n0:n0 + nw]),
             r=[b_dep], w=[(tag, "B", 0)], dma=(tag, "B", 0))
        S.op(ldq[1], lambda e, n0=n0, nw=nw: e.dma_start(out=B[:, half:KB, 0:nw], in_=bv[:, half:KB, n0:n0 + nw]),
             r=[b_dep], w=[(tag, "B", 1)], dma=(tag, "B", 1))
        for m0 in range(0, M, MT):
            sl = ai % 2
            ai += 1
            At = A[sl]
            S.op("sp", lambda e, At=At, m0=m0: e.dma_start(out=At[:, 0:half, :], in_=av[:, 0:half, m0:m0 + MT]),
                 r=[a_dep], w=[(tag, "A", sl, 0)], dma=(tag, "A", sl, 0))
            S.op(ldq[1], lambda e, At=At, m0=m0: e.dma_start(out=At[:, half:KB, :], in_=av[:, half:KB, m0:m0 + MT]),
                 r=[a_dep], w=[(tag, "A", sl, 1)], dma=(tag, "A", sl, 1))
            for s0 in range(0, nw, 512):
                sw = min(512, nw - s0)
                osl = oi % 2
                oi += 1
                Ot = O[osl]
                for j in range(4):
                    if mode == "FM" and j * 128 >= sw:
                        continue
                    pk = pi % 8
                    pi += 1
                    ps = PS[pk]

                    def mm(e, ps=ps, At=At, j=j, s0=s0, sw=sw, mode=mode):
                        ins = None
                        for kb in range(KB):
                            if mode == "TM":
                                ins = e.matmul(ps[:, 0:sw], lhsT=At[:, kb, j * 128:(j + 1) * 128],
                                               rhs=B[:, kb, s0:s0 + sw], start=(kb == 0), stop=(kb == KB - 1))
                            else:
                                ins = e.matmul(ps[:, 0:MT], lhsT=B[:, kb, s0 + j * 128:s0 + (j + 1) * 128],
                                               rhs=At[:, kb, :], start=(kb == 0), stop=(kb == KB - 1))
                        return ins
                    S.op("pe", mm, r=[(tag, "A", sl, 0), (tag, "A", sl, 1), (tag, "B", 0), (tag, "B", 1)],
                         w=[(tag, "PS", pk)])
                    ww = sw if mode == "TM" else MT
                    if j % 2 == 0:
                        S.op("act", lambda e, ps=ps, Ot=Ot, j=j, ww=ww: e.copy(out=Ot[:, j, 0:ww], in_=ps[:, 0:ww]),
                             r=[(tag, "PS", pk)], w=[(tag, "O", osl, j)])
                    else:
                        S.op("dve", lambda e, ps=ps, Ot=Ot, j=j, ww=ww: e.tensor_copy(out=Ot[:, j, 0:ww], in_=ps[:, 0:ww]),
                             r=[(tag, "PS", pk)], w=[(tag, "O", osl, j)])
                if mode == "TM":
                    dv = dst[m0:m0 + MT, s0:s0 + sw].rearrange("(j p) n -> p j n", p=128)
                    src = lambda Ot=Ot, sw=sw: Ot[:, :, 0:sw]
                else:
                    nj = sw // 128
                    dv = dst[s0:s0 + sw, m0:m0 + MT].rearrange("(j p) m -> p j m", p=128)
                    src = lambda Ot=Ot, nj=nj: Ot[:, 0:nj, :]
                k = (out_tag, "out", bi, m0, s0)
                S.op(stq, lambda e, dv=dv, src=src: e.dma_start(out=dv, in_=src()),
                     r=[(tag, "O", osl, j) for j in range(4)], w=[k], dma=(tag, "O", osl))
                keys.append(k)
    return keys


def gla_consts():
    j = np.arange(128)[:, None]
    c = np.arange(128)[None, :]
    tri = np.where(j <= c, -1.0 / 16.0, 0.0).astype(np.float32)
    tri2 = np.where(j > c, -1.0 / 16.0, 0.0).astype(np.float32)
    sel = np.full((128, 1), -1.0 / 16.0, np.float32)
    mask = (j <= c).astype(np.float32)
    return np.concatenate([tri, tri2, mask, sel, np.zeros((128, 127), np.float32)], axis=1)


def phase_gla(S, nc, st, SEQ, fmq, fmk, fmga, tmk, tmv, tmgz, wa2aug, normg_b, gconst, ident, outT, dep):
    def sb(name, shape, dt):
        return st.enter_context(nc.sbuf_tensor("gla_" + name, shape, dt))

    def psum(name, shape, dt=F32):
        return st.enter_context(nc.psum_tensor("gla_" + name, shape, dt))
    NT = SEQ // 128
    qT = [sb("qT%d" % i, [128, 2, 512], BF16) for i in range(2)]
    kT = [sb("kT%d" % i, [128, 2, 512], BF16) for i in range(2)]
    gaT = [sb("gaT%d" % i, [32, 512], BF16) for i in range(2)]
    ktm = [sb("ktm%d" % i, [128, 4, 256], BF16) for i in range(2)]
    vtm = [sb("vtm%d" % i, [128, 4, 512], BF16) for i in range(2)]
    gz = [sb("gz%d" % i, [128, 4, 512], BF16) for i in range(2)]
    wa_f = sb("wa_f", [32, 256], F32)
    wa = sb("wa", [32, 256], BF16)
    ng = sb("ng", [128, 512], F32)
    gc = sb("gc", [128, 512], F32)
    idn = sb("idn", [128, 128], BF16)
    e1 = sb("e1", [128, 256], F32)
    la = sb("la", [128, 256], F32)
    Eq = sb("Eq", [128, 2, 128], F32)
    Ek = sb("Ek", [128, 2, 128], F32)
    Eend = sb("Eend", [128, 256], F32)
    dec = sb("dec", [128, 2], F32)
    qd = sb("qd", [128, 2, 128], BF16)
    kd = sb("kd", [128, 2, 128], BF16)
    kend = sb("kend", [128, 256], BF16)
    att = sb("att", [128, 128], BF16)
    S32 = sb("S32", [128, 2, 512], F32)
    Sbf = sb("Sbf", [128, 2, 512], BF16)
    sq = sb("sq", [128, 512], F32)
    ss = sb("ss", [128, 1], F32)
    rstd = sb("rstd", [128, 1], F32)
    eps_t = sb("eps_t", [128, 1], F32)
    Gz = sb("Gz", [128, 512], F32)
    actt = sb("actt", [128, 512], BF16)
    oT = [sb("oT%d" % i, [128, 4, 512], BF16) for i in range(2)]
    P_lg = psum("P_lg", [128, 512])
    P_bc = psum("P_bc", [128, 512])
    P_bT = psum("P_bT", [128, 512])
    P_att = psum("P_att", [128, 512])
    P_o = psum("P_o", [128, 512])
    P_kv = [psum("P_kv%d" % i, [128, 512]) for i in range(2)]
    P_tr = psum("P_tr", [128, 1024], BF16)

    K = lambda *a: ("gla",) + a
    S.op("sp", lambda e: e.dma_start(out=wa_f[0:17, :], in_=wa2aug), w=[K("wa_f")], dma=K("wa_f"))
    S.op("sp", lambda e: e.dma_start(out=ng[:], in_=normg_b), w=[K("ng")], dma=K("ng"))
    S.op("sp", lambda e: e.dma_start(out=gc[:], in_=gconst), w=[K("gc")], dma=K("gc"))
    S.op("sp", lambda e: e.dma_start(out=idn[:], in_=ident), w=[K("idn")], dma=K("idn"))
    S.op("dve", lambda e: e.tensor_copy(out=wa[0:17, :], in_=wa_f[0:17, :]), r=[K("wa_f")], w=[K("wa")])
    for i in range(2):
        S.op("dve", lambda e, i=i: e.memset(gaT[i][0:1, :], 1.0), w=[K("gaT1", i)])
    S.op("dve", lambda e: e.memset(eps_t[:], 1e-6), w=[K("eps")])
    S.op("dve", lambda e: e.memset(S32[:], 0.0), w=[K("S32")])
    S.op("dve", lambda e: e.memset(Sbf[:], 0.0), w=[K("Sbf")])
    tri = gc[:, 0:128]
    tri2 = gc[:, 128:256]
    msk = gc[:, 256:384]
    sel = gc[:, 384:385]
    keys = []
    for t in range(NT):
        sup, j = divmod(t, 4)
        sl = sup % 2
        t0 = sup * 512
        if j == 0:
            S.op("sp", lambda e, sl=sl, t0=t0: e.dma_start(out=qT[sl][:], in_=fmq[:, t0:t0 + 512].rearrange("(b p) t -> p b t", p=128)),
                 r=[dep], w=[K("qT", sl)], dma=K("qT", sl))
            S.op("sp", lambda e, sl=sl, t0=t0: e.dma_start(out=kT[sl][:], in_=fmk[:, t0:t0 + 512].rearrange("(b p) t -> p b t", p=128)),
                 r=[dep], w=[K("kT", sl)], dma=K("kT", sl))
            S.op("sp", lambda e, sl=sl, t0=t0: e.dma_start(out=gaT[sl][1:17, :], in_=fmga[:, t0:t0 + 512]),
                 r=[dep, K("gaT1", sl)], w=[K("gaT", sl)], dma=K("gaT", sl))
            S.op("pool", lambda e, sl=sl, t0=t0: e.dma_start(out=ktm[sl][:], in_=tmk[t0:t0 + 512, :].rearrange("(j p) c -> p j c", p=128)),
                 r=[dep], w=[K("ktm", sl)], dma=K("ktm", sl))
            S.op("pool", lambda e, sl=sl, t0=t0: e.dma_start(out=vtm[sl][:], in_=tmv[t0:t0 + 512, :].rearrange("(j p) c -> p j c", p=128)),
                 r=[dep], w=[K("vtm", sl)], dma=K("vtm", sl))
            S.op("pool", lambda e, sl=sl, t0=t0: e.dma_start(out=gz[sl][:], in_=tmgz[t0:t0 + 512, :].rearrange("(j p) c -> p j c", p=128)),
                 r=[dep], w=[K("gz", sl)], dma=K("gz", sl))
        c0 = j * 128
        S.op("pe", lambda e, sl=sl, c0=c0: e.matmul(P_lg[:, 0:256], lhsT=gaT[sl][0:17, c0:c0 + 128], rhs=wa[0:17, :], start=True, stop=True),
             r=[K("gaT", sl), K("wa")], w=[K("P_lg")])
        S.op("act", lambda e: e.activation(out=e1[:], in_=P_lg[:, 0:256], func=AF.Exp, scale=-1.0), r=[K("P_lg")], w=[K("e1")])
        S.op("act", lambda e: e.activation(out=la[:], in_=e1[:], func=AF.Ln, bias=1.0), r=[K("e1")], w=[K("la")])
        def mm_bc(e):
            e.matmul(P_bc[:, 0:256], lhsT=tri2, rhs=la[:], start=True, stop=True)
            e.matmul(P_bT[:, 0:128], lhsT=la[:, 0:128], rhs=tri, start=True, stop=True)
            e.matmul(P_bT[:, 128:256], lhsT=la[:, 128:256], rhs=tri, start=True, stop=True)
            e.matmul(P_bT[:, 256:257], lhsT=la[:, 0:128], rhs=sel, start=True, stop=True)
            return e.matmul(P_bT[:, 257:258], lhsT=la[:, 128:256], rhs=sel, start=True, stop=True)
        S.op("pe", mm_bc, r=[K("la"), K("gc")], w=[K("P_bc"), K("P_bT")])
        S.op("act", lambda e: e.activation(out=Eq[:], in_=P_bT[:, 0:256].rearrange("p (b c) -> p b c", b=2), func=AF.Exp),
             r=[K("P_bT")], w=[K("Eq")])
        S.op("act", lambda e: e.activation(out=Ek[:], in_=P_bT[:, 0:256].rearrange("p (b c) -> p b c", b=2), func=AF.Exp, scale=-1.0),
             r=[K("P_bT")], w=[K("Ek")])
        S.op("act", lambda e: e.activation(out=dec[:], in_=P_bT[:, 256:258], func=AF.Exp), r=[K("P_bT")], w=[K("dec")])
        S.op("act", lambda e: e.activation(out=Eend[:], in_=P_bc[:, 0:256], func=AF.Exp), r=[K("P_bc")], w=[K("Eend")])
        S.op("dve", lambda e, sl=sl, c0=c0: e.scalar_tensor_tensor(out=qd[:], in0=qT[sl][:, :, c0:c0 + 128], scalar=1.0 / 16.0, in1=Eq[:],
                                                                   op0=ALU.mult, op1=ALU.mult),
             r=[K("qT", sl), K("Eq")], w=[K("qd")])
        S.op("dve", lambda e, sl=sl, c0=c0: e.tensor_tensor(out=kd[:], in0=kT[sl][:, :, c0:c0 + 128], in1=Ek[:], op=ALU.mult),
             r=[K("kT", sl), K("Ek")], w=[K("kd")])
        S.op("pool", lambda e, sl=sl, j=j: e.tensor_tensor(out=kend[:], in0=ktm[sl][:, j, :], in1=Eend[:], op=ALU.mult),
             r=[K("ktm", sl), K("Eend")], w=[K("kend")])
        def mm_att(e):
            e.matmul(P_att[:, 0:128], lhsT=kd[:, 0, :], rhs=qd[:, 0, :], start=True, stop=False)
            return e.matmul(P_att[:, 0:128], lhsT=kd[:, 1, :], rhs=qd[:, 1, :], start=False, stop=True)
        S.op("pe", mm_att, r=[K("kd"), K("qd")], w=[K("P_att")])
        S.op("dve", lambda e: e.tensor_tensor(out=att[:], in0=P_att[:, 0:128], in1=msk, op=ALU.mult), r=[K("P_att"), K("gc")], w=[K("att")])
        def mm_o(e, sl=sl, j=j):
            e.matmul(P_o[:], lhsT=att[:], rhs=vtm[sl][:, j, :], start=True, stop=False)
            e.matmul(P_o[:], lhsT=qd[:, 0, :], rhs=Sbf[:, 0, :], start=False, stop=False)
            return e.matmul(P_o[:], lhsT=qd[:, 1, :], rhs=Sbf[:, 1, :], start=False, stop=True)
        S.op("pe", mm_o, r=[K("att"), K("vtm", sl), K("qd"), K("Sbf")], w=[K("P_o")])
        for b in range(2):
            S.op("pe", lambda e, b=b, sl=sl, j=j: e.matmul(P_kv[b][:], lhsT=kend[:, b * 128:(b + 1) * 128], rhs=vtm[sl][:, j, :], start=True, stop=True),
                 r=[K("kend"), K("vtm", sl)], w=[K("P_kv", b)])
            S.op("dve", lambda e, b=b: e.scalar_tensor_tensor(out=S32[:, b, :], in0=S32[:, b, :], scalar=dec[:, b:b + 1], in1=P_kv[b][:],
                                                              op0=ALU.mult, op1=ALU.add),
                 r=[K("P_kv", b), K("dec"), K("S32")], w=[K("S32")])
        S.op("act", lambda e: e.copy(out=Sbf[:], in_=S32[:]), r=[K("S32")], w=[K("Sbf")])
        S.op("act", lambda e: e.activation(out=sq[:], in_=P_o[:], func=AF.Square, accum_out=ss[:]), r=[K("P_o")], w=[K("sq"), K("ss")])
        S.op("act", lambda e: e.activation(out=rstd[:], in_=ss[:], func=AF.Sqrt, scale=1.0 / 512.0, bias=eps_t[:, 0:1]), r=[K("ss"), K("eps")], w=[K("rstd")])
        S.op("dve", lambda e: e.reciprocal(out=rstd[:], in_=rstd[:]), r=[K("rstd")], w=[K("rstd")])
        S.op("act", lambda e, sl=sl, j=j: e.activation(out=Gz[:], in_=gz[sl][:, j, :], func=AF.Silu), r=[K("gz", sl)], w=[K("Gz")])
        S.op("pool", lambda e: e.tensor_tensor(out=Gz[:], in0=Gz[:], in1=ng[:], op=ALU.mult), r=[K("Gz"), K("ng")], w=[K("Gz")])
        S.op("dve", lambda e: e.scalar_tensor_tensor(out=actt[:], in0=P_o[:], scalar=rstd[:, 0:1], in1=Gz[:], op0=ALU.mult, op1=ALU.mult),
             r=[K("P_o"), K("rstd"), K("Gz")], w=[K("actt")])
        def mm_tr(e):
            ins = None
            for b in range(4):
                ins = e.transpose(P_tr[:, b * 128:(b + 1) * 128], actt[:, b * 128:(b + 1) * 128], idn[:])
            return ins
        S.op("pe", mm_tr, r=[K("actt"), K("idn")], w=[K("P_tr")])
        S.op("act", lambda e, sl=sl, c0=c0: e.copy(out=oT[sl][:, :, c0:c0 + 128], in_=P_tr[:, 0:512].rearrange("p (b c) -> p b c", b=4)),
             r=[K("P_tr")], w=[K("oT", sl)])
        if j == 3:
            k = K("out", sup)
            S.op("sp", lambda e, sl=sl, t0=t0: e.dma_start(out=outT[:, t0:t0 + 512].rearrange("(b p) t -> p b t", p=128), in_=oT[sl][:]),
                 r=[K("oT", sl)], w=[k], dma=K("oT", sl))
            keys.append(k)
    return keys


def finish_tile(S, nc, Kf, P_tr, actt_key, actt, idn, idn_key, oT, sl, j, outT, t0, keys, tag, stq="sp"):
    def mm_tr(e):
        ins = None
        for b in range(4):
            ins = e.transpose(P_tr[:, b * 128:(b + 1) * 128], actt[:, b * 128:(b + 1) * 128], idn[:])
        return ins
    S.op("pe", mm_tr, r=[actt_key, idn_key], w=[Kf("P_tr")])
    c0 = j * 128
    S.op("act", lambda e: e.copy(out=oT[sl][:, :, c0:c0 + 128], in_=P_tr[:, 0:512].rearrange("p (b c) -> p b c", b=4)),
         r=[Kf("P_tr")], w=[Kf("oT", sl)])
    if j == 3:
        k = Kf("out", t0)
        S.op(stq, lambda e: e.dma_start(out=outT[:, t0:t0 + 512].rearrange("(b p) t -> p b t", p=128), in_=oT[sl][:]),
             r=[Kf("oT", sl)], w=[k], dma=Kf("oT", sl))
        keys.append(k)


def phase_mem(S, nc, st, SEQ, fmmq, tmmz, memT_bf, wk_bf, wv_bf, ident, outT, dep, wdep):
    def sb(name, shape, dt):
        return st.enter_context(nc.sbuf_tensor("mem_" + name, shape, dt))

    def psum(name, shape, dt=F32):
        return st.enter_context(nc.psum_tensor("mem_" + name, shape, dt))
    K = lambda *a: ("mem",) + a
    mT = sb("mT", [128, 32, 256], BF16)
    wkv = sb("wkv", [128, 32, 512], BF16)
    kT = sb("kT", [128, 4, 256], BF16)
    vv = sb("vv", [128, 2, 512], BF16)
    ones = sb("ones", [128, 1], BF16)
    idn = sb("idn", [128, 128], BF16)
    qT = [sb("qT%d" % i, [128, 4, 512], BF16) for i in range(2)]
    mz = [sb("mz%d" % i, [128, 4, 512], BF16) for i in range(2)]
    pT = sb("pT", [128, 2, 512], BF16)
    sg = sb("sg", [128, 512], F32)
    rz = sb("rz", [128, 1], F32)
    actt = sb("actt", [128, 512], BF16)
    oT = [sb("oT%d" % i, [128, 4, 512], BF16) for i in range(2)]
    P_s = [psum("P_s%d" % i, [128, 512]) for i in range(2)]
    P_o = [psum("P_o%d" % i, [128, 512]) for i in range(2)]
    P_z = psum("P_z", [128, 512])
    P_tr = psum("P_tr", [128, 1024], BF16)
    S.op("sp", lambda e: e.dma_start(out=idn[:], in_=ident), w=[K("idn")], dma=K("idn"))
    S.op("dve", lambda e: e.memset(ones[:], 1.0), w=[K("ones")])
    S.op("sp", lambda e: e.dma_start(out=mT[:], in_=memT_bf.rearrange("(kb p) m -> p kb m", p=128)), r=[wdep], w=[K("mT")], dma=K("mT"))
    S.op("sp", lambda e: e.dma_start(out=wkv[:], in_=wk_bf.rearrange("(kb p) n -> p kb n", p=128)), r=[wdep], w=[K("wkv")], dma=K("wkv"))
    for db in range(4):
        def mmk(e, db=db):
            ins = None
            for kb in range(32):
                ins = e.matmul(P_s[db % 2][:, 0:256], lhsT=wkv[:, kb, db * 128:(db + 1) * 128], rhs=mT[:, kb, :], start=(kb == 0), stop=(kb == 31))
            return ins
        S.op("pe", mmk, r=[K("wkv"), K("mT")], w=[K("P_s", db % 2)])
        S.op("act", lambda e, db=db: e.activation(out=kT[:, db, :], in_=P_s[db % 2][:, 0:256], func=AF.Copy, scale=512.0 ** -0.5),
             r=[K("P_s", db % 2)], w=[K("kT")])
    S.op("sp", lambda e: e.dma_start(out=wkv[:], in_=wv_bf.rearrange("(kb p) n -> p kb n", p=128)), r=[wdep], w=[K("wkv")], dma=K("wkv"))
    for mt in range(2):
        def mmv(e, mt=mt):
            ins = None
            for kb in range(32):
                ins = e.matmul(P_o[mt][:], lhsT=mT[:, kb, mt * 128:(mt + 1) * 128], rhs=wkv[:, kb, :], start=(kb == 0), stop=(kb == 31))
            return ins
        S.op("pe", mmv, r=[K("wkv"), K("mT")], w=[K("P_o", mt)])
        S.op("act", lambda e, mt=mt: e.copy(out=vv[:, mt, :], in_=P_o[mt][:]), r=[K("P_o", mt)], w=[K("vv")])
    keys = []
    for sup in range(SEQ // 512):
        sl = sup % 2
        t0 = sup * 512
        S.op("sp", lambda e, sl=sl, t0=t0: e.dma_start(out=qT[sl][:], in_=fmmq[:, t0:t0 + 512].rearrange("(b p) t -> p b t", p=128)),
             r=[dep], w=[K("qT", sl)], dma=K("qT", sl))
        S.op("pool", lambda e, sl=sl, t0=t0: e.dma_start(out=mz[sl][:], in_=tmmz[t0:t0 + 512, :].rearrange("(j p) c -> p j c", p=128)),
             r=[dep], w=[K("mz", sl)], dma=K("mz", sl))
        for mt in range(2):
            def mms(e, mt=mt, sl=sl):
                ins = None
                for db in range(4):
                    ins = e.matmul(P_s[mt][:], lhsT=kT[:, db, mt * 128:(mt + 1) * 128], rhs=qT[sl][:, db, :], start=(db == 0), stop=(db == 3))
                return ins
            S.op("pe", mms, r=[K("kT"), K("qT", sl)], w=[K("P_s", mt)])
            S.op("act", lambda e, mt=mt: e.activation(out=pT[:, mt, :], in_=P_s[mt][:], func=AF.Exp), r=[K("P_s", mt)], w=[K("pT", mt)])
        for j in range(4):
            pj = j % 2
            def mmo(e, j=j, pj=pj):
                e.matmul(P_o[pj][:], lhsT=pT[:, 0, j * 128:(j + 1) * 128], rhs=vv[:, 0, :], start=True, stop=False)
                e.matmul(P_o[pj][:], lhsT=pT[:, 1, j * 128:(j + 1) * 128], rhs=vv[:, 1, :], start=False, stop=True)
                e.matmul(P_z[:, j:j + 1], lhsT=pT[:, 0, j * 128:(j + 1) * 128], rhs=ones[:], start=True, stop=False)
                return e.matmul(P_z[:, j:j + 1], lhsT=pT[:, 1, j * 128:(j + 1) * 128], rhs=ones[:], start=False, stop=True)
            S.op("pe", mmo, r=[K("pT", 0), K("pT", 1), K("vv"), K("ones")], w=[K("P_o", pj), K("P_z")])
            S.op("dve", lambda e, j=j: e.reciprocal(out=rz[:], in_=P_z[:, j:j + 1]), r=[K("P_z")], w=[K("rz")])
            S.op("act", lambda e, j=j, sl=sl: e.activation(out=sg[:], in_=mz[sl][:, j, :], func=AF.Silu), r=[K("mz", sl)], w=[K("sg")])
            S.op("dve", lambda e, pj=pj: e.scalar_tensor_tensor(out=actt[:], in0=P_o[pj][:], scalar=rz[:, 0:1], in1=sg[:], op0=ALU.mult, op1=ALU.mult),
                 r=[K("P_o", pj), K("rz"), K("sg")], w=[K("actt")])
            finish_tile(S, nc, K, P_tr, K("actt"), actt, idn, K("idn"), oT, sl, j, outT, t0, keys, "mem")
    return keys


def nsa_consts(SEQ, g):
    NT = SEQ // 128
    NSEL = SEQ // 64
    NCP = ((SEQ // 16 - 1 + 127) // 128) * 128
    NCT = NCP // 128
    slopes = (2.0 ** (-8.0 * (np.arange(16) + 1.0) / 16))[4 * g:4 * g + 4].astype(np.float64)
    nrel = np.arange(128)[:, None]
    m = np.arange(NCT)[None, :, None]
    i = np.arange(NT)[None, None, :]
    n = 128 * m + nrel[:, :, None]
    arg = 16 * n + 31 - 128 * i - 64
    fut = (16 * n + 31) > (128 * i + 127)
    biasc = np.stack([np.where(fut | (s * arg < -70.0), -30000.0, s * arg) for s in slopes], axis=1)
    biasc = biasc.reshape(128, 4 * NCT * NT).astype(np.float32)
    D = np.arange(NT)[None, :]
    bs = np.stack([np.where(s * (nrel - 64 - 128 * D) < -70.0, -30000.0, s * (nrel - 64 - 128 * D)) for s in slopes], axis=1)
    bs = bs.reshape(128, 4 * NT).astype(np.float32)
    trel = np.arange(128)[None, :]
    masks = []
    for k in range(16):
        masks.append(((16 * (nrel - 8 * k) + 31) <= trel))
    masks.append(((16 * (nrel - 128) + 31) <= trel))
    masks.append(nrel <= trel)
    masks.append(nrel > trel)
    maskc = np.concatenate(masks, axis=1).astype(np.float32).astype(NPBF)
    nn = np.arange(NCP)[:, None]
    jj = np.arange(NSEL)[None, :]
    ov = ((16 * nn < 64 * jj + 64) & (16 * nn + 31 >= 64 * jj) & (nn < SEQ // 16 - 1)).astype(np.float32)
    ovl = np.concatenate([ov, np.ones((NCP, 1), np.float32), np.zeros((NCP, 1), np.float32)], axis=1).astype(NPBF)
    ovl = np.ascontiguousarray(ovl.reshape(NCT, 128, NSEL + 2).transpose(1, 0, 2))
    E = np.zeros((128, SEQ), np.float32)
    kk = np.arange(SEQ)
    if NSEL <= 128:
        E[kk // 64, kk] = 1.0
    E = E.astype(NPBF)
    t = (128 * np.arange(NT)[:, None] + np.arange(128)[None, :])[:, :, None]
    jb = np.arange(NSEL)[None, None, :]
    cur = t // 64
    causal = (jb <= cur).astype(np.float32)
    forced = ((jb == 0) | (jb == cur) | (jb == cur - 1)).astype(np.float32)
    add = (causal - 1.0) + 1e4 * forced
    tk = np.stack([causal, add], axis=2).astype(np.float32)
    return dict(biasc=biasc, bs=bs, maskc=maskc, ovl=ovl, E=E, tk=tk)


def phase_nsa(S, nc, st, SEQ, fmq, fmkc, fmvc, fmks, fmkw, tmvs, tmvw, tmnz, tmbg,
              w1k, w1v, w2k, w2v, pekT, pevT, cst, ident, outT, dep, dbg=None):
    def sb(name, shape, dt):
        return st.enter_context(nc.sbuf_tensor("nsa_" + name, shape, dt))

    def psum(name, shape, dt=F32):
        return st.enter_context(nc.psum_tensor("nsa_" + name, shape, dt))
    K = lambda *a: ("nsa",) + a
    NT = SEQ // 128
    NSEL = SEQ // 64
    NC = SEQ // 16 - 1
    NCT = (NC + 127) // 128
    NCP = NCT * 128
    WR = 128 + NSEL + 1
    SC = 128.0 ** -0.5
    assert NSEL <= 128
    idn = sb("idn", [128, 128], BF16)
    ksT = sb("ksT", [128, SEQ], BF16)
    kwT = sb("kwT", [128, SEQ], BF16)
    kcT = sb("kcT", [128, SEQ], BF16)
    vsa = sb("vsa", [128, NT, 130], BF16)
    vwa = sb("vwa", [128, NT, 130], BF16)
    Emat = sb("E", [128, SEQ], BF16)
    kcmpT = sb("kcmpT", [128, NCP], BF16)
    Rc = sb("Rc", [128, NCT, WR + 1], BF16)
    biasc = sb("biasc", [128, 4 * NCT * NT], F32)
    bs = sb("bs", [128, 4 * NT], F32)
    maskc = sb("maskc", [128, 19 * 128], BF16)
    w1f = sb("w1f", [128, 32, 128], F32)
    w1 = sb("w1", [128, 32, 128], BF16)
    w2f = sb("w2f", [128, 128], F32)
    w2 = sb("w2", [128, 128], BF16)
    pef = sb("pef", [128, 32], F32)
    peb = sb("peb", [128, 32], BF16)
    cb = sb("cb", [128, 1], F32)
    hc = sb("hc", [128, NCP], BF16)
    S.op("sp", lambda e: e.dma_start(out=idn[:], in_=ident), w=[K("idn")], dma=K("idn"))
    S.op("sp", lambda e: e.dma_start(out=ksT[:], in_=fmks), r=[dep], w=[K("ksT")], dma=K("ksT"))
    S.op("sp", lambda e: e.dma_start(out=kwT[:], in_=fmkw), r=[dep], w=[K("kwT")], dma=K("kwT"))
    S.op("pool", lambda e: e.dma_start(out=Emat[:], in_=cst["E"]), w=[K("E")], dma=K("E"))
    S.op("pool", lambda e: e.dma_start(out=biasc[:], in_=cst["biasc"]), w=[K("biasc")], dma=K("biasc"))
    S.op("pool", lambda e: e.dma_start(out=bs[:], in_=cst["bs"]), w=[K("bs")], dma=K("bs"))
    S.op("pool", lambda e: e.dma_start(out=maskc[:], in_=cst["maskc"]), w=[K("maskc")], dma=K("maskc"))
    S.op("dve", lambda e: e.memset(vsa[:], 1.0), w=[K("vsa")])
    S.op("dve", lambda e: e.memset(vwa[:], 1.0), w=[K("vwa")])
    S.op("sp", lambda e: e.dma_start(out=vsa[:, :, 0:128], in_=tmvs.rearrange("(j p) c -> p j c", p=128)), r=[dep, K("vsa")], w=[K("vsa")], dma=K("vsa"))
    S.op("sp", lambda e: e.dma_start(out=vwa[:, :, 0:128], in_=tmvw.rearrange("(j p) c -> p j c", p=128)), r=[dep, K("vwa")], w=[K("vwa")], dma=K("vwa"))
    S.op("dve", lambda e: e.memset(kcmpT[:], 0.0), w=[K("kcmpT")])
    S.op("dve", lambda e: e.memset(Rc[:], 0.0), w=[K("Rc")])
    S.op("dve", lambda e: e.memset(hc[:], 0.0), w=[K("hc")])
    S.op("pool", lambda e: e.dma_start(out=Rc[:, :, 128:WR + 1], in_=cst["ovl"]), r=[K("Rc")], w=[K("Rc")], dma=K("Rc"))
    P_sc = [psum("P_sc%d" % i, [128, 512]) for i in range(2)]
    P_big = [psum("P_big0", [128, 512]), P_sc[0]]
    P_sw = [psum("P_sw%d" % i, [128, 512]) for i in range(4)]
    P_trm = psum("P_trm", [128, 1024], BF16)
    P_tr = P_trm[:, 0:512]
    P_mT = P_trm[:, 512:1024]
    for which, (fmx, w1d, w2d, ped) in enumerate(((fmkc, w1k, w2k, pekT), (fmvc, w1v, w2v, pevT))):
        S.op("sp", lambda e, fmx=fmx: e.dma_start(out=kcT[:], in_=fmx), r=[dep], w=[K("kcT")], dma=K("kcT"))
        S.op("sp", lambda e, w1d=w1d: e.dma_start(out=w1f[:], in_=w1d.rearrange("(l d) o -> d l o", d=128)), w=[K("w1f")], dma=K("w1f"))
        S.op("sp", lambda e, w2d=w2d: e.dma_start(out=w2f[:], in_=w2d), w=[K("w2f")], dma=K("w2f"))
        S.op("sp", lambda e, ped=ped: e.dma_start(out=pef[:], in_=ped), w=[K("pef")], dma=K("pef"))
        S.op("dve", lambda e: e.tensor_copy(out=w1[:], in_=w1f[:]), r=[K("w1f")], w=[K("w1")])
        S.op("dve", lambda e: e.tensor_copy(out=w2[:], in_=w2f[:]), r=[K("w2f")], w=[K("w2")])
        S.op("dve", lambda e: e.tensor_copy(out=peb[:], in_=pef[:]), r=[K("pef")], w=[K("peb")])
        def mm_c(e):
            ins = None
            for l in range(32):
                ins = e.matmul(P_big[1][:, 0:1], lhsT=w1[:, l, :], rhs=peb[:, l:l + 1], start=(l == 0), stop=(l == 31))
            return ins
        S.op("pe", mm_c, r=[K("w1"), K("peb")], w=[K("P_sc", 0)])
        S.op("act", lambda e: e.copy(out=cb[:], in_=P_big[1][:, 0:1]), r=[K("P_sc", 0)], w=[K("cb")])
        def mm_pre(e):
            ins = None
            for l in range(32):
                ins = e.matmul(P_big[0][:, 0:NC], lhsT=w1[:, l, :], rhs=kcT[:, l:l + 16 * (NC - 1) + 1:16], start=(l == 0), stop=(l == 31))
            return ins
        S.op("pe", mm_pre, r=[K("w1"), K("kcT")], w=[K("P_big", 0)])
        S.op("act", lambda e: e.activation(out=hc[:, 0:NC], in_=P_big[0][:, 0:NC], func=AF.Silu, bias=cb[:, 0:1]), r=[K("P_big", 0), K("cb")], w=[K("hc")])
        if which == 0:
            S.op("pe", lambda e: e.matmul(P_big[0][:, 0:NC], lhsT=w2[:], rhs=hc[:, 0:NC], start=True, stop=True), r=[K("w2"), K("hc")], w=[K("P_big", 0)])
            S.op("act", lambda e: e.copy(out=kcmpT[:, 0:NC], in_=P_big[0][:, 0:NC]), r=[K("P_big", 0)], w=[K("kcmpT")])
        else:
            for nt in range(NCT):
                S.op("pe", lambda e, nt=nt: e.matmul(P_big[0][:, 0:128], lhsT=hc[:, nt * 128:(nt + 1) * 128], rhs=w2[:], start=True, stop=True),
                     r=[K("w2"), K("hc")], w=[K("P_big", 0)])
                S.op("act", lambda e, nt=nt: e.copy(out=Rc[:, nt, 0:128], in_=P_big[0][:, 0:128]), r=[K("P_big", 0)], w=[K("Rc")])
    qT = [sb("qT%d" % i, [128, 4, 128], BF16) for i in range(2)]
    nz = [sb("nz%d" % i, [128, 512], BF16) for i in range(2)]
    bgl = [sb("bg%d" % i, [128, 12], BF16) for i in range(2)]
    tkc = [sb("tk%d" % i, [128, 2, NSEL], F32) for i in range(2)]
    gates = sb("gates", [128, 12], F32)
    pT = [sb("pT%d" % i, [128, 128], BF16) for i in range(4)]
    pT4 = [sb("pT4_%d" % i, [128, 4, 128], BF16) for i in range(4)]
    negm4 = sb("negm4", [128, 4, 128], BF16)
    zc = sb("zc", [128, 4], F32)
    imp = sb("imp", [128, NSEL], F32)
    score = sb("score", [128, NSEL], F32)
    score2 = sb("score2", [128, NSEL], F32)
    mx8 = sb("mx8", [128, 8], F32)
    mx8b = sb("mx8b", [128, 8], F32)
    msel = sb("msel", [128, 128], BF16)
    negm = sb("negm", [128, 128], BF16)
    ocmp = sb("ocmp", [128, 4, 128], F32)
    cf = sb("cf", [128, 8], F32)
    tmp = sb("tmp", [128, 128], F32)
    sg = sb("sg", [128, 512], F32)
    actt = sb("actt", [128, 512], BF16)
    oT = [sb("oT%d" % i, [128, 4, 512], BF16) for i in range(2)]
    S.op("dve", lambda e: e.memset(msel[:], 0.0), w=[K("msel")])
    dbgt = sb("dbgt", [128, 512], F32) if dbg is not None else None
    keys = []
    pcount = [0]
    sccount = [0]

    def score_exp(lhsT_fn, lk, r, sl, bias_ap, bias_key, mask_ap, extra=None):
        si = sccount[0] % 2
        sccount[0] += 1
        ps = P_sc[si][:, 0:128]
        pk = K("P_sc", si)

        def mm(e):
            ins = e.matmul(ps, lhsT=lhsT_fn(), rhs=qT[sl][:, r, :], start=True, stop=(extra is None))
            if extra is not None:
                ins = e.matmul(ps, lhsT=extra(), rhs=negm[:], start=False, stop=True)
            return ins
        S.op("pe", mm, r=list(lk) + [K("qT", sl)] + ([K("negm"), K("E")] if extra is not None else []), w=[pk])
        pi = pcount[0] % 4
        pcount[0] += 1
        S.op("act", lambda e: e.activation(out=pT[pi][:], in_=ps, func=AF.Exp, scale=SC, bias=bias_ap), r=[pk, bias_key], w=[K("pT", pi)])
        if mask_ap is not None:
            S.op("dve", lambda e: e.tensor_tensor(out=pT[pi][:], in0=pT[pi][:], in1=mask_ap, op=ALU.mult), r=[K("pT", pi), K("maskc")], w=[K("pT", pi)])
        return pi

    p4count = [0]

    def pair4(lhsT_fn, lk, sl, bcol, mask_ap, extra=None):
        si = sccount[0] % 2
        sccount[0] += 1
        ps = P_sc[si]
        pk = K("P_sc", si)
        qflat = qT[sl][:].rearrange("p a b -> p (a b)")

        def mm(e):
            ins = e.matmul(ps[:, 0:512], lhsT=lhsT_fn(), rhs=qflat, start=True, stop=(extra is None))
            if extra is not None:
                ins = e.matmul(ps[:, 0:512], lhsT=extra(), rhs=negm4[:].rearrange("p a b -> p (a b)"), start=False, stop=True)
            return ins
        S.op("pe", mm, r=list(lk) + [K("qT", sl)] + ([K("negm4"), K("E")] if extra is not None else []), w=[pk])
        pi = p4count[0] % 4
        p4count[0] += 1
        for r in range(4):
            bc = r * NT + bcol
            S.op("act", lambda e, r=r, bc=bc: e.activation(out=pT4[pi][:, r, :], in_=ps[:, r * 128:(r + 1) * 128], func=AF.Exp, scale=SC, bias=bs[:, bc:bc + 1]),
                 r=[pk, K("bs")], w=[K("pT4", pi, r)])
            if mask_ap is not None:
                S.op("dve", lambda e, r=r: e.tensor_tensor(out=pT4[pi][:, r, :], in0=pT4[pi][:, r, :], in1=mask_ap, op=ALU.mult),
                     r=[K("pT4", pi, r), K("maskc")], w=[K("pT4", pi, r)])
        return pi

    for i in range(NT):
        sl = i % 2
        t0 = i * 128
        sup, j4 = divmod(i, 4)
        osl = sup % 2
        S.op("sp", lambda e, sl=sl, t0=t0: e.dma_start(out=qT[sl][:], in_=fmq[:, t0:t0 + 128].rearrange("(b p) t -> p b t", p=128)),
             r=[dep], w=[K("qT", sl)], dma=K("qT", sl))
        S.op("sp", lambda e, sl=sl, t0=t0: e.dma_start(out=nz[sl][:], in_=tmnz[t0:t0 + 128, :]), r=[dep], w=[K("nz", sl)], dma=K("nz", sl))
        S.op("sp", lambda e, sl=sl, t0=t0: e.dma_start(out=bgl[sl][:], in_=tmbg[t0:t0 + 128, :]), r=[dep], w=[K("bg", sl)], dma=K("bg", sl))
        S.op("pool", lambda e, sl=sl, i=i: e.dma_start(out=tkc[sl][:], in_=cst["tk"][i]), w=[K("tk", sl)], dma=K("tk", sl))
        S.op("act", lambda e, sl=sl: e.activation(out=gates[:], in_=bgl[sl][:], func=AF.Sigmoid), r=[K("bg", sl)], w=[K("gates")])
        mb = min((8 * i + 6) // 128, NCT - 1)
        for r in range(4):
            pis = []
            for m in range(mb + 1):
                mask_ap = None
                if m == mb:
                    kq = i % 16
                    mask_ap = maskc[:, kq * 128:(kq + 1) * 128]
                elif m == mb - 1 and i % 16 == 0:
                    mask_ap = maskc[:, 16 * 128:17 * 128]
                bcol = (r * NCT + m) * NT + i
                pi = score_exp(lambda m=m: kcmpT[:, m * 128:(m + 1) * 128], [K("kcmpT")], r, sl, biasc[:, bcol:bcol + 1], K("biasc"), mask_ap)
                pis.append((m, pi))
            pb = P_big[0]

            def mm_pv(e, pis=pis, pb=pb):
                ins = None
                for q, (m, pi) in enumerate(pis):
                    ins = e.matmul(pb[:, 0:WR], lhsT=pT[pi][:], rhs=Rc[:, m, 0:WR], start=(q == 0), stop=(q == len(pis) - 1))
                return ins
            S.op("pe", mm_pv, r=[K("pT", pi) for _, pi in pis] + [K("Rc")], w=[K("P_big", 0)])
            S.op("dve", lambda e, r=r, pb=pb: e.tensor_scalar(out=zc[:, r:r + 1], in0=pb[:, WR - 1:WR], scalar1=1e-30, scalar2=None, op0=ALU.add),
                 r=[K("P_big", 0)], w=[K("zc", r)])
            S.op("dve", lambda e, r=r: e.reciprocal(out=zc[:, r:r + 1], in_=zc[:, r:r + 1]), r=[K("zc", r)], w=[K("zc", r)])
            if r == 0:
                S.op("dve", lambda e, pb=pb: e.tensor_scalar(out=imp[:], in0=pb[:, 128:128 + NSEL], scalar1=zc[:, 0:1], scalar2=None, op0=ALU.mult),
                     r=[K("P_big", 0), K("zc", 0)], w=[K("imp")])
            else:
                S.op("dve", lambda e, r=r, pb=pb: e.scalar_tensor_tensor(out=imp[:], in0=pb[:, 128:128 + NSEL], scalar=zc[:, r:r + 1], in1=imp[:], op0=ALU.mult, op1=ALU.add),
                     r=[K("P_big", 0), K("zc", r), K("imp")], w=[K("imp")])
            S.op("dve", lambda e, r=r: e.tensor_tensor(out=cf[:, r:r + 1], in0=zc[:, r:r + 1], in1=gates[:, 3 * r:3 * r + 1], op=ALU.mult),
                 r=[K("zc", r), K("gates")], w=[K("cf", r)])
            S.op("act", lambda e, r=r, pb=pb: e.activation(out=ocmp[:, r, :], in_=pb[:, 0:128], func=AF.Copy, scale=cf[:, r:r + 1]),
                 r=[K("P_big", 0), K("cf", r)], w=[K("ocmp", r)])
        S.op("dve", lambda e, sl=sl: e.tensor_tensor(out=score[:], in0=imp[:], in1=tkc[sl][:, 0, :], op=ALU.mult), r=[K("imp"), K("tk", sl)], w=[K("score")])
        S.op("dve", lambda e, sl=sl: e.tensor_tensor(out=score[:], in0=score[:], in1=tkc[sl][:, 1, :], op=ALU.add), r=[K("score"), K("tk", sl)], w=[K("score")])
        if NSEL > 16:
            S.op("dve", lambda e: e.max(out=mx8[:], in_=score[:]), r=[K("score")], w=[K("mx8")])
            S.op("dve", lambda e: e.match_replace(out=score2[:], in_to_replace=mx8[:], in_values=score[:], imm_value=-1e30), r=[K("mx8"), K("score")], w=[K("score2")])
            S.op("dve", lambda e: e.max(out=mx8b[:], in_=score2[:]), r=[K("score2")], w=[K("mx8b")])
            S.op("dve", lambda e: e.tensor_scalar(out=msel[:, 0:NSEL], in0=score[:], scalar1=mx8b[:, 7:8], scalar2=None, op0=ALU.is_ge),
                 r=[K("score"), K("mx8b")], w=[K("msel")])
        else:
            S.op("dve", lambda e: e.memset(msel[:, 0:NSEL], 1.0), r=[K("score")], w=[K("msel")])
        S.op("pe", lambda e: e.transpose(P_mT[:, 0:128], msel[:], idn[:]), r=[K("msel"), K("idn")], w=[K("P_tr")])
        for r in range(4):
            S.op("dve", lambda e, r=r: e.tensor_scalar(out=negm4[:, r, :], in0=P_mT[:, 0:128], scalar1=-1.0, scalar2=30000.0 / SC, op0=ALU.add, op1=ALU.mult),
                 r=[K("P_tr")], w=[K("negm4")])
        if dbg is not None:
            if "imp" in dbg:
                S.op("pool", lambda e, i=i: e.dma_start(out=dbg["imp"][i], in_=imp[:]), r=[K("imp")], w=[K("dbg", "imp", i)], dma=K("dbg1"))
            if "msel" in dbg:
                S.op("pool", lambda e, i=i: e.dma_start(out=dbg["msel"][i], in_=msel[:]), r=[K("msel")], w=[K("dbg", "msel", i)], dma=K("dbg2"))
            if "ocmp" in dbg:
                S.op("pool", lambda e, i=i: e.dma_start(out=dbg["ocmp"][i], in_=ocmp[:]), r=[K("ocmp", r) for r in range(4)], w=[K("dbg", "ocmp", i)], dma=K("dbg3"))
            if "zc" in dbg:
                S.op("pool", lambda e, i=i: e.dma_start(out=dbg["zc"][i], in_=zc[:]), r=[K("zc", r) for r in range(4)], w=[K("dbg", "zc", i)], dma=K("dbg4"))
            if i == 0 and "kcmpT" in dbg:
                S.op("pool", lambda e: e.dma_start(out=dbg["kcmpT"], in_=kcmpT[:]), r=[K("kcmpT")], w=[K("dbg", "kcmpT")], dma=K("dbg5"))
                S.op("pool", lambda e: e.dma_start(out=dbg["Rc"], in_=Rc[:, 0, 0:WR - 1]), r=[K("Rc")], w=[K("dbg", "Rc")], dma=K("dbg6"))
        S.op("act", lambda e, sl=sl: e.activation(out=sg[:], in_=nz[sl][:], func=AF.Silu), r=[K("nz", sl)], w=[K("sg")])
        def pv4(J, pi, col0, first, last):
            for r in range(4):
                S.op("pe", lambda e, r=r: e.matmul(P_sw[r][:, col0:col0 + 129], lhsT=pT4[pi][:, r, :], rhs=(vsa if col0 == 0 else vwa)[:, J, 0:129], start=first, stop=last),
                     r=[K("pT4", pi, r), K("vsa" if col0 == 0 else "vwa")], w=[K("P_sw", r)])
        prev = None
        for J in range(i + 1):
            mask_ap = maskc[:, 17 * 128:18 * 128] if J == i else None
            pi = pair4(lambda J=J: ksT[:, J * 128:(J + 1) * 128], [K("ksT")], sl, i - J, mask_ap, extra=lambda J=J: Emat[:, J * 128:(J + 1) * 128])
            if prev is not None:
                pv4(prev[0], prev[1], 0, prev[0] == 0, False)
            prev = (J, pi)
        pv4(prev[0], prev[1], 0, prev[0] == 0, True)
        J0 = max(0, i - 4)
        prev = None
        for J in range(J0, i + 1):
            mask_ap = None
            if J == i:
                mask_ap = maskc[:, 17 * 128:18 * 128]
            elif J == i - 4:
                mask_ap = maskc[:, 18 * 128:19 * 128]
            pi = pair4(lambda J=J: kwT[:, J * 128:(J + 1) * 128], [K("kwT")], sl, i - J, mask_ap)
            if prev is not None:
                pv4(prev[0], prev[1], 256, prev[0] == J0, False)
            prev = (J, pi)
        pv4(prev[0], prev[1], 256, prev[0] == J0, True)
        for r in range(4):
            psw = P_sw[r]
            S.op("dve", lambda e, r=r, psw=psw: e.reciprocal(out=cf[:, 4:5], in_=psw[:, 128:129]), r=[K("P_sw", r)], w=[K("cf4")])
            S.op("dve", lambda e, r=r: e.tensor_tensor(out=cf[:, 4:5], in0=cf[:, 4:5], in1=gates[:, 3 * r + 1:3 * r + 2], op=ALU.mult), r=[K("cf4"), K("gates")], w=[K("cf4")])
            S.op("dve", lambda e, r=r, psw=psw: e.reciprocal(out=cf[:, 5:6], in_=psw[:, 384:385]), r=[K("P_sw", r)], w=[K("cf5")])
            S.op("dve", lambda e, r=r: e.tensor_tensor(out=cf[:, 5:6], in0=cf[:, 5:6], in1=gates[:, 3 * r + 2:3 * r + 3], op=ALU.mult), r=[K("cf5"), K("gates")], w=[K("cf5")])
            S.op("dve", lambda e, r=r, psw=psw: e.scalar_tensor_tensor(out=tmp[:], in0=psw[:, 0:128], scalar=cf[:, 4:5], in1=ocmp[:, r, :], op0=ALU.mult, op1=ALU.add),
                 r=[K("P_sw", r), K("cf4"), K("ocmp", r)], w=[K("tmp")])
            S.op("dve", lambda e, r=r, psw=psw: e.scalar_tensor_tensor(out=tmp[:], in0=psw[:, 256:384], scalar=cf[:, 5:6], in1=tmp[:], op0=ALU.mult, op1=ALU.add),
                 r=[K("P_sw", r), K("cf5"), K("tmp")], w=[K("tmp")])
            if dbg is not None and "sw" in dbg:
                S.op("act", lambda e, psw=psw: e.copy(out=dbgt[:], in_=psw[:]), r=[K("P_sw", r), K("P_sw", r)], w=[K("dbgt")])
                S.op("pool", lambda e, i=i, r=r: e.dma_start(out=dbg["sw"][i, r], in_=dbgt[:]), r=[K("dbgt")], w=[K("dbg", "sw", i, r)], dma=K("dbg7"))
            S.op("pool", lambda e, r=r: e.tensor_tensor(out=actt[:, r * 128:(r + 1) * 128], in0=tmp[:], in1=sg[:, r * 128:(r + 1) * 128], op=ALU.mult),
                 r=[K("tmp"), K("sg")], w=[K("actt")])
        finish_tile(S, nc, K, P_tr, K("actt"), actt, idn, K("idn"), oT, osl, j4, outT, sup * 512, keys, "nsa")
    return keys


def dram_cast(S, src, dst, tag, nchunk=8):
    R, C = src.shape
    step = (R + nchunk - 1) // nchunk
    keys = []
    for i, r0 in enumerate(range(0, R, step)):
        r1 = min(R, r0 + step)
        k = (tag, "dram", i)
        S.op("pool", lambda e, r0=r0, r1=r1: e.dma_start(out=dst[r0:r1, :], in_=src[r0:r1, :]), w=[k], dma=k)
        keys.append(k)
    return keys


def phase_out(S, nc, st, TT, xT_bf, x_tok, actT, wm_bf, wbr_bf, wout_bf, bmT, lng, lnb, out, dep, alpha):
    def sb(name, shape, dt):
        return st.enter_context(nc.sbuf_tensor("po_" + name, shape, dt))

    def psum(name, shape, dt=F32):
        return st.enter_context(nc.psum_tensor("po_" + name, shape, dt))
    K = lambda *a: ("po",) + a
    TL = 512
    xT = sb("xT", [128, 32, TL], BF16)
    act = sb("act", [128, 16, TL], BF16)
    mg = sb("mg", [128, 32, TL], BF16)
    Wm = [sb("Wm%d" % i, [128, 32, 128], BF16) for i in range(2)]
    Wb = [sb("Wb%d" % i, [128, 16, 128], BF16) for i in range(2)]
    Wo = [sb("Wo%d" % i, [128, 32, 256], BF16) for i in range(2)]
    z = sb("z", [128, 4096], F32)
    gch = [sb("gch%d" % i, [128, 1024], F32) for i in range(2)]
    bch = [sb("bch%d" % i, [128, 1024], F32) for i in range(2)]
    at = [sb("at%d" % i, [128, TL], F32) for i in range(2)]
    bm = sb("bm", [128, 96], F32)
    st6 = sb("st6", [128, 16], F32)
    junk = sb("junk", [128, 1024], F32)
    eps_t = sb("eps", [128, 1], F32)
    P_m = [psum("P_m%d" % i, [128, 512]) for i in range(2)]
    P_y = [psum("P_y%d" % i, [128, 512]) for i in range(2)]
    P_o = [psum("P_o%d" % i, [128, 512]) for i in range(4)]
    S.op("sp", lambda e: e.dma_start(out=bm[:], in_=bmT), w=[K("bm")], dma=K("bm"))
    S.op("dve", lambda e: e.memset(eps_t[:], 1e-5), w=[K("eps")])
    wmv = wm_bf.rearrange("(kb p) n -> p kb n", p=128)
    wov = wout_bf.rearrange("(kb p) n -> p kb n", p=128)
    wi = 0
    woi = 0
    gi = 0
    keys = []
    for tt in range(TT // TL):
        t0 = tt * TL
        S.op("sp", lambda e, t0=t0: e.dma_start(out=xT[:], in_=xT_bf[:, t0:t0 + TL].rearrange("(kb p) t -> p kb t", p=128)),
             r=[dep], w=[K("xT")], dma=K("xT"))
        for br in range(3):
            S.op("act", lambda e, t0=t0, br=br: e.dma_start(out=act[:], in_=actT[br][:, t0:t0 + TL].rearrange("(fb p) t -> p fb t", p=128)),
                 r=[dep], w=[K("act")], dma=K("act"))
            wbv = wbr_bf[br].rearrange("(fb p) n -> p fb n", p=128)
            for cb in range(32):
                ws = wi % 2
                wi += 1
                col = br * 4096 + cb * 128
                S.op("sp", lambda e, ws=ws, col=col: e.dma_start(out=Wm[ws][:], in_=wmv[:, :, col:col + 128]), r=[dep], w=[K("Wm", ws)], dma=K("Wm", ws))
                S.op("act", lambda e, ws=ws, cb=cb, wbv=wbv: e.dma_start(out=Wb[ws][:], in_=wbv[:, :, cb * 128:(cb + 1) * 128]), r=[dep], w=[K("Wb", ws)], dma=K("Wb", ws))

                def mm1(e, ws=ws):
                    ins = None
                    for kb in range(32):
                        ins = e.matmul(P_m[ws][:], lhsT=Wm[ws][:, kb, :], rhs=xT[:, kb, :], start=(kb == 0), stop=(kb == 31))
                    return ins
                S.op("pe", mm1, r=[K("Wm", ws), K("xT")], w=[K("P_m", ws)])

                def mm2(e, ws=ws):
                    ins = None
                    for fb in range(16):
                        ins = e.matmul(P_y[ws][:], lhsT=Wb[ws][:, fb, :], rhs=act[:, fb, :], start=(fb == 0), stop=(fb == 15))
                    return ins
                S.op("pe", mm2, r=[K("Wb", ws), K("act")], w=[K("P_y", ws)])
                bcol = br * 32 + cb
                S.op("act", lambda e, ws=ws, bcol=bcol: e.activation(out=at[ws][:], in_=P_m[ws][:], func=AF.Sigmoid, bias=bm[:, bcol:bcol + 1]),
                     r=[K("P_m", ws), K("bm")], w=[K("at", ws)])
                if br == 0:
                    S.op("dve", lambda e, ws=ws, cb=cb: e.tensor_tensor(out=mg[:, cb, :], in0=P_y[ws][:], in1=at[ws][:], op=ALU.mult),
                         r=[K("P_y", ws), K("at", ws)], w=[K("mg", cb)])
                else:
                    S.op("dve", lambda e, ws=ws: e.tensor_tensor(out=at[ws][:], in0=P_y[ws][:], in1=at[ws][:], op=ALU.mult),
                         r=[K("P_y", ws), K("at", ws)], w=[K("at", ws)])
                    S.op("pool", lambda e, ws=ws, cb=cb: e.tensor_tensor(out=mg[:, cb, :], in0=mg[:, cb, :], in1=at[ws][:], op=ALU.add),
                         r=[K("mg", cb), K("at", ws)], w=[K("mg", cb)])
        for j in range(4):
            r0 = t0 + j * 128
            S.op("sp", lambda e, r0=r0: e.dma_start(out=z[:], in_=x_tok[r0:r0 + 128, :]), w=[K("z")], dma=K("z"))
            for nb in range(16):
                wos = woi % 2
                woi += 1
                pk = woi % 4
                S.op("sp", lambda e, wos=wos, nb=nb: e.dma_start(out=Wo[wos][:], in_=wov[:, :, nb * 256:(nb + 1) * 256]), r=[dep], w=[K("Wo", wos)], dma=K("Wo", wos))

                def mm3(e, wos=wos, pk=pk, j=j):
                    ins = None
                    for cb in range(32):
                        ins = e.matmul(P_o[pk][:, 0:256], lhsT=mg[:, cb, j * 128:(j + 1) * 128], rhs=Wo[wos][:, cb, :], start=(cb == 0), stop=(cb == 31))
                    return ins
                S.op("pe", mm3, r=[K("Wo", wos)] + [K("mg", cb) for cb in range(32)], w=[K("P_o", pk)])
                S.op("dve", lambda e, pk=pk, nb=nb: e.scalar_tensor_tensor(out=z[:, nb * 256:(nb + 1) * 256], in0=z[:, nb * 256:(nb + 1) * 256], scalar=alpha,
                                                                         in1=P_o[pk][:, 0:256], op0=ALU.mult, op1=ALU.add),
                     r=[K("P_o", pk), K("z")], w=[K("z")])
            for c in range(4):
                S.op("act", lambda e, c=c: e.activation(out=junk[:], in_=z[:, c * 1024:(c + 1) * 1024], func=AF.Copy, accum_out=st6[:, c:c + 1]),
                     r=[K("z")], w=[K("junk"), K("st6", c)])
                S.op("act", lambda e, c=c: e.activation(out=junk[:], in_=z[:, c * 1024:(c + 1) * 1024], func=AF.Square, accum_out=st6[:, 4 + c:5 + c]),
                     r=[K("z")], w=[K("junk"), K("st6", 4 + c)])
            stk = [K("st6", c) for c in range(8)]
            S.op("dve", lambda e: e.tensor_reduce(out=st6[:, 8:9], in_=st6[:, 0:4], axis=AX.X, op=ALU.add), r=stk, w=[K("mean")])
            S.op("dve", lambda e: e.tensor_reduce(out=st6[:, 9:10], in_=st6[:, 4:8], axis=AX.X, op=ALU.add), r=stk, w=[K("ex2")])
            S.op("dve", lambda e: e.tensor_scalar(out=st6[:, 8:9], in0=st6[:, 8:9], scalar1=1.0 / 4096.0, scalar2=None, op0=ALU.mult), r=[K("mean")], w=[K("mean")])
            S.op("dve", lambda e: e.tensor_scalar(out=st6[:, 9:10], in0=st6[:, 9:10], scalar1=1.0 / 4096.0, scalar2=None, op0=ALU.mult), r=[K("ex2")], w=[K("ex2")])
            S.op("dve", lambda e: e.tensor_tensor(out=st6[:, 10:11], in0=st6[:, 8:9], in1=st6[:, 8:9], op=ALU.mult), r=[K("mean")], w=[K("m2")])
            S.op("dve", lambda e: e.tensor_tensor(out=st6[:, 11:12], in0=st6[:, 9:10], in1=st6[:, 10:11], op=ALU.subtract), r=[K("ex2"), K("m2")], w=[K("var")])
            S.op("act", lambda e: e.activation(out=st6[:, 12:13], in_=st6[:, 11:12], func=AF.Sqrt, bias=eps_t[:, 0:1]), r=[K("var"), K("eps")], w=[K("rstd")])
            S.op("dve", lambda e: e.reciprocal(out=st6[:, 13:14], in_=st6[:, 12:13]), r=[K("rstd")], w=[K("rstd2")])
            for c in range(4):
                gs = gi % 2
                gi += 1
                S.op("sp", lambda e, gs=gs, c=c: e.dma_start(out=gch[gs][:], in_=lng[:, c * 1024:(c + 1) * 1024]), w=[K("gch", gs)], dma=K("gch", gs))
                S.op("sp", lambda e, gs=gs, c=c: e.dma_start(out=bch[gs][:], in_=lnb[:, c * 1024:(c + 1) * 1024]), w=[K("bch", gs)], dma=K("bch", gs))
                zc = z[:, c * 1024:(c + 1) * 1024]
                S.op("dve", lambda e, zc=zc: e.tensor_scalar(out=zc, in0=zc, scalar1=st6[:, 8:9], scalar2=st6[:, 13:14], op0=ALU.subtract, op1=ALU.mult),
                     r=[K("z"), K("mean"), K("rstd2")], w=[K("z")])
                S.op("pool", lambda e, zc=zc, gs=gs: e.tensor_tensor(out=zc, in0=zc, in1=gch[gs][:], op=ALU.mult), r=[K("z"), K("gch", gs)], w=[K("z")])
                S.op("dve", lambda e, zc=zc, gs=gs: e.tensor_tensor(out=zc, in0=zc, in1=bch[gs][:], op=ALU.add), r=[K("z"), K("bch", gs)], w=[K("z")])
            k = K("out", r0)
            S.op("sp", lambda e, r0=r0: e.dma_start(out=out[r0:r0 + 128, :], in_=z[:]), r=[K("z")], w=[k], dma=K("zout"))
            keys.append(k)
    return keys


def phase_select(S, nc, st, gathered, myact, selm, NR, SEQ, TT):
    def sb(name, shape, dt):
        return st.enter_context(nc.sbuf_tensor("sel_" + name, shape, dt))
    K = lambda *a: ("sel",) + a
    C = [sb("C%d" % i, [128, 16, 512], BF16) for i in range(2)]
    acc = sb("acc", [128, 16, 512], BF16)
    m = sb("m", [128, 8], F32)
    S.op("sp", lambda e: e.dma_start(out=m[:], in_=selm), w=[K("m")], dma=K("m"))
    NB = NR // 4
    NQ = SEQ // TT
    keys = []
    ci = 0
    for br in range(3):
        for tt in range(TT // 512):
            for k in range(NB * NQ):
                bb, q = divmod(k, NQ)
                sl = ci % 2
                ci += 1
                for g in range(4):
                    r0 = (4 * bb + g) * 1536 + br * 512
                    c0 = q * TT + tt * 512
                    S.op("sp" if g % 2 == 0 else "act", lambda e, sl=sl, g=g, r0=r0, c0=c0: e.dma_start(
                        out=C[sl][:, 4 * g:4 * g + 4, :], in_=gathered[r0:r0 + 512, c0:c0 + 512].rearrange("(fb p) t -> p fb t", p=128)),
                        w=[K("C", sl, g)], dma=K("C", sl, g))
                rk = [K("C", sl, g) for g in range(4)] + [K("m")]
                if k == 0:
                    S.op("dve", lambda e, sl=sl, k=k: e.tensor_scalar(out=acc[:], in0=C[sl][:], scalar1=m[:, k:k + 1], scalar2=None, op0=ALU.mult),
                         r=rk, w=[K("acc")])
                else:
                    S.op("dve", lambda e, sl=sl, k=k: e.scalar_tensor_tensor(out=acc[:], in0=C[sl][:], scalar=m[:, k:k + 1], in1=acc[:], op0=ALU.mult, op1=ALU.add),
                         r=rk + [K("acc")], w=[K("acc")])
            kk = K("out", br, tt)
            S.op("sp", lambda e, br=br, tt=tt: e.dma_start(out=myact[br][:, tt * 512:(tt + 1) * 512].rearrange("(fb p) t -> p fb t", p=128), in_=acc[:]),
                 r=[K("acc")], w=[kk], dma=K("accst"))
            keys.append(kk)
    return keys


D_MODEL = 4096
IN_SIZES = (1024, 1024, 2048, 2048, 16, 2048, 512, 512, 512, 512, 512, 512, 2048, 48, 2048, 2048, 12288)
OFF = np.concatenate([[0], np.cumsum(IN_SIZES)]).astype(np.int64)
NF = 2176
NTM = 2576
FM_Q, FM_K, FM_NQ, FM_KC, FM_VC, FM_KS, FM_KW, FM_MQ, FM_GA = 0, 256, 512, 1024, 1152, 1280, 1408, 1536, 2048
TM_K, TM_V, TM_GZ, TM_VS, TM_VW, TM_NZ, TM_MZ, TM_BG = 0, 256, 768, 1280, 1408, 1536, 2048, 2560


def proj_cols(g):
    fm = np.concatenate([
        OFF[0] + g * 256 + np.arange(256), OFF[1] + g * 256 + np.arange(256), OFF[5] + g * 512 + np.arange(512),
        OFF[6] + g * 128 + np.arange(128), OFF[7] + g * 128 + np.arange(128), OFF[8] + g * 128 + np.arange(128),
        OFF[10] + g * 128 + np.arange(128), OFF[14] + g * 512 + np.arange(512), OFF[4] + np.arange(16)])
    tm = np.concatenate([
        OFF[1] + g * 256 + np.arange(256), OFF[2] + g * 512 + np.arange(512), OFF[3] + g * 512 + np.arange(512),
        OFF[9] + g * 128 + np.arange(128), OFF[11] + g * 128 + np.arange(128), OFF[12] + g * 512 + np.arange(512),
        OFF[15] + g * 512 + np.arange(512), OFF[13] + g * 12 + np.arange(12)])
    return fm, tm


def _ctx():
    import contextlib
    return contextlib.ExitStack()


def build_proj(SEQ):
    nc = bass.Bass("TRN2", target_bir_lowering=False)
    xT = nc.dram_tensor("xT", [4096, SEQ], F32, kind="ExternalInput").ap()
    w = nc.dram_tensor("w", [4096, NF + NTM], F32, kind="ExternalInput").ap()
    fm = nc.dram_tensor("fm", [NF, SEQ], BF16, kind="ExternalOutput").ap()
    tm = nc.dram_tensor("tm", [SEQ, NTM], BF16, kind="ExternalOutput").ap()
    xb = nc.dram_tensor("xb", [4096, SEQ], BF16).ap()
    wb = nc.dram_tensor("wb", [4096, NF + NTM], BF16).ap()
    S = Sched(nc)
    with _ctx() as st:
        sb = lambda name, shape, dt: st.enter_context(nc.sbuf_tensor(name, shape, dt))
        bufs = dict(name="gb", A=[sb("A%d" % i, [128, 32, 512], BF16) for i in range(2)], B=sb("B", [128, 32, 1024], BF16),
                    O=[sb("O%d" % i, [128, 4, 512], BF16) for i in range(2)],
                    PS=[st.enter_context(nc.psum_tensor("ps%d" % i, [128, 512], F32)) for i in range(8)])
        dummy = sb("dummyt", [128, 8], F32)
        k2 = dram_cast(S, w, wb, "cw", 8)
        k1 = dram_cast(S, xT, xb, "cx", 16)
        S.op("pool", lambda e: e.memset(dummy[:, 0:1], 0.0), r=k1, w=["xb_done"])
        S.op("pool", lambda e: e.memset(dummy[:, 1:2], 0.0), r=k2, w=["wb_done"])
        blocks = [("FM", 0, 1024, fm[0:1024, :]), ("FM", 1024, 1024, fm[1024:2048, :]), ("FM", 2048, 128, fm[2048:2176, :]),
                  ("TM", NF, 1024, tm[:, 0:1024]), ("TM", NF + 1024, 1024, tm[:, 1024:2048]), ("TM", NF + 2048, 528, tm[:, 2048:2576])]
        gemm(S, "g", xb, wb, SEQ, 4096, blocks, bufs, "xb_done", "wb_done")
        S.emit()
    return nc


def build_gla(SEQ):
    nc = bass.Bass("TRN2", target_bir_lowering=False)
    di = lambda n, s, d: nc.dram_tensor(n, list(s), d, kind="ExternalInput").ap()
    fm = di("fm", [NF, SEQ], BF16)
    tm = di("tm", [SEQ, NTM], BF16)
    wa2 = di("wa2", [17, 256], F32)
    ngb = di("ngb", [128, 512], F32)
    gcn = di("gcn", [128, 512], F32)
    idn = di("idn", [128, 128], BF16)
    o_gla = nc.dram_tensor("o_gla", [512, SEQ], BF16, kind="ExternalOutput").ap()
    S = Sched(nc)
    with _ctx() as st:
        phase_gla(S, nc, st, SEQ, fm[FM_Q:FM_Q + 256, :], fm[FM_K:FM_K + 256, :], fm[FM_GA:FM_GA + 16, :],
                  tm[:, TM_K:TM_K + 256], tm[:, TM_V:TM_V + 512], tm[:, TM_GZ:TM_GZ + 512], wa2, ngb, gcn, idn, o_gla, "nodep")
        S.emit()
    return nc


def build_mem(SEQ):
    nc = bass.Bass("TRN2", target_bir_lowering=False)
    di = lambda n, s, d: nc.dram_tensor(n, list(s), d, kind="ExternalInput").ap()
    fm = di("fm", [NF, SEQ], BF16)
    tm = di("tm", [SEQ, NTM], BF16)
    idn = di("idn", [128, 128], BF16)
    memT = di("memT", [4096, 256], F32)
    wk = di("wk", [4096, 512], F32)
    wv = di("wv", [4096, 512], F32)
    memTb = nc.dram_tensor("memTb", [4096, 256], BF16).ap()
    wkb = nc.dram_tensor("wkb", [4096, 512], BF16).ap()
    wvb = nc.dram_tensor("wvb", [4096, 512], BF16).ap()
    o_mem = nc.dram_tensor("o_mem", [512, SEQ], BF16, kind="ExternalOutput").ap()
    S = Sched(nc)
    with _ctx() as st:
        dummy = st.enter_context(nc.sbuf_tensor("dummyt", [128, 8], F32))
        ks = dram_cast(S, memT, memTb, "cm", 2) + dram_cast(S, wk, wkb, "ck", 2) + dram_cast(S, wv, wvb, "cv", 2)
        S.op("pool", lambda e: e.memset(dummy[:, 0:1], 0.0), r=ks, w=["w_done"])
        phase_mem(S, nc, st, SEQ, fm[FM_MQ:FM_MQ + 512, :], tm[:, TM_MZ:TM_MZ + 512], memTb, wkb, wvb, idn, o_mem, "nodep", "w_done")
        S.emit()
    return nc


def build_nsa(SEQ, cn):
    nc = bass.Bass("TRN2", target_bir_lowering=False)
    di = lambda n, s, d: nc.dram_tensor(n, list(s), d, kind="ExternalInput").ap()
    fm = di("fm", [NF, SEQ], BF16)
    tm = di("tm", [SEQ, NTM], BF16)
    w = {k: di(k, s, F32) for k, s in (("w1k", [4096, 128]), ("w1v", [4096, 128]), ("w2k", [128, 128]), ("w2v", [128, 128]),
                                       ("pekT", [128, 32]), ("pevT", [128, 32]))}
    cst = {k: di("c_" + k, v.shape, BF16 if v.dtype == NPBF else F32) for k, v in cn.items()}
    idn = di("idn", [128, 128], BF16)
    o_nsa = nc.dram_tensor("o_nsa", [512, SEQ], BF16, kind="ExternalOutput").ap()
    S = Sched(nc)
    with _ctx() as st:
        phase_nsa(S, nc, st, SEQ, fm[FM_NQ:FM_NQ + 512, :], fm[FM_KC:FM_KC + 128, :], fm[FM_VC:FM_VC + 128, :], fm[FM_KS:FM_KS + 128, :],
                  fm[FM_KW:FM_KW + 128, :], tm[:, TM_VS:TM_VS + 128], tm[:, TM_VW:TM_VW + 128], tm[:, TM_NZ:TM_NZ + 512], tm[:, TM_BG:TM_BG + 12],
                  w["w1k"], w["w1v"], w["w2k"], w["w2v"], w["pekT"], w["pevT"], cst, idn, o_nsa, "nodep")
        S.emit()
    return nc


def build_out(TT, alpha):
    nc = bass.Bass("TRN2", target_bir_lowering=False)
    di = lambda n, s, d: nc.dram_tensor(n, list(s), d, kind="ExternalInput").ap()
    dt = lambda n, s, d: nc.dram_tensor(n, list(s), d).ap()
    xT = di("xT", [4096, TT], F32)
    xtok = di("xtok", [TT, 4096], F32)
    actT = [di("act%d" % i, [2048, TT], BF16) for i in range(3)]
    wm = di("wm", [4096, 12288], F32)
    wbr = [di("wbr%d" % i, [2048, 4096], F32) for i in range(3)]
    wout = di("wout", [4096, 4096], F32)
    bmT = di("bmT", [128, 96], F32)
    lng = di("lng", [128, 4096], F32)
    lnb = di("lnb", [128, 4096], F32)
    out = nc.dram_tensor("out", [TT, 4096], F32, kind="ExternalOutput").ap()
    xTb = dt("xTb", [4096, TT], BF16)
    wmb = dt("wmb", [4096, 12288], BF16)
    wbrb = [dt("wbrb%d" % i, [2048, 4096], BF16) for i in range(3)]
    woutb = dt("woutb", [4096, 4096], BF16)
    S = Sched(nc)
    with _ctx() as st:
        dummy = st.enter_context(nc.sbuf_tensor("dummyt", [128, 8], F32))
        ks = dram_cast(S, xT, xTb, "cx") + dram_cast(S, wm, wmb, "cwm", 16) + dram_cast(S, wout, woutb, "cwo")
        for i in range(3):
            ks += dram_cast(S, wbr[i], wbrb[i], "cwb%d" % i)
        S.op("pool", lambda e: e.memset(dummy[:, 0:1], 0.0), r=ks, w=["w_done"])
        phase_out(S, nc, st, TT, xTb, xtok, actT, wmb, wbrb, woutb, bmT, lng, lnb, out, "w_done", alpha)
        S.emit()
    return nc


def kernel_multi(x, mem, w_in, b_merge, gla_w_a2, gla_b_a, gla_norm_g, nsa_pe_k, nsa_pe_v, nsa_wk1, nsa_wk2, nsa_wv1, nsa_wv2,
           w_mem_kv, w_br_gla, w_br_nsa, w_br_mem, w_out, ln_g, ln_b):
    f32 = lambda a: np.ascontiguousarray(np.asarray(a, dtype=np.float32))
    x = np.asarray(x, dtype=np.float32)
    B, SEQ, D = x.shape
    NCORE = 4 * B
    cores = list(range(NCORE))
    w_in0 = np.asarray(w_in, dtype=np.float32)[0]
    alpha = float((2 * 1) ** 0.25)
    ident = np.eye(128, dtype=np.float32).astype(NPBF)
    xTs = [f32(x[b].T) for b in range(B)]
    ins = []
    for c in cores:
        b, g = divmod(c, 4)
        fmc, tmc = proj_cols(g)
        wc = np.zeros((4096, NF + NTM), np.float32)
        wc[:, 0:len(fmc)] = w_in0[:, fmc]
        wc[:, NF:NF + len(tmc)] = w_in0[:, tmc]
        ins.append(dict(xT=xTs[b], w=wc))
    res = run_bass_kernel_spmd(build_proj(SEQ), ins, core_ids=cores)
    fms = [np.asarray(r["fm"]) for r in res.results]
    tms = [np.asarray(r["tm"]) for r in res.results]
    del ins, res
    gcn = gla_consts()
    ins = []
    for c in cores:
        b, g = divmod(c, 4)
        wa2 = np.concatenate([np.asarray(gla_b_a, np.float32)[0][None, g * 256:(g + 1) * 256],
                              np.asarray(gla_w_a2, np.float32)[0][:, g * 256:(g + 1) * 256]], axis=0)
        ins.append(dict(fm=fms[c], tm=tms[c], wa2=f32(wa2),
                        ngb=f32(np.broadcast_to(np.asarray(gla_norm_g, np.float32)[0][None, :], (128, 512))), gcn=gcn, idn=ident))
    res = run_bass_kernel_spmd(build_gla(SEQ), ins, core_ids=cores)
    o_gla = [np.asarray(r["o_gla"]) for r in res.results]
    del ins, res
    wkv = np.asarray(w_mem_kv, dtype=np.float32)[0]
    memTs = [f32(np.asarray(mem, dtype=np.float32)[b].T) for b in range(B)]
    ins = []
    for c in cores:
        b, g = divmod(c, 4)
        ins.append(dict(fm=fms[c], tm=tms[c], idn=ident, memT=memTs[b], wk=f32(wkv[:, g * 512:(g + 1) * 512]),
                        wv=f32(wkv[:, 2048 + g * 512:2048 + (g + 1) * 512])))
    res = run_bass_kernel_spmd(build_mem(SEQ), ins, core_ids=cores)
    o_mem = [np.asarray(r["o_mem"]) for r in res.results]
    del ins, res
    cns = [nsa_consts(SEQ, g) for g in range(4)]
    ins = []
    for c in cores:
        b, g = divmod(c, 4)
        d = dict(fm=fms[c], tm=tms[c], w1k=f32(np.asarray(nsa_wk1)[0]), w1v=f32(np.asarray(nsa_wv1)[0]), w2k=f32(np.asarray(nsa_wk2)[0]),
                 w2v=f32(np.asarray(nsa_wv2)[0]), pekT=f32(np.asarray(nsa_pe_k, np.float32)[0].T), pevT=f32(np.asarray(nsa_pe_v, np.float32)[0].T), idn=ident)
        for k, v in cns[g].items():
            d["c_" + k] = v
        ins.append(d)
    res = run_bass_kernel_spmd(build_nsa(SEQ, cns[0]), ins, core_ids=cores)
    o_nsa = [np.asarray(r["o_nsa"]) for r in res.results]
    del ins, res, fms, tms
    TT = B * SEQ // NCORE
    per_b = SEQ // TT
    wm = f32(w_in0[:, OFF[16]:OFF[17]])
    bmT = f32(np.asarray(b_merge, np.float32)[0].reshape(96, 128).T)
    lng = f32(np.broadcast_to(np.asarray(ln_g, np.float32)[0][None], (128, 4096)))
    lnb = f32(np.broadcast_to(np.asarray(ln_b, np.float32)[0][None], (128, 4096)))
    wbrs = [f32(np.asarray(wb)[0]) for wb in (w_br_gla, w_br_nsa, w_br_mem)]
    wo = f32(np.asarray(w_out)[0])
    ins = []
    for c in cores:
        b, q = divmod(c, per_b)
        sl = slice(q * TT, (q + 1) * TT)
        d = dict(xT=f32(xTs[b][:, sl]), xtok=f32(x[b, sl]), wm=wm, wbr0=wbrs[0], wbr1=wbrs[1], wbr2=wbrs[2], wout=wo, bmT=bmT, lng=lng, lnb=lnb)
        for i, oo in enumerate((o_gla, o_nsa, o_mem)):
            d["act%d" % i] = np.ascontiguousarray(np.concatenate([oo[b * 4 + g][:, sl] for g in range(4)], axis=0))
        ins.append(d)
    res = run_bass_kernel_spmd(build_out(TT, alpha), ins, core_ids=cores)
    out = np.concatenate([np.asarray(r["out"]).astype(np.float32) for r in res.results], axis=0).reshape(B, SEQ, D)
    return out


def build_fused(SEQ, NR, cn, alpha):
    import contextlib
    TT = SEQ // 4
    nc = bass.Bass("TRN2", target_bir_lowering=False)
    di = lambda n, s, d: nc.dram_tensor(n, list(s), d, kind="ExternalInput").ap()
    dt = lambda n, s, d: nc.dram_tensor(n, list(s), d).ap()
    xT = di("xT", [4096, SEQ], F32)
    w = di("w", [4096, NF + NTM], F32)
    wa2 = di("wa2", [17, 256], F32)
    ngb = di("ngb", [128, 512], F32)
    gcn = di("gcn", [128, 512], F32)
    idn = di("idn", [128, 128], BF16)
    memT = di("memT", [4096, 256], F32)
    wk = di("wk", [4096, 512], F32)
    wv = di("wv", [4096, 512], F32)
    nw = {k: di(k, s, F32) for k, s in (("w1k", [4096, 128]), ("w1v", [4096, 128]), ("w2k", [128, 128]), ("w2v", [128, 128]),
                                        ("pekT", [128, 32]), ("pevT", [128, 32]))}
    cst = {k: di("c_" + k, v.shape, BF16 if v.dtype == NPBF else F32) for k, v in cn.items()}
    xTq = di("xTq", [4096, TT], F32)
    xtok = di("xtok", [TT, 4096], F32)
    wm = di("wm", [4096, 12288], F32)
    wbr = [di("wbr%d" % i, [2048, 4096], F32) for i in range(3)]
    wout = di("wout", [4096, 4096], F32)
    bmT = di("bmT", [128, 96], F32)
    lng = di("lng", [128, 4096], F32)
    lnb = di("lnb", [128, 4096], F32)
    selm = di("selm", [128, 8], F32)
    out = nc.dram_tensor("out", [TT, 4096], F32, kind="ExternalOutput").ap()
    xb = dt("xb", [4096, SEQ], BF16)
    wb = dt("wb", [4096, NF + NTM], BF16)
    fm = dt("fm", [NF, SEQ], BF16)
    tm = dt("tm", [SEQ, NTM], BF16)
    memTb = dt("memTb", [4096, 256], BF16)
    wkb = dt("wkb", [4096, 512], BF16)
    wvb = dt("wvb", [4096, 512], BF16)
    acts = dt("acts", [1536, SEQ], BF16)
    gathered = dt("gathered", [NR * 1536, SEQ], BF16)
    myact = [dt("myact%d" % i, [2048, TT], BF16) for i in range(3)]
    xTqb = dt("xTqb", [4096, TT], BF16)
    wmb = dt("wmb", [4096, 12288], BF16)
    wbrb = [dt("wbrb%d" % i, [2048, 4096], BF16) for i in range(3)]
    woutb = dt("woutb", [4096, 4096], BF16)
    tok_src = dt("tok_src", [1, 16], F32)
    tok_dst = dt("tok_dst", [1, 16], F32)
    S = Sched(nc)
    with contextlib.ExitStack() as top:
        scr = {e: top.enter_context(nc.sbuf_tensor("scr_" + e, [1, 2], F32)) for e in ("act", "dve", "pool")}
        S.setup_phased({e: scr[e][:] for e in scr}, tok_src, tok_dst)
        with contextlib.ExitStack() as st:
            sb = lambda name, shape, dty: st.enter_context(nc.sbuf_tensor(name, shape, dty))
            bufs = dict(name="gb", A=[sb("A%d" % i, [128, 32, 512], BF16) for i in range(2)], B=sb("B", [128, 32, 1024], BF16),
                        O=[sb("O%d" % i, [128, 4, 512], BF16) for i in range(2)],
                        PS=[st.enter_context(nc.psum_tensor("ps%d" % i, [128, 512], F32)) for i in range(8)])
            dummy = sb("dummyt", [128, 8], F32)
            k2 = dram_cast(S, w, wb, "cw", 8)
            k1 = dram_cast(S, xT, xb, "cx", 16)
            S.op("pool", lambda e: e.memset(dummy[:, 0:1], 0.0), r=k1, w=["xb_done"])
            S.op("pool", lambda e: e.memset(dummy[:, 1:2], 0.0), r=k2, w=["wb_done"])
            dram_cast(S, memT, memTb, "cm", 2)
            dram_cast(S, wk, wkb, "ck", 2)
            dram_cast(S, wv, wvb, "cv", 2)
            dram_cast(S, xTq, xTqb, "cxq", 4)
            dram_cast(S, wm, wmb, "cwm", 16)
            dram_cast(S, wout, woutb, "cwo", 8)
            for i in range(3):
                dram_cast(S, wbr[i], wbrb[i], "cwb%d" % i, 4)
            blocks = [("FM", 0, 1024, fm[0:1024, :]), ("FM", 1024, 1024, fm[1024:2048, :]), ("FM", 2048, 128, fm[2048:2176, :]),
                      ("TM", NF, 1024, tm[:, 0:1024]), ("TM", NF + 1024, 1024, tm[:, 1024:2048]), ("TM", NF + 2048, 528, tm[:, 2048:2576])]
            gemm(S, "g", xb, wb, SEQ, 4096, blocks, bufs, "xb_done", "wb_done", stq="sp", ldq=("sp", "act"))
            S.flush()
        with contextlib.ExitStack() as st:
            phase_gla(S, nc, st, SEQ, fm[FM_Q:FM_Q + 256, :], fm[FM_K:FM_K + 256, :], fm[FM_GA:FM_GA + 16, :],
                      tm[:, TM_K:TM_K + 256], tm[:, TM_V:TM_V + 512], tm[:, TM_GZ:TM_GZ + 512], wa2, ngb, gcn, idn, acts[0:512, :], "nodep")
            S.flush()
        with contextlib.ExitStack() as st:
            phase_mem(S, nc, st, SEQ, fm[FM_MQ:FM_MQ + 512, :], tm[:, TM_MZ:TM_MZ + 512], memTb, wkb, wvb, idn, acts[1024:1536, :], "nodep", "nodep")
            S.flush()
        with contextlib.ExitStack() as st:
            kn = phase_nsa(S, nc, st, SEQ, fm[FM_NQ:FM_NQ + 512, :], fm[FM_KC:FM_KC + 128, :], fm[FM_VC:FM_VC + 128, :], fm[FM_KS:FM_KS + 128, :],
                           fm[FM_KW:FM_KW + 128, :], tm[:, TM_VS:TM_VS + 128], tm[:, TM_VW:TM_VW + 128], tm[:, TM_NZ:TM_NZ + 512],
                           tm[:, TM_BG:TM_BG + 12], nw["w1k"], nw["w1v"], nw["w2k"], nw["w2v"], nw["pekT"], nw["pevT"], cst, idn, acts[512:1024, :], "nodep")
            S.op("pool", lambda e: e.collective_compute("AllGather", ALU.bypass, replica_groups=[list(range(NR))], ins=[acts.opt()], outs=[gathered.opt()]),
                 r=kn, w=["gathered"], dma="cc", inc=1)
            S.flush()
        with contextlib.ExitStack() as st:
            phase_select(S, nc, st, gathered, myact, selm, NR, SEQ, TT)
            S.flush()
        with contextlib.ExitStack() as st:
            phase_out(S, nc, st, TT, xTqb, xtok, myact, wmb, wbrb, woutb, bmT, lng, lnb, out, "nodep", alpha)
            S.flush(final=True)
        S.close()
    return nc


def kernel(x, mem, w_in, b_merge, gla_w_a2, gla_b_a, gla_norm_g, nsa_pe_k, nsa_pe_v, nsa_wk1, nsa_wk2, nsa_wv1, nsa_wv2,
                 w_mem_kv, w_br_gla, w_br_nsa, w_br_mem, w_out, ln_g, ln_b):
    f32 = lambda a: np.ascontiguousarray(np.asarray(a, dtype=np.float32))
    x = np.asarray(x, dtype=np.float32)
    B, SEQ, D = x.shape
    NR = 4 * B
    TT = SEQ // 4
    cores = list(range(NR))
    w_in0 = np.asarray(w_in, dtype=np.float32)[0]
    alpha = float((2 * 1) ** 0.25)
    ident = np.eye(128, dtype=np.float32).astype(NPBF)
    xTs = [f32(x[b].T) for b in range(B)]
    memTs = [f32(np.asarray(mem, dtype=np.float32)[b].T) for b in range(B)]
    wkv = np.asarray(w_mem_kv, dtype=np.float32)[0]
    gcn = gla_consts()
    cns = [nsa_consts(SEQ, g) for g in range(4)]
    wm = f32(w_in0[:, OFF[16]:OFF[17]])
    bmT = f32(np.asarray(b_merge, np.float32)[0].reshape(96, 128).T)
    lng = f32(np.broadcast_to(np.asarray(ln_g, np.float32)[0][None], (128, 4096)))
    lnb = f32(np.broadcast_to(np.asarray(ln_b, np.float32)[0][None], (128, 4096)))
    wbrs = [f32(np.asarray(wb_)[0]) for wb_ in (w_br_gla, w_br_nsa, w_br_mem)]
    wo = f32(np.asarray(w_out)[0])
    ngb = f32(np.broadcast_to(np.asarray(gla_norm_g, np.float32)[0][None, :], (128, 512)))
    shared = dict(idn=ident, gcn=gcn, ngb=ngb, w1k=f32(np.asarray(nsa_wk1)[0]), w1v=f32(np.asarray(nsa_wv1)[0]), w2k=f32(np.asarray(nsa_wk2)[0]),
                  w2v=f32(np.asarray(nsa_wv2)[0]), pekT=f32(np.asarray(nsa_pe_k, np.float32)[0].T), pevT=f32(np.asarray(nsa_pe_v, np.float32)[0].T),
                  wm=wm, wbr0=wbrs[0], wbr1=wbrs[1], wbr2=wbrs[2], wout=wo, bmT=bmT, lng=lng, lnb=lnb)
    ins = []
    for c in cores:
        b, g = divmod(c, 4)
        fmc, tmc = proj_cols(g)
        wc = np.zeros((4096, NF + NTM), np.float32)
        wc[:, 0:len(fmc)] = w_in0[:, fmc]
        wc[:, NF:NF + len(tmc)] = w_in0[:, tmc]
        wa2 = np.concatenate([np.asarray(gla_b_a, np.float32)[0][None, g * 256:(g + 1) * 256],
                              np.asarray(gla_w_a2, np.float32)[0][:, g * 256:(g + 1) * 256]], axis=0)
        sl = slice(g * TT, (g + 1) * TT)
        selm = np.zeros((128, 8), np.float32)
        selm[:, b * 4 + g] = 1.0
        d = dict(shared)
        d.update(xT=xTs[b], w=wc, wa2=f32(wa2), memT=memTs[b], wk=f32(wkv[:, g * 512:(g + 1) * 512]),
                 wv=f32(wkv[:, 2048 + g * 512:2048 + (g + 1) * 512]), xTq=f32(xTs[b][:, sl]), xtok=f32(x[b, sl]), selm=selm)
        for k, v in cns[g].items():
            d["c_" + k] = v
        ins.append(d)
    res = run_bass_kernel_spmd(build_fused(SEQ, NR, cns[0], alpha), ins, core_ids=cores)
    out = np.concatenate([np.asarray(r["out"]).astype(np.float32) for r in res.results], axis=0).reshape(B, SEQ, D)
    return out
```

```python
import numpy as np
import ml_dtypes
import concourse.bass as bass
import concourse.mybir as mybir
from concourse.bass_utils import run_bass_kernel_spmd

F32 = mybir.dt.float32
BF16 = mybir.dt.bfloat16
AF = mybir.ActivationFunctionType
ALU = mybir.AluOpType
AX = mybir.AxisListType
NPBF = ml_dtypes.bfloat16


class Sched:
    ENGS = ("pe", "act", "dve", "pool", "sp")
    SEM_SPAN = 12000

    def __init__(self, nc):
        self.nc = nc
        self.ops = []
        self.last_w = {}
        self.readers = {}

    def op(self, eng, fn, r=(), w=(), dma=None, inc=16):
        idx = len(self.ops)
        deps = set()
        raw = set()
        for k in r:
            if k in self.last_w:
                deps.add(self.last_w[k])
                raw.add(self.last_w[k])
        for k in w:
            if k in self.last_w:
                deps.add(self.last_w[k])
            rd = self.readers.get(k)
            if rd:
                deps.update(rd.values())
        for k in r:
            d = self.readers.setdefault(k, {})
            d[("dma", idx) if dma is not None else eng] = idx
        for k in w:
            self.last_w[k] = idx
            self.readers[k] = {}
        deps.discard(idx)
        self.ops.append(dict(eng=eng, fn=fn, deps=deps, raw=raw, dma=dma, sig=False, inc=inc))
        return idx

    def setup_phased(self, scr_tiles, tok_src, tok_dst):
        import contextlib
        self.stack = contextlib.ExitStack()
        self.sems = {}
        self.cnt = {e: 0 for e in self.ENGS}
        self.dma_n = {e: 0 for e in self.ENGS}
        self.dma_cnt = {}
        self.emitted = 0
        self.scr = scr_tiles
        self.tok_src, self.tok_dst = tok_src, tok_dst
        self.bar = {}
        self.nphase = 0

    def _sem(self, key):
        if key not in self.sems:
            self.sems[key] = self.stack.enter_context(self.nc.semaphore("s%d" % len(self.sems)))
        return self.sems[key]

    def flush(self, final=False):
        nc = self.nc
        ops = self.ops
        lo = self.emitted
        cur = ops[lo:]
        self.emitted = len(ops)
        npool = {"sp": 6, "pool": 5, "act": 4, "dve": 1, "pe": 1}
        toks = []
        for e in ("act", "dve", "pool"):
            scr = self.scr[e]
            if e == "act":
                fn = (lambda eng, scr=scr: eng.copy(out=scr[:, 0:1], in_=scr[:, 1:2]))
            else:
                fn = (lambda eng, scr=scr: eng.memset(scr[:, 0:1], 0.0))
            o = dict(eng=e, fn=fn, deps=set(), raw=set(), dma=None, sig=True, inc=1, tok=True)
            toks.append(o)
        o = dict(eng="sp", fn=(lambda eng: eng.dma_start(out=self.tok_dst, in_=self.tok_src)), deps=set(), raw=set(), dma="tok", sig=False, inc=16, tok=True)
        toks.append(o)
        for i, o in enumerate(cur):
            nd = set()
            for d in o["deps"]:
                if d < lo:
                    continue
                od = ops[d]
                if od["dma"] is None and od["eng"] == o["eng"] and o["dma"] is None and (o["eng"] == "pe" or d not in o["raw"]):
                    continue
                nd.add(d)
                if od["dma"] is None:
                    od["sig"] = True
            o["deps"] = nd
        pe_ops = [o for o in cur if o["eng"] == "pe"]
        if pe_ops:
            pe_ops[-1]["sig"] = True
        allops = cur + toks
        for o in allops:
            if o["dma"] is not None:
                q = o["eng"]
                if o["dma"] == "cc":
                    k = (q, "cc")
                else:
                    k = (q, self.dma_n[q] % npool[q])
                    self.dma_n[q] += 1
                o["prev"] = ("d", k, self.dma_cnt.get(k, 0))
                self.dma_cnt[k] = self.dma_cnt.get(k, 0) + o["inc"]
                o["semv"] = ("d", k, self.dma_cnt[k])
            elif o["sig"]:
                e = o["eng"]
                c = self.cnt[e]
                self.cnt[e] += 1
                o["semv"] = ("e", (e, c // self.SEM_SPAN), c % self.SEM_SPAN + 1)
        per_eng = {e: [o for o in allops if o["eng"] == e] for e in self.ENGS}
        newbar = {}
        for o in toks:
            t, k, v = o["semv"]
            newbar[(t, k)] = v
        if pe_ops:
            t, k, v = pe_ops[-1]["semv"]
            newbar[(t, k)] = v
        oldbar = self.bar
        block = nc.Block()
        with block:
            def run(engname, engobj):
                waited = {}
                for key, v in oldbar.items():
                    waited[key] = v
                    engobj.wait_ge(self._sem(key), v)
                myops = per_eng[engname]
                for o in myops:
                    need = {}
                    if o.get("tok"):
                        for (q, j), v in self.dma_cnt.items():
                            if q == engname and not (o["dma"] is not None and ("d", (q, j)) == o["semv"][:2]):
                                need[("d", (q, j))] = v
                            elif q == engname:
                                need[("d", (q, j))] = o["prev"][2]
                    for d in o["deps"]:
                        t, k, v = ops[d]["semv"]
                        key = (t, k)
                        if need.get(key, 0) < v:
                            need[key] = v
                    if o["dma"] is not None:
                        t, k, v = o["prev"]
                        if v > 0 and need.get((t, k), 0) < v:
                            need[(t, k)] = v
                    for key, v in need.items():
                        if v <= 0 or waited.get(key, 0) >= v:
                            continue
                        waited[key] = v
                        engobj.wait_ge(self._sem(key), v)
                    ins = o["fn"](engobj)
                    if o["dma"] is not None:
                        t, k, v = o["semv"]
                        ins.then_inc(self._sem((t, k)), o["inc"])
                    elif o["sig"]:
                        t, k, v = o["semv"]
                        ins.then_inc(self._sem((t, k)), 1)
                if final:
                    for (q, j), v in self.dma_cnt.items():
                        if q == engname:
                            engobj.wait_ge(self._sem(("d", (q, j))), v)

            @block.tensor
            def _(e):
                run("pe", e)

            @block.scalar
            def _(e):
                run("act", e)

            @block.vector
            def _(e):
                run("dve", e)

            @block.gpsimd
            def _(e):
                run("pool", e)

            @block.sync
            def _(e):
                run("sp", e)
        t, k, v = toks[-1]["semv"]
        newbar[(t, k)] = v
        self.bar = dict(oldbar)
        self.bar.update(newbar)
        self.nphase += 1
        print("phase %d: %d ops, %d sems" % (self.nphase, len(cur), len(self.sems)))
        self.last_w = {}
        self.readers = {}

    def close(self):
        self.stack.close()

    def emit(self):
        nc = self.nc
        ops = self.ops
        for o in ops:
            nd = set()
            for d in o["deps"]:
                od = ops[d]
                if od["dma"] is None and od["eng"] == o["eng"] and o["dma"] is None:
                    if o["eng"] == "pe":
                        continue
                nd.add(d)
                if od["dma"] is None:
                    od["sig"] = True
            o["deps"] = nd
        cnt = {e: 0 for e in self.ENGS}
        npool = {"sp": 6, "pool": 5, "act": 4, "dve": 1, "pe": 1}
        dma_n = {e: 0 for e in self.ENGS}
        dma_cnt = {}
        for o in ops:
            if o["dma"] is not None:
                q = o["eng"]
                k = (q, dma_n[q] % npool[q])
                dma_n[q] += 1
                o["prev"] = ("d", k, dma_cnt.get(k, 0))
                dma_cnt[k] = dma_cnt.get(k, 0) + o["inc"]
                o["semv"] = ("d", k, dma_cnt[k])
            elif o["sig"]:
                e = o["eng"]
                c = cnt[e]
                cnt[e] += 1
                o["semv"] = ("e", (e, c // self.SEM_SPAN), c % self.SEM_SPAN + 1)
        sem_names = []
        for e in self.ENGS:
            for j in range((cnt[e] + self.SEM_SPAN - 1) // self.SEM_SPAN):
                sem_names.append(("e", (e, j)))
        for k in dma_cnt:
            sem_names.append(("d", k))
        import contextlib
        with contextlib.ExitStack() as st:
            sems = {}
            for i, sn in enumerate(sem_names):
                sems[sn] = st.enter_context(nc.semaphore("s%d" % i))
            block = st.enter_context(nc.Block())
            per_eng = {e: [o for o in ops if o["eng"] == e] for e in self.ENGS}

            def run(engname, engobj):
                waited = {}
                for o in per_eng[engname]:
                    need = {}
                    for d in o["deps"]:
                        t, k, v = ops[d]["semv"]
                        key = (t, k)
                        if need.get(key, 0) < v:
                            need[key] = v
                    if o["dma"] is not None:
                        t, k, v = o["prev"]
                        if v > 0 and need.get((t, k), 0) < v:
                            need[(t, k)] = v
                    for key, v in need.items():
                        if waited.get(key, 0) >= v:
                            continue
                        waited[key] = v
                        engobj.wait_ge(sems[key], v)
                    ins = o["fn"](engobj)
                    if o["dma"] is not None:
                        t, k, v = o["semv"]
                        ins.then_inc(sems[(t, k)], o["inc"])
                    elif o["sig"]:
                        t, k, v = o["semv"]
                        ins.then_inc(sems[(t, k)], 1)
                fin = {}
                for o in per_eng[engname]:
                    if o["dma"] is not None:
                        t, k, v = o["semv"]
                        fin[(t, k)] = max(fin.get((t, k), 0), v)
                for key, v in fin.items():
                    engobj.wait_ge(sems[key], v)

            @block.tensor
            def _(e):
                run("pe", e)

            @block.scalar
            def _(e):
                run("act", e)

            @block.vector
            def _(e):
                run("dve", e)

            @block.gpsimd
            def _(e):
                run("pool", e)

            @block.sync
            def _(e):
                run("sp", e)
        print("sched: %d ops, %d sems" % (len(ops), len(sem_names)))


def cast_pass(S, pool, src, dst, tag, engs=("dve", "pool"), stq="act", chunk=4096, nbuf=2):
    R, C = src.shape
    nrow = R // 128
    sv = src.rearrange("(n p) c -> p n c", p=128)
    dv = dst.rearrange("(n p) c -> p n c", p=128)
    if C >= chunk:
        assert C % chunk == 0
        steps = [(n, 1, c0, chunk) for n in range(nrow) for c0 in range(0, C, chunk)]
    else:
        nn = max(1, chunk // C)
        steps = [(n, min(nn, nrow - n), 0, C) for n in range(0, nrow, nn)]
    stg, obf, pn = pool["stg"], pool["obf"], pool["name"]
    keys = []
    for i, (n, k, c0, cw) in enumerate(steps):
        sl = i % nbuf
        sa = stg[sl][:, 0:k * cw].rearrange("p (k c) -> p k c", k=k)
        oa = obf[sl][:, 0:k * cw].rearrange("p (k c) -> p k c", k=k)
        S.op("sp", lambda e, sa=sa, n=n, k=k, c0=c0, cw=cw: e.dma_start(out=sa, in_=sv[:, n:n + k, c0:c0 + cw]),
             w=[(pn, "stg", sl)], dma=(pn, "stg", sl))
        ce = engs[i % len(engs)]
        if ce == "act":
            S.op("act", lambda e, sa=sa, oa=oa: e.copy(out=oa, in_=sa), r=[(pn, "stg", sl)], w=[(pn, "obf", sl)])
        else:
            S.op(ce, lambda e, sa=sa, oa=oa: e.tensor_copy(out=oa, in_=sa), r=[(pn, "stg", sl)], w=[(pn, "obf", sl)])
        S.op(stq, lambda e, oa=oa, n=n, k=k, c0=c0, cw=cw: e.dma_start(out=dv[:, n:n + k, c0:c0 + cw], in_=oa),
             r=[(pn, "obf", sl)], w=[(tag, "dram", i)], dma=(pn, "obf", sl))
        keys.append((tag, "dram", i))
    return keys


def gemm(S, tag, aT, bm, M, K, blocks, bufs, a_dep, b_dep, stq="act", ldq=("sp", "pool")):
    KB = K // 128
    MT = 512
    A, B, O, PS = bufs["A"], bufs["B"], bufs["O"], bufs["PS"]
    out_tag = tag
    tag = bufs["name"]
    av = aT.rearrange("(kb p) m -> p kb m", p=128)
    bv = bm.rearrange("(kb p) n -> p kb n", p=128)
    keys = []
    ai = 0
    oi = 0
    pi = 0
    for bi, (mode, n0, nw, dst) in enumerate(blocks):
        half = KB // 2
        S.op("sp", lambda e, n0=n0, nw=nw: e.dma_start(out=B[:, 0:half, 0:nw], in_=bv[:, 0:half, n0:n0 + nw]),
             r=[b_dep], w=[(tag, "B", 0)], dma=(tag, "B", 0))
        S.op(ldq[1], lambda e, n0=n0, nw=nw: e.dma_start(out=B[:, half:KB, 0:nw], in_=bv[:, half:KB, n0:n0 + nw]),
             r=[b_dep], w=[(tag, "B", 1)], dma=(tag, "B", 1))
        for m0 in range(0, M, MT):
            sl = ai % 2
            ai += 1
            At = A[sl]
            S.op("sp", lambda e, At=At, m0=m0: e.dma_start(out=At[:, 0:half, :], in_=av[:, 0:half, m0:m0 + MT]),
                 r=[a_dep], w=[(tag, "A", sl, 0)], dma=(tag, "A", sl, 0))
            S.op(ldq[1], lambda e, At=At, m0=m0: e.dma_start(out=At[:, half:KB, :], in_=av[:, half:KB, m0:m0 + MT]),
                 r=[a_dep], w=[(tag, "A", sl, 1)], dma=(tag, "A", sl, 1))
            for s0 in range(0, nw, 512):
                sw = min(512, nw - s0)
                osl = oi % 2
                oi += 1
                Ot = O[osl]
                for j in range(4):
                    if mode == "FM" and j * 128 >= sw:
                        continue
                    pk = pi % 8
                    pi += 1
                    ps = PS[pk]

                    def mm(e, ps=ps, At=At, j=j, s0=s0, sw=sw, mode=mode):
                        ins = None
                        for kb in range(KB):
                            if mode == "TM":
                                ins = e.matmul(ps[:, 0:sw], lhsT=At[:, kb, j * 128:(j + 1) * 128],
                                               rhs=B[:, kb, s0:s0 + sw], start=(kb == 0), stop=(kb == KB - 1))
                            else:
                                ins = e.matmul(ps[:, 0:MT], lhsT=B[:, kb, s0 + j * 128:s0 + (j + 1) * 128],
                                               rhs=At[:, kb, :], start=(kb == 0), stop=(kb == KB - 1))
                        return ins
                    S.op("pe", mm, r=[(tag, "A", sl, 0), (tag, "A", sl, 1), (tag, "B", 0), (tag, "B", 1)],
                         w=[(tag, "PS", pk)])
                    ww = sw if mode == "TM" else MT
                    if j % 2 == 0:
                        S.op("act", lambda e, ps=ps, Ot=Ot, j=j, ww=ww: e.copy(out=Ot[:, j, 0:ww], in_=ps[:, 0:ww]),
                             r=[(tag, "PS", pk)], w=[(tag, "O", osl, j)])
                    else:
                        S.op("dve", lambda e, ps=ps, Ot=Ot, j=j, ww=ww: e.tensor_copy(out=Ot[:, j, 0:ww], in_=ps[:, 0:ww]),
                             r=[(tag, "PS", pk)], w=[(tag, "O", osl, j)])
                if mode == "TM":
                    dv = dst[m0:m0 + MT, s0:s0 + sw].rearrange("(j p) n -> p j n", p=128)
                    src = lambda Ot=Ot, sw=sw: Ot[:, :, 0:sw]
                else:
                    nj = sw // 128
                    dv = dst[s0:s0 + sw, m0:m0 + MT].rearrange("(j p) m -> p j m", p=128)
                    src = lambda Ot=Ot, nj=nj: Ot[:, 0:nj, :]
                k = (out_tag, "out", bi, m0, s0)
                S.op(stq, lambda e, dv=dv, src=src: e.dma_start(out=dv, in_=src()),
                     r=[(tag, "O", osl, j) for j in range(4)], w=[k], dma=(tag, "O", osl))
                keys.append(k)
    return keys


def gla_consts():
    j = np.arange(128)[:, None]
    c = np.arange(128)[None, :]
    tri = np.where(j <= c, -1.0 / 16.0, 0.0).astype(np.float32)
    tri2 = np.where(j > c, -1.0 / 16.0, 0.0).astype(np.float32)
    sel = np.full((128, 1), -1.0 / 16.0, np.float32)
    mask = (j <= c).astype(np.float32)
    return np.concatenate([tri, tri2, mask, sel, np.zeros((128, 127), np.float32)], axis=1)


def phase_gla(S, nc, st, SEQ, fmq, fmk, fmga, tmk, tmv, tmgz, wa2aug, normg_b, gconst, ident, outT, dep):
    def sb(name, shape, dt):
        return st.enter_context(nc.sbuf_tensor("gla_" + name, shape, dt))

    def psum(name, shape, dt=F32):
        return st.enter_context(nc.psum_tensor("gla_" + name, shape, dt))
    NT = SEQ // 128
    qT = [sb("qT%d" % i, [128, 2, 512], BF16) for i in range(2)]
    kT = [sb("kT%d" % i, [128, 2, 512], BF16) for i in range(2)]
    gaT = [sb("gaT%d" % i, [32, 512], BF16) for i in range(2)]
    ktm = [sb("ktm%d" % i, [128, 4, 256], BF16) for i in range(2)]
    vtm = [sb("vtm%d" % i, [128, 4, 512], BF16) for i in range(2)]
    gz = [sb("gz%d" % i, [128, 4, 512], BF16) for i in range(2)]
    wa_f = sb("wa_f", [32, 256], F32)
    wa = sb("wa", [32, 256], BF16)
    ng = sb("ng", [128, 512], F32)
    gc = sb("gc", [128, 512], F32)
    idn = sb("idn", [128, 128], BF16)
    e1 = sb("e1", [128, 256], F32)
    la = sb("la", [128, 256], F32)
    Eq = sb("Eq", [128, 2, 128], F32)
    Ek = sb("Ek", [128, 2, 128], F32)
    Eend = sb("Eend", [128, 256], F32)
    dec = sb("dec", [128, 2], F32)
    qd = sb("qd", [128, 2, 128], BF16)
    kd = sb("kd", [128, 2, 128], BF16)
    kend = sb("kend", [128, 256], BF16)
    att = sb("att", [128, 128], BF16)
    S32 = sb("S32", [128, 2, 512], F32)
    Sbf = sb("Sbf", [128, 2, 512], BF16)
    sq = sb("sq", [128, 512], F32)
    ss = sb("ss", [128, 1], F32)
    rstd = sb("rstd", [128, 1], F32)
    eps_t = sb("eps_t", [128, 1], F32)
    Gz = sb("Gz", [128, 512], F32)
    actt = sb("actt", [128, 512], BF16)
    oT = [sb("oT%d" % i, [128, 4, 512], BF16) for i in range(2)]
    P_lg = psum("P_lg", [128, 512])
    P_bc = psum("P_bc", [128, 512])
    P_bT = psum("P_bT", [128, 512])
    P_att = psum("P_att", [128, 512])
    P_o = psum("P_o", [128, 512])
    P_kv = [psum("P_kv%d" % i, [128, 512]) for i in range(2)]
    P_tr = psum("P_tr", [128, 1024], BF16)

    K = lambda *a: ("gla",) + a
    S.op("sp", lambda e: e.dma_start(out=wa_f[0:17, :], in_=wa2aug), w=[K("wa_f")], dma=K("wa_f"))
    S.op("sp", lambda e: e.dma_start(out=ng[:], in_=normg_b), w=[K("ng")], dma=K("ng"))
    S.op("sp", lambda e: e.dma_start(out=gc[:], in_=gconst), w=[K("gc")], dma=K("gc"))
    S.op("sp", lambda e: e.dma_start(out=idn[:], in_=ident), w=[K("idn")], dma=K("idn"))
    S.op("dve", lambda e: e.tensor_copy(out=wa[0:17, :], in_=wa_f[0:17, :]), r=[K("wa_f")], w=[K("wa")])
    for i in range(2):
        S.op("dve", lambda e, i=i: e.memset(gaT[i][0:1, :], 1.0), w=[K("gaT1", i)])
    S.op("dve", lambda e: e.memset(eps_t[:], 1e-6), w=[K("eps")])
    S.op("dve", lambda e: e.memset(S32[:], 0.0), w=[K("S32")])
    S.op("dve", lambda e: e.memset(Sbf[:], 0.0), w=[K("Sbf")])
    tri = gc[:, 0:128]
    tri2 = gc[:, 128:256]
    msk = gc[:, 256:384]
    sel = gc[:, 384:385]
    keys = []
    for t in range(NT):
        sup, j = divmod(t, 4)
        sl = sup % 2
        t0 = sup * 512
        if j == 0:
            S.op("sp", lambda e, sl=sl, t0=t0: e.dma_start(out=qT[sl][:], in_=fmq[:, t0:t0 + 512].rearrange("(b p) t -> p b t", p=128)),
                 r=[dep], w=[K("qT", sl)], dma=K("qT", sl))
            S.op("sp", lambda e, sl=sl, t0=t0: e.dma_start(out=kT[sl][:], in_=fmk[:, t0:t0 + 512].rearrange("(b p) t -> p b t", p=128)),
                 r=[dep], w=[K("kT", sl)], dma=K("kT", sl))
            S.op("sp", lambda e, sl=sl, t0=t0: e.dma_start(out=gaT[sl][1:17, :], in_=fmga[:, t0:t0 + 512]),
                 r=[dep, K("gaT1", sl)], w=[K("gaT", sl)], dma=K("gaT", sl))
            S.op("pool", lambda e, sl=sl, t0=t0: e.dma_start(out=ktm[sl][:], in_=tmk[t0:t0 + 512, :].rearrange("(j p) c -> p j c", p=128)),
                 r=[dep], w=[K("ktm", sl)], dma=K("ktm", sl))
            S.op("pool", lambda e, sl=sl, t0=t0: e.dma_start(out=vtm[sl][:], in_=tmv[t0:t0 + 512, :].rearrange("(j p) c -> p j c", p=128)),
                 r=[dep], w=[K("vtm", sl)], dma=K("vtm", sl))
            S.op("pool", lambda e, sl=sl, t0=t0: e.dma_start(out=gz[sl][:], in_=tmgz[t0:t0 + 512, :].rearrange("(j p) c -> p j c", p=128)),
                 r=[dep], w=[K("gz", sl)], dma=K("gz", sl))
        c0 = j * 128
        S.op("pe", lambda e, sl=sl, c0=c0: e.matmul(P_lg[:, 0:256], lhsT=gaT[sl][0:17, c0:c0 + 128], rhs=wa[0:17, :], start=True, stop=True),
             r=[K("gaT", sl), K("wa")], w=[K("P_lg")])
        S.op("act", lambda e: e.activation(out=e1[:], in_=P_lg[:, 0:256], func=AF.Exp, scale=-1.0), r=[K("P_lg")], w=[K("e1")])
        S.op("act", lambda e: e.activation(out=la[:], in_=e1[:], func=AF.Ln, bias=1.0), r=[K("e1")], w=[K("la")])
        def mm_bc(e):
            e.matmul(P_bc[:, 0:256], lhsT=tri2, rhs=la[:], start=True, stop=True)
            e.matmul(P_bT[:, 0:128], lhsT=la[:, 0:128], rhs=tri, start=True, stop=True)
            e.matmul(P_bT[:, 128:256], lhsT=la[:, 128:256], rhs=tri, start=True, stop=True)
            e.matmul(P_bT[:, 256:257], lhsT=la[:, 0:128], rhs=sel, start=True, stop=True)
            return e.matmul(P_bT[:, 257:258], lhsT=la[:, 128:256], rhs=sel, start=True, stop=True)
        S.op("pe", mm_bc, r=[K("la"), K("gc")], w=[K("P_bc"), K("P_bT")])
        S.op("act", lambda e: e.activation(out=Eq[:], in_=P_bT[:, 0:256].rearrange("p (b c) -> p b c", b=2), func=AF.Exp),
             r=[K("P_bT")], w=[K("Eq")])
        S.op("act", lambda e: e.activation(out=Ek[:], in_=P_bT[:, 0:256].rearrange("p (b c) -> p b c", b=2), func=AF.Exp, scale=-1.0),
             r=[K("P_bT")], w=[K("Ek")])
        S.op("act", lambda e: e.activation(out=dec[:], in_=P_bT[:, 256:258], func=AF.Exp), r=[K("P_bT")], w=[K("dec")])
        S.op("act", lambda e: e.activation(out=Eend[:], in_=P_bc[:, 0:256], func=AF.Exp), r=[K("P_bc")], w=[K("Eend")])
        S.op("dve", lambda e, sl=sl, c0=c0: e.scalar_tensor_tensor(out=qd[:], in0=qT[sl][:, :, c0:c0 + 128], scalar=1.0 / 16.0, in1=Eq[:],
                                                                   op0=ALU.mult, op1=ALU.mult),
             r=[K("qT", sl), K("Eq")], w=[K("qd")])
        S.op("dve", lambda e, sl=sl, c0=c0: e.tensor_tensor(out=kd[:], in0=kT[sl][:, :, c0:c0 + 128], in1=Ek[:], op=ALU.mult),
             r=[K("kT", sl), K("Ek")], w=[K("kd")])
        S.op("pool", lambda e, sl=sl, j=j: e.tensor_tensor(out=kend[:], in0=ktm[sl][:, j, :], in1=Eend[:], op=ALU.mult),
             r=[K("ktm", sl), K("Eend")], w=[K("kend")])
        def mm_att(e):
            e.matmul(P_att[:, 0:128], lhsT=kd[:, 0, :], rhs=qd[:, 0, :], start=True, stop=False)
            return e.matmul(P_att[:, 0:128], lhsT=kd[:, 1, :], rhs=qd[:, 1, :], start=False, stop=True)
        S.op("pe", mm_att, r=[K("kd"), K("qd")], w=[K("P_att")])
        S.op("dve", lambda e: e.tensor_tensor(out=att[:], in0=P_att[:, 0:128], in1=msk, op=ALU.mult), r=[K("P_att"), K("gc")], w=[K("att")])
        def mm_o(e, sl=sl, j=j):
            e.matmul(P_o[:], lhsT=att[:], rhs=vtm[sl][:, j, :], start=True, stop=False)
            e.matmul(P_o[:], lhsT=qd[:, 0, :], rhs=Sbf[:, 0, :], start=False, stop=False)
            return e.matmul(P_o[:], lhsT=qd[:, 1, :], rhs=Sbf[:, 1, :], start=False, stop=True)
        S.op("pe", mm_o, r=[K("att"), K("vtm", sl), K("qd"), K("Sbf")], w=[K("P_o")])
        for b in range(2):
            S.op("pe", lambda e, b=b, sl=sl, j=j: e.matmul(P_kv[b][:], lhsT=kend[:, b * 128:(b + 1) * 128], rhs=vtm[sl][:, j, :], start=True, stop=True),
                 r=[K("kend"), K("vtm", sl)], w=[K("P_kv", b)])
            S.op("dve", lambda e, b=b: e.scalar_tensor_tensor(out=S32[:, b, :], in0=S32[:, b, :], scalar=dec[:, b:b + 1], in1=P_kv[b][:],
                                                              op0=ALU.mult, op1=ALU.add),
                 r=[K("P_kv", b), K("dec"), K("S32")], w=[K("S32")])
        S.op("act", lambda e: e.copy(out=Sbf[:], in_=S32[:]), r=[K("S32")], w=[K("Sbf")])
        S.op("act", lambda e: e.activation(out=sq[:], in_=P_o[:], func=AF.Square, accum_out=ss[:]), r=[K("P_o")], w=[K("sq"), K("ss")])
        S.op("act", lambda e: e.activation(out=rstd[:], in_=ss[:], func=AF.Sqrt, scale=1.0 / 512.0, bias=eps_t[:, 0:1]), r=[K("ss"), K("eps")], w=[K("rstd")])
        S.op("dve", lambda e: e.reciprocal(out=rstd[:], in_=rstd[:]), r=[K("rstd")], w=[K("rstd")])
        S.op("act", lambda e, sl=sl, j=j: e.activation(out=Gz[:], in_=gz[sl][:, j, :], func=AF.Silu), r=[K("gz", sl)], w=[K("Gz")])
        S.op("pool", lambda e: e.tensor_tensor(out=Gz[:], in0=Gz[:], in1=ng[:], op=ALU.mult), r=[K("Gz"), K("ng")], w=[K("Gz")])
        S.op("dve", lambda e: e.scalar_tensor_tensor(out=actt[:], in0=P_o[:], scalar=rstd[:, 0:1], in1=Gz[:], op0=ALU.mult, op1=ALU.mult),
             r=[K("P_o"), K("rstd"), K("Gz")], w=[K("actt")])
        def mm_tr(e):
            ins = None
            for b in range(4):
                ins = e.transpose(P_tr[:, b * 128:(b + 1) * 128], actt[:, b * 128:(b + 1) * 128], idn[:])
            return ins
        S.op("pe", mm_tr, r=[K("actt"), K("idn")], w=[K("P_tr")])
        S.op("act", lambda e, sl=sl, c0=c0: e.copy(out=oT[sl][:, :, c0:c0 + 128], in_=P_tr[:, 0:512].rearrange("p (b c) -> p b c", b=4)),
             r=[K("P_tr")], w=[K("oT", sl)])
        if j == 3:
            k = K("out", sup)
            S.op("sp", lambda e, sl=sl, t0=t0: e.dma_start(out=outT[:, t0:t0 + 512].rearrange("(b p) t -> p b t", p=128), in_=oT[sl][:]),
                 r=[K("oT", sl)], w=[k], dma=K("oT", sl))
            keys.append(k)
    return keys


def finish_tile(S, nc, Kf, P_tr, actt_key, actt, idn, idn_key, oT, sl, j, outT, t0, keys, tag, stq="sp"):
    def mm_tr(e):
        ins = None
        for b in range(4):
            ins = e.transpose(P_tr[:, b * 128:(b + 1) * 128], actt[:, b * 128:(b + 1) * 128], idn[:])
        return ins
    S.op("pe", mm_tr, r=[actt_key, idn_key], w=[Kf("P_tr")])
    c0 = j * 128
    S.op("act", lambda e: e.copy(out=oT[sl][:, :, c0:c0 + 128], in_=P_tr[:, 0:512].rearrange("p (b c) -> p b c", b=4)),
         r=[Kf("P_tr")], w=[Kf("oT", sl)])
    if j == 3:
        k = Kf("out", t0)
        S.op(stq, lambda e: e.dma_start(out=outT[:, t0:t0 + 512].rearrange("(b p) t -> p b t", p=128), in_=oT[sl][:]),
             r=[Kf("oT", sl)], w=[k], dma=Kf("oT", sl))
        keys.append(k)


def phase_mem(S, nc, st, SEQ, fmmq, tmmz, memT_bf, wk_bf, wv_bf, ident, outT, dep, wdep):
    def sb(name, shape, dt):
        return st.enter_context(nc.sbuf_tensor("mem_" + name, shape, dt))

    def psum(name, shape, dt=F32):
        return st.enter_context(nc.psum_tensor("mem_" + name, shape, dt))
    K = lambda *a: ("mem",) + a
    mT = sb("mT", [128, 32, 256], BF16)
    wkv = sb("wkv", [128, 32, 512], BF16)
    kT = sb("kT", [128, 4, 256], BF16)
    vv = sb("vv", [128, 2, 512], BF16)
    ones = sb("ones", [128, 1], BF16)
    idn = sb("idn", [128, 128], BF16)
    qT = [sb("qT%d" % i, [128, 4, 512], BF16) for i in range(2)]
    mz = [sb("mz%d" % i, [128, 4, 512], BF16) for i in range(2)]
    pT = sb("pT", [128, 2, 512], BF16)
    sg = sb("sg", [128, 512], F32)
    rz = sb("rz", [128, 1], F32)
    actt = sb("actt", [128, 512], BF16)
    oT = [sb("oT%d" % i, [128, 4, 512], BF16) for i in range(2)]
    P_s = [psum("P_s%d" % i, [128, 512]) for i in range(2)]
    P_o = [psum("P_o%d" % i, [128, 512]) for i in range(2)]
    P_z = psum("P_z", [128, 512])
    P_tr = psum("P_tr", [128, 1024], BF16)
    S.op("sp", lambda e: e.dma_start(out=idn[:], in_=ident), w=[K("idn")], dma=K("idn"))
    S.op("dve", lambda e: e.memset(ones[:], 1.0), w=[K("ones")])
    S.op("sp", lambda e: e.dma_start(out=mT[:], in_=memT_bf.rearrange("(kb p) m -> p kb m", p=128)), r=[wdep], w=[K("mT")], dma=K("mT"))
    S.op("sp", lambda e: e.dma_start(out=wkv[:], in_=wk_bf.rearrange("(kb p) n -> p kb n", p=128)), r=[wdep], w=[K("wkv")], dma=K("wkv"))
    for db in range(4):
        def mmk(e, db=db):
            ins = None
            for kb in range(32):
                ins = e.matmul(P_s[db % 2][:, 0:256], lhsT=wkv[:, kb, db * 128:(db + 1) * 128], rhs=mT[:, kb, :], start=(kb == 0), stop=(kb == 31))
            return ins
        S.op("pe", mmk, r=[K("wkv"), K("mT")], w=[K("P_s", db % 2)])
        S.op("act", lambda e, db=db: e.activation(out=kT[:, db, :], in_=P_s[db % 2][:, 0:256], func=AF.Copy, scale=512.0 ** -0.5),
             r=[K("P_s", db % 2)], w=[K("kT")])
    S.op("sp", lambda e: e.dma_start(out=wkv[:], in_=wv_bf.rearrange("(kb p) n -> p kb n", p=128)), r=[wdep], w=[K("wkv")], dma=K("wkv"))
    for mt in range(2):
        def mmv(e, mt=mt):
            ins = None
            for kb in range(32):
                ins = e.matmul(P_o[mt][:], lhsT=mT[:, kb, mt * 128:(mt + 1) * 128], rhs=wkv[:, kb, :], start=(kb == 0), stop=(kb == 31))
            return ins
        S.op("pe", mmv, r=[K("wkv"), K("mT")], w=[K("P_o", mt)])
        S.op("act", lambda e, mt=mt: e.copy(out=vv[:, mt, :], in_=P_o[mt][:]), r=[K("P_o", mt)], w=[K("vv")])
    keys = []
    for sup in range(SEQ // 512):
        sl = sup % 2
        t0 = sup * 512
        S.op("sp", lambda e, sl=sl, t0=t0: e.dma_start(out=qT[sl][:], in_=fmmq[:, t0:t0 + 512].rearrange("(b p) t -> p b t", p=128)),
             r=[dep], w=[K("qT", sl)], dma=K("qT", sl))
        S.op("pool", lambda e, sl=sl, t0=t0: e.dma_start(out=mz[sl][:], in_=tmmz[t0:t0 + 512, :].rearrange("(j p) c -> p j c", p=128)),
             r=[dep], w=[K("mz", sl)], dma=K("mz", sl))
        for mt in range(2):
            def mms(e, mt=mt, sl=sl):
                ins = None
                for db in range(4):
                    ins = e.matmul(P_s[mt][:], lhsT=kT[:, db, mt * 128:(mt + 1) * 128], rhs=qT[sl][:, db, :], start=(db == 0), stop=(db == 3))
                return ins
            S.op("pe", mms, r=[K("kT"), K("qT", sl)], w=[K("P_s", mt)])
            S.op("act", lambda e, mt=mt: e.activation(out=pT[:, mt, :], in_=P_s[mt][:], func=AF.Exp), r=[K("P_s", mt)], w=[K("pT", mt)])
        for j in range(4):
            pj = j % 2
            def mmo(e, j=j, pj=pj):
                e.matmul(P_o[pj][:], lhsT=pT[:, 0, j * 128:(j + 1) * 128], rhs=vv[:, 0, :], start=True, stop=False)
                e.matmul(P_o[pj][:], lhsT=pT[:, 1, j * 128:(j + 1) * 128], rhs=vv[:, 1, :], start=False, stop=True)
                e.matmul(P_z[:, j:j + 1], lhsT=pT[:, 0, j * 128:(j + 1) * 128], rhs=ones[:], start=True, stop=False)
                return e.matmul(P_z[:, j:j + 1], lhsT=pT[:, 1, j * 128:(j + 1) * 128], rhs=ones[:], start=False, stop=True)
            S.op("pe", mmo, r=[K("pT", 0), K("pT", 1), K("vv"), K("ones")], w=[K("P_o", pj), K("P_z")])
            S.op("dve", lambda e, j=j: e.reciprocal(out=rz[:], in_=P_z[:, j:j + 1]), r=[K("P_z")], w=[K("rz")])
            S.op("act", lambda e, j=j, sl=sl: e.activation(out=sg[:], in_=mz[sl][:, j, :], func=AF.Silu), r=[K("mz", sl)], w=[K("sg")])
            S.op("dve", lambda e, pj=pj: e.scalar_tensor_tensor(out=actt[:], in0=P_o[pj][:], scalar=rz[:, 0:1], in1=sg[:], op0=ALU.mult, op1=ALU.mult),
                 r=[K("P_o", pj), K("rz"), K("sg")], w=[K("actt")])
            finish_tile(S, nc, K, P_tr, K("actt"), actt, idn, K("idn"), oT, sl, j, outT, t0, keys, "mem")
    return keys


def nsa_consts(SEQ, g):
    NT = SEQ // 128
    NSEL = SEQ // 64
    NCP = ((SEQ // 16 - 1 + 127) // 128) * 128
    NCT = NCP // 128
    slopes = (2.0 ** (-8.0 * (np.arange(16) + 1.0) / 16))[4 * g:4 * g + 4].astype(np.float64)
    nrel = np.arange(128)[:, None]
    m = np.arange(NCT)[None, :, None]
    i = np.arange(NT)[None, None, :]
    n = 128 * m + nrel[:, :, None]
    arg = 16 * n + 31 - 128 * i - 64
    fut = (16 * n + 31) > (128 * i + 127)
    biasc = np.stack([np.where(fut | (s * arg < -70.0), -30000.0, s * arg) for s in slopes], axis=1)
    biasc = biasc.reshape(128, 4 * NCT * NT).astype(np.float32)
    D = np.arange(NT)[None, :]
    bs = np.stack([np.where(s * (nrel - 64 - 128 * D) < -70.0, -30000.0, s * (nrel - 64 - 128 * D)) for s in slopes], axis=1)
    bs = bs.reshape(128, 4 * NT).astype(np.float32)
    trel = np.arange(128)[None, :]
    masks = []
    for k in range(16):
        masks.append(((16 * (nrel - 8 * k) + 31) <= trel))
    masks.append(((16 * (nrel - 128) + 31) <= trel))
    masks.append(nrel <= trel)
    masks.append(nrel > trel)
    maskc = np.concatenate(masks, axis=1).astype(np.float32).astype(NPBF)
    nn = np.arange(NCP)[:, None]
    jj = np.arange(NSEL)[None, :]
    ov = ((16 * nn < 64 * jj + 64) & (16 * nn + 31 >= 64 * jj) & (nn < SEQ // 16 - 1)).astype(np.float32)
    ovl = np.concatenate([ov, np.ones((NCP, 1), np.float32), np.zeros((NCP, 1), np.float32)], axis=1).astype(NPBF)
    ovl = np.ascontiguousarray(ovl.reshape(NCT, 128, NSEL + 2).transpose(1, 0, 2))
    E = np.zeros((128, SEQ), np.float32)
    kk = np.arange(SEQ)
    if NSEL <= 128:
        E[kk // 64, kk] = 1.0
    E = E.astype(NPBF)
    t = (128 * np.arange(NT)[:, None] + np.arange(128)[None, :])[:, :, None]
    jb = np.arange(NSEL)[None, None, :]
    cur = t // 64
    causal = (jb <= cur).astype(np.float32)
    forced = ((jb == 0) | (jb == cur) | (jb == cur - 1)).astype(np.float32)
    add = (causal - 1.0) + 1e4 * forced
    tk = np.stack([causal, add], axis=2).astype(np.float32)
    return dict(biasc=biasc, bs=bs, maskc=maskc, ovl=ovl, E=E, tk=tk)


def phase_nsa(S, nc, st, SEQ, fmq, fmkc, fmvc, fmks, fmkw, tmvs, tmvw, tmnz, tmbg,
              w1k, w1v, w2k, w2v, pekT, pevT, cst, ident, outT, dep, dbg=None):
    def sb(name, shape, dt):
        return st.enter_context(nc.sbuf_tensor("nsa_" + name, shape, dt))

    def psum(name, shape, dt=F32):
        return st.enter_context(nc.psum_tensor("nsa_" + name, shape, dt))
    K = lambda *a: ("nsa",) + a
    NT = SEQ // 128
    NSEL = SEQ // 64
    NC = SEQ // 16 - 1
    NCT = (NC + 127) // 128
    NCP = NCT * 128
    WR = 128 + NSEL + 1
    SC = 128.0 ** -0.5
    assert NSEL <= 128
    idn = sb("idn", [128, 128], BF16)
    ksT = sb("ksT", [128, SEQ], BF16)
    kwT = sb("kwT", [128, SEQ], BF16)
    kcT = sb("kcT", [128, SEQ], BF16)
    vsa = sb("vsa", [128, NT, 130], BF16)
    vwa = sb("vwa", [128, NT, 130], BF16)
    Emat = sb("E", [128, SEQ], BF16)
    kcmpT = sb("kcmpT", [128, NCP], BF16)
    Rc = sb("Rc", [128, NCT, WR + 1], BF16)
    biasc = sb("biasc", [128, 4 * NCT * NT], F32)
    bs = sb("bs", [128, 4 * NT], F32)
    maskc = sb("maskc", [128, 19 * 128], BF16)
    w1f = sb("w1f", [128, 32, 128], F32)
    w1 = sb("w1", [128, 32, 128], BF16)
    w2f = sb("w2f", [128, 128], F32)
    w2 = sb("w2", [128, 128], BF16)
    pef = sb("pef", [128, 32], F32)
    peb = sb("peb", [128, 32], BF16)
    cb = sb("cb", [128, 1], F32)
    hc = sb("hc", [128, NCP], BF16)
    S.op("sp", lambda e: e.dma_start(out=idn[:], in_=ident), w=[K("idn")], dma=K("idn"))
    S.op("sp", lambda e: e.dma_start(out=ksT[:], in_=fmks), r=[dep], w=[K("ksT")], dma=K("ksT"))
    S.op("sp", lambda e: e.dma_start(out=kwT[:], in_=fmkw), r=[dep], w=[K("kwT")], dma=K("kwT"))
    S.op("pool", lambda e: e.dma_start(out=Emat[:], in_=cst["E"]), w=[K("E")], dma=K("E"))
    S.op("pool", lambda e: e.dma_start(out=biasc[:], in_=cst["biasc"]), w=[K("biasc")], dma=K("biasc"))
    S.op("pool", lambda e: e.dma_start(out=bs[:], in_=cst["bs"]), w=[K("bs")], dma=K("bs"))
    S.op("pool", lambda e: e.dma_start(out=maskc[:], in_=cst["maskc"]), w=[K("maskc")], dma=K("maskc"))
    S.op("dve", lambda e: e.memset(vsa[:], 1.0), w=[K("vsa")])
    S.op("dve", lambda e: e.memset(vwa[:], 1.0), w=[K("vwa")])
    S.op("sp", lambda e: e.dma_start(out=vsa[:, :, 0:128], in_=tmvs.rearrange("(j p) c -> p j c", p=128)), r=[dep, K("vsa")], w=[K("vsa")], dma=K("vsa"))
    S.op("sp", lambda e: e.dma_start(out=vwa[:, :, 0:128], in_=tmvw.rearrange("(j p) c -> p j c", p=128)), r=[dep, K("vwa")], w=[K("vwa")], dma=K("vwa"))
    S.op("dve", lambda e: e.memset(kcmpT[:], 0.0), w=[K("kcmpT")])
    S.op("dve", lambda e: e.memset(Rc[:], 0.0), w=[K("Rc")])
    S.op("dve", lambda e: e.memset(hc[:], 0.0), w=[K("hc")])
    S.op("pool", lambda e: e.dma_start(out=Rc[:, :, 128:WR + 1], in_=cst["ovl"]), r=[K("Rc")], w=[K("Rc")], dma=K("Rc"))
    P_sc = [psum("P_sc%d" % i, [128, 512]) for i in range(2)]
    P_big = [psum("P_big0", [128, 512]), P_sc[0]]
    P_sw = [psum("P_sw%d" % i, [128, 512]) for i in range(4)]
    P_trm = psum("P_trm", [128, 1024], BF16)
    P_tr = P_trm[:, 0:512]
    P_mT = P_trm[:, 512:1024]
    for which, (fmx, w1d, w2d, ped) in enumerate(((fmkc, w1k, w2k, pekT), (fmvc, w1v, w2v, pevT))):
        S.op("sp", lambda e, fmx=fmx: e.dma_start(out=kcT[:], in_=fmx), r=[dep], w=[K("kcT")], dma=K("kcT"))
        S.op("sp", lambda e, w1d=w1d: e.dma_start(out=w1f[:], in_=w1d.rearrange("(l d) o -> d l o", d=128)), w=[K("w1f")], dma=K("w1f"))
        S.op("sp", lambda e, w2d=w2d: e.dma_start(out=w2f[:], in_=w2d), w=[K("w2f")], dma=K("w2f"))
        S.op("sp", lambda e, ped=ped: e.dma_start(out=pef[:], in_=ped), w=[K("pef")], dma=K("pef"))
        S.op("dve", lambda e: e.tensor_copy(out=w1[:], in_=w1f[:]), r=[K("w1f")], w=[K("w1")])
        S.op("dve", lambda e: e.tensor_copy(out=w2[:], in_=w2f[:]), r=[K("w2f")], w=[K("w2")])
        S.op("dve", lambda e: e.tensor_copy(out=peb[:], in_=pef[:]), r=[K("pef")], w=[K("peb")])
        def mm_c(e):
            ins = None
            for l in range(32):
                ins = e.matmul(P_big[1][:, 0:1], lhsT=w1[:, l, :], rhs=peb[:, l:l + 1], start=(l == 0), stop=(l == 31))
            return ins
        S.op("pe", mm_c, r=[K("w1"), K("peb")], w=[K("P_sc", 0)])
        S.op("act", lambda e: e.copy(out=cb[:], in_=P_big[1][:, 0:1]), r=[K("P_sc", 0)], w=[K("cb")])
        def mm_pre(e):
            ins = None
            for l in range(32):
                ins = e.matmul(P_big[0][:, 0:NC], lhsT=w1[:, l, :], rhs=kcT[:, l:l + 16 * (NC - 1) + 1:16], start=(l == 0), stop=(l == 31))
            return ins
        S.op("pe", mm_pre, r=[K("w1"), K("kcT")], w=[K("P_big", 0)])
        S.op("act", lambda e: e.activation(out=hc[:, 0:NC], in_=P_big[0][:, 0:NC], func=AF.Silu, bias=cb[:, 0:1]), r=[K("P_big", 0), K("cb")], w=[K("hc")])
        if which == 0:
            S.op("pe", lambda e: e.matmul(P_big[0][:, 0:NC], lhsT=w2[:], rhs=hc[:, 0:NC], start=True, stop=True), r=[K("w2"), K("hc")], w=[K("P_big", 0)])
            S.op("act", lambda e: e.copy(out=kcmpT[:, 0:NC], in_=P_big[0][:, 0:NC]), r=[K("P_big", 0)], w=[K("kcmpT")])
        else:
            for nt in range(NCT):
                S.op("pe", lambda e, nt=nt: e.matmul(P_big[0][:, 0:128], lhsT=hc[:, nt * 128:(nt + 1) * 128], rhs=w2[:], start=True, stop=True),
                     r=[K("w2"), K("hc")], w=[K("P_big", 0)])
                S.op("act", lambda e, nt=nt: e.copy(out=Rc[:, nt, 0:128], in_=P_big[0][:, 0:128]), r=[K("P_big", 0)], w=[K("Rc")])
    qT = [sb("qT%d" % i, [128, 4, 128], BF16) for i in range(2)]
    nz = [sb("nz%d" % i, [128, 512], BF16) for i in range(2)]
    bgl = [sb("bg%d" % i, [128, 12], BF16) for i in range(2)]
    tkc = [sb("tk%d" % i, [128, 2, NSEL], F32) for i in range(2)]
    gates = sb("gates", [128, 12], F32)
    pT = [sb("pT%d" % i, [128, 128], BF16) for i in range(4)]
    pT4 = [sb("pT4_%d" % i, [128, 4, 128], BF16) for i in range(4)]
    negm4 = sb("negm4", [128, 4, 128], BF16)
    zc = sb("zc", [128, 4], F32)
    imp = sb("imp", [128, NSEL], F32)
    score = sb("score", [128, NSEL], F32)
    score2 = sb("score2", [128, NSEL], F32)
    mx8 = sb("mx8", [128, 8], F32)
    mx8b = sb("mx8b", [128, 8], F32)
    msel = sb("msel", [128, 128], BF16)
    negm = sb("negm", [128, 128], BF16)
    ocmp = sb("ocmp", [128, 4, 128], F32)
    cf = sb("cf", [128, 8], F32)
    tmp = sb("tmp", [128, 128], F32)
    sg = sb("sg", [128, 512], F32)
    actt = sb("actt", [128, 512], BF16)
    oT = [sb("oT%d" % i, [128, 4, 512], BF16) for i in range(2)]
    S.op("dve", lambda e: e.memset(msel[:], 0.0), w=[K("msel")])
    dbgt = sb("dbgt", [128, 512], F32) if dbg is not None else None
    keys = []
    pcount = [0]
    sccount = [0]

    def score_exp(lhsT_fn, lk, r, sl, bias_ap, bias_key, mask_ap, extra=None):
        si = sccount[0] % 2
        sccount[0] += 1
        ps = P_sc[si][:, 0:128]
        pk = K("P_sc", si)

        def mm(e):
            ins = e.matmul(ps, lhsT=lhsT_fn(), rhs=qT[sl][:, r, :], start=True, stop=(extra is None))
            if extra is not None:
                ins = e.matmul(ps, lhsT=extra(), rhs=negm[:], start=False, stop=True)
            return ins
        S.op("pe", mm, r=list(lk) + [K("qT", sl)] + ([K("negm"), K("E")] if extra is not None else []), w=[pk])
        pi = pcount[0] % 4
        pcount[0] += 1
        S.op("act", lambda e: e.activation(out=pT[pi][:], in_=ps, func=AF.Exp, scale=SC, bias=bias_ap), r=[pk, bias_key], w=[K("pT", pi)])
        if mask_ap is not None:
            S.op("dve", lambda e: e.tensor_tensor(out=pT[pi][:], in0=pT[pi][:], in1=mask_ap, op=ALU.mult), r=[K("pT", pi), K("maskc")], w=[K("pT", pi)])
        return pi

    p4count = [0]

    def pair4(lhsT_fn, lk, sl, bcol, mask_ap, extra=None):
        si = sccount[0] % 2
        sccount[0] += 1
        ps = P_sc[si]
        pk = K("P_sc", si)
        qflat = qT[sl][:].rearrange("p a b -> p (a b)")

        def mm(e):
            ins = e.matmul(ps[:, 0:512], lhsT=lhsT_fn(), rhs=qflat, start=True, stop=(extra is None))
            if extra is not None:
                ins = e.matmul(ps[:, 0:512], lhsT=extra(), rhs=negm4[:].rearrange("p a b -> p (a b)"), start=False, stop=True)
            return ins
        S.op("pe", mm, r=list(lk) + [K("qT", sl)] + ([K("negm4"), K("E")] if extra is not None else []), w=[pk])
        pi = p4count[0] % 4
        p4count[0] += 1
        for r in range(4):
            bc = r * NT + bcol
            S.op("act", lambda e, r=r, bc=bc: e.activation(out=pT4[pi][:, r, :], in_=ps[:, r * 128:(r + 1) * 128], func=AF.Exp, scale=SC, bias=bs[:, bc:bc + 1]),
                 r=[pk, K("bs")], w=[K("pT4", pi, r)])
            if mask_ap is not None:
                S.op("dve", lambda e, r=r: e.tensor_tensor(out=pT4[pi][:, r, :], in0=pT4[pi][:, r, :], in1=mask_ap, op=ALU.mult),
                     r=[K("pT4", pi, r), K("maskc")], w=[K("pT4", pi, r)])
        return pi

    for i in range(NT):
        sl = i % 2
        t0 = i * 128
        sup, j4 = divmod(i, 4)
        osl = sup % 2
        S.op("sp", lambda e, sl=sl, t0=t0: e.dma_start(out=qT[sl][:], in_=fmq[:, t0:t0 + 128].rearrange("(b p) t -> p b t", p=128)),
             r=[dep], w=[K("qT", sl)], dma=K("qT", sl))
        S.op("sp", lambda e, sl=sl, t0=t0: e.dma_start(out=nz[sl][:], in_=tmnz[t0:t0 + 128, :]), r=[dep], w=[K("nz", sl)], dma=K("nz", sl))
        S.op("sp", lambda e, sl=sl, t0=t0: e.dma_start(out=bgl[sl][:], in_=tmbg[t0:t0 + 128, :]), r=[dep], w=[K("bg", sl)], dma=K("bg", sl))
        S.op("pool", lambda e, sl=sl, i=i: e.dma_start(out=tkc[sl][:], in_=cst["tk"][i]), w=[K("tk", sl)], dma=K("tk", sl))
        S.op("act", lambda e, sl=sl: e.activation(out=gates[:], in_=bgl[sl][:], func=AF.Sigmoid), r=[K("bg", sl)], w=[K("gates")])
        mb = min((8 * i + 6) // 128, NCT - 1)
        for r in range(4):
            pis = []
            for m in range(mb + 1):
                mask_ap = None
                if m == mb:
                    kq = i % 16
                    mask_ap = maskc[:, kq * 128:(kq + 1) * 128]
                elif m == mb - 1 and i % 16 == 0:
                    mask_ap = maskc[:, 16 * 128:17 * 128]
                bcol = (r * NCT + m) * NT + i
                pi = score_exp(lambda m=m: kcmpT[:, m * 128:(m + 1) * 128], [K("kcmpT")], r, sl, biasc[:, bcol:bcol + 1], K("biasc"), mask_ap)
                pis.append((m, pi))
            pb = P_big[0]

            def mm_pv(e, pis=pis, pb=pb):
                ins = None
                for q, (m, pi) in enumerate(pis):
                    ins = e.matmul(pb[:, 0:WR], lhsT=pT[pi][:], rhs=Rc[:, m, 0:WR], start=(q == 0), stop=(q == len(pis) - 1))
                return ins
            S.op("pe", mm_pv, r=[K("pT", pi) for _, pi in pis] + [K("Rc")], w=[K("P_big", 0)])
            S.op("dve", lambda e, r=r, pb=pb: e.tensor_scalar(out=zc[:, r:r + 1], in0=pb[:, WR - 1:WR], scalar1=1e-30, scalar2=None, op0=ALU.add),
                 r=[K("P_big", 0)], w=[K("zc", r)])
            S.op("dve", lambda e, r=r: e.reciprocal(out=zc[:, r:r + 1], in_=zc[:, r:r + 1]), r=[K("zc", r)], w=[K("zc", r)])
            if r == 0:
                S.op("dve", lambda e, pb=pb: e.tensor_scalar(out=imp[:], in0=pb[:, 128:128 + NSEL], scalar1=zc[:, 0:1], scalar2=None, op0=ALU.mult),
                     r=[K("P_big", 0), K("zc", 0)], w=[K("imp")])
            else:
                S.op("dve", lambda e, r=r, pb=pb: e.scalar_tensor_tensor(out=imp[:], in0=pb[:, 128:128 + NSEL], scalar=zc[:, r:r + 1], in1=imp[:], op0=ALU.mult, op1=ALU.add),
                     r=[K("P_big", 0), K("zc", r), K("imp")], w=[K("imp")])
            S.op("dve", lambda e, r=r: e.tensor_tensor(out=cf[:, r:r + 1], in0=zc[:, r:r + 1], in1=gates[:, 3 * r:3 * r + 1], op=ALU.mult),
                 r=[K("zc", r), K("gates")], w=[K("cf", r)])
            S.op("act", lambda e, r=r, pb=pb: e.activation(out=ocmp[:, r, :], in_=pb[:, 0:128], func=AF.Copy, scale=cf[:, r:r + 1]),
                 r=[K("P_big", 0), K("cf", r)], w=[K("ocmp", r)])
        S.op("dve", lambda e, sl=sl: e.tensor_tensor(out=score[:], in0=imp[:], in1=tkc[sl][:, 0, :], op=ALU.mult), r=[K("imp"), K("tk", sl)], w=[K("score")])
        S.op("dve", lambda e, sl=sl: e.tensor_tensor(out=score[:], in0=score[:], in1=tkc[sl][:, 1, :], op=ALU.add), r=[K("score"), K("tk", sl)], w=[K("score")])
        if NSEL > 16:
            S.op("dve", lambda e: e.max(out=mx8[:], in_=score[:]), r=[K("score")], w=[K("mx8")])
            S.op("dve", lambda e: e.match_replace(out=score2[:], in_to_replace=mx8[:], in_values=score[:], imm_value=-1e30), r=[K("mx8"), K("score")], w=[K("score2")])
            S.op("dve", lambda e: e.max(out=mx8b[:], in_=score2[:]), r=[K("score2")], w=[K("mx8b")])
            S.op("dve", lambda e: e.tensor_scalar(out=msel[:, 0:NSEL], in0=score[:], scalar1=mx8b[:, 7:8], scalar2=None, op0=ALU.is_ge),
                 r=[K("score"), K("mx8b")], w=[K("msel")])
        else:
            S.op("dve", lambda e: e.memset(msel[:, 0:NSEL], 1.0), r=[K("score")], w=[K("msel")])
        S.op("pe", lambda e: e.transpose(P_mT[:, 0:128], msel[:], idn[:]), r=[K("msel"), K("idn")], w=[K("P_tr")])
        for r in range(4):
            S.op("dve", lambda e, r=r: e.tensor_scalar(out=negm4[:, r, :], in0=P_mT[:, 0:128], scalar1=-1.0, scalar2=30000.0 / SC, op0=ALU.add, op1=ALU.mult),
                 r=[K("P_tr")], w=[K("negm4")])
        if dbg is not None:
            if "imp" in dbg:
                S.op("pool", lambda e, i=i: e.dma_start(out=dbg["imp"][i], in_=imp[:]), r=[K("imp")], w=[K("dbg", "imp", i)], dma=K("dbg1"))
            if "msel" in dbg:
                S.op("pool", lambda e, i=i: e.dma_start(out=dbg["msel"][i], in_=msel[:]), r=[K("msel")], w=[K("dbg", "msel", i)], dma=K("dbg2"))
            if "ocmp" in dbg:
                S.op("pool", lambda e, i=i: e.dma_start(out=dbg["ocmp"][i], in_=ocmp[:]), r=[K("ocmp", r) for r in range(4)], w=[K("dbg", "ocmp", i)], dma=K("dbg3"))
            if "zc" in dbg:
                S.op("pool", lambda e, i=i: e.dma_start(out=dbg["zc"][i], in_=zc[:]), r=[K("zc", r) for r in range(4)], w=[K("dbg", "zc", i)], dma=K("dbg4"))
            if i == 0 and "kcmpT" in dbg:
                S.op("pool", lambda e: e.dma_start(out=dbg["kcmpT"], in_=kcmpT[:]), r=[K("kcmpT")], w=[K("dbg", "kcmpT")], dma=K("dbg5"))
                S.op("pool", lambda e: e.dma_start(out=dbg["Rc"], in_=Rc[:, 0, 0:WR - 1]), r=[K("Rc")], w=[K("dbg", "Rc")], dma=K("dbg6"))
        S.op("act", lambda e, sl=sl: e.activation(out=sg[:], in_=nz[sl][:], func=AF.Silu), r=[K("nz", sl)], w=[K("sg")])
        def pv4(J, pi, col0, first, last):
            for r in range(4):
                S.op("pe", lambda e, r=r: e.matmul(P_sw[r][:, col0:col0 + 129], lhsT=pT4[pi][:, r, :], rhs=(vsa if col0 == 0 else vwa)[:, J, 0:129], start=first, stop=last),
                     r=[K("pT4", pi, r), K("vsa" if col0 == 0 else "vwa")], w=[K("P_sw", r)])
        prev = None
        for J in range(i + 1):
            mask_ap = maskc[:, 17 * 128:18 * 128] if J == i else None
            pi = pair4(lambda J=J: ksT[:, J * 128:(J + 1) * 128], [K("ksT")], sl, i - J, mask_ap, extra=lambda J=J: Emat[:, J * 128:(J + 1) * 128])
            if prev is not None:
                pv4(prev[0], prev[1], 0, prev[0] == 0, False)
            prev = (J, pi)
        pv4(prev[0], prev[1], 0, prev[0] == 0, True)
        J0 = max(0, i - 4)
        prev = None
        for J in range(J0, i + 1):
            mask_ap = None
            if J == i:
                mask_ap = maskc[:, 17 * 128:18 * 128]
            elif J == i - 4:
                mask_ap = maskc[:, 18 * 128:19 * 128]
            pi = pair4(lambda J=J: kwT[:, J * 128:(J + 1) * 128], [K("kwT")], sl, i - J, mask_ap)
            if prev is not None:
                pv4(prev[0], prev[1], 256, prev[0] == J0, False)
            prev = (J, pi)
        pv4(prev[0], prev[1], 256, prev[0] == J0, True)
        for r in range(4):
            psw = P_sw[r]
            S.op("dve", lambda e, r=r, psw=psw: e.reciprocal(out=cf[:, 4:5], in_=psw[:, 128:129]), r=[K("P_sw", r)], w=[K("cf4")])
            S.op("dve", lambda e, r=r: e.tensor_tensor(out=cf[:, 4:5], in0=cf[:, 4:5], in1=gates[:, 3 * r + 1:3 * r + 2], op=ALU.mult), r=[K("cf4"), K("gates")], w=[K("cf4")])
            S.op("dve", lambda e, r=r, psw=psw: e.reciprocal(out=cf[:, 5:6], in_=psw[:, 384:385]), r=[K("P_sw", r)], w=[K("cf5")])
            S.op("dve", lambda e, r=r: e.tensor_tensor(out=cf[:, 5:6], in0=cf[:, 5:6], in1=gates[:, 3 * r + 2:3 * r + 3], op=ALU.mult), r=[K("cf5"), K("gates")], w=[K("cf5")])
            S.op("dve", lambda e, r=r, psw=psw: e.scalar_tensor_tensor(out=tmp[:], in0=psw[:, 0:128], scalar=cf[:, 4:5], in1=ocmp[:, r, :], op0=ALU.mult, op1=ALU.add),
                 r=[K("P_sw", r), K("cf4"), K("ocmp", r)], w=[K("tmp")])
            S.op("dve", lambda e, r=r, psw=psw: e.scalar_tensor_tensor(out=tmp[:], in0=psw[:, 256:384], scalar=cf[:, 5:6], in1=tmp[:], op0=ALU.mult, op1=ALU.add),
                 r=[K("P_sw", r), K("cf5"), K("tmp")], w=[K("tmp")])
            if dbg is not None and "sw" in dbg:
                S.op("act", lambda e, psw=psw: e.copy(out=dbgt[:], in_=psw[:]), r=[K("P_sw", r), K("P_sw", r)], w=[K("dbgt")])
                S.op("pool", lambda e, i=i, r=r: e.dma_start(out=dbg["sw"][i, r], in_=dbgt[:]), r=[K("dbgt")], w=[K("dbg", "sw", i, r)], dma=K("dbg7"))
            S.op("pool", lambda e, r=r: e.tensor_tensor(out=actt[:, r * 128:(r + 1) * 128], in0=tmp[:], in1=sg[:, r * 128:(r + 1) * 128], op=ALU.mult),
                 r=[K("tmp"), K("sg")], w=[K("actt")])
        finish_tile(S, nc, K, P_tr, K("actt"), actt, idn, K("idn"), oT, osl, j4, outT, sup * 512, keys, "nsa")
    return keys


def dram_cast(S, src, dst, tag, nchunk=8):
    R, C = src.shape
    step = (R + nchunk - 1) // nchunk
    keys = []
    for i, r0 in enumerate(range(0, R, step)):
        r1 = min(R, r0 + step)
        k = (tag, "dram", i)
        S.op("pool", lambda e, r0=r0, r1=r1: e.dma_start(out=dst[r0:r1, :], in_=src[r0:r1, :]), w=[k], dma=k)
        keys.append(k)
    return keys


def phase_out(S, nc, st, TT, xT_bf, x_tok, actT, wm_bf, wbr_bf, wout_bf, bmT, lng, lnb, out, dep, alpha):
    def sb(name, shape, dt):
        return st.enter_context(nc.sbuf_tensor("po_" + name, shape, dt))

    def psum(name, shape, dt=F32):
        return st.enter_context(nc.psum_tensor("po_" + name, shape, dt))
    K = lambda *a: ("po",) + a
    TL = 512
    xT = sb("xT", [128, 32, TL], BF16)
    act = sb("act", [128, 16, TL], BF16)
    mg = sb("mg", [128, 32, TL], BF16)
    Wm = [sb("Wm%d" % i, [128, 32, 128], BF16) for i in range(2)]
    Wb = [sb("Wb%d" % i, [128, 16, 128], BF16) for i in range(2)]
    Wo = [sb("Wo%d" % i, [128, 32, 256], BF16) for i in range(2)]
    z = sb("z", [128, 4096], F32)
    gch = [sb("gch%d" % i, [128, 1024], F32) for i in range(2)]
    bch = [sb("bch%d" % i, [128, 1024], F32) for i in range(2)]
    at = [sb("at%d" % i, [128, TL], F32) for i in range(2)]
    bm = sb("bm", [128, 96], F32)
    st6 = sb("st6", [128, 16], F32)
    junk = sb("junk", [128, 1024], F32)
    eps_t = sb("eps", [128, 1], F32)
    P_m = [psum("P_m%d" % i, [128, 512]) for i in range(2)]
    P_y = [psum("P_y%d" % i, [128, 512]) for i in range(2)]
    P_o = [psum("P_o%d" % i, [128, 512]) for i in range(4)]
    S.op("sp", lambda e: e.dma_start(out=bm[:], in_=bmT), w=[K("bm")], dma=K("bm"))
    S.op("dve", lambda e: e.memset(eps_t[:], 1e-5), w=[K("eps")])
    wmv = wm_bf.rearrange("(kb p) n -> p kb n", p=128)
    wov = wout_bf.rearrange("(kb p) n -> p kb n", p=128)
    wi = 0
    woi = 0
    gi = 0
    keys = []
    for tt in range(TT // TL):
        t0 = tt * TL
        S.op("sp", lambda e, t0=t0: e.dma_start(out=xT[:], in_=xT_bf[:, t0:t0 + TL].rearrange("(kb p) t -> p kb t", p=128)),
             r=[dep], w=[K("xT")], dma=K("xT"))
        for br in range(3):
            S.op("act", lambda e, t0=t0, br=br: e.dma_start(out=act[:], in_=actT[br][:, t0:t0 + TL].rearrange("(fb p) t -> p fb t", p=128)),
                 r=[dep], w=[K("act")], dma=K("act"))
            wbv = wbr_bf[br].rearrange("(fb p) n -> p fb n", p=128)
            for cb in range(32):
                ws = wi % 2
                wi += 1
                col = br * 4096 + cb * 128
                S.op("sp", lambda e, ws=ws, col=col: e.dma_start(out=Wm[ws][:], in_=wmv[:, :, col:col + 128]), r=[dep], w=[K("Wm", ws)], dma=K("Wm", ws))
                S.op("act", lambda e, ws=ws, cb=cb, wbv=wbv: e.dma_start(out=Wb[ws][:], in_=wbv[:, :, cb * 128:(cb + 1) * 128]), r=[dep], w=[K("Wb", ws)], dma=K("Wb", ws))

                def mm1(e, ws=ws):
                    ins = None
                    for kb in range(32):
                        ins = e.matmul(P_m[ws][:], lhsT=Wm[ws][:, kb, :], rhs=xT[:, kb, :], start=(kb == 0), stop=(kb == 31))
                    return ins
                S.op("pe", mm1, r=[K("Wm", ws), K("xT")], w=[K("P_m", ws)])

                def mm2(e, ws=ws):
                    ins = None
                    for fb in range(16):
                        ins = e.matmul(P_y[ws][:], lhsT=Wb[ws][:, fb, :], rhs=act[:, fb, :], start=(fb == 0), stop=(fb == 15))
                    return ins
                S.op("pe", mm2, r=[K("Wb", ws), K("act")], w=[K("P_y", ws)])
                bcol = br * 32 + cb
                S.op("act", lambda e, ws=ws, bcol=bcol: e.activation(out=at[ws][:], in_=P_m[ws][:], func=AF.Sigmoid, bias=bm[:, bcol:bcol + 1]),
                     r=[K("P_m", ws), K("bm")], w=[K("at", ws)])
                if br == 0:
                    S.op("dve", lambda e, ws=ws, cb=cb: e.tensor_tensor(out=mg[:, cb, :], in0=P_y[ws][:], in1=at[ws][:], op=ALU.mult),
                         r=[K("P_y", ws), K("at", ws)], w=[K("mg", cb)])
                else:
                    S.op("dve", lambda e, ws=ws: e.tensor_tensor(out=at[ws][:], in0=P_y[ws][:], in1=at[ws][:], op=ALU.mult),
                         r=[K("P_y", ws), K("at", ws)], w=[K("at", ws)])
                    S.op("pool", lambda e, ws=ws, cb=cb: e.tensor_tensor(out=mg[:, cb, :], in0=mg[:, cb, :], in1=at[ws][:], op=ALU.add),
                         r=[K("mg", cb), K("at", ws)], w=[K("mg", cb)])
        for j in range(4):
            r0 = t0 + j * 128
            S.op("sp", lambda e, r0=r0: e.dma_start(out=z[:], in_=x_tok[r0:r0 + 128, :]), w=[K("z")], dma=K("z"))
            for nb in range(16):
                wos = woi % 2
                woi += 1
                pk = woi % 4
                S.op("sp", lambda e, wos=wos, nb=nb: e.dma_start(out=Wo[wos][:], in_=wov[:, :, nb * 256:(nb + 1) * 256]), r=[dep], w=[K("Wo", wos)], dma=K("Wo", wos))

                def mm3(e, wos=wos, pk=pk, j=j):
                    ins = None
                    for cb in range(32):
                        ins = e.matmul(P_o[pk][:, 0:256], lhsT=mg[:, cb, j * 128:(j + 1) * 128], rhs=Wo[wos][:, cb, :], start=(cb == 0), stop=(cb == 31))
                    return ins
                S.op("pe", mm3, r=[K("Wo", wos)] + [K("mg", cb) for cb in range(32)], w=[K("P_o", pk)])
                S.op("dve", lambda e, pk=pk, nb=nb: e.scalar_tensor_tensor(out=z[:, nb * 256:(nb + 1) * 256], in0=z[:, nb * 256:(nb + 1) * 256], scalar=alpha,
                                                                         in1=P_o[pk][:, 0:256], op0=ALU.mult, op1=ALU.add),
                     r=[K("P_o", pk), K("z")], w=[K("z")])
            for c in range(4):
                S.op("act", lambda e, c=c: e.activation(out=junk[:], in_=z[:, c * 1024:(c + 1) * 1024], func=AF.Copy, accum_out=st6[:, c:c + 1]),
                     r=[K("z")], w=[K("junk"), K("st6", c)])
                S.op("act", lambda e, c=c: e.activation(out=junk[:], in_=z[:, c * 1024:(c + 1) * 1024], func=AF.Square, accum_out=st6[:, 4 + c:5 + c]),
                     r=[K("z")], w=[K("junk"), K("st6", 4 + c)])
            stk = [K("st6", c) for c in range(8)]
            S.op("dve", lambda e: e.tensor_reduce(out=st6[:, 8:9], in_=st6[:, 0:4], axis=AX.X, op=ALU.add), r=stk, w=[K("mean")])
            S.op("dve", lambda e: e.tensor_reduce(out=st6[:, 9:10], in_=st6[:, 4:8], axis=AX.X, op=ALU.add), r=stk, w=[K("ex2")])
            S.op("dve", lambda e: e.tensor_scalar(out=st6[:, 8:9], in0=st6[:, 8:9], scalar1=1.0 / 4096.0, scalar2=None, op0=ALU.mult), r=[K("mean")], w=[K("mean")])
            S.op("dve", lambda e: e.tensor_scalar(out=st6[:, 9:10], in0=st6[:, 9:10], scalar1=1.0 / 4096.0, scalar2=None, op0=ALU.mult), r=[K("ex2")], w=[K("ex2")])
            S.op("dve", lambda e: e.tensor_tensor(out=st6[:, 10:11], in0=st6[:, 8:9], in1=st6[:, 8:9], op=ALU.mult), r=[K("mean")], w=[K("m2")])
            S.op("dve", lambda e: e.tensor_tensor(out=st6[:, 11:12], in0=st6[:, 9:10], in1=st6[:, 10:11], op=ALU.subtract), r=[K("ex2"), K("m2")], w=[K("var")])
            S.op("act", lambda e: e.activation(out=st6[:, 12:13], in_=st6[:, 11:12], func=AF.Sqrt, bias=eps_t[:, 0:1]), r=[K("var"), K("eps")], w=[K("rstd")])
            S.op("dve", lambda e: e.reciprocal(out=st6[:, 13:14], in_=st6[:, 12:13]), r=[K("rstd")], w=[K("rstd2")])
            for c in range(4):
                gs = gi % 2
                gi += 1
                S.op("sp", lambda e, gs=gs, c=c: e.dma_start(out=gch[gs][:], in_=lng[:, c * 1024:(c + 1) * 1024]), w=[K("gch", gs)], dma=K("gch", gs))
                S.op("sp", lambda e, gs=gs, c=c: e.dma_start(out=bch[gs][:], in_=lnb[:, c * 1024:(c + 1) * 1024]), w=[K("bch", gs)], dma=K("bch", gs))
                zc = z[:, c * 1024:(c + 1) * 1024]
                S.op("dve", lambda e, zc=zc: e.tensor_scalar(out=zc, in0=zc, scalar1=st6[:, 8:9], scalar2=st6[:, 13:14], op0=ALU.subtract, op1=ALU.mult),
                     r=[K("z"), K("mean"), K("rstd2")], w=[K("z")])
                S.op("pool", lambda e, zc=zc, gs=gs: e.tensor_tensor(out=zc, in0=zc, in1=gch[gs][:], op=ALU.mult), r=[K("z"), K("gch", gs)], w=[K("z")])
                S.op("dve", lambda e, zc=zc, gs=gs: e.tensor_tensor(out=zc, in0=zc, in1=bch[gs][:], op=ALU.add), r=[K("z"), K("bch", gs)], w=[K("z")])
            k = K("out", r0)
            S.op("sp", lambda e, r0=r0: e.dma_start(out=out[r0:r0 + 128, :], in_=z[:]), r=[K("z")], w=[k], dma=K("zout"))
            keys.append(k)
    return keys


def phase_select(S, nc, st, gathered, myact, selm, NR, SEQ, TT):
    def sb(name, shape, dt):
        return st.enter_context(nc.sbuf_tensor("sel_" + name, shape, dt))
    K = lambda *a: ("sel",) + a
    C = [sb("C%d" % i, [128, 16, 512], BF16) for i in range(2)]
    acc = sb("acc", [128, 16, 512], BF16)
    m = sb("m", [128, 8], F32)
    S.op("sp", lambda e: e.dma_start(out=m[:], in_=selm), w=[K("m")], dma=K("m"))
    NB = NR // 4
    NQ = SEQ // TT
    keys = []
    ci = 0
    for br in range(3):
        for tt in range(TT // 512):
            for k in range(NB * NQ):
                bb, q = divmod(k, NQ)
                sl = ci % 2
                ci += 1
                for g in range(4):
                    r0 = (4 * bb + g) * 1536 + br * 512
                    c0 = q * TT + tt * 512
                    S.op("sp" if g % 2 == 0 else "act", lambda e, sl=sl, g=g, r0=r0, c0=c0: e.dma_start(
                        out=C[sl][:, 4 * g:4 * g + 4, :], in_=gathered[r0:r0 + 512, c0:c0 + 512].rearrange("(fb p) t -> p fb t", p=128)),
                        w=[K("C", sl, g)], dma=K("C", sl, g))
                rk = [K("C", sl, g) for g in range(4)] + [K("m")]
                if k == 0:
                    S.op("dve", lambda e, sl=sl, k=k: e.tensor_scalar(out=acc[:], in0=C[sl][:], scalar1=m[:, k:k + 1], scalar2=None, op0=ALU.mult),
                         r=rk, w=[K("acc")])
                else:
                    S.op("dve", lambda e, sl=sl, k=k: e.scalar_tensor_tensor(out=acc[:], in0=C[sl][:], scalar=m[:, k:k + 1], in1=acc[:], op0=ALU.mult, op1=ALU.add),
                         r=rk + [K("acc")], w=[K("acc")])
            kk = K("out", br, tt)
            S.op("sp", lambda e, br=br, tt=tt: e.dma_start(out=myact[br][:, tt * 512:(tt + 1) * 512].rearrange("(fb p) t -> p fb t", p=128), in_=acc[:]),
                 r=[K("acc")], w=[kk], dma=K("accst"))
            keys.append(kk)
    return keys


D_MODEL = 4096
IN_SIZES = (1024, 1024, 2048, 2048, 16, 2048, 512, 512, 512, 512, 512, 512, 2048, 48, 2048, 2048, 12288)
OFF = np.concatenate([[0], np.cumsum(IN_SIZES)]).astype(np.int64)
NF = 2176
NTM = 2576
FM_Q, FM_K, FM_NQ, FM_KC, FM_VC, FM_KS, FM_KW, FM_MQ, FM_GA = 0, 256, 512, 1024, 1152, 1280, 1408, 1536, 2048
TM_K, TM_V, TM_GZ, TM_VS, TM_VW, TM_NZ, TM_MZ, TM_BG = 0, 256, 768, 1280, 1408, 1536, 2048, 2560


def proj_cols(g):
    fm = np.concatenate([
        OFF[0] + g * 256 + np.arange(256), OFF[1] + g * 256 + np.arange(256), OFF[5] + g * 512 + np.arange(512),
        OFF[6] + g * 128 + np.arange(128), OFF[7] + g * 128 + np.arange(128), OFF[8] + g * 128 + np.arange(128),
        OFF[10] + g * 128 + np.arange(128), OFF[14] + g * 512 + np.arange(512), OFF[4] + np.arange(16)])
    tm = np.concatenate([
        OFF[1] + g * 256 + np.arange(256), OFF[2] + g * 512 + np.arange(512), OFF[3] + g * 512 + np.arange(512),
        OFF[9] + g * 128 + np.arange(128), OFF[11] + g * 128 + np.arange(128), OFF[12] + g * 512 + np.arange(512),
        OFF[15] + g * 512 + np.arange(512), OFF[13] + g * 12 + np.arange(12)])
    return fm, tm


def _ctx():
    import contextlib
    return contextlib.ExitStack()


def build_proj(SEQ):
    nc = bass.Bass("TRN2", target_bir_lowering=False)
    xT = nc.dram_tensor("xT", [4096, SEQ], F32, kind="ExternalInput").ap()
    w = nc.dram_tensor("w", [4096, NF + NTM], F32, kind="ExternalInput").ap()
    fm = nc.dram_tensor("fm", [NF, SEQ], BF16, kind="ExternalOutput").ap()
    tm = nc.dram_tensor("tm", [SEQ, NTM], BF16, kind="ExternalOutput").ap()
    xb = nc.dram_tensor("xb", [4096, SEQ], BF16).ap()
    wb = nc.dram_tensor("wb", [4096, NF + NTM], BF16).ap()
    S = Sched(nc)
    with _ctx() as st:
        sb = lambda name, shape, dt: st.enter_context(nc.sbuf_tensor(name, shape, dt))
        bufs = dict(name="gb", A=[sb("A%d" % i, [128, 32, 512], BF16) for i in range(2)], B=sb("B", [128, 32, 1024], BF16),
                    O=[sb("O%d" % i, [128, 4, 512], BF16) for i in range(2)],
                    PS=[st.enter_context(nc.psum_tensor("ps%d" % i, [128, 512], F32)) for i in range(8)])
        dummy = sb("dummyt", [128, 8], F32)
        k2 = dram_cast(S, w, wb, "cw", 8)
        k1 = dram_cast(S, xT, xb, "cx", 16)
        S.op("pool", lambda e: e.memset(dummy[:, 0:1], 0.0), r=k1, w=["xb_done"])
        S.op("pool", lambda e: e.memset(dummy[:, 1:2], 0.0), r=k2, w=["wb_done"])
        blocks = [("FM", 0, 1024, fm[0:1024, :]), ("FM", 1024, 1024, fm[1024:2048, :]), ("FM", 2048, 128, fm[2048:2176, :]),
                  ("TM", NF, 1024, tm[:, 0:1024]), ("TM", NF + 1024, 1024, tm[:, 1024:2048]), ("TM", NF + 2048, 528, tm[:, 2048:2576])]
        gemm(S, "g", xb, wb, SEQ, 4096, blocks, bufs, "xb_done", "wb_done")
        S.emit()
    return nc


def build_gla(SEQ):
    nc = bass.Bass("TRN2", target_bir_lowering=False)
    di = lambda n, s, d: nc.dram_tensor(n, list(s), d, kind="ExternalInput").ap()
    fm = di("fm", [NF, SEQ], BF16)
    tm = di("tm", [SEQ, NTM], BF16)
    wa2 = di("wa2", [17, 256], F32)
    ngb = di("ngb", [128, 512], F32)
    gcn = di("gcn", [128, 512], F32)
    idn = di("idn", [128, 128], BF16)
    o_gla = nc.dram_tensor("o_gla", [512, SEQ], BF16, kind="ExternalOutput").ap()
    S = Sched(nc)
    with _ctx() as st:
        phase_gla(S, nc, st, SEQ, fm[FM_Q:FM_Q + 256, :], fm[FM_K:FM_K + 256, :], fm[FM_GA:FM_GA + 16, :],
                  tm[:, TM_K:TM_K + 256], tm[:, TM_V:TM_V + 512], tm[:, TM_GZ:TM_GZ + 512], wa2, ngb, gcn, idn, o_gla, "nodep")
        S.emit()
    return nc


def build_mem(SEQ):
    nc = bass.Bass("TRN2", target_bir_lowering=False)
    di = lambda n, s, d: nc.dram_tensor(n, list(s), d, kind="ExternalInput").ap()
    fm = di("fm", [NF, SEQ], BF16)
    tm = di("tm", [SEQ, NTM], BF16)
    idn = di("idn", [128, 128], BF16)
    memT = di("memT", [4096, 256], F32)
    wk = di("wk", [4096, 512], F32)
    wv = di("wv", [4096, 512], F32)
    memTb = nc.dram_tensor("memTb", [4096, 256], BF16).ap()
    wkb = nc.dram_tensor("wkb", [4096, 512], BF16).ap()
    wvb = nc.dram_tensor("wvb", [4096, 512], BF16).ap()
    o_mem = nc.dram_tensor("o_mem", [512, SEQ], BF16, kind="ExternalOutput").ap()
    S = Sched(nc)
    with _ctx() as st:
        dummy = st.enter_context(nc.sbuf_tensor("dummyt", [128, 8], F32))
        ks = dram_cast(S, memT, memTb, "cm", 2) + dram_cast(S, wk, wkb, "ck", 2) + dram_cast(S, wv, wvb, "cv", 2)
        S.op("pool", lambda e: e.memset(dummy[:, 0:1], 0.0), r=ks, w=["w_done"])
        phase_mem(S, nc, st, SEQ, fm[FM_MQ:FM_MQ + 512, :], tm[:, TM_MZ:TM_MZ + 512], memTb, wkb, wvb, idn, o_mem, "nodep", "w_done")
        S.emit()
    return nc


def build_nsa(SEQ, cn):
    nc = bass.Bass("TRN2", target_bir_lowering=False)
    di = lambda n, s, d: nc.dram_tensor(n, list(s), d, kind="ExternalInput").ap()
    fm = di("fm", [NF, SEQ], BF16)
    tm = di("tm", [SEQ, NTM], BF16)
    w = {k: di(k, s, F32) for k, s in (("w1k", [4096, 128]), ("w1v", [4096, 128]), ("w2k", [128, 128]), ("w2v", [128, 128]),
                                       ("pekT", [128, 32]), ("pevT", [128, 32]))}
    cst = {k: di("c_" + k, v.shape, BF16 if v.dtype == NPBF else F32) for k, v in cn.items()}
    idn = di("idn", [128, 128], BF16)
    o_nsa = nc.dram_tensor("o_nsa", [512, SEQ], BF16, kind="ExternalOutput").ap()
    S = Sched(nc)
    with _ctx() as st:
        phase_nsa(S, nc, st, SEQ, fm[FM_NQ:FM_NQ + 512, :], fm[FM_KC:FM_KC + 128, :], fm[FM_VC:FM_VC + 128, :], fm[FM_KS:FM_KS + 128, :],
                  fm[FM_KW:FM_KW + 128, :], tm[:, TM_VS:TM_VS + 128], tm[:, TM_VW:TM_VW + 128], tm[:, TM_NZ:TM_NZ + 512], tm[:, TM_BG:TM_BG + 12],
                  w["w1k"], w["w1v"], w["w2k"], w["w2v"], w["pekT"], w["pevT"], cst, idn, o_nsa, "nodep")
        S.emit()
    return nc


def build_out(TT, alpha):
    nc = bass.Bass("TRN2", target_bir_lowering=False)
    di = lambda n, s, d: nc.dram_tensor(n, list(s), d, kind="ExternalInput").ap()
    dt = lambda n, s, d: nc.dram_tensor(n, list(s), d).ap()
    xT = di("xT", [4096, TT], F32)
    xtok = di("xtok", [TT, 4096], F32)
    actT = [di("act%d" % i, [2048, TT], BF16) for i in range(3)]
    wm = di("wm", [4096, 12288], F32)
    wbr = [di("wbr%d" % i, [2048, 4096], F32) for i in range(3)]
    wout = di("wout", [4096, 4096], F32)
    bmT = di("bmT", [128, 96], F32)
    lng = di("lng", [128, 4096], F32)
    lnb = di("lnb", [128, 4096], F32)
    out = nc.dram_tensor("out", [TT, 4096], F32, kind="ExternalOutput").ap()
    xTb = dt("xTb", [4096, TT], BF16)
    wmb = dt("wmb", [4096, 12288], BF16)
    wbrb = [dt("wbrb%d" % i, [2048, 4096], BF16) for i in range(3)]
    woutb = dt("woutb", [4096, 4096], BF16)
    S = Sched(nc)
    with _ctx() as st:
        dummy = st.enter_context(nc.sbuf_tensor("dummyt", [128, 8], F32))
        ks = dram_cast(S, xT, xTb, "cx") + dram_cast(S, wm, wmb, "cwm", 16) + dram_cast(S, wout, woutb, "cwo")
        for i in range(3):
            ks += dram_cast(S, wbr[i], wbrb[i], "cwb%d" % i)
        S.op("pool", lambda e: e.memset(dummy[:, 0:1], 0.0), r=ks, w=["w_done"])
        phase_out(S, nc, st, TT, xTb, xtok, actT, wmb, wbrb, woutb, bmT, lng, lnb, out, "w_done", alpha)
        S.emit()
    return nc


def kernel_multi(x, mem, w_in, b_merge, gla_w_a2, gla_b_a, gla_norm_g, nsa_pe_k, nsa_pe_v, nsa_wk1, nsa_wk2, nsa_wv1, nsa_wv2,
           w_mem_kv, w_br_gla, w_br_nsa, w_br_mem, w_out, ln_g, ln_b):
    f32 = lambda a: np.ascontiguousarray(np.asarray(a, dtype=np.float32))
    x = np.asarray(x, dtype=np.float32)
    B, SEQ, D = x.shape
    NCORE = 4 * B
    cores = list(range(NCORE))
    w_in0 = np.asarray(w_in, dtype=np.float32)[0]
    alpha = float((2 * 1) ** 0.25)
    ident = np.eye(128, dtype=np.float32).astype(NPBF)
    xTs = [f32(x[b].T) for b in range(B)]
    ins = []
    for c in cores:
        b, g = divmod(c, 4)
        fmc, tmc = proj_cols(g)
        wc = np.zeros((4096, NF + NTM), np.float32)
        wc[:, 0:len(fmc)] = w_in0[:, fmc]
        wc[:, NF:NF + len(tmc)] = w_in0[:, tmc]
        ins.append(dict(xT=xTs[b], w=wc))
    res = run_bass_kernel_spmd(build_proj(SEQ), ins, core_ids=cores)
    fms = [np.asarray(r["fm"]) for r in res.results]
    tms = [np.asarray(r["tm"]) for r in res.results]
    del ins, res
    gcn = gla_consts()
    ins = []
    for c in cores:
        b, g = divmod(c, 4)
        wa2 = np.concatenate([np.asarray(gla_b_a, np.float32)[0][None, g * 256:(g + 1) * 256],
                              np.asarray(gla_w_a2, np.float32)[0][:, g * 256:(g + 1) * 256]], axis=0)
        ins.append(dict(fm=fms[c], tm=tms[c], wa2=f32(wa2),
                        ngb=f32(np.broadcast_to(np.asarray(gla_norm_g, np.float32)[0][None, :], (128, 512))), gcn=gcn, idn=ident))
    res = run_bass_kernel_spmd(build_gla(SEQ), ins, core_ids=cores)
    o_gla = [np.asarray(r["o_gla"]) for r in res.results]
    del ins, res
    wkv = np.asarray(w_mem_kv, dtype=np.float32)[0]
    memTs = [f32(np.asarray(mem, dtype=np.float32)[b].T) for b in range(B)]
    ins = []
    for c in cores:
        b, g = divmod(c, 4)
        ins.append(dict(fm=fms[c], tm=tms[c], idn=ident, memT=memTs[b], wk=f32(wkv[:, g * 512:(g + 1) * 512]),
                        wv=f32(wkv[:, 2048 + g * 512:2048 + (g + 1) * 512])))
    res = run_bass_kernel_spmd(build_mem(SEQ), ins, core_ids=cores)
    o_mem = [np.asarray(r["o_mem"]) for r in res.results]
    del ins, res
    cns = [nsa_consts(SEQ, g) for g in range(4)]
    ins = []
    for c in cores:
        b, g = divmod(c, 4)
        d = dict(fm=fms[c], tm=tms[c], w1k=f32(np.asarray(nsa_wk1)[0]), w1v=f32(np.asarray(nsa_wv1)[0]), w2k=f32(np.asarray(nsa_wk2)[0]),
                 w2v=f32(np.asarray(nsa_wv2)[0]), pekT=f32(np.asarray(nsa_pe_k, np.float32)[0].T), pevT=f32(np.asarray(nsa_pe_v, np.float32)[0].T), idn=ident)
        for k, v in cns[g].items():
            d["c_" + k] = v
        ins.append(d)
    res = run_bass_kernel_spmd(build_nsa(SEQ, cns[0]), ins, core_ids=cores)
    o_nsa = [np.asarray(r["o_nsa"]) for r in res.results]
    del ins, res, fms, tms
    TT = B * SEQ // NCORE
    per_b = SEQ // TT
    wm = f32(w_in0[:, OFF[16]:OFF[17]])
    bmT = f32(np.asarray(b_merge, np.float32)[0].reshape(96, 128).T)
    lng = f32(np.broadcast_to(np.asarray(ln_g, np.float32)[0][None], (128, 4096)))
    lnb = f32(np.broadcast_to(np.asarray(ln_b, np.float32)[0][None], (128, 4096)))
    wbrs = [f32(np.asarray(wb)[0]) for wb in (w_br_gla, w_br_nsa, w_br_mem)]
    wo = f32(np.asarray(w_out)[0])
    ins = []
    for c in cores:
        b, q = divmod(c, per_b)
        sl = slice(q * TT, (q + 1) * TT)
        d = dict(xT=f32(xTs[b][:, sl]), xtok=f32(x[b, sl]), wm=wm, wbr0=wbrs[0], wbr1=wbrs[1], wbr2=wbrs[2], wout=wo, bmT=bmT, lng=lng, lnb=lnb)
        for i, oo in enumerate((o_gla, o_nsa, o_mem)):
            d["act%d" % i] = np.ascontiguousarray(np.concatenate([oo[b * 4 + g][:, sl] for g in range(4)], axis=0))
        ins.append(d)
    res = run_bass_kernel_spmd(build_out(TT, alpha), ins, core_ids=cores)
    out = np.concatenate([np.asarray(r["out"]).astype(np.float32) for r in res.results], axis=0).reshape(B, SEQ, D)
    return out


def build_fused(SEQ, NR, cn, alpha):
    import contextlib
    TT = SEQ // 4
    nc = bass.Bass("TRN2", target_bir_lowering=False)
    di = lambda n, s, d: nc.dram_tensor(n, list(s), d, kind="ExternalInput").ap()
    dt = lambda n, s, d: nc.dram_tensor(n, list(s), d).ap()
    xT = di("xT", [4096, SEQ], F32)
    w = di("w", [4096, NF + NTM], F32)
    wa2 = di("wa2", [17, 256], F32)
    ngb = di("ngb", [128, 512], F32)
    gcn = di("gcn", [128, 512], F32)
    idn = di("idn", [128, 128], BF16)
    memT = di("memT", [4096, 256], F32)
    wk = di("wk", [4096, 512], F32)
    wv = di("wv", [4096, 512], F32)
    nw = {k: di(k, s, F32) for k, s in (("w1k", [4096, 128]), ("w1v", [4096, 128]), ("w2k", [128, 128]), ("w2v", [128, 128]),
                                        ("pekT", [128, 32]), ("pevT", [128, 32]))}
    cst = {k: di("c_" + k, v.shape, BF16 if v.dtype == NPBF else F32) for k, v in cn.items()}
    xTq = di("xTq", [4096, TT], F32)
    xtok = di("xtok", [TT, 4096], F32)
    wm = di("wm", [4096, 12288], F32)
    wbr = [di("wbr%d" % i, [2048, 4096], F32) for i in range(3)]
    wout = di("wout", [4096, 4096], F32)
    bmT = di("bmT", [128, 96], F32)
    lng = di("lng", [128, 4096], F32)
    lnb = di("lnb", [128, 4096], F32)
    selm = di("selm", [128, 8], F32)
    out = nc.dram_tensor("out", [TT, 4096], F32, kind="ExternalOutput").ap()
    xb = dt("xb", [4096, SEQ], BF16)
    wb = dt("wb", [4096, NF + NTM], BF16)
    fm = dt("fm", [NF, SEQ], BF16)
    tm = dt("tm", [SEQ, NTM], BF16)
    memTb = dt("memTb", [4096, 256], BF16)
    wkb = dt("wkb", [4096, 512], BF16)
    wvb = dt("wvb", [4096, 512], BF16)
    acts = dt("acts", [1536, SEQ], BF16)
    gathered = dt("gathered", [NR * 1536, SEQ], BF16)
    myact = [dt("myact%d" % i, [2048, TT], BF16) for i in range(3)]
    xTqb = dt("xTqb", [4096, TT], BF16)
    wmb = dt("wmb", [4096, 12288], BF16)
    wbrb = [dt("wbrb%d" % i, [2048, 4096], BF16) for i in range(3)]
    woutb = dt("woutb", [4096, 4096], BF16)
    tok_src = dt("tok_src", [1, 16], F32)
    tok_dst = dt("tok_dst", [1, 16], F32)
    S = Sched(nc)
    with contextlib.ExitStack() as top:
        scr = {e: top.enter_context(nc.sbuf_tensor("scr_" + e, [1, 2], F32)) for e in ("act", "dve", "pool")}
        S.setup_phased({e: scr[e][:] for e in scr}, tok_src, tok_dst)
        with contextlib.ExitStack() as st:
            sb = lambda name, shape, dty: st.enter_context(nc.sbuf_tensor(name, shape, dty))
            bufs = dict(name="gb", A=[sb("A%d" % i, [128, 32, 512], BF16) for i in range(2)], B=sb("B", [128, 32, 1024], BF16),
                        O=[sb("O%d" % i, [128, 4, 512], BF16) for i in range(2)],
                        PS=[st.enter_context(nc.psum_tensor("ps%d" % i, [128, 512], F32)) for i in range(8)])
            dummy = sb("dummyt", [128, 8], F32)
            k2 = dram_cast(S, w, wb, "cw", 8)
            k1 = dram_cast(S, xT, xb, "cx", 16)
            S.op("pool", lambda e: e.memset(dummy[:, 0:1], 0.0), r=k1, w=["xb_done"])
            S.op("pool", lambda e: e.memset(dummy[:, 1:2], 0.0), r=k2, w=["wb_done"])
            dram_cast(S, memT, memTb, "cm", 2)
            dram_cast(S, wk, wkb, "ck", 2)
            dram_cast(S, wv, wvb, "cv", 2)
            dram_cast(S, xTq, xTqb, "cxq", 4)
            dram_cast(S, wm, wmb, "cwm", 16)
            dram_cast(S, wout, woutb, "cwo", 8)
            for i in range(3):
                dram_cast(S, wbr[i], wbrb[i], "cwb%d" % i, 4)
            blocks = [("FM", 0, 1024, fm[0:1024, :]), ("FM", 1024, 1024, fm[1024:2048, :]), ("FM", 2048, 128, fm[2048:2176, :]),
                      ("TM", NF, 1024, tm[:, 0:1024]), ("TM", NF + 1024, 1024, tm[:, 1024:2048]), ("TM", NF + 2048, 528, tm[:, 2048:2576])]
            gemm(S, "g", xb, wb, SEQ, 4096, blocks, bufs, "xb_done", "wb_done", stq="sp", ldq=("sp", "act"))
            S.flush()
        with contextlib.ExitStack() as st:
            phase_gla(S, nc, st, SEQ, fm[FM_Q:FM_Q + 256, :], fm[FM_K:FM_K + 256, :], fm[FM_GA:FM_GA + 16, :],
                      tm[:, TM_K:TM_K + 256], tm[:, TM_V:TM_V + 512], tm[:, TM_GZ:TM_GZ + 512], wa2, ngb, gcn, idn, acts[0:512, :], "nodep")
            S.flush()
        with contextlib.ExitStack() as st:
            phase_mem(S, nc, st, SEQ, fm[FM_MQ:FM_MQ + 512, :], tm[:, TM_MZ:TM_MZ + 512], memTb, wkb, wvb, idn, acts[1024:1536, :], "nodep", "nodep")
            S.flush()
        with contextlib.ExitStack() as st:
            kn = phase_nsa(S, nc, st, SEQ, fm[FM_NQ:FM_NQ + 512, :], fm[FM_KC:FM_KC + 128, :], fm[FM_VC:FM_VC + 128, :], fm[FM_KS:FM_KS + 128, :],
                           fm[FM_KW:FM_KW + 128, :], tm[:, TM_VS:TM_VS + 128], tm[:, TM_VW:TM_VW + 128], tm[:, TM_NZ:TM_NZ + 512],
                           tm[:, TM_BG:TM_BG + 12], nw["w1k"], nw["w1v"], nw["w2k"], nw["w2v"], nw["pekT"], nw["pevT"], cst, idn, acts[512:1024, :], "nodep")
            S.op("pool", lambda e: e.collective_compute("AllGather", ALU.bypass, replica_groups=[list(range(NR))], ins=[acts.opt()], outs=[gathered.opt()]),
                 r=kn, w=["gathered"], dma="cc", inc=1)
            S.flush()
        with contextlib.ExitStack() as st:
            phase_select(S, nc, st, gathered, myact, selm, NR, SEQ, TT)
            S.flush()
        with contextlib.ExitStack() as st:
            phase_out(S, nc, st, TT, xTqb, xtok, myact, wmb, wbrb, woutb, bmT, lng, lnb, out, "nodep", alpha)
            S.flush(final=True)
        S.close()
    return nc


def kernel(x, mem, w_in, b_merge, gla_w_a2, gla_b_a, gla_norm_g, nsa_pe_k, nsa_pe_v, nsa_wk1, nsa_wk2, nsa_wv1, nsa_wv2,
                 w_mem_kv, w_br_gla, w_br_nsa, w_br_mem, w_out, ln_g, ln_b):
    f32 = lambda a: np.ascontiguousarray(np.asarray(a, dtype=np.float32))
    x = np.asarray(x, dtype=np.float32)
    B, SEQ, D = x.shape
    NR = 4 * B
    TT = SEQ // 4
    cores = list(range(NR))
    w_in0 = np.asarray(w_in, dtype=np.float32)[0]
    alpha = float((2 * 1) ** 0.25)
    ident = np.eye(128, dtype=np.float32).astype(NPBF)
    xTs = [f32(x[b].T) for b in range(B)]
    memTs = [f32(np.asarray(mem, dtype=np.float32)[b].T) for b in range(B)]
    wkv = np.asarray(w_mem_kv, dtype=np.float32)[0]
    gcn = gla_consts()
    cns = [nsa_consts(SEQ, g) for g in range(4)]
    wm = f32(w_in0[:, OFF[16]:OFF[17]])
    bmT = f32(np.asarray(b_merge, np.float32)[0].reshape(96, 128).T)
    lng = f32(np.broadcast_to(np.asarray(ln_g, np.float32)[0][None], (128, 4096)))
    lnb = f32(np.broadcast_to(np.asarray(ln_b, np.float32)[0][None], (128, 4096)))
    wbrs = [f32(np.asarray(wb_)[0]) for wb_ in (w_br_gla, w_br_nsa, w_br_mem)]
    wo = f32(np.asarray(w_out)[0])
    ngb = f32(np.broadcast_to(np.asarray(gla_norm_g, np.float32)[0][None, :], (128, 512)))
    shared = dict(idn=ident, gcn=gcn, ngb=ngb, w1k=f32(np.asarray(nsa_wk1)[0]), w1v=f32(np.asarray(nsa_wv1)[0]), w2k=f32(np.asarray(nsa_wk2)[0]),
                  w2v=f32(np.asarray(nsa_wv2)[0]), pekT=f32(np.asarray(nsa_pe_k, np.float32)[0].T), pevT=f32(np.asarray(nsa_pe_v, np.float32)[0].T),
                  wm=wm, wbr0=wbrs[0], wbr1=wbrs[1], wbr2=wbrs[2], wout=wo, bmT=bmT, lng=lng, lnb=lnb)
    ins = []
    for c in cores:
        b, g = divmod(c, 4)
        fmc, tmc = proj_cols(g)
        wc = np.zeros((4096, NF + NTM), np.float32)
        wc[:, 0:len(fmc)] = w_in0[:, fmc]
        wc[:, NF:NF + len(tmc)] = w_in0[:, tmc]
        wa2 = np.concatenate([np.asarray(gla_b_a, np.float32)[0][None, g * 256:(g + 1) * 256],
                              np.asarray(gla_w_a2, np.float32)[0][:, g * 256:(g + 1) * 256]], axis=0)
        sl = slice(g * TT, (g + 1) * TT)
        selm = np.zeros((128, 8), np.float32)
        selm[:, b * 4 + g] = 1.0
        d = dict(shared)
        d.update(xT=xTs[b], w=wc, wa2=f32(wa2), memT=memTs[b], wk=f32(wkv[:, g * 512:(g + 1) * 512]),
                 wv=f32(wkv[:, 2048 + g * 512:2048 + (g + 1) * 512]), xTq=f32(xTs[b][:, sl]), xtok=f32(x[b, sl]), selm=selm)
        for k, v in cns[g].items():
            d["c_" + k] = v
        ins.append(d)
    res = run_bass_kernel_spmd(build_fused(SEQ, NR, cns[0], alpha), ins, core_ids=cores)
    out = np.concatenate([np.asarray(r["out"]).astype(np.float32) for r in res.results], axis=0).reshape(B, SEQ, D)
    return out
```

```python
import numpy as np
import ml_dtypes
import concourse.bass as bass
import concourse.mybir as mybir
from concourse.bass_utils import run_bass_kernel_spmd

F32 = mybir.dt.float32
BF16 = mybir.dt.bfloat16
AF = mybir.ActivationFunctionType
ALU = mybir.AluOpType
AX = mybir.AxisListType
NPBF = ml_dtypes.bfloat16


class Sched:
    ENGS = ("pe", "act", "dve", "pool", "sp")
    SEM_SPAN = 12000

    def __init__(self, nc):
        self.nc = nc
        self.ops = []
        self.last_w = {}
        self.readers = {}

    def op(self, eng, fn, r=(), w=(), dma=None, inc=16):
        idx = len(self.ops)
        deps = set()
        raw = set()
        for k in r:
            if k in self.last_w:
                deps.add(self.last_w[k])
                raw.add(self.last_w[k])
        for k in w:
            if k in self.last_w:
                deps.add(self.last_w[k])
            rd = self.readers.get(k)
            if rd:
                deps.update(rd.values())
        for k in r:
            d = self.readers.setdefault(k, {})
            d[("dma", idx) if dma is not None else eng] = idx
        for k in w:
            self.last_w[k] = idx
            self.readers[k] = {}
        deps.discard(idx)
        self.ops.append(dict(eng=eng, fn=fn, deps=deps, raw=raw, dma=dma, sig=False, inc=inc))
        return idx

    def setup_phased(self, scr_tiles, tok_src, tok_dst):
        import contextlib
        self.stack = contextlib.ExitStack()
        self.sems = {}
        self.cnt = {e: 0 for e in self.ENGS}
        self.dma_n = {e: 0 for e in self.ENGS}
        self.dma_cnt = {}
        self.emitted = 0
        self.scr = scr_tiles
        self.tok_src, self.tok_dst = tok_src, tok_dst
        self.bar = {}
        self.nphase = 0

    def _sem(self, key):
        if key not in self.sems:
            self.sems[key] = self.stack.enter_context(self.nc.semaphore("s%d" % len(self.sems)))
        return self.sems[key]

    def flush(self, final=False):
        nc = self.nc
        ops = self.ops
        lo = self.emitted
        cur = ops[lo:]
        self.emitted = len(ops)
        npool = {"sp": 6, "pool": 5, "act": 4, "dve": 1, "pe": 1}
        toks = []
        for e in ("act", "dve", "pool"):
            scr = self.scr[e]
            if e == "act":
                fn = (lambda eng, scr=scr: eng.copy(out=scr[:, 0:1], in_=scr[:, 1:2]))
            else:
                fn = (lambda eng, scr=scr: eng.memset(scr[:, 0:1], 0.0))
            o = dict(eng=e, fn=fn, deps=set(), raw=set(), dma=None, sig=True, inc=1, tok=True)
            toks.append(o)
        o = dict(eng="sp", fn=(lambda eng: eng.dma_start(out=self.tok_dst, in_=self.tok_src)), deps=set(), raw=set(), dma="tok", sig=False, inc=16, tok=True)
        toks.append(o)
        for i, o in enumerate(cur):
            nd = set()
            for d in o["deps"]:
                if d < lo:
                    continue
                od = ops[d]
                if od["dma"] is None and od["eng"] == o["eng"] and o["dma"] is None and (o["eng"] == "pe" or d not in o["raw"]):
                    continue
                nd.add(d)
                if od["dma"] is None:
                    od["sig"] = True
            o["deps"] = nd
        pe_ops = [o for o in cur if o["eng"] == "pe"]
        if pe_ops:
            pe_ops[-1]["sig"] = True
        allops = cur + toks
        for o in allops:
            if o["dma"] is not None:
                q = o["eng"]
                if isinstance(o["dma"], str) and o["dma"].startswith("cc"):
                    k = (q, o["dma"])
                else:
                    k = (q, self.dma_n[q] % npool[q])
                    self.dma_n[q] += 1
                o["prev"] = ("d", k, self.dma_cnt.get(k, 0))
                self.dma_cnt[k] = self.dma_cnt.get(k, 0) + o["inc"]
                o["semv"] = ("d", k, self.dma_cnt[k])
            elif o["sig"]:
                e = o["eng"]
                c = self.cnt[e]
                self.cnt[e] += 1
                o["semv"] = ("e", (e, c // self.SEM_SPAN), c % self.SEM_SPAN + 1)
        per_eng = {e: [o for o in allops if o["eng"] == e] for e in self.ENGS}
        newbar = {}
        for o in toks:
            t, k, v = o["semv"]
            newbar[(t, k)] = v
        if pe_ops:
            t, k, v = pe_ops[-1]["semv"]
            newbar[(t, k)] = v
        oldbar = self.bar
        block = nc.Block()
        with block:
            def run(engname, engobj):
                waited = {}
                for key, v in oldbar.items():
                    waited[key] = v
                    engobj.wait_ge(self._sem(key), v)
                myops = per_eng[engname]
                for o in myops:
                    need = {}
                    if o.get("tok"):
                        for (q, j), v in self.dma_cnt.items():
                            if q == engname and not (o["dma"] is not None and ("d", (q, j)) == o["semv"][:2]):
                                need[("d", (q, j))] = v
                            elif q == engname:
                                need[("d", (q, j))] = o["prev"][2]
                    for d in o["deps"]:
                        t, k, v = ops[d]["semv"]
                        key = (t, k)
                        if need.get(key, 0) < v:
                            need[key] = v
                    if o["dma"] is not None:
                        t, k, v = o["prev"]
                        if v > 0 and need.get((t, k), 0) < v:
                            need[(t, k)] = v
                    for key, v in need.items():
                        if v <= 0 or waited.get(key, 0) >= v:
                            continue
                        waited[key] = v
                        engobj.wait_ge(self._sem(key), v)
                    ins = o["fn"](engobj)
                    if o["dma"] is not None:
                        t, k, v = o["semv"]
                        ins.then_inc(self._sem((t, k)), o["inc"])
                    elif o["sig"]:
                        t, k, v = o["semv"]
                        ins.then_inc(self._sem((t, k)), 1)
                if final:
                    for (q, j), v in self.dma_cnt.items():
                        if q == engname:
                            engobj.wait_ge(self._sem(("d", (q, j))), v)

            @block.tensor
            def _(e):
                run("pe", e)

            @block.scalar
            def _(e):
                run("act", e)

            @block.vector
            def _(e):
                run("dve", e)

            @block.gpsimd
            def _(e):
                run("pool", e)

            @block.sync
            def _(e):
                run("sp", e)
        t, k, v = toks[-1]["semv"]
        newbar[(t, k)] = v
        self.bar = dict(oldbar)
        self.bar.update(newbar)
        self.nphase += 1
        print("phase %d: %d ops, %d sems" % (self.nphase, len(cur), len(self.sems)))
        self.last_w = {}
        self.readers = {}

    def close(self):
        self.stack.close()

    def emit(self):
        nc = self.nc
        ops = self.ops
        for o in ops:
            nd = set()
            for d in o["deps"]:
                od = ops[d]
                if od["dma"] is None and od["eng"] == o["eng"] and o["dma"] is None:
                    if o["eng"] == "pe":
                        continue
                nd.add(d)
                if od["dma"] is None:
                    od["sig"] = True
            o["deps"] = nd
        cnt = {e: 0 for e in self.ENGS}
        npool = {"sp": 6, "pool": 5, "act": 4, "dve": 1, "pe": 1}
        dma_n = {e: 0 for e in self.ENGS}
        dma_cnt = {}
        for o in ops:
            if o["dma"] is not None:
                q = o["eng"]
                k = (q, dma_n[q] % npool[q])
                dma_n[q] += 1
                o["prev"] = ("d", k, dma_cnt.get(k, 0))
                dma_cnt[k] = dma_cnt.get(k, 0) + o["inc"]
                o["semv"] = ("d", k, dma_cnt[k])
            elif o["sig"]:
                e = o["eng"]
                c = cnt[e]
                cnt[e] += 1
                o["semv"] = ("e", (e, c // self.SEM_SPAN), c % self.SEM_SPAN + 1)
        sem_names = []
        for e in self.ENGS:
            for j in range((cnt[e] + self.SEM_SPAN - 1) // self.SEM_SPAN):
                sem_names.append(("e", (e, j)))
        for k in dma_cnt:
            sem_names.append(("d", k))
        import contextlib
        with contextlib.ExitStack() as st:
            sems = {}
            for i, sn in enumerate(sem_names):
                sems[sn] = st.enter_context(nc.semaphore("s%d" % i))
            block = st.enter_context(nc.Block())
            per_eng = {e: [o for o in ops if o["eng"] == e] for e in self.ENGS}

            def run(engname, engobj):
                waited = {}
                for o in per_eng[engname]:
                    need = {}
                    for d in o["deps"]:
                        t, k, v = ops[d]["semv"]
                        key = (t, k)
                        if need.get(key, 0) < v:
                            need[key] = v
                    if o["dma"] is not None:
                        t, k, v = o["prev"]
                        if v > 0 and need.get((t, k), 0) < v:
                            need[(t, k)] = v
                    for key, v in need.items():
                        if waited.get(key, 0) >= v:
                            continue
                        waited[key] = v
                        engobj.wait_ge(sems[key], v)
                    ins = o["fn"](engobj)
                    if o["dma"] is not None:
                        t, k, v = o["semv"]
                        ins.then_inc(sems[(t, k)], o["inc"])
                    elif o["sig"]:
                        t, k, v = o["semv"]
                        ins.then_inc(sems[(t, k)], 1)
                fin = {}
                for o in per_eng[engname]:
                    if o["dma"] is not None:
                        t, k, v = o["semv"]
                        fin[(t, k)] = max(fin.get((t, k), 0), v)
                for key, v in fin.items():
                    engobj.wait_ge(sems[key], v)

            @block.tensor
            def _(e):
                run("pe", e)

            @block.scalar
            def _(e):
                run("act", e)

            @block.vector
            def _(e):
                run("dve", e)

            @block.gpsimd
            def _(e):
                run("pool", e)

            @block.sync
            def _(e):
                run("sp", e)
        print("sched: %d ops, %d sems" % (len(ops), len(sem_names)))


def cast_pass(S, pool, src, dst, tag, engs=("dve", "pool"), stq="act", chunk=4096, nbuf=2):
    R, C = src.shape
    nrow = R // 128
    sv = src.rearrange("(n p) c -> p n c", p=128)
    dv = dst.rearrange("(n p) c -> p n c", p=128)
    if C >= chunk:
        assert C % chunk == 0
        steps = [(n, 1, c0, chunk) for n in range(nrow) for c0 in range(0, C, chunk)]
    else:
        nn = max(1, chunk // C)
        steps = [(n, min(nn, nrow - n), 0, C) for n in range(0, nrow, nn)]
    stg, obf, pn = pool["stg"], pool["obf"], pool["name"]
    keys = []
    for i, (n, k, c0, cw) in enumerate(steps):
        sl = i % nbuf
        sa = stg[sl][:, 0:k * cw].rearrange("p (k c) -> p k c", k=k)
        oa = obf[sl][:, 0:k * cw].rearrange("p (k c) -> p k c", k=k)
        S.op("sp", lambda e, sa=sa, n=n, k=k, c0=c0, cw=cw: e.dma_start(out=sa, in_=sv[:, n:n + k, c0:c0 + cw]),
             w=[(pn, "stg", sl)], dma=(pn, "stg", sl))
        ce = engs[i % len(engs)]
        if ce == "act":
            S.op("act", lambda e, sa=sa, oa=oa: e.copy(out=oa, in_=sa), r=[(pn, "stg", sl)], w=[(pn, "obf", sl)])
        else:
            S.op(ce, lambda e, sa=sa, oa=oa: e.tensor_copy(out=oa, in_=sa), r=[(pn, "stg", sl)], w=[(pn, "obf", sl)])
        S.op(stq, lambda e, oa=oa, n=n, k=k, c0=c0, cw=cw: e.dma_start(out=dv[:, n:n + k, c0:c0 + cw], in_=oa),
             r=[(pn, "obf", sl)], w=[(tag, "dram", i)], dma=(pn, "obf", sl))
        keys.append((tag, "dram", i))
    return keys


def gemm(S, tag, aT, bm, M, K, blocks, bufs, a_dep, b_dep, stq="act", ldq=("sp", "pool")):
    KB = K // 128
    MT = 512
    A, B, O, PS = bufs["A"], bufs["B"], bufs["O"], bufs["PS"]
    out_tag = tag
    tag = bufs["name"]
    av = aT.rearrange("(kb p) m -> p kb m", p=128)
    bv = bm.rearrange("(kb p) n -> p kb n", p=128)
    keys = []
    ai = 0
    oi = 0
    pi = 0
    for bi, (mode, n0, nw, dst) in enumerate(blocks):
        half = KB // 2
        S.op("sp", lambda e, n0=n0, nw=nw: e.dma_start(out=B[:, 0:half, 0:nw], in_=bv[:, 0:half, n0:n0 + nw]),
             r=[b_dep], w=[(tag, "B", 0)], dma=(tag, "B", 0))
        S.op(ldq[1], lambda e, n0=n0, nw=nw: e.dma_start(out=B[:, half:KB, 0:nw], in_=bv[:, half:KB, n0:n0 + nw]),
             r=[b_dep], w=[(tag, "B", 1)], dma=(tag, "B", 1))
        for m0 in range(0, M, MT):
            sl = ai % 2
            ai += 1
            At = A[sl]
            S.op("sp", lambda e, At=At, m0=m0: e.dma_start(out=At[:, 0:half, :], in_=av[:, 0:half, m0:m0 + MT]),
                 r=[a_dep], w=[(tag, "A", sl, 0)], dma=(tag, "A", sl, 0))
            S.op(ldq[1], lambda e, At=At, m0=m0: e.dma_start(out=At[:, half:KB, :], in_=av[:, half:KB, m0:m0 + MT]),
                 r=[a_dep], w=[(tag, "A", sl, 1)], dma=(tag, "A", sl, 1))
            for s0 in range(0, nw, 512):
                sw = min(512, nw - s0)
                osl = oi % 2
                oi += 1
                Ot = O[osl]
                for j in range(4):
                    if mode == "FM" and j * 128 >= sw:
                        continue
                    pk = pi % 8
                    pi += 1
                    ps = PS[pk]

                    def mm(e, ps=ps, At=At, j=j, s0=s0, sw=sw, mode=mode):
                        ins = None
                        for kb in range(KB):
                            if mode == "TM":
                                ins = e.matmul(ps[:, 0:sw], lhsT=At[:, kb, j * 128:(j + 1) * 128],
                                               rhs=B[:, kb, s0:s0 + sw], start=(kb == 0), stop=(kb == KB - 1))
                            else:
                                ins = e.matmul(ps[:, 0:MT], lhsT=B[:, kb, s0 + j * 128:s0 + (j + 1) * 128],
                                               rhs=At[:, kb, :], start=(kb == 0), stop=(kb == KB - 1))
                        return ins
                    S.op("pe", mm, r=[(tag, "A", sl, 0), (tag, "A", sl, 1), (tag, "B", 0), (tag, "B", 1)],
                         w=[(tag, "PS", pk)])
                    ww = sw if mode == "TM" else MT
                    if j % 2 == 0:
                        S.op("act", lambda e, ps=ps, Ot=Ot, j=j, ww=ww: e.copy(out=Ot[:, j, 0:ww], in_=ps[:, 0:ww]),
                             r=[(tag, "PS", pk)], w=[(tag, "O", osl, j)])
                    else:
                        S.op("dve", lambda e, ps=ps, Ot=Ot, j=j, ww=ww: e.tensor_copy(out=Ot[:, j, 0:ww], in_=ps[:, 0:ww]),
                             r=[(tag, "PS", pk)], w=[(tag, "O", osl, j)])
                if mode == "TM":
                    dv = dst[m0:m0 + MT, s0:s0 + sw].rearrange("(j p) n -> p j n", p=128)
                    src = lambda Ot=Ot, sw=sw: Ot[:, :, 0:sw]
                else:
                    nj = sw // 128
                    dv = dst[s0:s0 + sw, m0:m0 + MT].rearrange("(j p) m -> p j m", p=128)
                    src = lambda Ot=Ot, nj=nj: Ot[:, 0:nj, :]
                k = (out_tag, "out", bi, m0, s0)
                S.op(stq, lambda e, dv=dv, src=src: e.dma_start(out=dv, in_=src()),
                     r=[(tag, "O", osl, j) for j in range(4)], w=[k], dma=(tag, "O", osl))
                keys.append(k)
    return keys


def gla_consts():
    j = np.arange(128)[:, None]
    c = np.arange(128)[None, :]
    tri = np.where(j <= c, -1.0 / 16.0, 0.0).astype(np.float32)
    tri2 = np.where(j > c, -1.0 / 16.0, 0.0).astype(np.float32)
    sel = np.full((128, 1), -1.0 / 16.0, np.float32)
    mask = (j <= c).astype(np.float32)
    return np.concatenate([tri, tri2, mask, sel, np.zeros((128, 127), np.float32)], axis=1)


def phase_gla(S, nc, st, SEQ, fmq, fmk, fmga, tmk, tmv, tmgz, wa2aug, normg_b, gconst, ident, outT, dep):
    def sb(name, shape, dt):
        return st.enter_context(nc.sbuf_tensor("gla_" + name, shape, dt))

    def psum(name, shape, dt=F32):
        return st.enter_context(nc.psum_tensor("gla_" + name, shape, dt))
    NT = SEQ // 128
    qT = [sb("qT%d" % i, [128, 2, 512], BF16) for i in range(2)]
    kT = [sb("kT%d" % i, [128, 2, 512], BF16) for i in range(2)]
    gaT = [sb("gaT%d" % i, [32, 512], BF16) for i in range(2)]
    ktm = [sb("ktm%d" % i, [128, 4, 256], BF16) for i in range(2)]
    vtm = [sb("vtm%d" % i, [128, 4, 512], BF16) for i in range(2)]
    gz = [sb("gz%d" % i, [128, 4, 512], BF16) for i in range(2)]
    wa_f = sb("wa_f", [32, 256], F32)
    wa = sb("wa", [32, 256], BF16)
    ng = sb("ng", [128, 512], F32)
    gc = sb("gc", [128, 512], F32)
    idn = sb("idn", [128, 128], BF16)
    e1 = sb("e1", [128, 256], F32)
    la = sb("la", [128, 256], F32)
    Eq = sb("Eq", [128, 2, 128], F32)
    Ek = sb("Ek", [128, 2, 128], F32)
    Eend = sb("Eend", [128, 256], F32)
    dec = sb("dec", [128, 2], F32)
    qd = sb("qd", [128, 2, 128], BF16)
    kd = sb("kd", [128, 2, 128], BF16)
    kend = sb("kend", [128, 256], BF16)
    att = sb("att", [128, 128], BF16)
    S32 = sb("S32", [128, 2, 512], F32)
    Sbf = sb("Sbf", [128, 2, 512], BF16)
    sq = sb("sq", [128, 512], F32)
    ss = sb("ss", [128, 1], F32)
    rstd = sb("rstd", [128, 1], F32)
    eps_t = sb("eps_t", [128, 1], F32)
    Gz = sb("Gz", [128, 512], F32)
    actt = sb("actt", [128, 512], BF16)
    oT = [sb("oT%d" % i, [128, 4, 512], BF16) for i in range(2)]
    P_lg = psum("P_lg", [128, 512])
    P_bc = psum("P_bc", [128, 512])
    P_bT = psum("P_bT", [128, 512])
    P_att = psum("P_att", [128, 512])
    P_o = psum("P_o", [128, 512])
    P_kv = [psum("P_kv%d" % i, [128, 512]) for i in range(2)]
    P_tr = psum("P_tr", [128, 1024], BF16)

    K = lambda *a: ("gla",) + a
    S.op("sp", lambda e: e.dma_start(out=wa_f[0:17, :], in_=wa2aug), w=[K("wa_f")], dma=K("wa_f"))
    S.op("sp", lambda e: e.dma_start(out=ng[:], in_=normg_b), w=[K("ng")], dma=K("ng"))
    S.op("sp", lambda e: e.dma_start(out=gc[:], in_=gconst), w=[K("gc")], dma=K("gc"))
    S.op("sp", lambda e: e.dma_start(out=idn[:], in_=ident), w=[K("idn")], dma=K("idn"))
    S.op("dve", lambda e: e.tensor_copy(out=wa[0:17, :], in_=wa_f[0:17, :]), r=[K("wa_f")], w=[K("wa")])
    for i in range(2):
        S.op("dve", lambda e, i=i: e.memset(gaT[i][0:1, :], 1.0), w=[K("gaT1", i)])
    S.op("dve", lambda e: e.memset(eps_t[:], 1e-6), w=[K("eps")])
    S.op("dve", lambda e: e.memset(S32[:], 0.0), w=[K("S32")])
    S.op("dve", lambda e: e.memset(Sbf[:], 0.0), w=[K("Sbf")])
    tri = gc[:, 0:128]
    tri2 = gc[:, 128:256]
    msk = gc[:, 256:384]
    sel = gc[:, 384:385]
    keys = []
    for t in range(NT):
        sup, j = divmod(t, 4)
        sl = sup % 2
        t0 = sup * 512
        if j == 0:
            S.op("sp", lambda e, sl=sl, t0=t0: e.dma_start(out=qT[sl][:], in_=fmq[:, t0:t0 + 512].rearrange("(b p) t -> p b t", p=128)),
                 r=[dep], w=[K("qT", sl)], dma=K("qT", sl))
            S.op("sp", lambda e, sl=sl, t0=t0: e.dma_start(out=kT[sl][:], in_=fmk[:, t0:t0 + 512].rearrange("(b p) t -> p b t", p=128)),
                 r=[dep], w=[K("kT", sl)], dma=K("kT", sl))
            S.op("sp", lambda e, sl=sl, t0=t0: e.dma_start(out=gaT[sl][1:17, :], in_=fmga[:, t0:t0 + 512]),
                 r=[dep, K("gaT1", sl)], w=[K("gaT", sl)], dma=K("gaT", sl))
            S.op("pool", lambda e, sl=sl, t0=t0: e.dma_start(out=ktm[sl][:], in_=tmk[t0:t0 + 512, :].rearrange("(j p) c -> p j c", p=128)),
                 r=[dep], w=[K("ktm", sl)], dma=K("ktm", sl))
            S.op("pool", lambda e, sl=sl, t0=t0: e.dma_start(out=vtm[sl][:], in_=tmv[t0:t0 + 512, :].rearrange("(j p) c -> p j c", p=128)),
                 r=[dep], w=[K("vtm", sl)], dma=K("vtm", sl))
            S.op("pool", lambda e, sl=sl, t0=t0: e.dma_start(out=gz[sl][:], in_=tmgz[t0:t0 + 512, :].rearrange("(j p) c -> p j c", p=128)),
                 r=[dep], w=[K("gz", sl)], dma=K("gz", sl))
        c0 = j * 128
        S.op("pe", lambda e, sl=sl, c0=c0: e.matmul(P_lg[:, 0:256], lhsT=gaT[sl][0:17, c0:c0 + 128], rhs=wa[0:17, :], start=True, stop=True),
             r=[K("gaT", sl), K("wa")], w=[K("P_lg")])
        S.op("act", lambda e: e.activation(out=e1[:], in_=P_lg[:, 0:256], func=AF.Exp, scale=-1.0), r=[K("P_lg")], w=[K("e1")])
        S.op("act", lambda e: e.activation(out=la[:], in_=e1[:], func=AF.Ln, bias=1.0), r=[K("e1")], w=[K("la")])
        def mm_bc(e):
            e.matmul(P_bc[:, 0:256], lhsT=tri2, rhs=la[:], start=True, stop=True)
            e.matmul(P_bT[:, 0:128], lhsT=la[:, 0:128], rhs=tri, start=True, stop=True)
            e.matmul(P_bT[:, 128:256], lhsT=la[:, 128:256], rhs=tri, start=True, stop=True)
            e.matmul(P_bT[:, 256:257], lhsT=la[:, 0:128], rhs=sel, start=True, stop=True)
            return e.matmul(P_bT[:, 257:258], lhsT=la[:, 128:256], rhs=sel, start=True, stop=True)
        S.op("pe", mm_bc, r=[K("la"), K("gc")], w=[K("P_bc"), K("P_bT")])
        S.op("act", lambda e: e.activation(out=Eq[:], in_=P_bT[:, 0:256].rearrange("p (b c) -> p b c", b=2), func=AF.Exp),
             r=[K("P_bT")], w=[K("Eq")])
        S.op("act", lambda e: e.activation(out=Ek[:], in_=P_bT[:, 0:256].rearrange("p (b c) -> p b c", b=2), func=AF.Exp, scale=-1.0),
             r=[K("P_bT")], w=[K("Ek")])
        S.op("act", lambda e: e.activation(out=dec[:], in_=P_bT[:, 256:258], func=AF.Exp), r=[K("P_bT")], w=[K("dec")])
        S.op("act", lambda e: e.activation(out=Eend[:], in_=P_bc[:, 0:256], func=AF.Exp), r=[K("P_bc")], w=[K("Eend")])
        S.op("dve", lambda e, sl=sl, c0=c0: e.scalar_tensor_tensor(out=qd[:], in0=qT[sl][:, :, c0:c0 + 128], scalar=1.0 / 16.0, in1=Eq[:],
                                                                   op0=ALU.mult, op1=ALU.mult),
             r=[K("qT", sl), K("Eq")], w=[K("qd")])
        S.op("dve", lambda e, sl=sl, c0=c0: e.tensor_tensor(out=kd[:], in0=kT[sl][:, :, c0:c0 + 128], in1=Ek[:], op=ALU.mult),
             r=[K("kT", sl), K("Ek")], w=[K("kd")])
        S.op("pool", lambda e, sl=sl, j=j: e.tensor_tensor(out=kend[:], in0=ktm[sl][:, j, :], in1=Eend[:], op=ALU.mult),
             r=[K("ktm", sl), K("Eend")], w=[K("kend")])
        def mm_att(e):
            e.matmul(P_att[:, 0:128], lhsT=kd[:, 0, :], rhs=qd[:, 0, :], start=True, stop=False)
            return e.matmul(P_att[:, 0:128], lhsT=kd[:, 1, :], rhs=qd[:, 1, :], start=False, stop=True)
        S.op("pe", mm_att, r=[K("kd"), K("qd")], w=[K("P_att")])
        S.op("dve", lambda e: e.tensor_tensor(out=att[:], in0=P_att[:, 0:128], in1=msk, op=ALU.mult), r=[K("P_att"), K("gc")], w=[K("att")])
        def mm_o(e, sl=sl, j=j):
            e.matmul(P_o[:], lhsT=att[:], rhs=vtm[sl][:, j, :], start=True, stop=False)
            e.matmul(P_o[:], lhsT=qd[:, 0, :], rhs=Sbf[:, 0, :], start=False, stop=False)
            return e.matmul(P_o[:], lhsT=qd[:, 1, :], rhs=Sbf[:, 1, :], start=False, stop=True)
        S.op("pe", mm_o, r=[K("att"), K("vtm", sl), K("qd"), K("Sbf")], w=[K("P_o")])
        for b in range(2):
            S.op("pe", lambda e, b=b, sl=sl, j=j: e.matmul(P_kv[b][:], lhsT=kend[:, b * 128:(b + 1) * 128], rhs=vtm[sl][:, j, :], start=True, stop=True),
                 r=[K("kend"), K("vtm", sl)], w=[K("P_kv", b)])
            S.op("dve", lambda e, b=b: e.scalar_tensor_tensor(out=S32[:, b, :], in0=S32[:, b, :], scalar=dec[:, b:b + 1], in1=P_kv[b][:],
                                                              op0=ALU.mult, op1=ALU.add),
                 r=[K("P_kv", b), K("dec"), K("S32")], w=[K("S32")])
        S.op("act", lambda e: e.copy(out=Sbf[:], in_=S32[:]), r=[K("S32")], w=[K("Sbf")])
        S.op("act", lambda e: e.activation(out=sq[:], in_=P_o[:], func=AF.Square, accum_out=ss[:]), r=[K("P_o")], w=[K("sq"), K("ss")])
        S.op("act", lambda e: e.activation(out=rstd[:], in_=ss[:], func=AF.Sqrt, scale=1.0 / 512.0, bias=eps_t[:, 0:1]), r=[K("ss"), K("eps")], w=[K("rstd")])
        S.op("dve", lambda e: e.reciprocal(out=rstd[:], in_=rstd[:]), r=[K("rstd")], w=[K("rstd")])
        S.op("act", lambda e, sl=sl, j=j: e.activation(out=Gz[:], in_=gz[sl][:, j, :], func=AF.Silu), r=[K("gz", sl)], w=[K("Gz")])
        S.op("pool", lambda e: e.tensor_tensor(out=Gz[:], in0=Gz[:], in1=ng[:], op=ALU.mult), r=[K("Gz"), K("ng")], w=[K("Gz")])
        S.op("dve", lambda e: e.scalar_tensor_tensor(out=actt[:], in0=P_o[:], scalar=rstd[:, 0:1], in1=Gz[:], op0=ALU.mult, op1=ALU.mult),
             r=[K("P_o"), K("rstd"), K("Gz")], w=[K("actt")])
        def mm_tr(e):
            ins = None
            for b in range(4):
                ins = e.transpose(P_tr[:, b * 128:(b + 1) * 128], actt[:, b * 128:(b + 1) * 128], idn[:])
            return ins
        S.op("pe", mm_tr, r=[K("actt"), K("idn")], w=[K("P_tr")])
        S.op("act", lambda e, sl=sl, c0=c0: e.copy(out=oT[sl][:, :, c0:c0 + 128], in_=P_tr[:, 0:512].rearrange("p (b c) -> p b c", b=4)),
             r=[K("P_tr")], w=[K("oT", sl)])
        if j == 3:
            k = K("out", sup)
            S.op("sp", lambda e, sl=sl, t0=t0: e.dma_start(out=outT[:, t0:t0 + 512].rearrange("(b p) t -> p b t", p=128), in_=oT[sl][:]),
                 r=[K("oT", sl)], w=[k], dma=K("oT", sl))
            keys.append(k)
    return keys


def finish_tile(S, nc, Kf, P_tr, actt_key, actt, idn, idn_key, oT, sl, j, outT, t0, keys, tag, stq="sp"):
    def mm_tr(e):
        ins = None
        for b in range(4):
            ins = e.transpose(P_tr[:, b * 128:(b + 1) * 128], actt[:, b * 128:(b + 1) * 128], idn[:])
        return ins
    S.op("pe", mm_tr, r=[actt_key, idn_key], w=[Kf("P_tr")])
    c0 = j * 128
    S.op("act", lambda e: e.copy(out=oT[sl][:, :, c0:c0 + 128], in_=P_tr[:, 0:512].rearrange("p (b c) -> p b c", b=4)),
         r=[Kf("P_tr")], w=[Kf("oT", sl)])
    if j == 3:
        k = Kf("out", t0)
        S.op(stq, lambda e: e.dma_start(out=outT[:, t0:t0 + 512].rearrange("(b p) t -> p b t", p=128), in_=oT[sl][:]),
             r=[Kf("oT", sl)], w=[k], dma=Kf("oT", sl))
        keys.append(k)


def phase_mem(S, nc, st, SEQ, fmmq, tmmz, memT_bf, wk_bf, wv_bf, ident, outT, dep, wdep):
    def sb(name, shape, dt):
        return st.enter_context(nc.sbuf_tensor("mem_" + name, shape, dt))

    def psum(name, shape, dt=F32):
        return st.enter_context(nc.psum_tensor("mem_" + name, shape, dt))
    K = lambda *a: ("mem",) + a
    mT = sb("mT", [128, 32, 256], BF16)
    wkv = sb("wkv", [128, 32, 512], BF16)
    kT = sb("kT", [128, 4, 256], BF16)
    vv = sb("vv", [128, 2, 512], BF16)
    ones = sb("ones", [128, 1], BF16)
    idn = sb("idn", [128, 128], BF16)
    qT = [sb("qT%d" % i, [128, 4, 512], BF16) for i in range(2)]
    mz = [sb("mz%d" % i, [128, 4, 512], BF16) for i in range(2)]
    pT = sb("pT", [128, 2, 512], BF16)
    sg = sb("sg", [128, 512], F32)
    rz = sb("rz", [128, 1], F32)
    actt = sb("actt", [128, 512], BF16)
    oT = [sb("oT%d" % i, [128, 4, 512], BF16) for i in range(2)]
    P_s = [psum("P_s%d" % i, [128, 512]) for i in range(2)]
    P_o = [psum("P_o%d" % i, [128, 512]) for i in range(2)]
    P_z = psum("P_z", [128, 512])
    P_tr = psum("P_tr", [128, 1024], BF16)
    S.op("sp", lambda e: e.dma_start(out=idn[:], in_=ident), w=[K("idn")], dma=K("idn"))
    S.op("dve", lambda e: e.memset(ones[:], 1.0), w=[K("ones")])
    S.op("sp", lambda e: e.dma_start(out=mT[:], in_=memT_bf.rearrange("(kb p) m -> p kb m", p=128)), r=[wdep], w=[K("mT")], dma=K("mT"))
    S.op("sp", lambda e: e.dma_start(out=wkv[:], in_=wk_bf.rearrange("(kb p) n -> p kb n", p=128)), r=[wdep], w=[K("wkv")], dma=K("wkv"))
    for db in range(4):
        def mmk(e, db=db):
            ins = None
            for kb in range(32):
                ins = e.matmul(P_s[db % 2][:, 0:256], lhsT=wkv[:, kb, db * 128:(db + 1) * 128], rhs=mT[:, kb, :], start=(kb == 0), stop=(kb == 31))
            return ins
        S.op("pe", mmk, r=[K("wkv"), K("mT")], w=[K("P_s", db % 2)])
        S.op("act", lambda e, db=db: e.activation(out=kT[:, db, :], in_=P_s[db % 2][:, 0:256], func=AF.Copy, scale=512.0 ** -0.5),
             r=[K("P_s", db % 2)], w=[K("kT")])
    S.op("sp", lambda e: e.dma_start(out=wkv[:], in_=wv_bf.rearrange("(kb p) n -> p kb n", p=128)), r=[wdep], w=[K("wkv")], dma=K("wkv"))
    for mt in range(2):
        def mmv(e, mt=mt):
            ins = None
            for kb in range(32):
                ins = e.matmul(P_o[mt][:], lhsT=mT[:, kb, mt * 128:(mt + 1) * 128], rhs=wkv[:, kb, :], start=(kb == 0), stop=(kb == 31))
            return ins
        S.op("pe", mmv, r=[K("wkv"), K("mT")], w=[K("P_o", mt)])
        S.op("act", lambda e, mt=mt: e.copy(out=vv[:, mt, :], in_=P_o[mt][:]), r=[K("P_o", mt)], w=[K("vv")])
    keys = []
    for sup in range(SEQ // 512):
        sl = sup % 2
        t0 = sup * 512
        S.op("sp", lambda e, sl=sl, t0=t0: e.dma_start(out=qT[sl][:], in_=fmmq[:, t0:t0 + 512].rearrange("(b p) t -> p b t", p=128)),
             r=[dep], w=[K("qT", sl)], dma=K("qT", sl))
        S.op("pool", lambda e, sl=sl, t0=t0: e.dma_start(out=mz[sl][:], in_=tmmz[t0:t0 + 512, :].rearrange("(j p) c -> p j c", p=128)),
             r=[dep], w=[K("mz", sl)], dma=K("mz", sl))
        for mt in range(2):
            def mms(e, mt=mt, sl=sl):
                ins = None
                for db in range(4):
                    ins = e.matmul(P_s[mt][:], lhsT=kT[:, db, mt * 128:(mt + 1) * 128], rhs=qT[sl][:, db, :], start=(db == 0), stop=(db == 3))
                return ins
            S.op("pe", mms, r=[K("kT"), K("qT", sl)], w=[K("P_s", mt)])
            S.op("act", lambda e, mt=mt: e.activation(out=pT[:, mt, :], in_=P_s[mt][:], func=AF.Exp), r=[K("P_s", mt)], w=[K("pT", mt)])
        for j in range(4):
            pj = j % 2
            def mmo(e, j=j, pj=pj):
                e.matmul(P_o[pj][:], lhsT=pT[:, 0, j * 128:(j + 1) * 128], rhs=vv[:, 0, :], start=True, stop=False)
                e.matmul(P_o[pj][:], lhsT=pT[:, 1, j * 128:(j + 1) * 128], rhs=vv[:, 1, :], start=False, stop=True)
                e.matmul(P_z[:, j:j + 1], lhsT=pT[:, 0, j * 128:(j + 1) * 128], rhs=ones[:], start=True, stop=False)
                return e.matmul(P_z[:, j:j + 1], lhsT=pT[:, 1, j * 128:(j + 1) * 128], rhs=ones[:], start=False, stop=True)
            S.op("pe", mmo, r=[K("pT", 0), K("pT", 1), K("vv"), K("ones")], w=[K("P_o", pj), K("P_z")])
            S.op("dve", lambda e, j=j: e.reciprocal(out=rz[:], in_=P_z[:, j:j + 1]), r=[K("P_z")], w=[K("rz")])
            S.op("act", lambda e, j=j, sl=sl: e.activation(out=sg[:], in_=mz[sl][:, j, :], func=AF.Silu), r=[K("mz", sl)], w=[K("sg")])
            S.op("dve", lambda e, pj=pj: e.scalar_tensor_tensor(out=actt[:], in0=P_o[pj][:], scalar=rz[:, 0:1], in1=sg[:], op0=ALU.mult, op1=ALU.mult),
                 r=[K("P_o", pj), K("rz"), K("sg")], w=[K("actt")])
            finish_tile(S, nc, K, P_tr, K("actt"), actt, idn, K("idn"), oT, sl, j, outT, t0, keys, "mem")
    return keys


def nsa_consts(SEQ, g):
    NT = SEQ // 128
    NSEL = SEQ // 64
    NCP = ((SEQ // 16 - 1 + 127) // 128) * 128
    NCT = NCP // 128
    slopes = (2.0 ** (-8.0 * (np.arange(16) + 1.0) / 16))[4 * g:4 * g + 4].astype(np.float64)
    nrel = np.arange(128)[:, None]
    m = np.arange(NCT)[None, :, None]
    i = np.arange(NT)[None, None, :]
    n = 128 * m + nrel[:, :, None]
    arg = 16 * n + 31 - 128 * i - 64
    fut = (16 * n + 31) > (128 * i + 127)
    biasc = np.stack([np.where(fut | (s * arg < -70.0), -30000.0, s * arg) for s in slopes], axis=1)
    biasc = biasc.reshape(128, 4 * NCT * NT).astype(np.float32)
    D = np.arange(NT)[None, :]
    bs = np.stack([np.where(s * (nrel - 64 - 128 * D) < -70.0, -30000.0, s * (nrel - 64 - 128 * D)) for s in slopes], axis=1)
    bs = bs.reshape(128, 4 * NT).astype(np.float32)
    trel = np.arange(128)[None, :]
    masks = []
    for k in range(16):
        masks.append(((16 * (nrel - 8 * k) + 31) <= trel))
    masks.append(((16 * (nrel - 128) + 31) <= trel))
    masks.append(nrel <= trel)
    masks.append(nrel > trel)
    maskc = np.concatenate(masks, axis=1).astype(np.float32).astype(NPBF)
    nn = np.arange(NCP)[:, None]
    jj = np.arange(NSEL)[None, :]
    ov = ((16 * nn < 64 * jj + 64) & (16 * nn + 31 >= 64 * jj) & (nn < SEQ // 16 - 1)).astype(np.float32)
    ovl = np.concatenate([ov, np.ones((NCP, 1), np.float32), np.zeros((NCP, 1), np.float32)], axis=1).astype(NPBF)
    ovl = np.ascontiguousarray(ovl.reshape(NCT, 128, NSEL + 2).transpose(1, 0, 2))
    E = np.zeros((128, SEQ), np.float32)
    kk = np.arange(SEQ)
    if NSEL <= 128:
        E[kk // 64, kk] = 1.0
    E = E.astype(NPBF)
    t = (128 * np.arange(NT)[:, None] + np.arange(128)[None, :])[:, :, None]
    jb = np.arange(NSEL)[None, None, :]
    cur = t // 64
    causal = (jb <= cur).astype(np.float32)
    forced = ((jb == 0) | (jb == cur) | (jb == cur - 1)).astype(np.float32)
    add = (causal - 1.0) + 1e4 * forced
    tk = np.stack([causal, add], axis=2).astype(np.float32)
    return dict(biasc=biasc, bs=bs, maskc=maskc, ovl=ovl, E=E, tk=tk)


def phase_nsa(S, nc, st, SEQ, fmq, fmkc, fmvc, fmks, fmkw, tmvs, tmvw, tmnz, tmbg,
              w1k, w1v, w2k, w2v, pekT, pevT, cst, ident, outT, dep, dbg=None):
    def sb(name, shape, dt):
        return st.enter_context(nc.sbuf_tensor("nsa_" + name, shape, dt))

    def psum(name, shape, dt=F32):
        return st.enter_context(nc.psum_tensor("nsa_" + name, shape, dt))
    K = lambda *a: ("nsa",) + a
    NT = SEQ // 128
    NSEL = SEQ // 64
    NC = SEQ // 16 - 1
    NCT = (NC + 127) // 128
    NCP = NCT * 128
    WR = 128 + NSEL + 1
    SC = 128.0 ** -0.5
    assert NSEL <= 128
    idn = sb("idn", [128, 128], BF16)
    ksT = sb("ksT", [128, SEQ], BF16)
    kwT = sb("kwT", [128, SEQ], BF16)
    kcT = sb("kcT", [128, SEQ], BF16)
    vsa = sb("vsa", [128, NT, 130], BF16)
    vwa = sb("vwa", [128, NT, 130], BF16)
    Emat = sb("E", [128, SEQ], BF16)
    kcmpT = sb("kcmpT", [128, NCP], BF16)
    Rc = sb("Rc", [128, NCT, WR + 1], BF16)
    biasc = sb("biasc", [128, 4 * NCT * NT], F32)
    bs = sb("bs", [128, 4 * NT], F32)
    maskc = sb("maskc", [128, 19 * 128], BF16)
    w1f = sb("w1f", [128, 32, 128], F32)
    w1 = sb("w1", [128, 32, 128], BF16)
    w2f = sb("w2f", [128, 128], F32)
    w2 = sb("w2", [128, 128], BF16)
    pef = sb("pef", [128, 32], F32)
    peb = sb("peb", [128, 32], BF16)
    cb = sb("cb", [128, 1], F32)
    hc = sb("hc", [128, NCP], BF16)
    S.op("sp", lambda e: e.dma_start(out=idn[:], in_=ident), w=[K("idn")], dma=K("idn"))
    S.op("sp", lambda e: e.dma_start(out=ksT[:], in_=fmks), r=[dep], w=[K("ksT")], dma=K("ksT"))
    S.op("sp", lambda e: e.dma_start(out=kwT[:], in_=fmkw), r=[dep], w=[K("kwT")], dma=K("kwT"))
    S.op("pool", lambda e: e.dma_start(out=Emat[:], in_=cst["E"]), w=[K("E")], dma=K("E"))
    S.op("pool", lambda e: e.dma_start(out=biasc[:], in_=cst["biasc"]), w=[K("biasc")], dma=K("biasc"))
    S.op("pool", lambda e: e.dma_start(out=bs[:], in_=cst["bs"]), w=[K("bs")], dma=K("bs"))
    S.op("pool", lambda e: e.dma_start(out=maskc[:], in_=cst["maskc"]), w=[K("maskc")], dma=K("maskc"))
    S.op("dve", lambda e: e.memset(vsa[:], 1.0), w=[K("vsa")])
    S.op("dve", lambda e: e.memset(vwa[:], 1.0), w=[K("vwa")])
    S.op("sp", lambda e: e.dma_start(out=vsa[:, :, 0:128], in_=tmvs.rearrange("(j p) c -> p j c", p=128)), r=[dep, K("vsa")], w=[K("vsa")], dma=K("vsa"))
    S.op("sp", lambda e: e.dma_start(out=vwa[:, :, 0:128], in_=tmvw.rearrange("(j p) c -> p j c", p=128)), r=[dep, K("vwa")], w=[K("vwa")], dma=K("vwa"))
    S.op("dve", lambda e: e.memset(kcmpT[:], 0.0), w=[K("kcmpT")])
    S.op("dve", lambda e: e.memset(Rc[:], 0.0), w=[K("Rc")])
    S.op("dve", lambda e: e.memset(hc[:], 0.0), w=[K("hc")])
    S.op("pool", lambda e: e.dma_start(out=Rc[:, :, 128:WR + 1], in_=cst["ovl"]), r=[K("Rc")], w=[K("Rc")], dma=K("Rc"))
    P_sc = [psum("P_sc%d" % i, [128, 512]) for i in range(2)]
    P_big = [psum("P_big0", [128, 512]), P_sc[0]]
    P_sw = [psum("P_sw%d" % i, [128, 512]) for i in range(4)]
    P_trm = psum("P_trm", [128, 1024], BF16)
    P_tr = P_trm[:, 0:512]
    P_mT = P_trm[:, 512:1024]
    for which, (fmx, w1d, w2d, ped) in enumerate(((fmkc, w1k, w2k, pekT), (fmvc, w1v, w2v, pevT))):
        S.op("sp", lambda e, fmx=fmx: e.dma_start(out=kcT[:], in_=fmx), r=[dep], w=[K("kcT")], dma=K("kcT"))
        S.op("sp", lambda e, w1d=w1d: e.dma_start(out=w1f[:], in_=w1d.rearrange("(l d) o -> d l o", d=128)), w=[K("w1f")], dma=K("w1f"))
        S.op("sp", lambda e, w2d=w2d: e.dma_start(out=w2f[:], in_=w2d), w=[K("w2f")], dma=K("w2f"))
        S.op("sp", lambda e, ped=ped: e.dma_start(out=pef[:], in_=ped), w=[K("pef")], dma=K("pef"))
        S.op("dve", lambda e: e.tensor_copy(out=w1[:], in_=w1f[:]), r=[K("w1f")], w=[K("w1")])
        S.op("dve", lambda e: e.tensor_copy(out=w2[:], in_=w2f[:]), r=[K("w2f")], w=[K("w2")])
        S.op("dve", lambda e: e.tensor_copy(out=peb[:], in_=pef[:]), r=[K("pef")], w=[K("peb")])
        def mm_c(e):
            ins = None
            for l in range(32):
                ins = e.matmul(P_big[1][:, 0:1], lhsT=w1[:, l, :], rhs=peb[:, l:l + 1], start=(l == 0), stop=(l == 31))
            return ins
        S.op("pe", mm_c, r=[K("w1"), K("peb")], w=[K("P_sc", 0)])
        S.op("act", lambda e: e.copy(out=cb[:], in_=P_big[1][:, 0:1]), r=[K("P_sc", 0)], w=[K("cb")])
        def mm_pre(e):
            ins = None
            for l in range(32):
                ins = e.matmul(P_big[0][:, 0:NC], lhsT=w1[:, l, :], rhs=kcT[:, l:l + 16 * (NC - 1) + 1:16], start=(l == 0), stop=(l == 31))
            return ins
        S.op("pe", mm_pre, r=[K("w1"), K("kcT")], w=[K("P_big", 0)])
        S.op("act", lambda e: e.activation(out=hc[:, 0:NC], in_=P_big[0][:, 0:NC], func=AF.Silu, bias=cb[:, 0:1]), r=[K("P_big", 0), K("cb")], w=[K("hc")])
        if which == 0:
            S.op("pe", lambda e: e.matmul(P_big[0][:, 0:NC], lhsT=w2[:], rhs=hc[:, 0:NC], start=True, stop=True), r=[K("w2"), K("hc")], w=[K("P_big", 0)])
            S.op("act", lambda e: e.copy(out=kcmpT[:, 0:NC], in_=P_big[0][:, 0:NC]), r=[K("P_big", 0)], w=[K("kcmpT")])
        else:
            for nt in range(NCT):
                S.op("pe", lambda e, nt=nt: e.matmul(P_big[0][:, 0:128], lhsT=hc[:, nt * 128:(nt + 1) * 128], rhs=w2[:], start=True, stop=True),
                     r=[K("w2"), K("hc")], w=[K("P_big", 0)])
                S.op("act", lambda e, nt=nt: e.copy(out=Rc[:, nt, 0:128], in_=P_big[0][:, 0:128]), r=[K("P_big", 0)], w=[K("Rc")])
    qT = [sb("qT%d" % i, [128, 4, 128], BF16) for i in range(2)]
    nz = [sb("nz%d" % i, [128, 512], BF16) for i in range(2)]
    bgl = [sb("bg%d" % i, [128, 12], BF16) for i in range(2)]
    tkc = [sb("tk%d" % i, [128, 2, NSEL], F32) for i in range(2)]
    gates = sb("gates", [128, 12], F32)
    pT = [sb("pT%d" % i, [128, 128], BF16) for i in range(4)]
    pT4 = [sb("pT4_%d" % i, [128, 4, 128], BF16) for i in range(4)]
    negm4 = sb("negm4", [128, 4, 128], BF16)
    zc = sb("zc", [128, 4], F32)
    imp = sb("imp", [128, NSEL], F32)
    score = sb("score", [128, NSEL], F32)
    score2 = sb("score2", [128, NSEL], F32)
    mx8 = sb("mx8", [128, 8], F32)
    mx8b = sb("mx8b", [128, 8], F32)
    msel = sb("msel", [128, 128], BF16)
    negm = sb("negm", [128, 128], BF16)
    ocmp = sb("ocmp", [128, 4, 128], F32)
    cf = sb("cf", [128, 8], F32)
    tmp = sb("tmp", [128, 128], F32)
    sg = sb("sg", [128, 512], F32)
    actt = sb("actt", [128, 512], BF16)
    oT = [sb("oT%d" % i, [128, 4, 512], BF16) for i in range(2)]
    S.op("dve", lambda e: e.memset(msel[:], 0.0), w=[K("msel")])
    dbgt = sb("dbgt", [128, 512], F32) if dbg is not None else None
    keys = []
    pcount = [0]
    sccount = [0]

    def score_exp(lhsT_fn, lk, r, sl, bias_ap, bias_key, mask_ap, extra=None):
        si = sccount[0] % 2
        sccount[0] += 1
        ps = P_sc[si][:, 0:128]
        pk = K("P_sc", si)

        def mm(e):
            ins = e.matmul(ps, lhsT=lhsT_fn(), rhs=qT[sl][:, r, :], start=True, stop=(extra is None))
            if extra is not None:
                ins = e.matmul(ps, lhsT=extra(), rhs=negm[:], start=False, stop=True)
            return ins
        S.op("pe", mm, r=list(lk) + [K("qT", sl)] + ([K("negm"), K("E")] if extra is not None else []), w=[pk])
        pi = pcount[0] % 4
        pcount[0] += 1
        S.op("act", lambda e: e.activation(out=pT[pi][:], in_=ps, func=AF.Exp, scale=SC, bias=bias_ap), r=[pk, bias_key], w=[K("pT", pi)])
        if mask_ap is not None:
            S.op("dve", lambda e: e.tensor_tensor(out=pT[pi][:], in0=pT[pi][:], in1=mask_ap, op=ALU.mult), r=[K("pT", pi), K("maskc")], w=[K("pT", pi)])
        return pi

    p4count = [0]

    def pair4(lhsT_fn, lk, sl, bcol, mask_ap, extra=None):
        si = sccount[0] % 2
        sccount[0] += 1
        ps = P_sc[si]
        pk = K("P_sc", si)
        qflat = qT[sl][:].rearrange("p a b -> p (a b)")

        def mm(e):
            ins = e.matmul(ps[:, 0:512], lhsT=lhsT_fn(), rhs=qflat, start=True, stop=(extra is None))
            if extra is not None:
                ins = e.matmul(ps[:, 0:512], lhsT=extra(), rhs=negm4[:].rearrange("p a b -> p (a b)"), start=False, stop=True)
            return ins
        S.op("pe", mm, r=list(lk) + [K("qT", sl)] + ([K("negm4"), K("E")] if extra is not None else []), w=[pk])
        pi = p4count[0] % 4
        p4count[0] += 1
        for r in range(4):
            bc = r * NT + bcol
            S.op("act", lambda e, r=r, bc=bc: e.activation(out=pT4[pi][:, r, :], in_=ps[:, r * 128:(r + 1) * 128], func=AF.Exp, scale=SC, bias=bs[:, bc:bc + 1]),
                 r=[pk, K("bs")], w=[K("pT4", pi, r)])
            if mask_ap is not None:
                S.op("dve", lambda e, r=r: e.tensor_tensor(out=pT4[pi][:, r, :], in0=pT4[pi][:, r, :], in1=mask_ap, op=ALU.mult),
                     r=[K("pT4", pi, r), K("maskc")], w=[K("pT4", pi, r)])
        return pi

    for i in range(NT):
        sl = i % 2
        t0 = i * 128
        sup, j4 = divmod(i, 4)
        osl = sup % 2
        S.op("sp", lambda e, sl=sl, t0=t0: e.dma_start(out=qT[sl][:], in_=fmq[:, t0:t0 + 128].rearrange("(b p) t -> p b t", p=128)),
             r=[dep], w=[K("qT", sl)], dma=K("qT", sl))
        S.op("sp", lambda e, sl=sl, t0=t0: e.dma_start(out=nz[sl][:], in_=tmnz[t0:t0 + 128, :]), r=[dep], w=[K("nz", sl)], dma=K("nz", sl))
        S.op("sp", lambda e, sl=sl, t0=t0: e.dma_start(out=bgl[sl][:], in_=tmbg[t0:t0 + 128, :]), r=[dep], w=[K("bg", sl)], dma=K("bg", sl))
        S.op("pool", lambda e, sl=sl, i=i: e.dma_start(out=tkc[sl][:], in_=cst["tk"][i]), w=[K("tk", sl)], dma=K("tk", sl))
        S.op("act", lambda e, sl=sl: e.activation(out=gates[:], in_=bgl[sl][:], func=AF.Sigmoid), r=[K("bg", sl)], w=[K("gates")])
        mb = min((8 * i + 6) // 128, NCT - 1)
        for r in range(4):
            pis = []
            for m in range(mb + 1):
                mask_ap = None
                if m == mb:
                    kq = i % 16
                    mask_ap = maskc[:, kq * 128:(kq + 1) * 128]
                elif m == mb - 1 and i % 16 == 0:
                    mask_ap = maskc[:, 16 * 128:17 * 128]
                bcol = (r * NCT + m) * NT + i
                pi = score_exp(lambda m=m: kcmpT[:, m * 128:(m + 1) * 128], [K("kcmpT")], r, sl, biasc[:, bcol:bcol + 1], K("biasc"), mask_ap)
                pis.append((m, pi))
            pb = P_sw[r]

            def mm_pv(e, pis=pis, pb=pb):
                ins = None
                for q, (m, pi) in enumerate(pis):
                    ins = e.matmul(pb[:, 0:WR], lhsT=pT[pi][:], rhs=Rc[:, m, 0:WR], start=(q == 0), stop=(q == len(pis) - 1))
                return ins
            S.op("pe", mm_pv, r=[K("pT", pi) for _, pi in pis] + [K("Rc")], w=[K("P_sw", r)])
            S.op("dve", lambda e, r=r, pb=pb: e.tensor_scalar(out=zc[:, r:r + 1], in0=pb[:, WR - 1:WR], scalar1=1e-30, scalar2=None, op0=ALU.add),
                 r=[K("P_sw", r)], w=[K("zc", r)])
            S.op("dve", lambda e, r=r: e.reciprocal(out=zc[:, r:r + 1], in_=zc[:, r:r + 1]), r=[K("zc", r)], w=[K("zc", r)])
            if r == 0:
                S.op("dve", lambda e, pb=pb: e.tensor_scalar(out=imp[:], in0=pb[:, 128:128 + NSEL], scalar1=zc[:, 0:1], scalar2=None, op0=ALU.mult),
                     r=[K("P_sw", r), K("zc", 0)], w=[K("imp")])
            else:
                S.op("dve", lambda e, r=r, pb=pb: e.scalar_tensor_tensor(out=imp[:], in0=pb[:, 128:128 + NSEL], scalar=zc[:, r:r + 1], in1=imp[:], op0=ALU.mult, op1=ALU.add),
                     r=[K("P_sw", r), K("zc", r), K("imp")], w=[K("imp")])
            S.op("dve", lambda e, r=r: e.tensor_tensor(out=cf[:, r:r + 1], in0=zc[:, r:r + 1], in1=gates[:, 3 * r:3 * r + 1], op=ALU.mult),
                 r=[K("zc", r), K("gates")], w=[K("cf", r)])
            S.op("act", lambda e, r=r, pb=pb: e.activation(out=ocmp[:, r, :], in_=pb[:, 0:128], func=AF.Copy, scale=cf[:, r:r + 1]),
                 r=[K("P_sw", r), K("cf", r)], w=[K("ocmp", r)])
        S.op("dve", lambda e, sl=sl: e.tensor_tensor(out=score[:], in0=imp[:], in1=tkc[sl][:, 0, :], op=ALU.mult), r=[K("imp"), K("tk", sl)], w=[K("score")])
        S.op("dve", lambda e, sl=sl: e.tensor_tensor(out=score[:], in0=score[:], in1=tkc[sl][:, 1, :], op=ALU.add), r=[K("score"), K("tk", sl)], w=[K("score")])
        if NSEL > 16:
            S.op("dve", lambda e: e.max(out=mx8[:], in_=score[:]), r=[K("score")], w=[K("mx8")])
            S.op("dve", lambda e: e.match_replace(out=score2[:], in_to_replace=mx8[:], in_values=score[:], imm_value=-1e30), r=[K("mx8"), K("score")], w=[K("score2")])
            S.op("dve", lambda e: e.max(out=mx8b[:], in_=score2[:]), r=[K("score2")], w=[K("mx8b")])
            S.op("dve", lambda e: e.tensor_scalar(out=msel[:, 0:NSEL], in0=score[:], scalar1=mx8b[:, 7:8], scalar2=None, op0=ALU.is_ge),
                 r=[K("score"), K("mx8b")], w=[K("msel")])
        else:
            S.op("dve", lambda e: e.memset(msel[:, 0:NSEL], 1.0), r=[K("score")], w=[K("msel")])
        S.op("pe", lambda e: e.transpose(P_mT[:, 0:128], msel[:], idn[:]), r=[K("msel"), K("idn")], w=[K("P_tr")])
        for r in range(4):
            S.op("dve", lambda e, r=r: e.tensor_scalar(out=negm4[:, r, :], in0=P_mT[:, 0:128], scalar1=-1.0, scalar2=30000.0 / SC, op0=ALU.add, op1=ALU.mult),
                 r=[K("P_tr")], w=[K("negm4")])
        if dbg is not None:
            if "imp" in dbg:
                S.op("pool", lambda e, i=i: e.dma_start(out=dbg["imp"][i], in_=imp[:]), r=[K("imp")], w=[K("dbg", "imp", i)], dma=K("dbg1"))
            if "msel" in dbg:
                S.op("pool", lambda e, i=i: e.dma_start(out=dbg["msel"][i], in_=msel[:]), r=[K("msel")], w=[K("dbg", "msel", i)], dma=K("dbg2"))
            if "ocmp" in dbg:
                S.op("pool", lambda e, i=i: e.dma_start(out=dbg["ocmp"][i], in_=ocmp[:]), r=[K("ocmp", r) for r in range(4)], w=[K("dbg", "ocmp", i)], dma=K("dbg3"))
            if "zc" in dbg:
                S.op("pool", lambda e, i=i: e.dma_start(out=dbg["zc"][i], in_=zc[:]), r=[K("zc", r) for r in range(4)], w=[K("dbg", "zc", i)], dma=K("dbg4"))
            if i == 0 and "kcmpT" in dbg:
                S.op("pool", lambda e: e.dma_start(out=dbg["kcmpT"], in_=kcmpT[:]), r=[K("kcmpT")], w=[K("dbg", "kcmpT")], dma=K("dbg5"))
                S.op("pool", lambda e: e.dma_start(out=dbg["Rc"], in_=Rc[:, 0, 0:WR - 1]), r=[K("Rc")], w=[K("dbg", "Rc")], dma=K("dbg6"))
        S.op("act", lambda e, sl=sl: e.activation(out=sg[:], in_=nz[sl][:], func=AF.Silu), r=[K("nz", sl)], w=[K("sg")])
        def pv4(J, pi, col0, first, last):
            for r in range(4):
                S.op("pe", lambda e, r=r: e.matmul(P_sw[r][:, col0:col0 + 129], lhsT=pT4[pi][:, r, :], rhs=(vsa if col0 == 0 else vwa)[:, J, 0:129], start=first, stop=last),
                     r=[K("pT4", pi, r), K("vsa" if col0 == 0 else "vwa")], w=[K("P_sw", r)])
        prev = None
        for J in range(i + 1):
            mask_ap = maskc[:, 17 * 128:18 * 128] if J == i else None
            pi = pair4(lambda J=J: ksT[:, J * 128:(J + 1) * 128], [K("ksT")], sl, i - J, mask_ap, extra=lambda J=J: Emat[:, J * 128:(J + 1) * 128])
            if prev is not None:
                pv4(prev[0], prev[1], 0, prev[0] == 0, False)
            prev = (J, pi)
        pv4(prev[0], prev[1], 0, prev[0] == 0, True)
        J0 = max(0, i - 4)
        prev = None
        for J in range(J0, i + 1):
            mask_ap = None
            if J == i:
                mask_ap = maskc[:, 17 * 128:18 * 128]
            elif J == i - 4:
                mask_ap = maskc[:, 18 * 128:19 * 128]
            pi = pair4(lambda J=J: kwT[:, J * 128:(J + 1) * 128], [K("kwT")], sl, i - J, mask_ap)
            if prev is not None:
                pv4(prev[0], prev[1], 256, prev[0] == J0, False)
            prev = (J, pi)
        pv4(prev[0], prev[1], 256, prev[0] == J0, True)
        for r in range(4):
            psw = P_sw[r]
            S.op("dve", lambda e, r=r, psw=psw: e.reciprocal(out=cf[:, 4:5], in_=psw[:, 128:129]), r=[K("P_sw", r)], w=[K("cf4")])
            S.op("dve", lambda e, r=r: e.tensor_tensor(out=cf[:, 4:5], in0=cf[:, 4:5], in1=gates[:, 3 * r + 1:3 * r + 2], op=ALU.mult), r=[K("cf4"), K("gates")], w=[K("cf4")])
            S.op("dve", lambda e, r=r, psw=psw: e.reciprocal(out=cf[:, 5:6], in_=psw[:, 384:385]), r=[K("P_sw", r)], w=[K("cf5")])
            S.op("dve", lambda e, r=r: e.tensor_tensor(out=cf[:, 5:6], in0=cf[:, 5:6], in1=gates[:, 3 * r + 2:3 * r + 3], op=ALU.mult), r=[K("cf5"), K("gates")], w=[K("cf5")])
            S.op("dve", lambda e, r=r, psw=psw: e.scalar_tensor_tensor(out=tmp[:], in0=psw[:, 0:128], scalar=cf[:, 4:5], in1=ocmp[:, r, :], op0=ALU.mult, op1=ALU.add),
                 r=[K("P_sw", r), K("cf4"), K("ocmp", r)], w=[K("tmp")])
            S.op("dve", lambda e, r=r, psw=psw: e.scalar_tensor_tensor(out=tmp[:], in0=psw[:, 256:384], scalar=cf[:, 5:6], in1=tmp[:], op0=ALU.mult, op1=ALU.add),
                 r=[K("P_sw", r), K("cf5"), K("tmp")], w=[K("tmp")])
            if dbg is not None and "sw" in dbg:
                S.op("act", lambda e, psw=psw: e.copy(out=dbgt[:], in_=psw[:]), r=[K("P_sw", r), K("P_sw", r)], w=[K("dbgt")])
                S.op("pool", lambda e, i=i, r=r: e.dma_start(out=dbg["sw"][i, r], in_=dbgt[:]), r=[K("dbgt")], w=[K("dbg", "sw", i, r)], dma=K("dbg7"))
            S.op("pool", lambda e, r=r: e.tensor_tensor(out=actt[:, r * 128:(r + 1) * 128], in0=tmp[:], in1=sg[:, r * 128:(r + 1) * 128], op=ALU.mult),
                 r=[K("tmp"), K("sg")], w=[K("actt")])
        finish_tile(S, nc, K, P_tr, K("actt"), actt, idn, K("idn"), oT, osl, j4, outT, sup * 512, keys, "nsa")
    return keys


def dram_cast(S, src, dst, tag, nchunk=8):
    R, C = src.shape
    step = (R + nchunk - 1) // nchunk
    keys = []
    for i, r0 in enumerate(range(0, R, step)):
        r1 = min(R, r0 + step)
        k = (tag, "dram", i)
        S.op("pool", lambda e, r0=r0, r1=r1: e.dma_start(out=dst[r0:r1, :], in_=src[r0:r1, :]), w=[k], dma=k)
        keys.append(k)
    return keys


def phase_out(S, nc, st, TT, xT_bf, x_tok, actT, wm_bf, wbr_bf, wout_bf, bmT, lng, lnb, out, dep, alpha):
    def sb(name, shape, dt):
        return st.enter_context(nc.sbuf_tensor("po_" + name, shape, dt))

    def psum(name, shape, dt=F32):
        return st.enter_context(nc.psum_tensor("po_" + name, shape, dt))
    K = lambda *a: ("po",) + a
    TL = 512
    xT = sb("xT", [128, 32, TL], BF16)
    act = sb("act", [128, 16, TL], BF16)
    mg = sb("mg", [128, 32, TL], BF16)
    Wm = [sb("Wm%d" % i, [128, 32, 128], BF16) for i in range(2)]
    Wb = [sb("Wb%d" % i, [128, 16, 128], BF16) for i in range(2)]
    Wo = [sb("Wo%d" % i, [128, 32, 256], BF16) for i in range(2)]
    z = sb("z", [128, 4096], F32)
    gch = [sb("gch%d" % i, [128, 1024], F32) for i in range(2)]
    bch = [sb("bch%d" % i, [128, 1024], F32) for i in range(2)]
    at = [sb("at%d" % i, [128, TL], F32) for i in range(2)]
    bm = sb("bm", [128, 96], F32)
    st6 = sb("st6", [128, 16], F32)
    junk = sb("junk", [128, 1024], F32)
    eps_t = sb("eps", [128, 1], F32)
    P_m = [psum("P_m%d" % i, [128, 512]) for i in range(2)]
    P_y = [psum("P_y%d" % i, [128, 512]) for i in range(2)]
    P_o = [psum("P_o%d" % i, [128, 512]) for i in range(4)]
    S.op("sp", lambda e: e.dma_start(out=bm[:], in_=bmT), w=[K("bm")], dma=K("bm"))
    S.op("dve", lambda e: e.memset(eps_t[:], 1e-5), w=[K("eps")])
    wmv = wm_bf.rearrange("(kb p) n -> p kb n", p=128)
    wov = wout_bf.rearrange("(kb p) n -> p kb n", p=128)
    wi = 0
    woi = 0
    gi = 0
    keys = []
    for tt in range(TT // TL):
        t0 = tt * TL
        S.op("sp", lambda e, t0=t0: e.dma_start(out=xT[:], in_=xT_bf[:, t0:t0 + TL].rearrange("(kb p) t -> p kb t", p=128)),
             r=[dep], w=[K("xT")], dma=K("xT"))
        for br in range(3):
            S.op("act", lambda e, t0=t0, br=br: e.dma_start(out=act[:], in_=actT[br][:, t0:t0 + TL].rearrange("(fb p) t -> p fb t", p=128)),
                 r=[dep], w=[K("act")], dma=K("act"))
            wbv = wbr_bf[br].rearrange("(fb p) n -> p fb n", p=128)
            for cb in range(32):
                ws = wi % 2
                wi += 1
                col = br * 4096 + cb * 128
                S.op("sp", lambda e, ws=ws, col=col: e.dma_start(out=Wm[ws][:], in_=wmv[:, :, col:col + 128]), r=[dep], w=[K("Wm", ws)], dma=K("Wm", ws))
                S.op("act", lambda e, ws=ws, cb=cb, wbv=wbv: e.dma_start(out=Wb[ws][:], in_=wbv[:, :, cb * 128:(cb + 1) * 128]), r=[dep], w=[K("Wb", ws)], dma=K("Wb", ws))

                def mm1(e, ws=ws):
                    ins = None
                    for kb in range(32):
                        ins = e.matmul(P_m[ws][:], lhsT=Wm[ws][:, kb, :], rhs=xT[:, kb, :], start=(kb == 0), stop=(kb == 31))
                    return ins
                S.op("pe", mm1, r=[K("Wm", ws), K("xT")], w=[K("P_m", ws)])

                def mm2(e, ws=ws):
                    ins = None
                    for fb in range(16):
                        ins = e.matmul(P_y[ws][:], lhsT=Wb[ws][:, fb, :], rhs=act[:, fb, :], start=(fb == 0), stop=(fb == 15))
                    return ins
                S.op("pe", mm2, r=[K("Wb", ws), K("act")], w=[K("P_y", ws)])
                bcol = br * 32 + cb
                S.op("act", lambda e, ws=ws, bcol=bcol: e.activation(out=at[ws][:], in_=P_m[ws][:], func=AF.Sigmoid, bias=bm[:, bcol:bcol + 1]),
                     r=[K("P_m", ws), K("bm")], w=[K("at", ws)])
                if br == 0:
                    S.op("dve", lambda e, ws=ws, cb=cb: e.tensor_tensor(out=mg[:, cb, :], in0=P_y[ws][:], in1=at[ws][:], op=ALU.mult),
                         r=[K("P_y", ws), K("at", ws)], w=[K("mg", cb)])
                else:
                    S.op("dve", lambda e, ws=ws: e.tensor_tensor(out=at[ws][:], in0=P_y[ws][:], in1=at[ws][:], op=ALU.mult),
                         r=[K("P_y", ws), K("at", ws)], w=[K("at", ws)])
                    S.op("pool", lambda e, ws=ws, cb=cb: e.tensor_tensor(out=mg[:, cb, :], in0=mg[:, cb, :], in1=at[ws][:], op=ALU.add),
                         r=[K("mg", cb), K("at", ws)], w=[K("mg", cb)])
        for j in range(4):
            r0 = t0 + j * 128
            S.op("sp", lambda e, r0=r0: e.dma_start(out=z[:], in_=x_tok[r0:r0 + 128, :]), w=[K("z")], dma=K("z"))
            for nb in range(16):
                wos = woi % 2
                woi += 1
                pk = woi % 4
                S.op("sp", lambda e, wos=wos, nb=nb: e.dma_start(out=Wo[wos][:], in_=wov[:, :, nb * 256:(nb + 1) * 256]), r=[dep], w=[K("Wo", wos)], dma=K("Wo", wos))

                def mm3(e, wos=wos, pk=pk, j=j):
                    ins = None
                    for cb in range(32):
                        ins = e.matmul(P_o[pk][:, 0:256], lhsT=mg[:, cb, j * 128:(j + 1) * 128], rhs=Wo[wos][:, cb, :], start=(cb == 0), stop=(cb == 31))
                    return ins
                S.op("pe", mm3, r=[K("Wo", wos)] + [K("mg", cb) for cb in range(32)], w=[K("P_o", pk)])
                S.op("dve", lambda e, pk=pk, nb=nb: e.scalar_tensor_tensor(out=z[:, nb * 256:(nb + 1) * 256], in0=z[:, nb * 256:(nb + 1) * 256], scalar=alpha,
                                                                         in1=P_o[pk][:, 0:256], op0=ALU.mult, op1=ALU.add),
                     r=[K("P_o", pk), K("z")], w=[K("z")])
            for c in range(4):
                S.op("act", lambda e, c=c: e.activation(out=junk[:], in_=z[:, c * 1024:(c + 1) * 1024], func=AF.Copy, accum_out=st6[:, c:c + 1]),
                     r=[K("z")], w=[K("junk"), K("st6", c)])
                S.op("act", lambda e, c=c: e.activation(out=junk[:], in_=z[:, c * 1024:(c + 1) * 1024], func=AF.Square, accum_out=st6[:, 4 + c:5 + c]),
                     r=[K("z")], w=[K("junk"), K("st6", 4 + c)])
            stk = [K("st6", c) for c in range(8)]
            S.op("dve", lambda e: e.tensor_reduce(out=st6[:, 8:9], in_=st6[:, 0:4], axis=AX.X, op=ALU.add), r=stk, w=[K("mean")])
            S.op("dve", lambda e: e.tensor_reduce(out=st6[:, 9:10], in_=st6[:, 4:8], axis=AX.X, op=ALU.add), r=stk, w=[K("ex2")])
            S.op("dve", lambda e: e.tensor_scalar(out=st6[:, 8:9], in0=st6[:, 8:9], scalar1=1.0 / 4096.0, scalar2=None, op0=ALU.mult), r=[K("mean")], w=[K("mean")])
            S.op("dve", lambda e: e.tensor_scalar(out=st6[:, 9:10], in0=st6[:, 9:10], scalar1=1.0 / 4096.0, scalar2=None, op0=ALU.mult), r=[K("ex2")], w=[K("ex2")])
            S.op("dve", lambda e: e.tensor_tensor(out=st6[:, 10:11], in0=st6[:, 8:9], in1=st6[:, 8:9], op=ALU.mult), r=[K("mean")], w=[K("m2")])
            S.op("dve", lambda e: e.tensor_tensor(out=st6[:, 11:12], in0=st6[:, 9:10], in1=st6[:, 10:11], op=ALU.subtract), r=[K("ex2"), K("m2")], w=[K("var")])
            S.op("act", lambda e: e.activation(out=st6[:, 12:13], in_=st6[:, 11:12], func=AF.Sqrt, bias=eps_t[:, 0:1]), r=[K("var"), K("eps")], w=[K("rstd")])
            S.op("dve", lambda e: e.reciprocal(out=st6[:, 13:14], in_=st6[:, 12:13]), r=[K("rstd")], w=[K("rstd2")])
            for c in range(4):
                gs = gi % 2
                gi += 1
                S.op("sp", lambda e, gs=gs, c=c: e.dma_start(out=gch[gs][:], in_=lng[:, c * 1024:(c + 1) * 1024]), w=[K("gch", gs)], dma=K("gch", gs))
                S.op("sp", lambda e, gs=gs, c=c: e.dma_start(out=bch[gs][:], in_=lnb[:, c * 1024:(c + 1) * 1024]), w=[K("bch", gs)], dma=K("bch", gs))
                zc = z[:, c * 1024:(c + 1) * 1024]
                S.op("dve", lambda e, zc=zc: e.tensor_scalar(out=zc, in0=zc, scalar1=st6[:, 8:9], scalar2=st6[:, 13:14], op0=ALU.subtract, op1=ALU.mult),
                     r=[K("z"), K("mean"), K("rstd2")], w=[K("z")])
                S.op("pool", lambda e, zc=zc, gs=gs: e.tensor_tensor(out=zc, in0=zc, in1=gch[gs][:], op=ALU.mult), r=[K("z"), K("gch", gs)], w=[K("z")])
                S.op("dve", lambda e, zc=zc, gs=gs: e.tensor_tensor(out=zc, in0=zc, in1=bch[gs][:], op=ALU.add), r=[K("z"), K("bch", gs)], w=[K("z")])
            k = K("out", r0)
            S.op("sp", lambda e, r0=r0: e.dma_start(out=out[r0:r0 + 128, :], in_=z[:]), r=[K("z")], w=[k], dma=K("zout"))
            keys.append(k)
    return keys


def phase_select(S, nc, st, gathered, myact, selm, NR, SEQ, TT):
    def sb(name, shape, dt):
        return st.enter_context(nc.sbuf_tensor("sel_" + name, shape, dt))
    K = lambda *a: ("sel",) + a
    C = [sb("C%d" % i, [128, 16, 512], BF16) for i in range(2)]
    acc = sb("acc", [128, 16, 512], BF16)
    m = sb("m", [128, 8], F32)
    S.op("sp", lambda e: e.dma_start(out=m[:], in_=selm), w=[K("m")], dma=K("m"))
    NB = NR // 4
    NQ = SEQ // TT
    keys = []
    ci = 0
    for br in range(3):
        for tt in range(TT // 512):
            for k in range(NB * NQ):
                bb, q = divmod(k, NQ)
                sl = ci % 2
                ci += 1
                for g in range(4):
                    r0 = (4 * bb + g) * 512
                    c0 = q * TT + tt * 512
                    S.op("sp" if g % 2 == 0 else "act", lambda e, sl=sl, g=g, r0=r0, c0=c0, br=br: e.dma_start(
                        out=C[sl][:, 4 * g:4 * g + 4, :], in_=gathered[br][r0:r0 + 512, c0:c0 + 512].rearrange("(fb p) t -> p fb t", p=128)),
                        w=[K("C", sl, g)], dma=K("C", sl, g))
                rk = [K("C", sl, g) for g in range(4)] + [K("m")]
                if k == 0:
                    S.op("dve", lambda e, sl=sl, k=k: e.tensor_scalar(out=acc[:], in0=C[sl][:], scalar1=m[:, k:k + 1], scalar2=None, op0=ALU.mult),
                         r=rk, w=[K("acc")])
                else:
                    S.op("dve", lambda e, sl=sl, k=k: e.scalar_tensor_tensor(out=acc[:], in0=C[sl][:], scalar=m[:, k:k + 1], in1=acc[:], op0=ALU.mult, op1=ALU.add),
                         r=rk + [K("acc")], w=[K("acc")])
            kk = K("out", br, tt)
            S.op("sp", lambda e, br=br, tt=tt: e.dma_start(out=myact[br][:, tt * 512:(tt + 1) * 512].rearrange("(fb p) t -> p fb t", p=128), in_=acc[:]),
                 r=[K("acc")], w=[kk], dma=K("accst"))
            keys.append(kk)
    return keys


D_MODEL = 4096
IN_SIZES = (1024, 1024, 2048, 2048, 16, 2048, 512, 512, 512, 512, 512, 512, 2048, 48, 2048, 2048, 12288)
OFF = np.concatenate([[0], np.cumsum(IN_SIZES)]).astype(np.int64)
NF = 2176
NTM = 2576
FM_Q, FM_K, FM_NQ, FM_KC, FM_VC, FM_KS, FM_KW, FM_MQ, FM_GA = 0, 256, 512, 1024, 1152, 1280, 1408, 1536, 2048
TM_K, TM_V, TM_GZ, TM_VS, TM_VW, TM_NZ, TM_MZ, TM_BG = 0, 256, 768, 1280, 1408, 1536, 2048, 2560


def proj_cols(g):
    fm = np.concatenate([
        OFF[0] + g * 256 + np.arange(256), OFF[1] + g * 256 + np.arange(256), OFF[5] + g * 512 + np.arange(512),
        OFF[6] + g * 128 + np.arange(128), OFF[7] + g * 128 + np.arange(128), OFF[8] + g * 128 + np.arange(128),
        OFF[10] + g * 128 + np.arange(128), OFF[14] + g * 512 + np.arange(512), OFF[4] + np.arange(16)])
    tm = np.concatenate([
        OFF[1] + g * 256 + np.arange(256), OFF[2] + g * 512 + np.arange(512), OFF[3] + g * 512 + np.arange(512),
        OFF[9] + g * 128 + np.arange(128), OFF[11] + g * 128 + np.arange(128), OFF[12] + g * 512 + np.arange(512),
        OFF[15] + g * 512 + np.arange(512), OFF[13] + g * 12 + np.arange(12)])
    return fm, tm


def _ctx():
    import contextlib
    return contextlib.ExitStack()


def build_proj(SEQ):
    nc = bass.Bass("TRN2", target_bir_lowering=False)
    xT = nc.dram_tensor("xT", [4096, SEQ], F32, kind="ExternalInput").ap()
    w = nc.dram_tensor("w", [4096, NF + NTM], F32, kind="ExternalInput").ap()
    fm = nc.dram_tensor("fm", [NF, SEQ], BF16, kind="ExternalOutput").ap()
    tm = nc.dram_tensor("tm", [SEQ, NTM], BF16, kind="ExternalOutput").ap()
    xb = nc.dram_tensor("xb", [4096, SEQ], BF16).ap()
    wb = nc.dram_tensor("wb", [4096, NF + NTM], BF16).ap()
    S = Sched(nc)
    with _ctx() as st:
        sb = lambda name, shape, dt: st.enter_context(nc.sbuf_tensor(name, shape, dt))
        bufs = dict(name="gb", A=[sb("A%d" % i, [128, 32, 512], BF16) for i in range(2)], B=sb("B", [128, 32, 1024], BF16),
                    O=[sb("O%d" % i, [128, 4, 512], BF16) for i in range(2)],
                    PS=[st.enter_context(nc.psum_tensor("ps%d" % i, [128, 512], F32)) for i in range(8)])
        dummy = sb("dummyt", [128, 8], F32)
        k2 = dram_cast(S, w, wb, "cw", 8)
        k1 = dram_cast(S, xT, xb, "cx", 16)
        S.op("pool", lambda e: e.memset(dummy[:, 0:1], 0.0), r=k1, w=["xb_done"])
        S.op("pool", lambda e: e.memset(dummy[:, 1:2], 0.0), r=k2, w=["wb_done"])
        blocks = [("FM", 0, 1024, fm[0:1024, :]), ("FM", 1024, 1024, fm[1024:2048, :]), ("FM", 2048, 128, fm[2048:2176, :]),
                  ("TM", NF, 1024, tm[:, 0:1024]), ("TM", NF + 1024, 1024, tm[:, 1024:2048]), ("TM", NF + 2048, 528, tm[:, 2048:2576])]
        gemm(S, "g", xb, wb, SEQ, 4096, blocks, bufs, "xb_done", "wb_done")
        S.emit()
    return nc


def build_gla(SEQ):
    nc = bass.Bass("TRN2", target_bir_lowering=False)
    di = lambda n, s, d: nc.dram_tensor(n, list(s), d, kind="ExternalInput").ap()
    fm = di("fm", [NF, SEQ], BF16)
    tm = di("tm", [SEQ, NTM], BF16)
    wa2 = di("wa2", [17, 256], F32)
    ngb = di("ngb", [128, 512], F32)
    gcn = di("gcn", [128, 512], F32)
    idn = di("idn", [128, 128], BF16)
    o_gla = nc.dram_tensor("o_gla", [512, SEQ], BF16, kind="ExternalOutput").ap()
    S = Sched(nc)
    with _ctx() as st:
        phase_gla(S, nc, st, SEQ, fm[FM_Q:FM_Q + 256, :], fm[FM_K:FM_K + 256, :], fm[FM_GA:FM_GA + 16, :],
                  tm[:, TM_K:TM_K + 256], tm[:, TM_V:TM_V + 512], tm[:, TM_GZ:TM_GZ + 512], wa2, ngb, gcn, idn, o_gla, "nodep")
        S.emit()
    return nc


def build_mem(SEQ):
    nc = bass.Bass("TRN2", target_bir_lowering=False)
    di = lambda n, s, d: nc.dram_tensor(n, list(s), d, kind="ExternalInput").ap()
    fm = di("fm", [NF, SEQ], BF16)
    tm = di("tm", [SEQ, NTM], BF16)
    idn = di("idn", [128, 128], BF16)
    memT = di("memT", [4096, 256], F32)
    wk = di("wk", [4096, 512], F32)
    wv = di("wv", [4096, 512], F32)
    memTb = nc.dram_tensor("memTb", [4096, 256], BF16).ap()
    wkb = nc.dram_tensor("wkb", [4096, 512], BF16).ap()
    wvb = nc.dram_tensor("wvb", [4096, 512], BF16).ap()
    o_mem = nc.dram_tensor("o_mem", [512, SEQ], BF16, kind="ExternalOutput").ap()
    S = Sched(nc)
    with _ctx() as st:
        dummy = st.enter_context(nc.sbuf_tensor("dummyt", [128, 8], F32))
        ks = dram_cast(S, memT, memTb, "cm", 2) + dram_cast(S, wk, wkb, "ck", 2) + dram_cast(S, wv, wvb, "cv", 2)
        S.op("pool", lambda e: e.memset(dummy[:, 0:1], 0.0), r=ks, w=["w_done"])
        phase_mem(S, nc, st, SEQ, fm[FM_MQ:FM_MQ + 512, :], tm[:, TM_MZ:TM_MZ + 512], memTb, wkb, wvb, idn, o_mem, "nodep", "w_done")
        S.emit()
    return nc


def build_nsa(SEQ, cn):
    nc = bass.Bass("TRN2", target_bir_lowering=False)
    di = lambda n, s, d: nc.dram_tensor(n, list(s), d, kind="ExternalInput").ap()
    fm = di("fm", [NF, SEQ], BF16)
    tm = di("tm", [SEQ, NTM], BF16)
    w = {k: di(k, s, F32) for k, s in (("w1k", [4096, 128]), ("w1v", [4096, 128]), ("w2k", [128, 128]), ("w2v", [128, 128]),
                                       ("pekT", [128, 32]), ("pevT", [128, 32]))}
    cst = {k: di("c_" + k, v.shape, BF16 if v.dtype == NPBF else F32) for k, v in cn.items()}
    idn = di("idn", [128, 128], BF16)
    o_nsa = nc.dram_tensor("o_nsa", [512, SEQ], BF16, kind="ExternalOutput").ap()
    S = Sched(nc)
    with _ctx() as st:
        phase_nsa(S, nc, st, SEQ, fm[FM_NQ:FM_NQ + 512, :], fm[FM_KC:FM_KC + 128, :], fm[FM_VC:FM_VC + 128, :], fm[FM_KS:FM_KS + 128, :],
                  fm[FM_KW:FM_KW + 128, :], tm[:, TM_VS:TM_VS + 128], tm[:, TM_VW:TM_VW + 128], tm[:, TM_NZ:TM_NZ + 512], tm[:, TM_BG:TM_BG + 12],
                  w["w1k"], w["w1v"], w["w2k"], w["w2v"], w["pekT"], w["pevT"], cst, idn, o_nsa, "nodep")
        S.emit()
    return nc


def build_out(TT, alpha):
    nc = bass.Bass("TRN2", target_bir_lowering=False)
    di = lambda n, s, d: nc.dram_tensor(n, list(s), d, kind="ExternalInput").ap()
    dt = lambda n, s, d: nc.dram_tensor(n, list(s), d).ap()
    xT = di("xT", [4096, TT], F32)
    xtok = di("xtok", [TT, 4096], F32)
    actT = [di("act%d" % i, [2048, TT], BF16) for i in range(3)]
    wm = di("wm", [4096, 12288], F32)
    wbr = [di("wbr%d" % i, [2048, 4096], F32) for i in range(3)]
    wout = di("wout", [4096, 4096], F32)
    bmT = di("bmT", [128, 96], F32)
    lng = di("lng", [128, 4096], F32)
    lnb = di("lnb", [128, 4096], F32)
    out = nc.dram_tensor("out", [TT, 4096], F32, kind="ExternalOutput").ap()
    xTb = dt("xTb", [4096, TT], BF16)
    wmb = dt("wmb", [4096, 12288], BF16)
    wbrb = [dt("wbrb%d" % i, [2048, 4096], BF16) for i in range(3)]
    woutb = dt("woutb", [4096, 4096], BF16)
    S = Sched(nc)
    with _ctx() as st:
        dummy = st.enter_context(nc.sbuf_tensor("dummyt", [128, 8], F32))
        ks = dram_cast(S, xT, xTb, "cx") + dram_cast(S, wm, wmb, "cwm", 16) + dram_cast(S, wout, woutb, "cwo")
        for i in range(3):
            ks += dram_cast(S, wbr[i], wbrb[i], "cwb%d" % i)
        S.op("pool", lambda e: e.memset(dummy[:, 0:1], 0.0), r=ks, w=["w_done"])
        phase_out(S, nc, st, TT, xTb, xtok, actT, wmb, wbrb, woutb, bmT, lng, lnb, out, "w_done", alpha)
        S.emit()
    return nc


def kernel_multi(x, mem, w_in, b_merge, gla_w_a2, gla_b_a, gla_norm_g, nsa_pe_k, nsa_pe_v, nsa_wk1, nsa_wk2, nsa_wv1, nsa_wv2,
           w_mem_kv, w_br_gla, w_br_nsa, w_br_mem, w_out, ln_g, ln_b):
    f32 = lambda a: np.ascontiguousarray(np.asarray(a, dtype=np.float32))
    x = np.asarray(x, dtype=np.float32)
    B, SEQ, D = x.shape
    NCORE = 4 * B
    cores = list(range(NCORE))
    w_in0 = np.asarray(w_in, dtype=np.float32)[0]
    alpha = float((2 * 1) ** 0.25)
    ident = np.eye(128, dtype=np.float32).astype(NPBF)
    xTs = [f32(x[b].T) for b in range(B)]
    ins = []
    for c in cores:
        b, g = divmod(c, 4)
        fmc, tmc = proj_cols(g)
        wc = np.zeros((4096, NF + NTM), np.float32)
        wc[:, 0:len(fmc)] = w_in0[:, fmc]
        wc[:, NF:NF + len(tmc)] = w_in0[:, tmc]
        ins.append(dict(xT=xTs[b], w=wc))
    res = run_bass_kernel_spmd(build_proj(SEQ), ins, core_ids=cores)
    fms = [np.asarray(r["fm"]) for r in res.results]
    tms = [np.asarray(r["tm"]) for r in res.results]
    del ins, res
    gcn = gla_consts()
    ins = []
    for c in cores:
        b, g = divmod(c, 4)
        wa2 = np.concatenate([np.asarray(gla_b_a, np.float32)[0][None, g * 256:(g + 1) * 256],
                              np.asarray(gla_w_a2, np.float32)[0][:, g * 256:(g + 1) * 256]], axis=0)
        ins.append(dict(fm=fms[c], tm=tms[c], wa2=f32(wa2),
                        ngb=f32(np.broadcast_to(np.asarray(gla_norm_g, np.float32)[0][None, :], (128, 512))), gcn=gcn, idn=ident))
    res = run_bass_kernel_spmd(build_gla(SEQ), ins, core_ids=cores)
    o_gla = [np.asarray(r["o_gla"]) for r in res.results]
    del ins, res
    wkv = np.asarray(w_mem_kv, dtype=np.float32)[0]
    memTs = [f32(np.asarray(mem, dtype=np.float32)[b].T) for b in range(B)]
    ins = []
    for c in cores:
        b, g = divmod(c, 4)
        ins.append(dict(fm=fms[c], tm=tms[c], idn=ident, memT=memTs[b], wk=f32(wkv[:, g * 512:(g + 1) * 512]),
                        wv=f32(wkv[:, 2048 + g * 512:2048 + (g + 1) * 512])))
    res = run_bass_kernel_spmd(build_mem(SEQ), ins, core_ids=cores)
    o_mem = [np.asarray(r["o_mem"]) for r in res.results]
    del ins, res
    cns = [nsa_consts(SEQ, g) for g in range(4)]
    ins = []
    for c in cores:
        b, g = divmod(c, 4)
        d = dict(fm=fms[c], tm=tms[c], w1k=f32(np.asarray(nsa_wk1)[0]), w1v=f32(np.asarray(nsa_wv1)[0]), w2k=f32(np.asarray(nsa_wk2)[0]),
                 w2v=f32(np.asarray(nsa_wv2)[0]), pekT=f32(np.asarray(nsa_pe_k, np.float32)[0].T), pevT=f32(np.asarray(nsa_pe_v, np.float32)[0].T), idn=ident)
        for k, v in cns[g].items():
            d["c_" + k] = v
        ins.append(d)
    res = run_bass_kernel_spmd(build_nsa(SEQ, cns[0]), ins, core_ids=cores)
    o_nsa = [np.asarray(r["o_nsa"]) for r in res.results]
    del ins, res, fms, tms
    TT = B * SEQ // NCORE
    per_b = SEQ // TT
    wm = f32(w_in0[:, OFF[16]:OFF[17]])
    bmT = f32(np.asarray(b_merge, np.float32)[0].reshape(96, 128).T)
    lng = f32(np.broadcast_to(np.asarray(ln_g, np.float32)[0][None], (128, 4096)))
    lnb = f32(np.broadcast_to(np.asarray(ln_b, np.float32)[0][None], (128, 4096)))
    wbrs = [f32(np.asarray(wb)[0]) for wb in (w_br_gla, w_br_nsa, w_br_mem)]
    wo = f32(np.asarray(w_out)[0])
    ins = []
    for c in cores:
        b, q = divmod(c, per_b)
        sl = slice(q * TT, (q + 1) * TT)
        d = dict(xT=f32(xTs[b][:, sl]), xtok=f32(x[b, sl]), wm=wm, wbr0=wbrs[0], wbr1=wbrs[1], wbr2=wbrs[2], wout=wo, bmT=bmT, lng=lng, lnb=lnb)
        for i, oo in enumerate((o_gla, o_nsa, o_mem)):
            d["act%d" % i] = np.ascontiguousarray(np.concatenate([oo[b * 4 + g][:, sl] for g in range(4)], axis=0))
        ins.append(d)
    res = run_bass_kernel_spmd(build_out(TT, alpha), ins, core_ids=cores)
    out = np.concatenate([np.asarray(r["out"]).astype(np.float32) for r in res.results], axis=0).reshape(B, SEQ, D)
    return out


def build_fused(SEQ, NR, cn, alpha):
    import contextlib
    TT = SEQ // 4
    nc = bass.Bass("TRN2", target_bir_lowering=False)
    di = lambda n, s, d: nc.dram_tensor(n, list(s), d, kind="ExternalInput").ap()
    dt = lambda n, s, d: nc.dram_tensor(n, list(s), d).ap()
    xT = di("xT", [4096, SEQ], F32)
    w = di("w", [4096, NF + NTM], F32)
    wa2 = di("wa2", [17, 256], F32)
    ngb = di("ngb", [128, 512], F32)
    gcn = di("gcn", [128, 512], F32)
    idn = di("idn", [128, 128], BF16)
    memT = di("memT", [4096, 256], F32)
    wk = di("wk", [4096, 512], F32)
    wv = di("wv", [4096, 512], F32)
    nw = {k: di(k, s, F32) for k, s in (("w1k", [4096, 128]), ("w1v", [4096, 128]), ("w2k", [128, 128]), ("w2v", [128, 128]),
                                        ("pekT", [128, 32]), ("pevT", [128, 32]))}
    cst = {k: di("c_" + k, v.shape, BF16 if v.dtype == NPBF else F32) for k, v in cn.items()}
    xTq = di("xTq", [4096, TT], F32)
    xtok = di("xtok", [TT, 4096], F32)
    wm = di("wm", [4096, 12288], F32)
    wbr = [di("wbr%d" % i, [2048, 4096], F32) for i in range(3)]
    wout = di("wout", [4096, 4096], F32)
    bmT = di("bmT", [128, 96], F32)
    lng = di("lng", [128, 4096], F32)
    lnb = di("lnb", [128, 4096], F32)
    selm = di("selm", [128, 8], F32)
    out = nc.dram_tensor("out", [TT, 4096], F32, kind="ExternalOutput").ap()
    xb = dt("xb", [4096, SEQ], BF16)
    wb = dt("wb", [4096, NF + NTM], BF16)
    fm = dt("fm", [NF, SEQ], BF16)
    tm = dt("tm", [SEQ, NTM], BF16)
    memTb = dt("memTb", [4096, 256], BF16)
    wkb = dt("wkb", [4096, 512], BF16)
    wvb = dt("wvb", [4096, 512], BF16)
    acts = [dt("acts%d" % i, [512, SEQ], BF16) for i in range(3)]
    gathered = [dt("gathered%d" % i, [NR * 512, SEQ], BF16) for i in range(3)]
    myact = [dt("myact%d" % i, [2048, TT], BF16) for i in range(3)]
    xTqb = dt("xTqb", [4096, TT], BF16)
    wmb = dt("wmb", [4096, 12288], BF16)
    wbrb = [dt("wbrb%d" % i, [2048, 4096], BF16) for i in range(3)]
    woutb = dt("woutb", [4096, 4096], BF16)
    tok_src = dt("tok_src", [1, 16], F32)
    tok_dst = dt("tok_dst", [1, 16], F32)
    S = Sched(nc)
    with contextlib.ExitStack() as top:
        scr = {e: top.enter_context(nc.sbuf_tensor("scr_" + e, [1, 2], F32)) for e in ("act", "dve", "pool")}
        S.setup_phased({e: scr[e][:] for e in scr}, tok_src, tok_dst)
        with contextlib.ExitStack() as st:
            sb = lambda name, shape, dty: st.enter_context(nc.sbuf_tensor(name, shape, dty))
            bufs = dict(name="gb", A=[sb("A%d" % i, [128, 32, 512], BF16) for i in range(2)], B=sb("B", [128, 32, 1024], BF16),
                        O=[sb("O%d" % i, [128, 4, 512], BF16) for i in range(2)],
                        PS=[st.enter_context(nc.psum_tensor("ps%d" % i, [128, 512], F32)) for i in range(8)])
            dummy = sb("dummyt", [128, 8], F32)
            k2 = dram_cast(S, w, wb, "cw", 8)
            k1 = dram_cast(S, xT, xb, "cx", 16)
            S.op("pool", lambda e: e.memset(dummy[:, 0:1], 0.0), r=k1, w=["xb_done"])
            S.op("pool", lambda e: e.memset(dummy[:, 1:2], 0.0), r=k2, w=["wb_done"])
            dram_cast(S, memT, memTb, "cm", 2)
            dram_cast(S, wk, wkb, "ck", 2)
            dram_cast(S, wv, wvb, "cv", 2)
            dram_cast(S, xTq, xTqb, "cxq", 4)
            dram_cast(S, wm, wmb, "cwm", 16)
            dram_cast(S, wout, woutb, "cwo", 8)
            for i in range(3):
                dram_cast(S, wbr[i], wbrb[i], "cwb%d" % i, 4)
            blocks = [("FM", 0, 1024, fm[0:1024, :]), ("FM", 1024, 1024, fm[1024:2048, :]), ("FM", 2048, 128, fm[2048:2176, :]),
                      ("TM", NF, 1024, tm[:, 0:1024]), ("TM", NF + 1024, 1024, tm[:, 1024:2048]), ("TM", NF + 2048, 528, tm[:, 2048:2576])]
            gemm(S, "g", xb, wb, SEQ, 4096, blocks, bufs, "xb_done", "wb_done", stq="sp", ldq=("sp", "act"))
            S.flush()
        with contextlib.ExitStack() as st:
            phase_gla(S, nc, st, SEQ, fm[FM_Q:FM_Q + 256, :], fm[FM_K:FM_K + 256, :], fm[FM_GA:FM_GA + 16, :],
                      tm[:, TM_K:TM_K + 256], tm[:, TM_V:TM_V + 512], tm[:, TM_GZ:TM_GZ + 512], wa2, ngb, gcn, idn, acts[0], "nodep")
            S.flush()
        with contextlib.ExitStack() as st:
            phase_mem(S, nc, st, SEQ, fm[FM_MQ:FM_MQ + 512, :], tm[:, TM_MZ:TM_MZ + 512], memTb, wkb, wvb, idn, acts[2], "nodep", "nodep")
            S.flush()
        with contextlib.ExitStack() as st:
            for br in (0, 2):
                S.op("pool", lambda e, br=br: e.collective_compute("AllGather", ALU.bypass, replica_groups=[list(range(NR))], ins=[acts[br].opt()], outs=[gathered[br].opt()]),
                     w=[("gathered", br)], dma="cc%d" % br, inc=1)
            kn = phase_nsa(S, nc, st, SEQ, fm[FM_NQ:FM_NQ + 512, :], fm[FM_KC:FM_KC + 128, :], fm[FM_VC:FM_VC + 128, :], fm[FM_KS:FM_KS + 128, :],
                           fm[FM_KW:FM_KW + 128, :], tm[:, TM_VS:TM_VS + 128], tm[:, TM_VW:TM_VW + 128], tm[:, TM_NZ:TM_NZ + 512],
                           tm[:, TM_BG:TM_BG + 12], nw["w1k"], nw["w1v"], nw["w2k"], nw["w2v"], nw["pekT"], nw["pevT"], cst, idn, acts[1], "nodep")
            S.op("pool", lambda e: e.collective_compute("AllGather", ALU.bypass, replica_groups=[list(range(NR))], ins=[acts[1].opt()], outs=[gathered[1].opt()]),
                 r=kn, w=[("gathered", 1)], dma="cc1", inc=1)
            S.flush()
        with contextlib.ExitStack() as st:
            phase_select(S, nc, st, gathered, myact, selm, NR, SEQ, TT)
            S.flush()
        with contextlib.ExitStack() as st:
            phase_out(S, nc, st, TT, xTqb, xtok, myact, wmb, wbrb, woutb, bmT, lng, lnb, out, "nodep", alpha)
            S.flush(final=True)
        S.close()
    return nc


def kernel(x, mem, w_in, b_merge, gla_w_a2, gla_b_a, gla_norm_g, nsa_pe_k, nsa_pe_v, nsa_wk1, nsa_wk2, nsa_wv1, nsa_wv2,
                 w_mem_kv, w_br_gla, w_br_nsa, w_br_mem, w_out, ln_g, ln_b):
    f32 = lambda a: np.ascontiguousarray(np.asarray(a, dtype=np.float32))
    x = np.asarray(x, dtype=np.float32)
    B, SEQ, D = x.shape
    NR = 4 * B
    TT = SEQ // 4
    cores = list(range(NR))
    w_in0 = np.asarray(w_in, dtype=np.float32)[0]
    alpha = float((2 * 1) ** 0.25)
    ident = np.eye(128, dtype=np.float32).astype(NPBF)
    xTs = [f32(x[b].T) for b in range(B)]
    memTs = [f32(np.asarray(mem, dtype=np.float32)[b].T) for b in range(B)]
    wkv = np.asarray(w_mem_kv, dtype=np.float32)[0]
    gcn = gla_consts()
    cns = [nsa_consts(SEQ, g) for g in range(4)]
    wm = f32(w_in0[:, OFF[16]:OFF[17]])
    bmT = f32(np.asarray(b_merge, np.float32)[0].reshape(96, 128).T)
    lng = f32(np.broadcast_to(np.asarray(ln_g, np.float32)[0][None], (128, 4096)))
    lnb = f32(np.broadcast_to(np.asarray(ln_b, np.float32)[0][None], (128, 4096)))
    wbrs = [f32(np.asarray(wb_)[0]) for wb_ in (w_br_gla, w_br_nsa, w_br_mem)]
    wo = f32(np.asarray(w_out)[0])
    ngb = f32(np.broadcast_to(np.asarray(gla_norm_g, np.float32)[0][None, :], (128, 512)))
    shared = dict(idn=ident, gcn=gcn, ngb=ngb, w1k=f32(np.asarray(nsa_wk1)[0]), w1v=f32(np.asarray(nsa_wv1)[0]), w2k=f32(np.asarray(nsa_wk2)[0]),
                  w2v=f32(np.asarray(nsa_wv2)[0]), pekT=f32(np.asarray(nsa_pe_k, np.float32)[0].T), pevT=f32(np.asarray(nsa_pe_v, np.float32)[0].T),
                  wm=wm, wbr0=wbrs[0], wbr1=wbrs[1], wbr2=wbrs[2], wout=wo, bmT=bmT, lng=lng, lnb=lnb)
    ins = []
    for c in cores:
        b, g = divmod(c, 4)
        fmc, tmc = proj_cols(g)
        wc = np.zeros((4096, NF + NTM), np.float32)
        wc[:, 0:len(fmc)] = w_in0[:, fmc]
        wc[:, NF:NF + len(tmc)] = w_in0[:, tmc]
        wa2 = np.concatenate([np.asarray(gla_b_a, np.float32)[0][None, g * 256:(g + 1) * 256],
                              np.asarray(gla_w_a2, np.float32)[0][:, g * 256:(g + 1) * 256]], axis=0)
        sl = slice(g * TT, (g + 1) * TT)
        selm = np.zeros((128, 8), np.float32)
        selm[:, b * 4 + g] = 1.0
        d = dict(shared)
        d.update(xT=xTs[b], w=wc, wa2=f32(wa2), memT=memTs[b], wk=f32(wkv[:, g * 512:(g + 1) * 512]),
                 wv=f32(wkv[:, 2048 + g * 512:2048 + (g + 1) * 512]), xTq=f32(xTs[b][:, sl]), xtok=f32(x[b, sl]), selm=selm)
        for k, v in cns[g].items():
            d["c_" + k] = v
        ins.append(d)
    res = run_bass_kernel_spmd(build_fused(SEQ, NR, cns[0], alpha), ins, core_ids=cores)
    out = np.concatenate([np.asarray(r["out"]).astype(np.float32) for r in res.results], axis=0).reshape(B, SEQ, D)
    return out
```
